# Optimizing a Trainium2 kernel written in Bass

```python
import math
import jax, jax.numpy as jnp
from jax import lax
import numpy as np

D_MODEL = 1024
BATCH = 8
SEQ = 2048
DEPTH = 4

N_MIXERS = 3
N_LAYERS_A = len(range(0, DEPTH, N_MIXERS))
N_LAYERS_B = len(range(1, DEPTH, N_MIXERS))
N_LAYERS_C = len(range(2, DEPTH, N_MIXERS))

DEEPNORM_ALPHA = (2.0 * DEPTH) ** 0.25
DEEPNORM_BETA = (8.0 * DEPTH) ** -0.25
LN_EPS = 1e-5

A_HEADS = 8
A_HEAD_DIM = D_MODEL // A_HEADS
MOBA_BLOCK = 256
MOBA_TOPK = 3
MOBA_Q_CHUNK = 16
REL_BUCKETS = 32
REL_MAX_DIST = 128

B_GROUPS = 8
B_WIDTH = 2 * D_MODEL
B_CHUNK = 128

C_HEADS = 4
C_KEY_DIM = D_MODEL // 2
C_VAL_DIM = D_MODEL
C_DK = C_KEY_DIM // C_HEADS
C_DV = C_VAL_DIM // C_HEADS
C_GATE_RANK = 16
C_GATE_NORMALIZER = 16.0
C_CHUNK = 64
C_IN_WIDTH = 2 * C_KEY_DIM + 2 * C_VAL_DIM + C_GATE_RANK

N_EXPERTS = 32
TOP_K = 4
D_EXPERT = D_MODEL
SWIGLU_LIMIT = 7.0
SWIGLU_ALPHA = 1.702
MOE_ROW_BLOCK = 128

kernel_name = "hybrid_moba_gmlp_gla_moe_deepnorm"


def layer_norm(x, g, b):
    xf = x.astype(jnp.float32)
    mu = xf.mean(-1, keepdims=True)
    var = jnp.square(xf - mu).mean(-1, keepdims=True)
    y = (xf - mu) * lax.rsqrt(var + LN_EPS) * g.astype(jnp.float32) + b.astype(jnp.float32)
    return y.astype(x.dtype)


def rms_norm(x, g):
    xf = x.astype(jnp.float32)
    return xf * lax.rsqrt(jnp.mean(xf * xf, -1, keepdims=True) + LN_EPS) * g.astype(jnp.float32)


def t5_bucket(rel):
    n = jnp.maximum(rel, 0)
    max_exact = REL_BUCKETS // 2
    nf = jnp.maximum(n, 1).astype(jnp.float32)
    large = max_exact + (jnp.log(nf / max_exact) / math.log(REL_MAX_DIST / max_exact)
                         * (REL_BUCKETS - max_exact)).astype(jnp.int32)
    large = jnp.minimum(large, REL_BUCKETS - 1)
    return jnp.where(n < max_exact, n, large)


def moba_attention(x, w_in, w_out, rel_bias):
    B, S, _ = x.shape
    H, Dh, BLK, QC = A_HEADS, A_HEAD_DIM, MOBA_BLOCK, MOBA_Q_CHUNK
    qkv = (x @ w_in).reshape(B, S, 3, H, Dh)
    q = qkv[:, :, 0].transpose(0, 2, 1, 3)
    k = qkv[:, :, 1].transpose(0, 2, 1, 3)
    v = qkv[:, :, 2].transpose(0, 2, 1, 3)
    nb = -(-S // BLK)
    pad = nb * BLK - S
    kb = jnp.pad(k, ((0, 0), (0, 0), (0, pad), (0, 0))).reshape(B, H, nb, BLK, Dh)
    vb = jnp.pad(v, ((0, 0), (0, 0), (0, pad), (0, 0))).reshape(B, H, nb, BLK, Dh)
    k_mean = kb.astype(jnp.float32).mean(axis=3)
    gate = jnp.einsum('bhsd,bhnd->bhsn', q.astype(jnp.float32), k_mean)
    pos = jnp.arange(S)
    past = jnp.arange(nb)[None, :] < (pos // BLK)[:, None]
    gate = jnp.where(past, gate, -jnp.inf)
    k_sel = min(MOBA_TOPK, nb)
    sel_score, sel_idx = lax.top_k(gate, k_sel)
    sel_valid = jnp.isfinite(sel_score)

    rel_t = rel_bias.T
    bi = jnp.arange(B)[:, None, None, None]
    hi = jnp.arange(H)[None, :, None, None]
    scale = Dh ** -0.5
    offs = jnp.arange(BLK)

    def attend_chunk(t0):
        qc = lax.dynamic_slice_in_dim(q, t0, QC, axis=2)
        idx = lax.dynamic_slice_in_dim(sel_idx, t0, QC, axis=2)
        valid = lax.dynamic_slice_in_dim(sel_valid, t0, QC, axis=2)
        qpos = t0 + jnp.arange(QC)
        kg = kb[bi, hi, idx]
        vg = vb[bi, hi, idx]
        s_sel = jnp.einsum('bhqd,bhqnjd->bhqnj', qc, kg).astype(jnp.float32) * scale
        rel_sel = qpos[None, None, :, None, None] - (idx[..., None] * BLK + offs)
        s_sel = jnp.where(valid[..., None],
                          s_sel + rel_t[hi[..., None], t5_bucket(rel_sel)], -jnp.inf)
        own = t0 // BLK
        ko = lax.dynamic_index_in_dim(kb, own, axis=2, keepdims=False)
        vo = lax.dynamic_index_in_dim(vb, own, axis=2, keepdims=False)
        rel_own = qpos[:, None] - (own * BLK + offs)[None, :]
        s_own = (jnp.einsum('bhqd,bhjd->bhqj', qc, ko).astype(jnp.float32) * scale
                 + rel_t[:, t5_bucket(rel_own)])
        s_own = jnp.where(rel_own >= 0, s_own, -jnp.inf)
        logits = jnp.concatenate([s_sel.reshape(B, H, QC, k_sel * BLK), s_own], axis=-1)
        p = jax.nn.softmax(logits, axis=-1).astype(v.dtype)
        p_sel = p[..., :k_sel * BLK].reshape(B, H, QC, k_sel, BLK)
        p_own = p[..., k_sel * BLK:]
        return (jnp.einsum('bhqnj,bhqnjd->bhqd', p_sel, vg)
                + jnp.einsum('bhqj,bhjd->bhqd', p_own, vo))

    o = lax.map(attend_chunk, jnp.arange(0, S, QC))
    o = o.transpose(1, 0, 3, 2, 4).reshape(B, S, H * Dh)
    return o @ w_out


def gmlp_mixer(x, w_in, ln_g, ln_b, w_s, b_s, w_out):
    B, S, _ = x.shape
    z = jax.nn.gelu(x @ w_in, approximate=False)
    u, v = jnp.split(z, 2, axis=-1)
    v = layer_norm(v, ln_g, ln_b)
    gw = B_WIDTH // B_GROUPS
    vc = v.reshape(B, S // B_CHUNK, B_CHUNK, B_GROUPS, gw)
    ws = w_s * jnp.tril(jnp.ones((B_CHUNK, B_CHUNK), w_s.dtype))
    mixed = jnp.einsum('gts,bcsge->bctge', ws, vc) + b_s.T[None, None, :, :, None]
    y = u * mixed.reshape(B, S, B_WIDTH)
    return y @ w_out


def gla_mixer(x, w_in, w_gk_up, b_gk, norm_g, w_out):
    B, S, _ = x.shape
    proj = x @ w_in
    splits = [C_KEY_DIM, 2 * C_KEY_DIM, 2 * C_KEY_DIM + C_VAL_DIM, 2 * C_KEY_DIM + 2 * C_VAL_DIM]
    q, k, v, g, gk_low = jnp.split(proj, splits, axis=-1)
    log_a = jax.nn.log_sigmoid((gk_low @ w_gk_up + b_gk).astype(jnp.float32)) / C_GATE_NORMALIZER
    nc = S // C_CHUNK

    def heads(t, dh):
        return t.astype(jnp.float32).reshape(B, nc, C_CHUNK, C_HEADS, dh).transpose(1, 0, 3, 2, 4)

    qh = heads(q, C_DK) * (C_DK ** -0.5)
    kh = heads(k, C_DK)
    vh = heads(v, C_DV)
    ah = heads(log_a, C_DK)
    causal = jnp.tril(jnp.ones((C_CHUNK, C_CHUNK), bool))

    def step(state, inp):
        qc, kc, vc, ac = inp
        b = jnp.cumsum(ac, axis=2)
        o_inter = jnp.einsum('bhck,bhkv->bhcv', qc * jnp.exp(b), state)
        diff = b[:, :, :, None, :] - b[:, :, None, :, :]
        diff = jnp.where(causal[None, None, :, :, None], diff, -jnp.inf)
        att = jnp.einsum('bhik,bhjk,bhijk->bhij', qc, kc, jnp.exp(diff))
        o_intra = jnp.einsum('bhij,bhjv->bhiv', att, vc)
        b_last = b[:, :, -1:, :]
        new_state = (jnp.exp(b_last[:, :, 0, :])[..., None] * state
                     + jnp.einsum('bhck,bhcv->bhkv', kc * jnp.exp(b_last - b), vc))
        return new_state, o_inter + o_intra

    state0 = jnp.zeros((B, C_HEADS, C_DK, C_DV), jnp.float32)
    _, o = lax.scan(step, state0, (qh, kh, vh, ah))
    o = o.transpose(1, 0, 3, 2, 4).reshape(B, S, C_HEADS, C_DV)
    o = rms_norm(o, norm_g) * jax.nn.silu(g.reshape(B, S, C_HEADS, C_DV).astype(jnp.float32))
    return o.reshape(B, S, C_VAL_DIM).astype(x.dtype) @ w_out


def moe_ffn(x, w_router, b_router, w_gu, b_gu, w_down, b_down):
    B, S, D = x.shape
    T = B * S
    xt = x.reshape(T, D)
    logits = (xt @ w_router + b_router).astype(jnp.float32)
    top_logit, top_e = lax.top_k(logits, TOP_K)
    gate = jax.nn.softmax(top_logit, axis=-1)
    n_assign = T * TOP_K
    flat_e = top_e.reshape(-1)
    flat_tok = jnp.arange(n_assign) // TOP_K
    order = jnp.argsort(flat_e)
    e_sorted = flat_e[order]
    tok_sorted = flat_tok[order]
    gate_sorted = gate.reshape(-1)[order]
    counts = jnp.zeros((N_EXPERTS,), jnp.int32).at[flat_e].add(1)
    padded = (counts + MOE_ROW_BLOCK - 1) // MOE_ROW_BLOCK * MOE_ROW_BLOCK
    start = jnp.cumsum(counts) - counts
    pend = jnp.cumsum(padded)
    pstart = pend - padded
    dest = pstart[e_sorted] + (jnp.arange(n_assign) - start[e_sorted])
    n_blocks = -(-n_assign // MOE_ROW_BLOCK) + N_EXPERTS
    n_rows = n_blocks * MOE_ROW_BLOCK
    row_tok = jnp.full((n_rows,), T, jnp.int32).at[dest].set(tok_sorted)
    x_pad = jnp.concatenate([xt, jnp.zeros((1, D), xt.dtype)], axis=0)
    xs = x_pad[row_tok].reshape(n_blocks, MOE_ROW_BLOCK, D)
    block_e = jnp.minimum(
        jnp.searchsorted(pend, jnp.arange(n_blocks) * MOE_ROW_BLOCK, side='right'), N_EXPERTS - 1)

    def expert_block(args):
        xb, e = args
        h = xb @ w_gu[e] + b_gu[e]
        h_gate, h_up = jnp.split(h, 2, axis=-1)
        h_gate = jnp.minimum(h_gate, SWIGLU_LIMIT)
        h_up = jnp.clip(h_up, -SWIGLU_LIMIT, SWIGLU_LIMIT)
        act = h_gate * jax.nn.sigmoid(SWIGLU_ALPHA * h_gate) * (h_up + 1.0)
        return act @ w_down[e] + b_down[e]

    ys = lax.map(expert_block, (xs, block_e)).reshape(n_rows, D)
    y_assign = ys[dest] * gate_sorted[:, None].astype(ys.dtype)
    y = jax.ops.segment_sum(y_assign, tok_sorted, num_segments=T)
    return y.reshape(B, S, D)


def setup_inputs(seed: int = 0) -> dict:
    key = jax.random.key(seed)
    ks = jax.random.split(key, 23)
    f32 = jnp.float32
    D = D_MODEL

    def nrm(k, shape, scale):
        return jax.random.normal(k, shape, f32) * scale

    return {
        "x": nrm(ks[0], (BATCH, SEQ, D), 1.0),
        "rel_bias": nrm(ks[1], (REL_BUCKETS, A_HEADS), 0.5),
        "a_w_in": nrm(ks[2], (N_LAYERS_A, D, 3 * D), D ** -0.5),
        "a_w_out": nrm(ks[3], (N_LAYERS_A, D, D), D ** -0.5 * DEEPNORM_BETA),
        "b_w_in": nrm(ks[4], (N_LAYERS_B, D, 2 * B_WIDTH), D ** -0.5),
        "b_ln_g": 1.0 + nrm(ks[5], (N_LAYERS_B, B_WIDTH), 0.02),
        "b_ln_b": nrm(ks[6], (N_LAYERS_B, B_WIDTH), 0.02),
        "b_w_s": nrm(ks[7], (N_LAYERS_B, B_GROUPS, B_CHUNK, B_CHUNK), B_CHUNK ** -0.5),
        "b_b_s": 1.0 + nrm(ks[8], (N_LAYERS_B, B_GROUPS, B_CHUNK), 0.02),
        "b_w_out": nrm(ks[9], (N_LAYERS_B, B_WIDTH, D), B_WIDTH ** -0.5 * DEEPNORM_BETA),
        "c_w_in": nrm(ks[10], (N_LAYERS_C, D, C_IN_WIDTH), D ** -0.5),
        "c_w_gk_up": nrm(ks[11], (N_LAYERS_C, C_GATE_RANK, C_KEY_DIM), C_GATE_RANK ** -0.5),
        "c_b_gk": nrm(ks[12], (N_LAYERS_C, C_KEY_DIM), 0.1),
        "c_norm_g": 1.0 + nrm(ks[13], (N_LAYERS_C, C_DV), 0.02),
        "c_w_out": nrm(ks[14], (N_LAYERS_C, C_VAL_DIM, D), C_VAL_DIM ** -0.5 * DEEPNORM_BETA),
        "ln_g": 1.0 + nrm(ks[15], (DEPTH, 2, D), 0.02),
        "ln_b": nrm(ks[16], (DEPTH, 2, D), 0.02),
        "moe_w_router": nrm(ks[17], (DEPTH, D, N_EXPERTS), D ** -0.5),
        "moe_b_router": nrm(ks[18], (DEPTH, N_EXPERTS), 0.01),
        "moe_w_gate_up": nrm(ks[19], (DEPTH, N_EXPERTS, D, 2 * D_EXPERT), D ** -0.5),
        "moe_b_gate_up": nrm(ks[20], (DEPTH, N_EXPERTS, 2 * D_EXPERT), 0.01),
        "moe_w_down": nrm(ks[21], (DEPTH, N_EXPERTS, D_EXPERT, D), D_EXPERT ** -0.5 * DEEPNORM_BETA),
        "moe_b_down": nrm(ks[22], (DEPTH, N_EXPERTS, D), 0.01),
    }


def reference(x, rel_bias, a_w_in, a_w_out, b_w_in, b_ln_g, b_ln_b, b_w_s, b_b_s, b_w_out,
              c_w_in, c_w_gk_up, c_b_gk, c_norm_g, c_w_out, ln_g, ln_b,
              moe_w_router, moe_b_router, moe_w_gate_up, moe_b_gate_up, moe_w_down, moe_b_down):
    h = x
    for i in range(DEPTH):
        j = i // N_MIXERS
        mixer = i % N_MIXERS
        if mixer == 0:
            t = moba_attention(h, a_w_in[j], a_w_out[j], rel_bias)
        elif mixer == 1:
            t = gmlp_mixer(h, b_w_in[j], b_ln_g[j], b_ln_b[j], b_w_s[j], b_b_s[j], b_w_out[j])
        else:
            t = gla_mixer(h, c_w_in[j], c_w_gk_up[j], c_b_gk[j], c_norm_g[j], c_w_out[j])
        h = layer_norm(DEEPNORM_ALPHA * h + t, ln_g[i, 0], ln_b[i, 0])
        f = moe_ffn(h, moe_w_router[i], moe_b_router[i], moe_w_gate_up[i], moe_b_gate_up[i],
                    moe_w_down[i], moe_b_down[i])
        h = layer_norm(DEEPNORM_ALPHA * h + f, ln_g[i, 1], ln_b[i, 1])
    return h
```

```python
import math
from contextlib import ExitStack
import numpy as np
import concourse.bass as bass
import concourse.mybir as mybir
from concourse.bass_utils import run_bass_kernel_spmd

F32 = mybir.dt.float32
BF16 = mybir.dt.bfloat16
ALU = mybir.AluOpType
AF = mybir.ActivationFunctionType
AX = mybir.AxisListType

ENGS = ("pe", "act", "dve", "pool", "sp")

D = 1024
S = 2048
NT = 16
DEPTH = 4
ALPHA = (2.0 * DEPTH) ** 0.25
EPS = 1e-5
NE = 32
NEG = -30000.0


class Buf:
    __slots__ = ("name", "lw", "rd")

    def __init__(self, name="b"):
        self.name = name
        self.lw = None
        self.rd = []


class Op:
    __slots__ = ("eng", "fn", "dma", "deps", "sem", "val", "needs_sig", "epoch")

    def __init__(self, eng, fn, dma, epoch=0):
        self.epoch = epoch
        self.eng = eng
        self.fn = fn
        self.dma = dma
        self.deps = []
        self.sem = None
        self.val = 0
        self.needs_sig = False


class Prog:
    def __init__(self, nc, n_dma_sems=(("sp", 24), ("pool", 24), ("act", 8))):
        self.nc = nc
        self.pending = {e: [] for e in ENGS}
        self.esem = {}
        self.ecount = {e: 0 for e in ENGS}
        self._ctx = []
        for e in ENGS:
            cm = nc.semaphore("s_" + e)
            self.esem[e] = cm.__enter__()
            self._ctx.append(cm)
        self.dsems = {}
        self.dcount = {}
        self.dlast = {}
        self.dnext = {}
        for q, n in n_dma_sems:
            lst = []
            for i in range(n):
                cm = nc.semaphore("d_%s_%d" % (q, i))
                lst.append(cm.__enter__())
                self._ctx.append(cm)
            self.dsems[q] = lst
            self.dcount[q] = [0] * n
            self.dlast[q] = [None] * n
            self.dnext[q] = 0
        self.known = {e: {} for e in ENGS}
        self.all_dma_since_barrier = []
        self.n_ops = 0
        self.epoch = 0

    def close(self):
        for cm in reversed(self._ctx):
            cm.__exit__(None, None, None)

    def add(self, eng, fn, R=(), W=(), dma=False):
        op = Op(eng, fn, dma, self.epoch)
        self.n_ops += 1
        deps = []
        rset = set(id(b) for b in R)
        for b in R:
            if b.lw is not None:
                deps.append((b.lw, True))
        for b in W:
            if b.lw is not None:
                deps.append((b.lw, id(b) in rset))
            for r in b.rd:
                deps.append((r, False))
        seen = set()
        for d, raw in deps:
            if d is op or d.epoch < self.epoch:
                continue
            key = id(d)
            if d.dma or dma:
                pass
            elif d.eng == eng:
                if eng == "pe" or not raw:
                    continue
            if key in seen:
                continue
            seen.add(key)
            op.deps.append(d)
            d.needs_sig = True
        for b in R:
            b.rd.append(op)
        for b in W:
            b.lw = op
            b.rd = []
        if dma:
            q = eng
            k = self.dnext[q]
            self.dnext[q] = (k + 1) % len(self.dsems[q])
            prev = self.dlast[q][k]
            if prev is not None and prev.epoch == self.epoch:
                op.deps.append(prev)
            self.dcount[q][k] += 16
            op.sem = self.dsems[q][k]
            op.val = self.dcount[q][k]
            self.dlast[q][k] = op
            op.needs_sig = True
            self.all_dma_since_barrier.append(op)
        self.pending[eng].append(op)
        return op

    def barrier(self):
        lasts = []
        for e in ENGS:
            for o in reversed(self.pending[e]):
                if not o.dma and o.fn is not None:
                    lasts.append(o)
                    break
        dmas = list(self.all_dma_since_barrier)
        self.all_dma_since_barrier = []
        for e in ENGS:
            op = Op(e, None, False, self.epoch)
            for d in lasts:
                if d.eng != e:
                    op.deps.append(d)
                    d.needs_sig = True
            for d in dmas:
                op.deps.append(d)
            self.pending[e].append(op)
        self.epoch += 1

    def flush(self):
        nc = self.nc
        self.barrier()
        for e in ENGS:
            for op in self.pending[e]:
                if op.dma or op.fn is None:
                    continue
                if op.needs_sig:
                    self.ecount[e] += 1
                    op.sem = self.esem[e]
                    op.val = self.ecount[e]
        pend = self.pending
        self.pending = {e: [] for e in ENGS}
        known = self.known

        def emit(e, eng):
            kn = known[e]
            for op in pend[e]:
                need = {}
                for d in op.deps:
                    assert d.sem is not None, "dep without signal"
                    sid = id(d.sem)
                    if kn.get(sid, 0) >= d.val:
                        continue
                    if sid not in need or need[sid][1] < d.val:
                        need[sid] = (d.sem, d.val)
                for sid, (sem, val) in need.items():
                    eng.wait_ge(sem, val)
                    kn[sid] = val
                if op.fn is None:
                    continue
                ins = op.fn(eng)
                if op.dma:
                    ins.then_inc(op.sem, 16)
                elif op.needs_sig:
                    ins.then_inc(op.sem, 1)

        with nc.Block() as blk:
            @blk.tensor
            def _(eng):
                emit("pe", eng)

            @blk.scalar
            def _(eng):
                emit("act", eng)

            @blk.vector
            def _(eng):
                emit("dve", eng)

            @blk.gpsimd
            def _(eng):
                emit("pool", eng)

            @blk.sync
            def _(eng):
                emit("sp", eng)


class Ctx:
    pass


def bcast_rows(ap_row, nparts=128):
    return ap_row.partition_broadcast(nparts)


def ln_tile(C, t, lng, lnb, blnp, router=None):
    P, nc = C.P, C.nc
    h, hT = C.h, C.hT
    bh, bhT = C.bh[t], C.bhT[t]
    st, mv, rs, nb = C.ln_st, C.ln_mv, C.ln_rs, C.ln_nb
    b_st = C.b_lnst
    for c in range(2):
        P.add("dve", lambda e, c=c: e.bn_stats(out=st[:, c, :], in_=h[:, t, c * 512:(c + 1) * 512]), R=[bh], W=[b_st])
    P.add("dve", lambda e: e.bn_aggr(out=mv[:], in_=st[:]), R=[b_st], W=[b_st])
    P.add("act", lambda e: e.activation(out=rs[:], in_=mv[:, 1:2], func=AF.Sqrt, bias=EPS, scale=1.0), R=[b_st], W=[b_st])
    P.add("dve", lambda e: e.reciprocal(out=rs[:], in_=rs[:]), R=[b_st], W=[b_st])
    P.add("dve", lambda e: e.tensor_scalar(out=h[:, t, :], in0=h[:, t, :], scalar1=mv[:, 0:1], scalar2=rs[:, 0:1],
                                           op0=ALU.subtract, op1=ALU.mult), R=[bh, b_st], W=[bh])
    P.add("pool", lambda e: e.tensor_tensor(out=h[:, t, :], in0=h[:, t, :], in1=lng[:], op=ALU.mult), R=[bh, blnp], W=[bh])
    P.add("pool", lambda e: e.tensor_tensor(out=h[:, t, :], in0=h[:, t, :], in1=lnb[:], op=ALU.add), R=[bh, blnp], W=[bh])
    transpose_tile(C, t, router)


def transpose_tile(C, t, router=None):
    P = C.P
    h, hT = C.h, C.hT
    bh, bhT = C.bh[t], C.bhT[t]
    for half in range(2):
        ps, bps = C.ps[C.tp_rr % 2], C.bps[C.tp_rr % 2]
        C.tp_rr += 1
        for kk in range(4):
            k = half * 4 + kk
            P.add("pe", lambda e, k=k, kk=kk, ps=ps: e.transpose(out=ps[:, kk * 128:(kk + 1) * 128], in_=h[:, t, k * 128:(k + 1) * 128],
                                                                  identity=C.ident[:]), R=[bh, C.b_const], W=[bps])
        P.add("act", lambda e, ps=ps, half=half: e.copy(out=hT[:, half * 4:half * 4 + 4, t * 128:(t + 1) * 128],
                                                        in_=ps[:].rearrange("p (k c) -> p k c", k=4)), R=[bps], W=[bhT])
        if router is not None:
            P.add("act", lambda e, ps=ps, half=half: e.copy(out=C.hT32[:, half * 4:half * 4 + 4, :],
                                                            in_=ps[:].rearrange("p (k c) -> p k c", k=4)), R=[bps], W=[C.b_hT32])
    if router is not None:
        router_tile(C, t, router)


def router_tile(C, t, R_):
    P = C.P
    ps, bps = C.ps[2], C.bps[2]
    wr, brb, b_r = R_["wr"], R_["brb"], R_["b"]
    lg, m8, ex, msk, sm = C.r_lg, C.r_m8, C.r_ex, C.r_msk, C.r_sm
    b = C.b_rt
    for k in range(8):
        P.add("pe", lambda e, k=k: e.matmul(ps[:, 0:NE], lhsT=C.hT32[:, k, :], rhs=wr[:, k, :], start=(k == 0), stop=(k == 7)),
              R=[C.b_hT32, b_r], W=[bps])
    P.add("dve", lambda e: e.tensor_tensor(out=lg[:], in0=ps[:, 0:NE], in1=brb[:], op=ALU.add), R=[bps, b_r], W=[b])
    P.add("dve", lambda e: e.max(out=m8[:], in_=lg[:]), R=[b], W=[b])
    P.add("dve", lambda e: e.tensor_scalar(out=msk[:], in0=lg[:], scalar1=m8[:, 3:4], scalar2=None, op0=ALU.is_ge), R=[b], W=[b])
    P.add("dve", lambda e: e.tensor_scalar(out=sm[:, 0:1], in0=m8[:, 0:1], scalar1=-1.0, scalar2=None, op0=ALU.mult), R=[b], W=[b])
    P.add("act", lambda e: e.activation(out=ex[:], in_=lg[:], func=AF.Exp, bias=sm[:, 0:1], scale=1.0), R=[b], W=[b])
    P.add("dve", lambda e: e.tensor_tensor(out=ex[:], in0=ex[:], in1=msk[:], op=ALU.mult), R=[b], W=[b])
    P.add("dve", lambda e: e.reduce_sum(out=sm[:, 1:2], in_=ex[:], axis=AX.X), R=[b], W=[b])
    P.add("dve", lambda e: e.reciprocal(out=sm[:, 1:2], in_=sm[:, 1:2]), R=[b], W=[b])
    P.add("dve", lambda e: e.tensor_scalar(out=C.gates[:, t, :], in0=ex[:], scalar1=sm[:, 1:2], scalar2=None, op0=ALU.mult),
          R=[b], W=[C.b_gates[t]])


def load_ln_params(C, lng, lnb, blnp, g_row, b_row):
    P = C.P
    P.add("sp", lambda e: e.dma_start(out=lng[:], in_=bcast_rows(g_row)), W=[blnp], dma=True)
    P.add("sp", lambda e: e.dma_start(out=lnb[:], in_=bcast_rows(b_row)), W=[blnp], dma=True)


def moe_phase(C, li, T):
    P, nc = C.P, C.nc
    h, hT = C.h, C.hT
    with ExitStack() as es:
        w1g = es.enter_context(nc.sbuf_tensor("m_w1g_%d" % li, [128, 2, 8, 512], BF16))
        w1u = es.enter_context(nc.sbuf_tensor("m_w1u_%d" % li, [128, 2, 8, 512], BF16))
        w2 = es.enter_context(nc.sbuf_tensor("m_w2_%d" % li, [128, 2, 4, 1024], BF16))
        actT = es.enter_context(nc.sbuf_tensor("m_actT_%d" % li, [128, 2, 4, 512], BF16))
        g1 = es.enter_context(nc.sbuf_tensor("m_g1_%d" % li, [128, 2, 512], F32))
        sg = es.enter_context(nc.sbuf_tensor("m_sg_%d" % li, [128, 2, 512], F32))
        u1 = es.enter_context(nc.sbuf_tensor("m_u1_%d" % li, [128, 2, 512], F32))
        braw = es.enter_context(nc.sbuf_tensor("m_braw_%d" % li, [128, 4, 128], F32))
        bguT = es.enter_context(nc.sbuf_tensor("m_bguT_%d" % li, [128, 512], F32))
        bd = es.enter_context(nc.sbuf_tensor("m_bd_%d" % li, [NE, D], F32))
        gT = es.enter_context(nc.sbuf_tensor("m_gT_%d" % li, [NE, 128], F32))
        lng = es.enter_context(nc.sbuf_tensor("m_lng_%d" % li, [128, D], F32))
        lnb = es.enter_context(nc.sbuf_tensor("m_lnb_%d" % li, [128, D], F32))
        b_w = [Buf("w%d" % i) for i in range(2)]
        b_act = [Buf() for _ in range(2)]
        b_g1 = [Buf() for _ in range(2)]
        b_sg = [Buf() for _ in range(2)]
        b_u1 = [Buf() for _ in range(2)]
        b_misc = Buf()
        b_gT = Buf()
        blnp = Buf()
        load_ln_params(C, lng, lnb, blnp, T["ln_g"][li, 1, :], T["ln_b"][li, 1, :])
        P.add("sp", lambda e: e.dma_start(out=braw[:], in_=T["moe_b_gate_up_%d" % li].rearrange("e (c p) -> (e c) p", p=128)
                                          .rearrange("(b r) p -> r b p", r=128)), W=[b_misc], dma=True)
        P.add("sp", lambda e: e.dma_start(out=bd[:], in_=T["moe_b_down_%d" % li]), W=[b_misc], dma=True)
        for bb in range(4):
            ps, bps = C.ps[bb % 2], C.bps[bb % 2]
            P.add("pe", lambda e, bb=bb, ps=ps: e.transpose(out=ps[:, 0:128], in_=braw[:, bb, :], identity=C.ident[:]),
                  R=[b_misc, C.b_const], W=[bps])
            P.add("act", lambda e, bb=bb, ps=ps: e.copy(out=bguT[:, bb * 128:(bb + 1) * 128], in_=ps[:, 0:128]), R=[bps], W=[b_misc])
        for t in range(NT):
            ps, bps = C.ps[2], C.bps[2]
            P.add("pe", lambda e, t=t: e.transpose(out=ps[0:NE, 0:128], in_=C.gates[:, t, :], identity=C.ident[:]),
                  R=[C.b_gates[t], C.b_const], W=[bps])
            P.add("act", lambda e: e.copy(out=gT[:], in_=ps[0:NE, 0:128]), R=[bps], W=[b_gT])
            for n in range(2):
                po, bpo = C.ps[n], C.bps[n]
                P.add("pe", lambda e, n=n, po=po: e.matmul(po[:], lhsT=gT[:], rhs=bd[:, n * 512:(n + 1) * 512], start=True, stop=True),
                      R=[b_gT, b_misc], W=[bpo])
                P.add("dve", lambda e, n=n, po=po, t=t: e.scalar_tensor_tensor(out=h[:, t, n * 512:(n + 1) * 512], in0=h[:, t, n * 512:(n + 1) * 512],
                                                                               scalar=ALPHA, in1=po[:], op0=ALU.mult, op1=ALU.add),
                      R=[bpo, C.bh[t]], W=[C.bh[t]])
        wgu = T["moe_w_gate_up_%d" % li]
        wd = T["moe_w_down_%d" % li]
        it = 0
        for ex in range(C.n_experts):
            for hf in range(2):
                s = it % 2
                it += 1
                bw = b_w[s]
                P.add("pool", lambda e, s=s, ex=ex, hf=hf: e.dma_start(
                    out=w1g[:, s], in_=wgu[ex, :, hf * 512:(hf + 1) * 512].rearrange("(k p) n -> p k n", p=128)), W=[bw], dma=True)
                P.add("pool", lambda e, s=s, ex=ex, hf=hf: e.dma_start(
                    out=w1u[:, s], in_=wgu[ex, :, 1024 + hf * 512:1024 + (hf + 1) * 512].rearrange("(k p) n -> p k n", p=128)), W=[bw], dma=True)
                P.add("pool", lambda e, s=s, ex=ex, hf=hf: e.dma_start(
                    out=w2[:, s], in_=wd[ex, hf * 512:(hf + 1) * 512, :].rearrange("(j p) n -> p j n", p=128)), W=[bw], dma=True)
                for g in range(4):
                    a = C.act_rr % 2
                    C.act_rr += 1
                    bhTg = [C.bhT[g * 4 + i] for i in range(4)]
                    for j in range(4):
                        q = C.q_rr % 2
                        C.q_rr += 1
                        pg, bpg = C.ps[3 + q], C.bps[3 + q]
                        pu, bpu = C.ps[5 + q], C.bps[5 + q]
                        for k in range(8):
                            P.add("pe", lambda e, k=k, j=j, s=s, g=g, pg=pg: e.matmul(pg[:], lhsT=w1g[:, s, k, j * 128:(j + 1) * 128],
                                                                                    rhs=hT[:, k, g * 512:(g + 1) * 512], start=(k == 0), stop=(k == 7)),
                                  R=[bw] + bhTg, W=[bpg])
                        for k in range(8):
                            P.add("pe", lambda e, k=k, j=j, s=s, g=g, pu=pu: e.matmul(pu[:], lhsT=w1u[:, s, k, j * 128:(j + 1) * 128],
                                                                                    rhs=hT[:, k, g * 512:(g + 1) * 512], start=(k == 0), stop=(k == 7)),
                                  R=[bw] + bhTg, W=[bpu])
                        cg = ex * 16 + hf * 4 + j
                        cu = ex * 16 + 8 + hf * 4 + j
                        P.add("dve", lambda e, q=q, pg=pg, cg=cg: e.tensor_scalar(out=g1[:, q, :], in0=pg[:], scalar1=bguT[:, cg:cg + 1], scalar2=7.0,
                                                                                 op0=ALU.add, op1=ALU.min), R=[bpg, b_misc], W=[b_g1[q]])
                        P.add("act", lambda e, q=q: e.activation(out=sg[:, q, :], in_=g1[:, q, :], func=AF.Sigmoid, scale=1.702), R=[b_g1[q]], W=[b_sg[q]])
                        P.add("dve", lambda e, q=q, pu=pu, cu=cu: e.tensor_scalar(out=u1[:, q, :], in0=pu[:], scalar1=bguT[:, cu:cu + 1], scalar2=7.0,
                                                                                 op0=ALU.add, op1=ALU.min), R=[bpu, b_misc], W=[b_u1[q]])
                        P.add("pool", lambda e, q=q: e.tensor_scalar(out=u1[:, q, :], in0=u1[:, q, :], scalar1=-7.0, scalar2=1.0,
                                                                     op0=ALU.max, op1=ALU.add), R=[b_u1[q]], W=[b_u1[q]])
                        P.add("pool", lambda e, q=q: e.tensor_tensor(out=sg[:, q, :], in0=sg[:, q, :], in1=g1[:, q, :], op=ALU.mult),
                              R=[b_sg[q], b_g1[q]], W=[b_sg[q]])
                        P.add("dve", lambda e, q=q, a=a, j=j: e.tensor_tensor(out=actT[:, a, j, :], in0=sg[:, q, :], in1=u1[:, q, :], op=ALU.mult),
                              R=[b_sg[q], b_u1[q]], W=[b_act[a]])
                    for tt in range(4):
                        t = g * 4 + tt
                        for n in range(2):
                            y = C.y_rr % 3
                            C.y_rr += 1
                            py, bpy = C.ps[(0, 1, 7)[y]], C.bps[(0, 1, 7)[y]]
                            for j in range(4):
                                P.add("pe", lambda e, j=j, a=a, s=s, tt=tt, n=n, py=py: e.matmul(py[:], lhsT=actT[:, a, j, tt * 128:(tt + 1) * 128],
                                                                                               rhs=w2[:, s, j, n * 512:(n + 1) * 512], start=(j == 0), stop=(j == 3)),
                                      R=[b_act[a], bw], W=[bpy])
                            P.add("dve", lambda e, t=t, n=n, py=py, ex=ex: e.scalar_tensor_tensor(
                                out=h[:, t, n * 512:(n + 1) * 512], in0=py[:], scalar=C.gates[:, t, ex:ex + 1], in1=h[:, t, n * 512:(n + 1) * 512],
                                op0=ALU.mult, op1=ALU.add), R=[bpy, C.b_gates[t], C.bh[t]], W=[C.bh[t]])
        for t in range(NT):
            ln_tile(C, t, lng, lnb, blnp, router=None)
        P.flush()


def out_proj_ln(C, li, T, srcT, nchunks, w_dram, R_):
    P, nc = C.P, C.nc
    h = C.h
    with ExitStack() as es:
        wo = es.enter_context(nc.sbuf_tensor("o_w_%d" % li, [128, nchunks, D], BF16))
        lng = es.enter_context(nc.sbuf_tensor("o_lng_%d" % li, [128, D], F32))
        lnb = es.enter_context(nc.sbuf_tensor("o_lnb_%d" % li, [128, D], F32))
        bw, blnp = Buf(), Buf()
        P.add("pool", lambda e: e.dma_start(out=wo[:], in_=w_dram.rearrange("(k p) n -> p k n", p=128)), W=[bw], dma=True)
        load_ln_params(C, lng, lnb, blnp, T["ln_g"][li, 0, :], T["ln_b"][li, 0, :])
        for t in range(NT):
            for n in range(2):
                po, bpo = C.ps[3 + n + 2 * (t % 2)], C.bps[3 + n + 2 * (t % 2)]
                for f in range(nchunks):
                    P.add("pe", lambda e, f=f, n=n, t=t, po=po: e.matmul(po[:], lhsT=srcT[:, f, t * 128:(t + 1) * 128], rhs=wo[:, f, n * 512:(n + 1) * 512],
                                                                      start=(f == 0), stop=(f == nchunks - 1)), R=[C.b_srcT, bw], W=[bpo])
                P.add("dve", lambda e, n=n, t=t, po=po: e.scalar_tensor_tensor(out=h[:, t, n * 512:(n + 1) * 512], in0=h[:, t, n * 512:(n + 1) * 512],
                                                                             scalar=ALPHA, in1=po[:], op0=ALU.mult, op1=ALU.add),
                      R=[bpo, C.bh[t]], W=[C.bh[t]])
            ln_tile(C, t, lng, lnb, blnp, router=R_)
        P.flush()


def moba_phase(C, li, T, R_):
    P, nc = C.P, C.nc
    h, hT = C.h, C.hT
    j = li // 3
    w_in = T["a_w_in_%d" % j]
    H = 8
    SCALE = 128 ** -0.5
    BIG = 30000.0
    with nc.sbuf_tensor("a_aT_%d" % li, [128, 8, S], BF16) as aT:
        C.b_srcT = Buf("aT")
        with ExitStack() as es:
            wq = es.enter_context(nc.sbuf_tensor("a_wq_%d" % li, [128, 2, 8, 384], BF16))
            qT = es.enter_context(nc.sbuf_tensor("a_qT_%d" % li, [128, 2, S], BF16))
            kT = es.enter_context(nc.sbuf_tensor("a_kT_%d" % li, [128, 2, S], BF16))
            V = es.enter_context(nc.sbuf_tensor("a_V_%d" % li, [128, 2, NT, 128], BF16))
            kms = es.enter_context(nc.sbuf_tensor("a_kms_%d" % li, [128, 8], F32))
            kmb = es.enter_context(nc.sbuf_tensor("a_kmb_%d" % li, [128, 8], BF16))
            g2 = es.enter_context(nc.sbuf_tensor("a_g2_%d" % li, [128, 128], F32))
            m8 = es.enter_context(nc.sbuf_tensor("a_m8_%d" % li, [128, 8], F32))
            bm = es.enter_context(nc.sbuf_tensor("a_bm_%d" % li, [128, 128], F32))
            cm = es.enter_context(nc.sbuf_tensor("a_cm_%d" % li, [128, 128], F32))
            vm = es.enter_context(nc.sbuf_tensor("a_vm_%d" % li, [128, 128], F32))
            vm2 = es.enter_context(nc.sbuf_tensor("a_vm2_%d" % li, [128, 128], F32))
            rel = es.enter_context(nc.sbuf_tensor("a_rel_%d" % li, [128, 8, 2, 128], F32))
            caus = es.enter_context(nc.sbuf_tensor("a_caus_%d" % li, [128, 128], F32))
            cb = es.enter_context(nc.sbuf_tensor("a_cb_%d" % li, [128, 8], F32))
            sc = es.enter_context(nc.sbuf_tensor("a_s_%d" % li, [128, 2, S], F32))
            pb = es.enter_context(nc.sbuf_tensor("a_p_%d" % li, [128, S], BF16))
            pT = es.enter_context(nc.sbuf_tensor("a_pT_%d" % li, [128, 2, 4, 128], BF16))
            stt = es.enter_context(nc.sbuf_tensor("a_st_%d" % li, [128, 2, 4], F32))
            b_wq = [Buf() for _ in range(2)]
            b_q = [Buf() for _ in range(2)]
            b_k = [Buf() for _ in range(2)]
            b_v = [Buf() for _ in range(2)]
            b_km, b_gate, b_cst = Buf(), Buf(), Buf()
            b_s = [Buf() for _ in range(2)]
            b_p = [Buf()] * 2
            b_pT = [Buf() for _ in range(2)]
            b_st = [Buf() for _ in range(2)]
            SK = ""
            if "rel" not in SK:
                P.add("sp", lambda e: e.dma_start(out=rel[:], in_=T["relT"].rearrange("h d i j -> i h d j")), W=[b_cst], dma=True)
            P.add("sp", lambda e: e.dma_start(out=caus[:], in_=T["causal"]), W=[b_cst], dma=True)
            P.add("sp", lambda e: e.dma_start(out=vm[:], in_=T["vmask"]), W=[b_cst], dma=True)
            P.add("sp", lambda e: e.dma_start(out=vm2[:], in_=T["vmask2"]), W=[b_cst], dma=True)
            if "cb" not in SK:
                P.add("sp", lambda e: e.dma_start(out=cb[:], in_=bcast_rows(T["rel_bias"][31, :])), W=[b_cst], dma=True)
            for hh in range(H):
                P.add("pool", lambda e, hh=hh: e.tensor_tensor(out=rel[:, hh, 0, :], in0=rel[:, hh, 0, :], in1=caus[:], op=ALU.add), R=[b_cst], W=[b_cst])
            allT = list(C.bhT)
            for hh in range(H):
                s = hh % 2
                for c3 in range(3):
                    P.add("pool", lambda e, s=s, c3=c3, hh=hh: e.dma_start(
                        out=wq[:, s, :, c3 * 128:(c3 + 1) * 128],
                        in_=w_in[:, c3 * 1024 + hh * 128:c3 * 1024 + (hh + 1) * 128].rearrange("(k p) n -> p k n", p=128)), W=[b_wq[s]], dma=True)
                for g in range(4):
                    pq, bpq = C.ps[3], C.bps[3]
                    pk, bpk = C.ps[4], C.bps[4]
                    for k in range(8):
                        P.add("pe", lambda e, k=k, g=g, s=s: e.matmul(pq[:], lhsT=wq[:, s, k, 0:128], rhs=hT[:, k, g * 512:(g + 1) * 512],
                                                                    start=(k == 0), stop=(k == 7)), R=[b_wq[s]] + allT[g * 4:g * 4 + 4], W=[bpq])
                    P.add("act", lambda e, g=g, s=s: e.copy(out=qT[:, s, g * 512:(g + 1) * 512], in_=pq[:]), R=[bpq], W=[b_q[s]])
                    for k in range(8):
                        P.add("pe", lambda e, k=k, g=g, s=s: e.matmul(pk[:], lhsT=wq[:, s, k, 128:256], rhs=hT[:, k, g * 512:(g + 1) * 512],
                                                                    start=(k == 0), stop=(k == 7)), R=[b_wq[s]] + allT[g * 4:g * 4 + 4], W=[bpk])
                    for b2 in range(2):
                        P.add("act", lambda e, g=g, s=s, b2=b2: e.activation(out=kT[:, s, g * 512 + b2 * 256:g * 512 + (b2 + 1) * 256],
                                                                           in_=pk[:, b2 * 256:(b2 + 1) * 256], func=AF.Copy,
                                                                           accum_out=kms[:, 2 * g + b2:2 * g + b2 + 1]), R=[bpk], W=[b_k[s], b_km])
                P.add("dve", lambda e: e.tensor_scalar(out=kmb[:], in0=kms[:], scalar1=1.0 / 256.0, scalar2=None, op0=ALU.mult), R=[b_km], W=[b_km])
                for g in range(4):
                    pv, bpv = C.ps[5], C.bps[5]
                    for tt in range(4):
                        t = g * 4 + tt
                        for k in range(8):
                            P.add("pe", lambda e, k=k, t=t, tt=tt, s=s: e.matmul(pv[:, tt * 128:(tt + 1) * 128], lhsT=hT[:, k, t * 128:(t + 1) * 128],
                                                                               rhs=wq[:, s, k, 256:384], start=(k == 0), stop=(k == 7)),
                                  R=[b_wq[s], C.bhT[t]], W=[bpv])
                    P.add("act", lambda e, g=g, s=s: e.copy(out=V[:, s, g * 4:g * 4 + 4, :], in_=pv[:].rearrange("p (a c) -> p a c", a=4)),
                          R=[bpv], W=[b_v[s]])
                MD = 9
                if MD < 2:
                    continue
                pg, bpg = C.ps[6], C.bps[6]
                for qt in range(NT):
                    P.add("pe", lambda e, qt=qt, s=s: e.matmul(pg[:, qt * 8:(qt + 1) * 8], lhsT=qT[:, s, qt * 128:(qt + 1) * 128], rhs=kmb[:],
                                                             start=True, stop=True), R=[b_q[s], b_km], W=[bpg])
                P.add("dve", lambda e: e.tensor_tensor(out=g2[:], in0=pg[:, 0:128], in1=vm[:], op=ALU.add), R=[bpg, b_cst], W=[b_gate])
                for qt in range(NT):
                    P.add("dve", lambda e, qt=qt: e.max(out=m8[:], in_=g2[:, qt * 8:(qt + 1) * 8]), R=[b_gate], W=[b_gate])
                    P.add("dve", lambda e, qt=qt: e.tensor_scalar(out=bm[:, qt * 8:(qt + 1) * 8], in0=g2[:, qt * 8:(qt + 1) * 8], scalar1=m8[:, 2:3],
                                                                  scalar2=BIG, op0=ALU.is_ge, op1=ALU.mult), R=[b_gate], W=[b_gate])
                P.add("dve", lambda e: e.tensor_tensor(out=bm[:], in0=bm[:], in1=vm2[:], op=ALU.add), R=[b_gate, b_cst], W=[b_gate])
                P.add("dve", lambda e, hh=hh: e.tensor_scalar(out=cm[:], in0=bm[:], scalar1=cb[:, hh:hh + 1], scalar2=None, op0=ALU.add),
                      R=[b_gate, b_cst], W=[b_gate])
                for qt in range(NT if MD >= 3 else 0):
                    qb = qt // 2
                    nk = qt + 1
                    z = qt % 2
                    for c in range((nk + 3) // 4):
                        w = min(4, nk - c * 4)
                        pss, bpss = C.ps[3 + (C.q_rr % 2)], C.bps[3 + (C.q_rr % 2)]
                        C.q_rr += 1
                        P.add("pe", lambda e, c=c, w=w, qt=qt, s=s, pss=pss: e.matmul(pss[:, 0:w * 128], lhsT=qT[:, s, qt * 128:(qt + 1) * 128],
                                                                                   rhs=kT[:, s, c * 512:c * 512 + w * 128], start=True, stop=True),
                              R=[b_q[s], b_k[s]], W=[bpss])
                        for i in range(w):
                            kt = c * 4 + i
                            n = kt // 2
                            src = pss[:, i * 128:(i + 1) * 128]
                            dst = sc[:, z, kt * 128:(kt + 1) * 128]
                            if kt == qt:
                                P.add("dve", lambda e, src=src, dst=dst, hh=hh: e.scalar_tensor_tensor(out=dst, in0=src, scalar=SCALE, in1=rel[:, hh, 0, :],
                                                                                                     op0=ALU.mult, op1=ALU.add), R=[bpss, b_cst], W=[b_s[z]])
                            elif kt == qt - 1:
                                P.add("dve", lambda e, src=src, dst=dst, hh=hh: e.scalar_tensor_tensor(out=dst, in0=src, scalar=SCALE, in1=rel[:, hh, 1, :],
                                                                                                     op0=ALU.mult, op1=ALU.add), R=[bpss, b_cst], W=[b_s[z]])
                                if n < qb:
                                    P.add("dve", lambda e, dst=dst, qt=qt, n=n: e.tensor_scalar(out=dst, in0=dst, scalar1=bm[:, qt * 8 + n:qt * 8 + n + 1],
                                                                                              scalar2=None, op0=ALU.add), R=[b_s[z], b_gate], W=[b_s[z]])
                            else:
                                P.add("dve", lambda e, src=src, dst=dst, qt=qt, n=n: e.tensor_scalar(out=dst, in0=src, scalar1=SCALE,
                                                                                                   scalar2=cm[:, qt * 8 + n:qt * 8 + n + 1],
                                                                                                   op0=ALU.mult, op1=ALU.add), R=[bpss, b_gate], W=[b_s[z]])
                    L = nk * 128
                    if MD < 4:
                        continue
                    P.add("dve", lambda e, z=z, L=L: e.reduce_max(out=stt[:, z, 0:1], in_=sc[:, z, 0:L], axis=AX.X), R=[b_s[z]], W=[b_st[z]])
                    P.add("dve", lambda e, z=z: e.tensor_scalar(out=stt[:, z, 1:2], in0=stt[:, z, 0:1], scalar1=-1.0, scalar2=None, op0=ALU.mult),
                          R=[b_st[z]], W=[b_st[z]])
                    P.add("act", lambda e, z=z, L=L: e.activation(out=sc[:, z, 0:L], in_=sc[:, z, 0:L], func=AF.Exp, bias=stt[:, z, 1:2], scale=1.0,
                                                                  accum_out=stt[:, z, 2:3]), R=[b_s[z], b_st[z]], W=[b_s[z], b_st[z]])
                    P.add("dve", lambda e, z=z: e.reciprocal(out=stt[:, z, 3:4], in_=stt[:, z, 2:3]), R=[b_st[z]], W=[b_st[z]])
                    P.add("pool", lambda e, z=z, L=L: e.tensor_scalar(out=pb[:, 0:L], in0=sc[:, z, 0:L], scalar1=stt[:, z, 3:4], scalar2=None, op0=ALU.mult),
                          R=[b_s[z], b_st[z]], W=[b_p[z]])
                    po, bpo = C.ps[5 + z], C.bps[5 + z]
                    if MD < 5:
                        continue
                    for c in range((nk + 3) // 4):
                        w = min(4, nk - c * 4)
                        y = C.y_rr % 2
                        C.y_rr += 1
                        ptp, bptp = C.ps[(0, 1)[y]], C.bps[(0, 1)[y]]
                        ptb = ptp[:].bitcast(BF16)
                        for i in range(w):
                            kt = c * 4 + i
                            P.add("pe", lambda e, i=i, kt=kt, z=z, ptb=ptb: e.transpose(out=ptb[:, i * 128:(i + 1) * 128], in_=pb[:, kt * 128:(kt + 1) * 128],
                                                                                      identity=C.identb[:]), R=[b_p[z], C.b_const], W=[bptp])
                        P.add("act", lambda e, w=w, y=y, ptb=ptb: e.copy(out=pT[:, y, 0:w, :], in_=ptb[:, 0:w * 128].rearrange("p (a c) -> p a c", a=w)),
                              R=[bptp], W=[b_pT[y]])
                        for i in range(w):
                            kt = c * 4 + i
                            P.add("pe", lambda e, i=i, kt=kt, y=y, s=s, qt=qt, nk=nk, po=po: e.matmul(po[:, 0:128], lhsT=V[:, s, kt, :], rhs=pT[:, y, i, :],
                                                                                                   start=(kt == 0), stop=(kt == nk - 1)),
                                  R=[b_v[s], b_pT[y]], W=[bpo])
                    P.add("act", lambda e, po=po, hh=hh, qt=qt: e.copy(out=aT[:, hh, qt * 128:(qt + 1) * 128], in_=po[:, 0:128]), R=[bpo], W=[C.b_srcT])
            P.flush()
        out_proj_ln(C, li, T, aT, 8, T["a_w_out_%d" % j], R_)


def gmlp_phase(C, li, T, R_):
    P, nc = C.P, C.nc
    h, hT = C.h, C.hT
    j = li // 3
    w_in = T["b_w_in_%d" % j]
    w_out = T["b_w_out_%d" % j]
    with ExitStack() as es:
        lng = es.enter_context(nc.sbuf_tensor("g_lng_%d" % li, [128, D], F32))
        lnb = es.enter_context(nc.sbuf_tensor("g_lnb_%d" % li, [128, D], F32))
        wi = es.enter_context(nc.sbuf_tensor("g_wi_%d" % li, [128, 2, 8, 512], BF16))
        wo = es.enter_context(nc.sbuf_tensor("g_wo_%d" % li, [128, 2, 4, 1024], BF16))
        yT = es.enter_context(nc.sbuf_tensor("g_yT_%d" % li, [128, 16, 256], BF16))
        v = es.enter_context(nc.sbuf_tensor("g_v_%d" % li, [128, 2, 2048], F32))
        vn = es.enter_context(nc.sbuf_tensor("g_vn_%d" % li, [128, 2, 2048], BF16))
        gb = es.enter_context(nc.sbuf_tensor("g_gb_%d" % li, [128, 2, 2048], F32))
        ws = es.enter_context(nc.sbuf_tensor("g_ws_%d" % li, [128, 8, 128], F32))
        wsT = es.enter_context(nc.sbuf_tensor("g_wsT_%d" % li, [128, 8, 128], BF16))
        tril = es.enter_context(nc.sbuf_tensor("g_tril_%d" % li, [128, 128], F32))
        bs = es.enter_context(nc.sbuf_tensor("g_bs_%d" % li, [1, 1024], F32))
        ones = es.enter_context(nc.sbuf_tensor("g_ones_%d" % li, [1, 128], F32))
        st = es.enter_context(nc.sbuf_tensor("g_st_%d" % li, [128, 4, 6], F32))
        mv = es.enter_context(nc.sbuf_tensor("g_mv_%d" % li, [128, 2], F32))
        rs = es.enter_context(nc.sbuf_tensor("g_rs_%d" % li, [128, 1], F32))
        blnp, b_cst, b_wsT, b_yT, b_st = Buf(), Buf(), Buf(), Buf(), Buf()
        b_wi = [Buf(), Buf()]
        b_wo = [Buf(), Buf()]
        b_v = [Buf(), Buf()]
        b_vn = [Buf(), Buf()]
        load_ln_params(C, lng, lnb, blnp, T["ln_g"][li, 0, :], T["ln_b"][li, 0, :])
        P.add("sp", lambda e: e.dma_start(out=gb[:, 0, :], in_=bcast_rows(T["b_ln_g_%d" % j])), W=[b_cst], dma=True)
        P.add("sp", lambda e: e.dma_start(out=gb[:, 1, :], in_=bcast_rows(T["b_ln_b_%d" % j])), W=[b_cst], dma=True)
        P.add("sp", lambda e: e.dma_start(out=ws[:], in_=T["b_w_s_%d" % j].rearrange("g t s -> t g s")), W=[b_cst], dma=True)
        P.add("sp", lambda e: e.dma_start(out=tril[:], in_=T["tril"]), W=[b_cst], dma=True)
        P.add("sp", lambda e: e.dma_start(out=bs[:], in_=T["b_b_s_%d" % j].rearrange("(o g) t -> o (g t)", o=1)), W=[b_cst], dma=True)
        P.add("pool", lambda e: e.memset(ones[:], 1.0), W=[b_cst])
        for g in range(8):
            P.add("dve", lambda e, g=g: e.tensor_tensor(out=ws[:, g, :], in0=ws[:, g, :], in1=tril[:], op=ALU.mult), R=[b_cst], W=[b_cst])
        for half in range(2):
            ps, bps = C.ps[half], C.bps[half]
            for gg in range(4):
                g = half * 4 + gg
                P.add("pe", lambda e, g=g, gg=gg, ps=ps: e.transpose(out=ps[:, gg * 128:(gg + 1) * 128], in_=ws[:, g, :], identity=C.ident[:]),
                      R=[b_cst, C.b_const], W=[bps])
            P.add("act", lambda e, ps=ps, half=half: e.copy(out=wsT[:, half * 4:half * 4 + 4, :], in_=ps[:].rearrange("p (a c) -> p a c", a=4)),
                  R=[bps], W=[b_wsT])
        it = 0
        iw = 0
        acc_banks = (5, 6, 7, 2)
        DBG = 9
        for tg in range(8 if DBG >= 2 else 0):
            c0 = tg * 256
            tiles = (2 * tg, 2 * tg + 1)
            bhTg = [C.bhT[t] for t in tiles]
            for cg in range(8):
                s = it % 2
                it += 1
                P.add("pool", lambda e, s=s, cg=cg: e.dma_start(out=wi[:, s], in_=w_in[:, cg * 512:(cg + 1) * 512].rearrange("(k p) n -> p k n", p=128)),
                      W=[b_wi[s]], dma=True)
                if cg < 4:
                    for jj in range(4):
                        fc = cg * 4 + jj
                        q = C.q_rr % 2
                        C.q_rr += 1
                        pu, bpu = C.ps[3 + q], C.bps[3 + q]
                        for k in range(8):
                            P.add("pe", lambda e, k=k, jj=jj, s=s, pu=pu, c0=c0: e.matmul(pu[:, 0:256], lhsT=wi[:, s, k, jj * 128:(jj + 1) * 128],
                                                                                      rhs=hT[:, k, c0:c0 + 256], start=(k == 0), stop=(k == 7)),
                                  R=[b_wi[s]] + bhTg, W=[bpu])
                        P.add("act", lambda e, fc=fc, pu=pu: e.activation(out=yT[:, fc, :], in_=pu[:, 0:256], func=AF.Gelu), R=[bpu], W=[b_yT])
                else:
                    for ti in range(2):
                        t = tiles[ti]
                        q = C.q_rr % 2
                        C.q_rr += 1
                        pv, bpv = C.ps[3 + q], C.bps[3 + q]
                        for k in range(8):
                            P.add("pe", lambda e, k=k, t=t, s=s, pv=pv: e.matmul(pv[:], lhsT=hT[:, k, t * 128:(t + 1) * 128], rhs=wi[:, s, k, :],
                                                                               start=(k == 0), stop=(k == 7)), R=[b_wi[s], C.bhT[t]], W=[bpv])
                        P.add("act", lambda e, ti=ti, cg=cg, pv=pv: e.activation(out=v[:, ti, (cg - 4) * 512:(cg - 3) * 512], in_=pv[:], func=AF.Gelu),
                              R=[bpv], W=[b_v[ti]])
            for ti in range(2 if DBG >= 3 else 0):
                for c in range(4):
                    P.add("dve", lambda e, c=c, ti=ti: e.bn_stats(out=st[:, c, :], in_=v[:, ti, c * 512:(c + 1) * 512]), R=[b_v[ti]], W=[b_st])
                P.add("dve", lambda e: e.bn_aggr(out=mv[:], in_=st[:]), R=[b_st], W=[b_st])
                P.add("act", lambda e: e.activation(out=rs[:], in_=mv[:, 1:2], func=AF.Sqrt, bias=EPS, scale=1.0), R=[b_st], W=[b_st])
                P.add("dve", lambda e: e.reciprocal(out=rs[:], in_=rs[:]), R=[b_st], W=[b_st])
                P.add("dve", lambda e, ti=ti: e.tensor_scalar(out=v[:, ti, :], in0=v[:, ti, :], scalar1=mv[:, 0:1], scalar2=rs[:, 0:1],
                                                              op0=ALU.subtract, op1=ALU.mult), R=[b_v[ti], b_st], W=[b_v[ti]])
                P.add("pool", lambda e, ti=ti: e.tensor_tensor(out=v[:, ti, :], in0=v[:, ti, :], in1=gb[:, 0, :], op=ALU.mult), R=[b_v[ti], b_cst], W=[b_v[ti]])
                P.add("pool", lambda e, ti=ti: e.tensor_tensor(out=vn[:, ti, :], in0=v[:, ti, :], in1=gb[:, 1, :], op=ALU.add), R=[b_v[ti], b_cst], W=[b_vn[ti]])
            for ti in range(2 if DBG >= 4 else 0):
                for bk in range(4):
                    q = C.q_rr % 2
                    C.q_rr += 1
                    pm, bpm = C.ps[3 + q], C.bps[3 + q]
                    for jj in range(4):
                        fc = bk * 4 + jj
                        g = fc // 2
                        P.add("pe", lambda e, jj=jj, fc=fc, g=g, ti=ti, pm=pm: e.matmul(pm[:, jj * 128:(jj + 1) * 128], lhsT=vn[:, ti, fc * 128:(fc + 1) * 128],
                                                                                      rhs=wsT[:, g, :], start=True, stop=False), R=[b_vn[ti], b_wsT], W=[bpm])
                        P.add("pe", lambda e, jj=jj, g=g, pm=pm: e.matmul(pm[:, jj * 128:(jj + 1) * 128], lhsT=ones[0:1, :], rhs=bs[0:1, g * 128:(g + 1) * 128],
                                                                        start=False, stop=True), R=[b_cst], W=[bpm])
                    P.add("dve", lambda e, bk=bk, ti=ti, pm=pm: e.tensor_tensor(out=yT[:, bk * 4:bk * 4 + 4, ti * 128:(ti + 1) * 128],
                                                                              in0=pm[:].rearrange("p (a c) -> p a c", a=4),
                                                                              in1=yT[:, bk * 4:bk * 4 + 4, ti * 128:(ti + 1) * 128], op=ALU.mult),
                          R=[bpm, b_yT], W=[b_yT])
            if DBG < 5:
                continue
            for wc in range(4):
                s2 = iw % 2
                iw += 1
                P.add("pool", lambda e, s2=s2, wc=wc: e.dma_start(out=wo[:, s2], in_=w_out[wc * 512:(wc + 1) * 512, :].rearrange("(f p) n -> p f n", p=128)),
                      W=[b_wo[s2]], dma=True)
                for ti in range(2):
                    for n in range(2):
                        bi = acc_banks[ti * 2 + n]
                        po, bpo = C.ps[bi], C.bps[bi]
                        for ff in range(4):
                            P.add("pe", lambda e, wc=wc, ff=ff, ti=ti, n=n, s2=s2, po=po: e.matmul(
                                po[:], lhsT=yT[:, wc * 4 + ff, ti * 128:(ti + 1) * 128], rhs=wo[:, s2, ff, n * 512:(n + 1) * 512],
                                start=(wc == 0 and ff == 0), stop=(wc == 3 and ff == 3)), R=[b_yT, b_wo[s2]], W=[bpo])
            for ti in range(2):
                t = tiles[ti]
                for n in range(2):
                    bi = acc_banks[ti * 2 + n]
                    po, bpo = C.ps[bi], C.bps[bi]
                    P.add("dve", lambda e, n=n, t=t, po=po: e.scalar_tensor_tensor(out=h[:, t, n * 512:(n + 1) * 512], in0=h[:, t, n * 512:(n + 1) * 512],
                                                                                 scalar=ALPHA, in1=po[:], op0=ALU.mult, op1=ALU.add),
                          R=[bpo, C.bh[t]], W=[C.bh[t]])
            for ti in range(2 if DBG >= 6 else 0):
                ln_tile(C, tiles[ti], lng, lnb, blnp, router=(R_ if DBG >= 7 else None))
        P.flush()


def gla_phase(C, li, T, R_):
    P, nc = C.P, C.nc
    h, hT = C.h, C.hT
    j = li // 3
    w_in = T["c_w_in_%d" % j]
    SCALE = 128 ** -0.5
    with nc.sbuf_tensor("c_oT_%d" % li, [128, 8, S], BF16) as oT:
        C.b_srcT = Buf("oT")
        with ExitStack() as es:
            wq = es.enter_context(nc.sbuf_tensor("c_wq_%d" % li, [128, 2, 8, 768], BF16))
            wg = es.enter_context(nc.sbuf_tensor("c_wg_%d" % li, [128, 8, 16], BF16))
            gkT = es.enter_context(nc.sbuf_tensor("c_gkT_%d" % li, [32, S], F32))
            wgk = es.enter_context(nc.sbuf_tensor("c_wgk_%d" % li, [32, 512], F32))
            ng = es.enter_context(nc.sbuf_tensor("c_ng_%d" % li, [128, 256], F32))
            tri = es.enter_context(nc.sbuf_tensor("c_tri_%d" % li, [128, 128], F32))
            su = es.enter_context(nc.sbuf_tensor("c_su_%d" % li, [128, 128], F32))
            cmk = es.enter_context(nc.sbuf_tensor("c_cm_%d" % li, [128, 128], F32))
            qk = es.enter_context(nc.sbuf_tensor("c_qk_%d" % li, [128, 2, 2, 128], F32))
            kt = es.enter_context(nc.sbuf_tensor("c_kt_%d" % li, [128, 2, 128], F32))
            vt = es.enter_context(nc.sbuf_tensor("c_vt_%d" % li, [128, 2, 256], BF16))
            gs = es.enter_context(nc.sbuf_tensor("c_gs_%d" % li, [128, 2, 256], F32))
            nl = es.enter_context(nc.sbuf_tensor("c_nl_%d" % li, [128, 2, 128], F32))
            ex = es.enter_context(nc.sbuf_tensor("c_ex_%d" % li, [128, 2, 3, 128], F32))
            qz = es.enter_context(nc.sbuf_tensor("c_qz_%d" % li, [128, 2, 2, 128], BF16))
            ke = es.enter_context(nc.sbuf_tensor("c_ke_%d" % li, [128, 2, 128], BF16))
            krz = es.enter_context(nc.sbuf_tensor("c_krz_%d" % li, [128, 2, 2, 128], BF16))
            att = es.enter_context(nc.sbuf_tensor("c_att_%d" % li, [128, 2, 128], BF16))
            St = es.enter_context(nc.sbuf_tensor("c_S_%d" % li, [128, 256], F32))
            Sb = es.enter_context(nc.sbuf_tensor("c_Sb_%d" % li, [128, 5, 256], BF16))
            jk = es.enter_context(nc.sbuf_tensor("c_jk_%d" % li, [128, 256], F32))
            ot = es.enter_context(nc.sbuf_tensor("c_ot_%d" % li, [128, 2, 256], F32))
            og = es.enter_context(nc.sbuf_tensor("c_og_%d" % li, [128, 2, 256], BF16))
            ss = es.enter_context(nc.sbuf_tensor("c_ss_%d" % li, [128, 2, 2], F32))
            b_cst, b_gk, b_S, b_jk = Buf(), Buf(), Buf(), Buf()
            b_wq = [Buf(), Buf()]
            b_qk = [Buf(), Buf()]
            b_kt = [Buf(), Buf()]
            b_vt = [Buf(), Buf()]
            b_gs = [Buf(), Buf()]
            b_nl = [Buf(), Buf()]
            b_ex = [Buf(), Buf()]
            b_qz = [Buf(), Buf()]
            b_ke = [Buf(), Buf()]
            b_krz = [Buf(), Buf()]
            b_att = [Buf(), Buf()]
            b_Sb = [Buf() for _ in range(5)]
            b_ot = [Buf(), Buf()]
            b_og = [Buf(), Buf()]
            b_ss = [Buf(), Buf()]
            P.add("pool", lambda e: e.dma_start(out=wg[:], in_=w_in[:, 3072:3088].rearrange("(k p) n -> p k n", p=128)), W=[b_cst], dma=True)
            P.add("sp", lambda e: e.dma_start(out=wgk[0:16, :], in_=T["c_w_gk_up_%d" % j]), W=[b_cst], dma=True)
            P.add("sp", lambda e: e.dma_start(out=wgk[16:17, :], in_=T["c_b_gk_%d" % j].rearrange("(o n) -> o n", o=1)), W=[b_cst], dma=True)
            P.add("sp", lambda e: e.dma_start(out=ng[:], in_=bcast_rows(T["c_norm_g_%d" % j])), W=[b_cst], dma=True)
            P.add("sp", lambda e: e.dma_start(out=tri[:], in_=T["gla_tri"]), W=[b_cst], dma=True)
            P.add("sp", lambda e: e.dma_start(out=su[:], in_=T["gla_su"]), W=[b_cst], dma=True)
            P.add("sp", lambda e: e.dma_start(out=cmk[:], in_=T["gla_cm"]), W=[b_cst], dma=True)
            P.add("pool", lambda e: e.memset(gkT[:], 1.0), W=[b_gk])
            for z in range(2):
                P.add("pool", lambda e, z=z: e.memset(qz[:, z], 0.0), W=[b_qz[z]])
                P.add("pool", lambda e, z=z: e.memset(krz[:, z], 0.0), W=[b_krz[z]])
            for g4 in range(4):
                pg, bpg = C.ps[3 + g4 % 2], C.bps[3 + g4 % 2]
                for k in range(8):
                    P.add("pe", lambda e, k=k, g4=g4, pg=pg: e.matmul(pg[0:16, :], lhsT=wg[:, k, :], rhs=hT[:, k, g4 * 512:(g4 + 1) * 512],
                                                                    start=(k == 0), stop=(k == 7)), R=[b_cst] + C.bhT[g4 * 4:g4 * 4 + 4], W=[bpg])
                P.add("act", lambda e, g4=g4, pg=pg: e.copy(out=gkT[0:16, g4 * 512:(g4 + 1) * 512], in_=pg[0:16, :]), R=[bpg], W=[b_gk])

            def front(hd, s, t):
                z = t % 2
                tc = slice(t * 128, (t + 1) * 128)
                bhT = C.bhT[t]
                pa, bpa = C.ps[3], C.bps[3]
                for k in range(8):
                    P.add("pe", lambda e, k=k: e.matmul(pa[:, 0:128], lhsT=wq[:, s, k, 0:128], rhs=hT[:, k, tc], start=(k == 0), stop=(k == 7)),
                          R=[b_wq[s], bhT], W=[bpa])
                for k in range(8):
                    P.add("pe", lambda e, k=k: e.matmul(pa[:, 128:256], lhsT=wq[:, s, k, 128:256], rhs=hT[:, k, tc], start=(k == 0), stop=(k == 7)),
                          R=[b_wq[s], bhT], W=[bpa])
                for k in range(8):
                    P.add("pe", lambda e, k=k: e.matmul(pa[:, 256:384], lhsT=hT[:, k, tc], rhs=wq[:, s, k, 128:256], start=(k == 0), stop=(k == 7)),
                          R=[b_wq[s], bhT], W=[bpa])
                P.add("pe", lambda e: e.matmul(pa[:, 384:512], lhsT=gkT[0:17, tc], rhs=wgk[0:17, hd * 128:(hd + 1) * 128], start=True, stop=True),
                      R=[b_gk, b_cst], W=[bpa])
                P.add("act", lambda e: e.copy(out=qk[:, z], in_=pa[:, 0:256].rearrange("p (a c) -> p a c", a=2)), R=[bpa], W=[b_qk[z]])
                P.add("act", lambda e: e.copy(out=kt[:, z], in_=pa[:, 256:384]), R=[bpa], W=[b_kt[z]])
                P.add("act", lambda e: e.activation(out=nl[:, z], in_=pa[:, 384:512], func=AF.Exp, scale=-1.0), R=[bpa], W=[b_nl[z]])
                P.add("act", lambda e: e.activation(out=nl[:, z], in_=nl[:, z], func=AF.Ln, bias=1.0, scale=1.0), R=[b_nl[z]], W=[b_nl[z]])
                pb_, bpb = C.ps[4], C.bps[4]
                for k in range(8):
                    P.add("pe", lambda e, k=k: e.matmul(pb_[:], lhsT=hT[:, k, tc], rhs=wq[:, s, k, 256:768], start=(k == 0), stop=(k == 7)),
                          R=[b_wq[s], bhT], W=[bpb])
                P.add("act", lambda e: e.copy(out=vt[:, z], in_=pb_[:, 0:256]), R=[bpb], W=[b_vt[z]])
                P.add("act", lambda e: e.activation(out=gs[:, z], in_=pb_[:, 256:512], func=AF.Silu), R=[bpb], W=[b_gs[z]])
                pc, bpc = C.ps[5], C.bps[5]
                P.add("pe", lambda e: e.matmul(pc[:, 0:128], lhsT=nl[:, z], rhs=tri[:], start=True, stop=True), R=[b_nl[z], b_cst], W=[bpc])
                P.add("pe", lambda e: e.matmul(pc[:, 128:256], lhsT=su[:], rhs=nl[:, z], start=True, stop=True), R=[b_nl[z], b_cst], W=[bpc])
                P.add("act", lambda e: e.activation(out=ex[:, z, 0, :], in_=pc[:, 0:128], func=AF.Exp, scale=-1.0 / 16.0), R=[bpc], W=[b_ex[z]])
                P.add("act", lambda e: e.activation(out=ex[:, z, 1, :], in_=pc[:, 0:128], func=AF.Exp, scale=1.0 / 16.0), R=[bpc], W=[b_ex[z]])
                P.add("act", lambda e: e.activation(out=ex[:, z, 2, :], in_=pc[:, 128:256], func=AF.Exp, scale=-1.0 / 16.0), R=[bpc], W=[b_ex[z]])
                for c in range(2):
                    cs = slice(c * 64, (c + 1) * 64)
                    P.add("dve", lambda e, c=c, cs=cs: e.scalar_tensor_tensor(out=qz[:, z, c, cs], in0=qk[:, z, 0, cs], scalar=SCALE, in1=ex[:, z, 0, cs],
                                                                            op0=ALU.mult, op1=ALU.mult), R=[b_qk[z], b_ex[z]], W=[b_qz[z]])
                P.add("pool", lambda e: e.tensor_tensor(out=ke[:, z], in0=qk[:, z, 1], in1=ex[:, z, 1, :], op=ALU.mult), R=[b_qk[z], b_ex[z]], W=[b_ke[z]])
                for c in range(2):
                    rows = slice(c * 64, (c + 1) * 64)
                    P.add("pool", lambda e, c=c, rows=rows: e.tensor_tensor(out=krz[rows, z, c, :], in0=kt[rows, z], in1=ex[rows, z, 2, :], op=ALU.mult),
                          R=[b_kt[z], b_ex[z]], W=[b_krz[z]])
                P.add("pe", lambda e: e.matmul(pc[:, 256:384], lhsT=ke[:, z], rhs=qz[:, z, 0, :], start=True, stop=False), R=[b_ke[z], b_qz[z]], W=[bpc])
                P.add("pe", lambda e: e.matmul(pc[:, 256:384], lhsT=ke[:, z], rhs=qz[:, z, 1, :], start=False, stop=True), R=[b_ke[z], b_qz[z]], W=[bpc])
                P.add("dve", lambda e: e.tensor_tensor(out=att[:, z], in0=pc[:, 256:384], in1=cmk[:], op=ALU.mult), R=[bpc, b_cst], W=[b_att[z]])
                pd, bpd = C.ps[6], C.bps[6]
                P.add("pe", lambda e: e.matmul(pd[:, 0:256], lhsT=krz[:, z, 0, :], rhs=vt[:, z], start=True, stop=True), R=[b_krz[z], b_vt[z]], W=[bpd])
                P.add("pe", lambda e: e.matmul(pd[:, 256:512], lhsT=krz[:, z, 1, :], rhs=vt[:, z], start=True, stop=True), R=[b_krz[z], b_vt[z]], W=[bpd])
                P.add("dve", lambda e: e.scalar_tensor_tensor(out=St[:], in0=St[:], scalar=ex[:, z, 0, 63:64], in1=pd[:, 0:256], op0=ALU.mult, op1=ALU.add),
                      R=[b_S, b_ex[z], bpd], W=[b_S])
                P.add("act", lambda e: e.copy(out=Sb[:, 3 + z, :], in_=St[:]), R=[b_S], W=[b_Sb[3 + z]])
                P.add("dve", lambda e: e.scalar_tensor_tensor(out=St[:], in0=St[:], scalar=ex[:, z, 0, 127:128], in1=pd[:, 256:512], op0=ALU.mult, op1=ALU.add),
                      R=[b_S, b_ex[z], bpd], W=[b_S])
                P.add("act", lambda e: e.copy(out=Sb[:, (t + 1) % 3, :], in_=St[:]), R=[b_S], W=[b_Sb[(t + 1) % 3]])

            def back(hd, s, t):
                z = t % 2
                tc = slice(t * 128, (t + 1) * 128)
                po, bpo = C.ps[7], C.bps[7]
                P.add("pe", lambda e: e.matmul(po[:, 0:256], lhsT=qz[:, z, 0, :], rhs=Sb[:, t % 3, :], start=True, stop=False), R=[b_qz[z], b_Sb[t % 3]], W=[bpo])
                P.add("pe", lambda e: e.matmul(po[:, 0:256], lhsT=qz[:, z, 1, :], rhs=Sb[:, 3 + z, :], start=False, stop=False), R=[b_qz[z], b_Sb[3 + z]], W=[bpo])
                P.add("pe", lambda e: e.matmul(po[:, 0:256], lhsT=att[:, z], rhs=vt[:, z], start=False, stop=True), R=[b_att[z], b_vt[z]], W=[bpo])
                P.add("act", lambda e: e.activation(out=jk[:], in_=po[:, 0:256], func=AF.Square, accum_out=ss[:, z, 0:1]), R=[bpo], W=[b_jk, b_ss[z]])
                P.add("act", lambda e: e.activation(out=ss[:, z, 1:2], in_=ss[:, z, 0:1], func=AF.Sqrt, bias=EPS, scale=1.0 / 256.0), R=[b_ss[z]], W=[b_ss[z]])
                P.add("dve", lambda e: e.reciprocal(out=ss[:, z, 1:2], in_=ss[:, z, 1:2]), R=[b_ss[z]], W=[b_ss[z]])
                P.add("dve", lambda e: e.scalar_tensor_tensor(out=ot[:, z], in0=po[:, 0:256], scalar=ss[:, z, 1:2], in1=ng[:], op0=ALU.mult, op1=ALU.mult),
                      R=[bpo, b_ss[z], b_cst], W=[b_ot[z]])
                P.add("pool", lambda e: e.tensor_tensor(out=og[:, z], in0=ot[:, z], in1=gs[:, z], op=ALU.mult), R=[b_ot[z], b_gs[z]], W=[b_og[z]])
                ptp, bptp = C.ps[z], C.bps[z]
                ptb = ptp[:].bitcast(BF16)
                for c2 in range(2):
                    P.add("pe", lambda e, c2=c2: e.transpose(out=ptb[:, c2 * 128:(c2 + 1) * 128], in_=og[:, z, c2 * 128:(c2 + 1) * 128], identity=C.identb[:]),
                          R=[b_og[z], C.b_const], W=[bptp])
                P.add("act", lambda e: e.copy(out=oT[:, hd * 2:hd * 2 + 2, tc], in_=ptb[:, 0:256].rearrange("p (a c) -> p a c", a=2)), R=[bptp], W=[C.b_srcT])

            for hd in range(4):
                s = hd % 2
                for (c0_, w_, d0) in ((hd * 128, 128, 0), (512 + hd * 128, 128, 128), (1024 + hd * 256, 256, 256), (2048 + hd * 256, 256, 512)):
                    P.add("pool", lambda e, c0_=c0_, w_=w_, d0=d0, s=s: e.dma_start(
                        out=wq[:, s, :, d0:d0 + w_], in_=w_in[:, c0_:c0_ + w_].rearrange("(k p) n -> p k n", p=128)), W=[b_wq[s]], dma=True)
                P.add("pool", lambda e: e.memset(St[:], 0.0), W=[b_S])
                P.add("pool", lambda e: e.memset(Sb[:, 0, :], 0.0), W=[b_Sb[0]])
                front(hd, s, 0)
                for t in range(NT):
                    if t + 1 < NT:
                        front(hd, s, t + 1)
                    back(hd, s, t)
            P.flush()
        out_proj_ln(C, li, T, oT, 8, T["c_w_out_%d" % j], R_)


def build_program(mode="full", n_experts=NE, layers=(0, 1, 2, 3)):
    nc = bass.Bass("TRN2", target_bir_lowering=False)
    T = {}

    def din(name, shape, dt=F32):
        T[name] = nc.dram_tensor(name, list(shape), dt, kind="ExternalInput").ap()

    do_mixer = mode in ("full", "mixer_only")
    do_moe = mode in ("full", "moe_only")
    din("x", (S, D))
    din("ident", (128, 128))
    din("identb", (128, 128), BF16)
    din("ln_g", (DEPTH, 2, D))
    din("ln_b", (DEPTH, 2, D))
    mixers = sorted(set(li % 3 for li in layers)) if do_mixer else []
    if 0 in mixers:
        din("rel_bias", (32, 8))
        din("relT", (8, 2, 128, 128))
        din("causal", (128, 128))
        din("vmask", (128, 128))
        din("vmask2", (128, 128))
    for li in layers:
        if do_mixer:
            j = li // 3
            if li % 3 == 0:
                din("a_w_in_%d" % j, (D, 3 * D))
                din("a_w_out_%d" % j, (D, D))
            elif li % 3 == 1:
                din("b_w_in_%d" % j, (D, 4 * D))
                din("b_ln_g_%d" % j, (2 * D,))
                din("b_ln_b_%d" % j, (2 * D,))
                din("b_w_s_%d" % j, (8, 128, 128))
                din("b_b_s_%d" % j, (8, 128))
                din("b_w_out_%d" % j, (2 * D, D))
                din("tril", (128, 128))
            else:
                din("c_w_in_%d" % j, (D, 3088))
                din("c_w_gk_up_%d" % j, (16, 512))
                din("c_b_gk_%d" % j, (512,))
                din("c_norm_g_%d" % j, (256,))
                din("c_w_out_%d" % j, (D, D))
                din("gla_tri", (128, 128))
                din("gla_su", (128, 128))
                din("gla_cm", (128, 128))
        din("moe_w_router_%d" % li, (D, NE))
        din("moe_b_router_%d" % li, (NE,))
        if do_moe:
            din("moe_w_gate_up_%d" % li, (NE, D, 2 * D))
            din("moe_b_gate_up_%d" % li, (NE, 2 * D))
            din("moe_w_down_%d" % li, (NE, D, D))
            din("moe_b_down_%d" % li, (NE, D))
    out = nc.dram_tensor("out", [S, D], F32, kind="ExternalOutput").ap()

    C = Ctx()
    C.nc = nc
    C.n_experts = n_experts
    C.P = P = Prog(nc)
    C.tp_rr = C.act_rr = C.q_rr = C.y_rr = 0
    with ExitStack() as es:
        h = es.enter_context(nc.sbuf_tensor("h", [128, NT, D], F32))
        hT = es.enter_context(nc.sbuf_tensor("hT", [128, 8, S], BF16))
        ident = es.enter_context(nc.sbuf_tensor("ident_s", [128, 128], F32))
        identb = es.enter_context(nc.sbuf_tensor("identb_s", [128, 128], BF16))
        hT32 = es.enter_context(nc.sbuf_tensor("hT32", [128, 8, 128], F32))
        gates = es.enter_context(nc.sbuf_tensor("gates", [128, NT, NE], F32))
        ln_st = es.enter_context(nc.sbuf_tensor("ln_st", [128, 2, 6], F32))
        ln_mv = es.enter_context(nc.sbuf_tensor("ln_mv", [128, 2], F32))
        ln_rs = es.enter_context(nc.sbuf_tensor("ln_rs", [128, 1], F32))
        r_lg = es.enter_context(nc.sbuf_tensor("r_lg", [128, NE], F32))
        r_m8 = es.enter_context(nc.sbuf_tensor("r_m8", [128, 8], F32))
        r_ex = es.enter_context(nc.sbuf_tensor("r_ex", [128, NE], F32))
        r_msk = es.enter_context(nc.sbuf_tensor("r_msk", [128, NE], F32))
        r_sm = es.enter_context(nc.sbuf_tensor("r_sm", [128, 2], F32))
        r_wr = es.enter_context(nc.sbuf_tensor("r_wr", [128, 8, NE], F32))
        r_brb = es.enter_context(nc.sbuf_tensor("r_brb", [128, NE], F32))
        C.h, C.hT, C.ident, C.identb, C.hT32, C.gates = h, hT, ident, identb, hT32, gates
        C.ln_st, C.ln_mv, C.ln_rs, C.ln_nb = ln_st, ln_mv, ln_rs, None
        C.r_lg, C.r_m8, C.r_ex, C.r_msk, C.r_sm = r_lg, r_m8, r_ex, r_msk, r_sm
        C.bh = [Buf("h%d" % t) for t in range(NT)]
        C.bhT = [Buf("hT%d" % t) for t in range(NT)]
        C.b_gates = [Buf() for t in range(NT)]
        C.b_const = Buf("const")
        C.b_lnst = Buf()
        C.b_hT32 = Buf()
        C.b_rt = Buf()
        ps_cms = [nc.psum_tensor("ps%d" % i, [128, 512], F32) for i in range(8)]
        C.ps = [cm.__enter__() for cm in ps_cms]
        C.bps = [Buf("ps%d" % i) for i in range(8)]

        P.add("sp", lambda e: e.dma_start(out=ident[:], in_=T["ident"]), W=[C.b_const], dma=True)
        P.add("sp", lambda e: e.dma_start(out=identb[:], in_=T["identb"]), W=[C.b_const], dma=True)
        for t in range(NT):
            P.add("sp", lambda e, t=t: e.dma_start(out=h[:, t, :], in_=T["x"][t * 128:(t + 1) * 128, :]), W=[C.bh[t]], dma=True)
        first = True
        for li in layers:
            b_r = Buf()
            P.add("sp", lambda e, li=li: e.dma_start(out=r_wr[:], in_=T["moe_w_router_%d" % li].rearrange("(k p) n -> p k n", p=128)), W=[b_r], dma=True)
            P.add("sp", lambda e, li=li: e.dma_start(out=r_brb[:], in_=bcast_rows(T["moe_b_router_%d" % li])), W=[b_r], dma=True)
            R_ = {"wr": r_wr, "brb": r_brb, "b": b_r}
            if first:
                for t in range(NT):
                    transpose_tile(C, t, None if do_mixer else R_)
                P.flush()
                first = False
            if do_mixer:
                if li % 3 == 0:
                    moba_phase(C, li, T, R_)
                elif li % 3 == 1:
                    gmlp_phase(C, li, T, R_)
                else:
                    gla_phase(C, li, T, R_)
            if do_moe:
                moe_phase(C, li, T)

        for t in range(NT):
            P.add("sp", lambda e, t=t: e.dma_start(out=out[t * 128:(t + 1) * 128, :], in_=h[:, t, :]), R=[C.bh[t]], dma=True)
        P.flush()
        for cm in reversed(ps_cms):
            cm.__exit__(None, None, None)
    P.close()
    return nc


def t5_bucket_np(rel):
    n = np.maximum(rel, 0)
    nf = np.maximum(n, 1).astype(np.float32)
    large = 16 + (np.log(nf / np.float32(16)) / np.float32(math.log(128 / 16)) * np.float32(16)).astype(np.int32)
    large = np.minimum(large, 31)
    return np.where(n < 16, n, large)


def host_constants(rel_bias=None):
    import ml_dtypes
    c = {"ident": np.eye(128, dtype=np.float32), "identb": np.eye(128, dtype=np.float32).astype(ml_dtypes.bfloat16)}
    i = np.arange(128)[:, None]
    jj = np.arange(128)[None, :]
    c["causal"] = np.where(jj <= i, 0.0, NEG).astype(np.float32)
    c["tril"] = (jj <= i).astype(np.float32)
    qt = np.arange(16)[:, None]
    n = np.arange(8)[None, :]
    valid = n < (qt // 2)
    vm = np.where(valid, 0.0, -1e30).astype(np.float32).reshape(1, 128)
    c["vmask"] = np.broadcast_to(vm, (128, 128)).copy()
    vm2 = np.where(valid, -30000.0, -1e30).astype(np.float32).reshape(1, 128)
    c["vmask2"] = np.broadcast_to(vm2, (128, 128)).copy()
    if rel_bias is not None:
        relT = np.empty((8, 2, 128, 128), np.float32)
        for d in range(2):
            bk = t5_bucket_np(d * 128 + i - jj)
            for hh in range(8):
                relT[hh, d] = rel_bias[bk, hh]
        c["relT"] = relT
    same = (i // 64) == (jj // 64)
    c["gla_tri"] = (same & (i <= jj)).astype(np.float32)
    c["gla_su"] = (same & (i > jj)).astype(np.float32)
    c["gla_cm"] = (same & (i <= jj)).astype(np.float32)
    return c


def make_in_map(A, x_core, consts, layers=(0, 1, 2, 3), mode="full"):
    do_mixer = mode in ("full", "mixer_only")
    do_moe = mode in ("full", "moe_only")
    im = {"x": np.ascontiguousarray(x_core), "ln_g": A["ln_g"], "ln_b": A["ln_b"]}
    im.update(consts)
    if "rel_bias" in A:
        im["rel_bias"] = A["rel_bias"]
    for li in layers:
        j = li // 3
        if do_mixer:
            if li % 3 == 0:
                im["a_w_in_%d" % j] = A["a_w_in"][j]
                im["a_w_out_%d" % j] = A["a_w_out"][j]
            elif li % 3 == 1:
                for n in ("b_w_in", "b_ln_g", "b_ln_b", "b_w_s", "b_b_s", "b_w_out"):
                    im["%s_%d" % (n, j)] = A[n][j]
            else:
                for n in ("c_w_in", "c_w_gk_up", "c_b_gk", "c_norm_g", "c_w_out"):
                    im["%s_%d" % (n, j)] = A[n][j]
        im["moe_w_router_%d" % li] = A["moe_w_router"][li]
        im["moe_b_router_%d" % li] = A["moe_b_router"][li]
        if do_moe:
            for n in ("moe_w_gate_up", "moe_b_gate_up", "moe_w_down", "moe_b_down"):
                im["%s_%d" % (n, li)] = A[n][li]
    return im


def kernel(**inputs):
    A = {k: np.asarray(v) for k, v in inputs.items()}
    nc = build_program("full")
    consts = host_constants(A["rel_bias"])
    in_maps = [make_in_map(A, A["x"][c], consts) for c in range(8)]
    res = run_bass_kernel_spmd(nc, in_maps, core_ids=list(range(8)))
    out = np.stack([np.asarray(res.results[c]["out"]) for c in range(8)], axis=0)
    return out.astype(np.float32)
```

```python
import math
from contextlib import ExitStack
import numpy as np
import concourse.bass as bass
import concourse.mybir as mybir
from concourse.bass_utils import run_bass_kernel_spmd

F32 = mybir.dt.float32
BF16 = mybir.dt.bfloat16
ALU = mybir.AluOpType
AF = mybir.ActivationFunctionType
AX = mybir.AxisListType

ENGS = ("pe", "act", "dve", "pool", "sp")

D = 1024
S = 2048
NT = 16
DEPTH = 4
ALPHA = (2.0 * DEPTH) ** 0.25
EPS = 1e-5
NE = 32
NEG = -30000.0


class Buf:
    __slots__ = ("name", "lw", "rd")

    def __init__(self, name="b"):
        self.name = name
        self.lw = None
        self.rd = []


class Op:
    __slots__ = ("eng", "fn", "dma", "deps", "sem", "val", "needs_sig", "epoch")

    def __init__(self, eng, fn, dma, epoch=0):
        self.epoch = epoch
        self.eng = eng
        self.fn = fn
        self.dma = dma
        self.deps = []
        self.sem = None
        self.val = 0
        self.needs_sig = False


class Prog:
    def __init__(self, nc, n_dma_sems=(("sp", 24), ("pool", 24), ("act", 8))):
        self.nc = nc
        self.pending = {e: [] for e in ENGS}
        self.esem = {}
        self.ecount = {e: 0 for e in ENGS}
        self._ctx = []
        for e in ENGS:
            cm = nc.semaphore("s_" + e)
            self.esem[e] = cm.__enter__()
            self._ctx.append(cm)
        self.dsems = {}
        self.dcount = {}
        self.dlast = {}
        self.dnext = {}
        for q, n in n_dma_sems:
            lst = []
            for i in range(n):
                cm = nc.semaphore("d_%s_%d" % (q, i))
                lst.append(cm.__enter__())
                self._ctx.append(cm)
            self.dsems[q] = lst
            self.dcount[q] = [0] * n
            self.dlast[q] = [None] * n
            self.dnext[q] = 0
        self.known = {e: {} for e in ENGS}
        self.all_dma_since_barrier = []
        self.n_ops = 0
        self.epoch = 0

    def close(self):
        for cm in reversed(self._ctx):
            cm.__exit__(None, None, None)

    def add(self, eng, fn, R=(), W=(), dma=False):
        op = Op(eng, fn, dma, self.epoch)
        self.n_ops += 1
        deps = []
        rset = set(id(b) for b in R)
        for b in R:
            if b.lw is not None:
                deps.append((b.lw, True))
        for b in W:
            if b.lw is not None:
                deps.append((b.lw, id(b) in rset))
            for r in b.rd:
                deps.append((r, False))
        seen = set()
        for d, raw in deps:
            if d is op or d.epoch < self.epoch:
                continue
            key = id(d)
            if d.dma or dma:
                pass
            elif d.eng == eng:
                if eng == "pe" or not raw:
                    continue
            if key in seen:
                continue
            seen.add(key)
            op.deps.append(d)
            d.needs_sig = True
        for b in R:
            b.rd.append(op)
        for b in W:
            b.lw = op
            b.rd = []
        if dma:
            q = eng
            k = self.dnext[q]
            self.dnext[q] = (k + 1) % len(self.dsems[q])
            prev = self.dlast[q][k]
            if prev is not None and prev.epoch == self.epoch:
                op.deps.append(prev)
            self.dcount[q][k] += 16
            op.sem = self.dsems[q][k]
            op.val = self.dcount[q][k]
            self.dlast[q][k] = op
            op.needs_sig = True
            self.all_dma_since_barrier.append(op)
        self.pending[eng].append(op)
        return op

    def barrier(self):
        lasts = []
        for e in ENGS:
            for o in reversed(self.pending[e]):
                if not o.dma and o.fn is not None:
                    lasts.append(o)
                    break
        dmas = list(self.all_dma_since_barrier)
        self.all_dma_since_barrier = []
        for e in ENGS:
            op = Op(e, None, False, self.epoch)
            for d in lasts:
                if d.eng != e:
                    op.deps.append(d)
                    d.needs_sig = True
            for d in dmas:
                op.deps.append(d)
            self.pending[e].append(op)
        self.epoch += 1

    def flush(self):
        nc = self.nc
        self.barrier()
        for e in ENGS:
            for op in self.pending[e]:
                if op.dma or op.fn is None:
                    continue
                if op.needs_sig:
                    self.ecount[e] += 1
                    op.sem = self.esem[e]
                    op.val = self.ecount[e]
        pend = self.pending
        self.pending = {e: [] for e in ENGS}
        known = self.known

        def emit(e, eng):
            kn = known[e]
            for op in pend[e]:
                need = {}
                for d in op.deps:
                    assert d.sem is not None, "dep without signal"
                    sid = id(d.sem)
                    if kn.get(sid, 0) >= d.val:
                        continue
                    if sid not in need or need[sid][1] < d.val:
                        need[sid] = (d.sem, d.val)
                for sid, (sem, val) in need.items():
                    eng.wait_ge(sem, val)
                    kn[sid] = val
                if op.fn is None:
                    continue
                ins = op.fn(eng)
                if op.dma:
                    ins.then_inc(op.sem, 16)
                elif op.needs_sig:
                    ins.then_inc(op.sem, 1)

        with nc.Block() as blk:
            @blk.tensor
            def _(eng):
                emit("pe", eng)

            @blk.scalar
            def _(eng):
                emit("act", eng)

            @blk.vector
            def _(eng):
                emit("dve", eng)

            @blk.gpsimd
            def _(eng):
                emit("pool", eng)

            @blk.sync
            def _(eng):
                emit("sp", eng)


class Ctx:
    pass


def bcast_rows(ap_row, nparts=128):
    return ap_row.partition_broadcast(nparts)


def ln_tile(C, t, lng, lnb, blnp, router=None):
    P, nc = C.P, C.nc
    h, hT = C.h, C.hT
    bh, bhT = C.bh[t], C.bhT[t]
    st, mv, rs, nb = C.ln_st, C.ln_mv, C.ln_rs, C.ln_nb
    b_st = C.b_lnst
    for c in range(2):
        P.add("dve", lambda e, c=c: e.bn_stats(out=st[:, c, :], in_=h[:, t, c * 512:(c + 1) * 512]), R=[bh], W=[b_st])
    P.add("dve", lambda e: e.bn_aggr(out=mv[:], in_=st[:]), R=[b_st], W=[b_st])
    P.add("act", lambda e: e.activation(out=rs[:], in_=mv[:, 1:2], func=AF.Sqrt, bias=EPS, scale=1.0), R=[b_st], W=[b_st])
    P.add("dve", lambda e: e.reciprocal(out=rs[:], in_=rs[:]), R=[b_st], W=[b_st])
    P.add("dve", lambda e: e.tensor_scalar(out=h[:, t, :], in0=h[:, t, :], scalar1=mv[:, 0:1], scalar2=rs[:, 0:1],
                                           op0=ALU.subtract, op1=ALU.mult), R=[bh, b_st], W=[bh])
    P.add("pool", lambda e: e.tensor_tensor(out=h[:, t, :], in0=h[:, t, :], in1=lng[:], op=ALU.mult), R=[bh, blnp], W=[bh])
    P.add("pool", lambda e: e.tensor_tensor(out=h[:, t, :], in0=h[:, t, :], in1=lnb[:], op=ALU.add), R=[bh, blnp], W=[bh])
    transpose_tile(C, t, router)


def transpose_tile(C, t, router=None):
    P = C.P
    h, hT = C.h, C.hT
    bh, bhT = C.bh[t], C.bhT[t]
    for half in range(2):
        ps, bps = C.ps[C.tp_rr % 2], C.bps[C.tp_rr % 2]
        C.tp_rr += 1
        for kk in range(4):
            k = half * 4 + kk
            P.add("pe", lambda e, k=k, kk=kk, ps=ps: e.transpose(out=ps[:, kk * 128:(kk + 1) * 128], in_=h[:, t, k * 128:(k + 1) * 128],
                                                                  identity=C.ident[:]), R=[bh, C.b_const], W=[bps])
        P.add("act", lambda e, ps=ps, half=half: e.copy(out=hT[:, half * 4:half * 4 + 4, t * 128:(t + 1) * 128],
                                                        in_=ps[:].rearrange("p (k c) -> p k c", k=4)), R=[bps], W=[bhT])
        if router is not None:
            P.add("act", lambda e, ps=ps, half=half: e.copy(out=C.hT32[:, half * 4:half * 4 + 4, :],
                                                            in_=ps[:].rearrange("p (k c) -> p k c", k=4)), R=[bps], W=[C.b_hT32])
    if router is not None:
        router_tile(C, t, router)


def router_tile(C, t, R_):
    P = C.P
    ps, bps = C.ps[2], C.bps[2]
    wr, brb, b_r = R_["wr"], R_["brb"], R_["b"]
    lg, m8, ex, msk, sm = C.r_lg, C.r_m8, C.r_ex, C.r_msk, C.r_sm
    b = C.b_rt
    for k in range(8):
        P.add("pe", lambda e, k=k: e.matmul(ps[:, 0:NE], lhsT=C.hT32[:, k, :], rhs=wr[:, k, :], start=(k == 0), stop=(k == 7)),
              R=[C.b_hT32, b_r], W=[bps])
    P.add("dve", lambda e: e.tensor_tensor(out=lg[:], in0=ps[:, 0:NE], in1=brb[:], op=ALU.add), R=[bps, b_r], W=[b])
    P.add("dve", lambda e: e.max(out=m8[:], in_=lg[:]), R=[b], W=[b])
    P.add("dve", lambda e: e.tensor_scalar(out=msk[:], in0=lg[:], scalar1=m8[:, 3:4], scalar2=None, op0=ALU.is_ge), R=[b], W=[b])
    P.add("dve", lambda e: e.tensor_scalar(out=sm[:, 0:1], in0=m8[:, 0:1], scalar1=-1.0, scalar2=None, op0=ALU.mult), R=[b], W=[b])
    P.add("act", lambda e: e.activation(out=ex[:], in_=lg[:], func=AF.Exp, bias=sm[:, 0:1], scale=1.0), R=[b], W=[b])
    P.add("dve", lambda e: e.tensor_tensor(out=ex[:], in0=ex[:], in1=msk[:], op=ALU.mult), R=[b], W=[b])
    P.add("dve", lambda e: e.reduce_sum(out=sm[:, 1:2], in_=ex[:], axis=AX.X), R=[b], W=[b])
    P.add("dve", lambda e: e.reciprocal(out=sm[:, 1:2], in_=sm[:, 1:2]), R=[b], W=[b])
    P.add("dve", lambda e: e.tensor_scalar(out=C.gates[:, t, :], in0=ex[:], scalar1=sm[:, 1:2], scalar2=None, op0=ALU.mult),
          R=[b], W=[C.b_gates[t]])


def load_ln_params(C, lng, lnb, blnp, g_row, b_row):
    P = C.P
    P.add("sp", lambda e: e.dma_start(out=lng[:], in_=bcast_rows(g_row)), W=[blnp], dma=True)
    P.add("sp", lambda e: e.dma_start(out=lnb[:], in_=bcast_rows(b_row)), W=[blnp], dma=True)


def moe_phase(C, li, T):
    P, nc = C.P, C.nc
    h, hT = C.h, C.hT
    with ExitStack() as es:
        w1g = es.enter_context(nc.sbuf_tensor("m_w1g_%d" % li, [128, 2, 8, 512], BF16))
        w1u = es.enter_context(nc.sbuf_tensor("m_w1u_%d" % li, [128, 2, 8, 512], BF16))
        w2 = es.enter_context(nc.sbuf_tensor("m_w2_%d" % li, [128, 2, 4, 1024], BF16))
        actT = es.enter_context(nc.sbuf_tensor("m_actT_%d" % li, [128, 2, 4, 512], BF16))
        g1 = es.enter_context(nc.sbuf_tensor("m_g1_%d" % li, [128, 2, 512], F32))
        sg = es.enter_context(nc.sbuf_tensor("m_sg_%d" % li, [128, 2, 512], F32))
        u1 = es.enter_context(nc.sbuf_tensor("m_u1_%d" % li, [128, 2, 512], F32))
        braw = es.enter_context(nc.sbuf_tensor("m_braw_%d" % li, [128, 4, 128], F32))
        bguT = es.enter_context(nc.sbuf_tensor("m_bguT_%d" % li, [128, 512], F32))
        bd = es.enter_context(nc.sbuf_tensor("m_bd_%d" % li, [NE, D], F32))
        gT = es.enter_context(nc.sbuf_tensor("m_gT_%d" % li, [NE, 128], F32))
        lng = es.enter_context(nc.sbuf_tensor("m_lng_%d" % li, [128, D], F32))
        lnb = es.enter_context(nc.sbuf_tensor("m_lnb_%d" % li, [128, D], F32))
        b_w = [Buf("w%d" % i) for i in range(2)]
        b_act = [Buf() for _ in range(2)]
        b_g1 = [Buf() for _ in range(2)]
        b_sg = [Buf() for _ in range(2)]
        b_u1 = [Buf() for _ in range(2)]
        b_misc = Buf()
        b_gT = Buf()
        blnp = Buf()
        load_ln_params(C, lng, lnb, blnp, T["ln_g"][li, 1, :], T["ln_b"][li, 1, :])
        P.add("sp", lambda e: e.dma_start(out=braw[:], in_=T["moe_b_gate_up_%d" % li].rearrange("e (c p) -> (e c) p", p=128)
                                          .rearrange("(b r) p -> r b p", r=128)), W=[b_misc], dma=True)
        P.add("sp", lambda e: e.dma_start(out=bd[:], in_=T["moe_b_down_%d" % li]), W=[b_misc], dma=True)
        for bb in range(4):
            ps, bps = C.ps[bb % 2], C.bps[bb % 2]
            P.add("pe", lambda e, bb=bb, ps=ps: e.transpose(out=ps[:, 0:128], in_=braw[:, bb, :], identity=C.ident[:]),
                  R=[b_misc, C.b_const], W=[bps])
            P.add("act", lambda e, bb=bb, ps=ps: e.copy(out=bguT[:, bb * 128:(bb + 1) * 128], in_=ps[:, 0:128]), R=[bps], W=[b_misc])
        for t in range(NT):
            ps, bps = C.ps[2], C.bps[2]
            P.add("pe", lambda e, t=t: e.transpose(out=ps[0:NE, 0:128], in_=C.gates[:, t, :], identity=C.ident[:]),
                  R=[C.b_gates[t], C.b_const], W=[bps])
            P.add("act", lambda e: e.copy(out=gT[:], in_=ps[0:NE, 0:128]), R=[bps], W=[b_gT])
            for n in range(2):
                po, bpo = C.ps[n], C.bps[n]
                P.add("pe", lambda e, n=n, po=po: e.matmul(po[:], lhsT=gT[:], rhs=bd[:, n * 512:(n + 1) * 512], start=True, stop=True),
                      R=[b_gT, b_misc], W=[bpo])
                P.add("dve", lambda e, n=n, po=po, t=t: e.scalar_tensor_tensor(out=h[:, t, n * 512:(n + 1) * 512], in0=h[:, t, n * 512:(n + 1) * 512],
                                                                               scalar=ALPHA, in1=po[:], op0=ALU.mult, op1=ALU.add),
                      R=[bpo, C.bh[t]], W=[C.bh[t]])
        bg3 = bguT[:].rearrange("p (e c) -> p e c", c=16)
        P.add("dve", lambda e: e.tensor_scalar(out=bg3[:, :, 8:16], in0=bg3[:, :, 8:16], scalar1=1.0, scalar2=None, op0=ALU.add), R=[b_misc], W=[b_misc])
        wgu = T["moe_w_gate_up_%d" % li]
        wd = T["moe_w_down_%d" % li]

        def w1_unit(ex, hf, s, g, a):
            bw = b_w[s]
            bhTg = [C.bhT[g * 4 + i] for i in range(4)]
            for j in range(4):
                q = C.q_rr % 2
                C.q_rr += 1
                pg, bpg = C.ps[3 + q], C.bps[3 + q]
                pu, bpu = C.ps[5 + q], C.bps[5 + q]
                for k in range(8):
                    P.add("pe", lambda e, k=k, j=j, pg=pg: e.matmul(pg[:], lhsT=w1g[:, s, k, j * 128:(j + 1) * 128],
                                                                  rhs=hT[:, k, g * 512:(g + 1) * 512], start=(k == 0), stop=(k == 7)),
                          R=[bw] + bhTg, W=[bpg])
                for k in range(8):
                    P.add("pe", lambda e, k=k, j=j, pu=pu: e.matmul(pu[:], lhsT=w1u[:, s, k, j * 128:(j + 1) * 128],
                                                                  rhs=hT[:, k, g * 512:(g + 1) * 512], start=(k == 0), stop=(k == 7)),
                          R=[bw] + bhTg, W=[bpu])
                cg = ex * 16 + hf * 4 + j
                cu = ex * 16 + 8 + hf * 4 + j
                P.add("dve", lambda e, q=q, pg=pg, cg=cg: e.tensor_scalar(out=g1[:, q, :], in0=pg[:], scalar1=bguT[:, cg:cg + 1], scalar2=7.0,
                                                                         op0=ALU.add, op1=ALU.min), R=[bpg, b_misc], W=[b_g1[q]])
                P.add("act", lambda e, q=q: e.activation(out=sg[:, q, :], in_=g1[:, q, :], func=AF.Sigmoid, scale=1.702), R=[b_g1[q]], W=[b_sg[q]])
                P.add("dve", lambda e, q=q, pu=pu, cu=cu: e.tensor_scalar(out=u1[:, q, :], in0=pu[:], scalar1=bguT[:, cu:cu + 1], scalar2=8.0,
                                                                         op0=ALU.add, op1=ALU.min), R=[bpu, b_misc], W=[b_u1[q]])
                P.add("dve", lambda e, q=q: e.tensor_tensor(out=sg[:, q, :], in0=sg[:, q, :], in1=g1[:, q, :], op=ALU.mult),
                      R=[b_sg[q], b_g1[q]], W=[b_sg[q]])
                P.add("dve", lambda e, q=q, j=j: e.scalar_tensor_tensor(out=actT[:, a, j, :], in0=u1[:, q, :], scalar=-6.0, in1=sg[:, q, :],
                                                                       op0=ALU.max, op1=ALU.mult), R=[b_sg[q], b_u1[q]], W=[b_act[a]])

        def w2_unit(ex, hf, s, g, a):
            bw = b_w[s]
            for tt in range(4):
                t = g * 4 + tt
                for n in range(2):
                    y = C.y_rr % 3
                    C.y_rr += 1
                    py, bpy = C.ps[(0, 1, 7)[y]], C.bps[(0, 1, 7)[y]]
                    for j in range(4):
                        P.add("pe", lambda e, j=j, tt=tt, n=n, py=py: e.matmul(py[:], lhsT=actT[:, a, j, tt * 128:(tt + 1) * 128],
                                                                             rhs=w2[:, s, j, n * 512:(n + 1) * 512], start=(j == 0), stop=(j == 3)),
                              R=[b_act[a], bw], W=[bpy])
                    P.add("dve", lambda e, t=t, n=n, py=py: e.scalar_tensor_tensor(
                        out=h[:, t, n * 512:(n + 1) * 512], in0=py[:], scalar=C.gates[:, t, ex:ex + 1], in1=h[:, t, n * 512:(n + 1) * 512],
                        op0=ALU.mult, op1=ALU.add), R=[bpy, C.b_gates[t], C.bh[t]], W=[C.bh[t]])

        it = 0
        prev = None
        for ex in range(C.n_experts):
            for hf in range(2):
                s = it % 2
                it += 1
                bw = b_w[s]
                P.add("pool", lambda e, s=s, ex=ex, hf=hf: e.dma_start(
                    out=w1g[:, s], in_=wgu[ex, :, hf * 512:(hf + 1) * 512].rearrange("(k p) n -> p k n", p=128)), W=[bw], dma=True)
                P.add("pool", lambda e, s=s, ex=ex, hf=hf: e.dma_start(
                    out=w1u[:, s], in_=wgu[ex, :, 1024 + hf * 512:1024 + (hf + 1) * 512].rearrange("(k p) n -> p k n", p=128)), W=[bw], dma=True)
                P.add("pool", lambda e, s=s, ex=ex, hf=hf: e.dma_start(
                    out=w2[:, s], in_=wd[ex, hf * 512:(hf + 1) * 512, :].rearrange("(j p) n -> p j n", p=128)), W=[bw], dma=True)
                for g in range(4):
                    a = C.act_rr % 2
                    C.act_rr += 1
                    w1_unit(ex, hf, s, g, a)
                    if prev is not None:
                        w2_unit(*prev)
                    prev = (ex, hf, s, g, a)
        if prev is not None:
            w2_unit(*prev)
        for t in range(NT):
            ln_tile(C, t, lng, lnb, blnp, router=None)
        P.flush()


def out_proj_ln(C, li, T, srcT, nchunks, w_dram, R_):
    P, nc = C.P, C.nc
    h = C.h
    with ExitStack() as es:
        wo = es.enter_context(nc.sbuf_tensor("o_w_%d" % li, [128, nchunks, D], BF16))
        lng = es.enter_context(nc.sbuf_tensor("o_lng_%d" % li, [128, D], F32))
        lnb = es.enter_context(nc.sbuf_tensor("o_lnb_%d" % li, [128, D], F32))
        bw, blnp = Buf(), Buf()
        P.add("pool", lambda e: e.dma_start(out=wo[:], in_=w_dram.rearrange("(k p) n -> p k n", p=128)), W=[bw], dma=True)
        load_ln_params(C, lng, lnb, blnp, T["ln_g"][li, 0, :], T["ln_b"][li, 0, :])
        for t in range(NT):
            for n in range(2):
                po, bpo = C.ps[3 + n + 2 * (t % 2)], C.bps[3 + n + 2 * (t % 2)]
                for f in range(nchunks):
                    P.add("pe", lambda e, f=f, n=n, t=t, po=po: e.matmul(po[:], lhsT=srcT[:, f, t * 128:(t + 1) * 128], rhs=wo[:, f, n * 512:(n + 1) * 512],
                                                                      start=(f == 0), stop=(f == nchunks - 1)), R=[C.b_srcT, bw], W=[bpo])
                P.add("dve", lambda e, n=n, t=t, po=po: e.scalar_tensor_tensor(out=h[:, t, n * 512:(n + 1) * 512], in0=h[:, t, n * 512:(n + 1) * 512],
                                                                             scalar=ALPHA, in1=po[:], op0=ALU.mult, op1=ALU.add),
                      R=[bpo, C.bh[t]], W=[C.bh[t]])
            ln_tile(C, t, lng, lnb, blnp, router=R_)
        P.flush()


def moba_phase(C, li, T, R_):
    P, nc = C.P, C.nc
    h, hT = C.h, C.hT
    j = li // 3
    w_in = T["a_w_in_%d" % j]
    H = 8
    SCALE = 128 ** -0.5
    BIG = 30000.0
    with nc.sbuf_tensor("a_aT_%d" % li, [128, 8, S], BF16) as aT:
        C.b_srcT = Buf("aT")
        with ExitStack() as es:
            wq = es.enter_context(nc.sbuf_tensor("a_wq_%d" % li, [128, 2, 8, 384], BF16))
            qT = es.enter_context(nc.sbuf_tensor("a_qT_%d" % li, [128, 2, S], BF16))
            kT = es.enter_context(nc.sbuf_tensor("a_kT_%d" % li, [128, 2, S], BF16))
            V = es.enter_context(nc.sbuf_tensor("a_V_%d" % li, [128, 2, NT, 128], BF16))
            kms = es.enter_context(nc.sbuf_tensor("a_kms_%d" % li, [128, 8], F32))
            kmb = es.enter_context(nc.sbuf_tensor("a_kmb_%d" % li, [128, 8], BF16))
            g2 = es.enter_context(nc.sbuf_tensor("a_g2_%d" % li, [128, 128], F32))
            m8 = es.enter_context(nc.sbuf_tensor("a_m8_%d" % li, [128, 8], F32))
            bm = es.enter_context(nc.sbuf_tensor("a_bm_%d" % li, [128, 128], F32))
            cm = es.enter_context(nc.sbuf_tensor("a_cm_%d" % li, [128, 128], F32))
            vm = es.enter_context(nc.sbuf_tensor("a_vm_%d" % li, [128, 128], F32))
            vm2 = es.enter_context(nc.sbuf_tensor("a_vm2_%d" % li, [128, 128], F32))
            rel = es.enter_context(nc.sbuf_tensor("a_rel_%d" % li, [128, 8, 2, 128], F32))
            caus = es.enter_context(nc.sbuf_tensor("a_caus_%d" % li, [128, 128], F32))
            cb = es.enter_context(nc.sbuf_tensor("a_cb_%d" % li, [128, 8], F32))
            sc = es.enter_context(nc.sbuf_tensor("a_s_%d" % li, [128, 2, S], F32))
            pb = es.enter_context(nc.sbuf_tensor("a_p_%d" % li, [128, S], BF16))
            pT = es.enter_context(nc.sbuf_tensor("a_pT_%d" % li, [128, 2, 4, 128], BF16))
            stt = es.enter_context(nc.sbuf_tensor("a_st_%d" % li, [128, 2, 4], F32))
            b_wq = [Buf() for _ in range(2)]
            b_q = [Buf() for _ in range(2)]
            b_k = [Buf() for _ in range(2)]
            b_v = [Buf() for _ in range(2)]
            b_km, b_gate, b_cst = Buf(), Buf(), Buf()
            b_s = [Buf() for _ in range(2)]
            b_p = [Buf()] * 2
            b_pT = [Buf() for _ in range(2)]
            b_st = [Buf() for _ in range(2)]
            SK = ""
            if "rel" not in SK:
                P.add("sp", lambda e: e.dma_start(out=rel[:], in_=T["relT"].rearrange("h d i j -> i h d j")), W=[b_cst], dma=True)
            P.add("sp", lambda e: e.dma_start(out=caus[:], in_=T["causal"]), W=[b_cst], dma=True)
            P.add("sp", lambda e: e.dma_start(out=vm[:], in_=T["vmask"]), W=[b_cst], dma=True)
            P.add("sp", lambda e: e.dma_start(out=vm2[:], in_=T["vmask2"]), W=[b_cst], dma=True)
            if "cb" not in SK:
                P.add("sp", lambda e: e.dma_start(out=cb[:], in_=bcast_rows(T["rel_bias"][31, :])), W=[b_cst], dma=True)
            for hh in range(H):
                P.add("pool", lambda e, hh=hh: e.tensor_tensor(out=rel[:, hh, 0, :], in0=rel[:, hh, 0, :], in1=caus[:], op=ALU.add), R=[b_cst], W=[b_cst])
            allT = list(C.bhT)
            for hh in range(H):
                s = hh % 2
                for c3 in range(3):
                    P.add("pool", lambda e, s=s, c3=c3, hh=hh: e.dma_start(
                        out=wq[:, s, :, c3 * 128:(c3 + 1) * 128],
                        in_=w_in[:, c3 * 1024 + hh * 128:c3 * 1024 + (hh + 1) * 128].rearrange("(k p) n -> p k n", p=128)), W=[b_wq[s]], dma=True)
                for g in range(4):
                    pq, bpq = C.ps[3], C.bps[3]
                    pk, bpk = C.ps[4], C.bps[4]
                    for k in range(8):
                        P.add("pe", lambda e, k=k, g=g, s=s: e.matmul(pq[:], lhsT=wq[:, s, k, 0:128], rhs=hT[:, k, g * 512:(g + 1) * 512],
                                                                    start=(k == 0), stop=(k == 7)), R=[b_wq[s]] + allT[g * 4:g * 4 + 4], W=[bpq])
                    P.add("act", lambda e, g=g, s=s: e.copy(out=qT[:, s, g * 512:(g + 1) * 512], in_=pq[:]), R=[bpq], W=[b_q[s]])
                    for k in range(8):
                        P.add("pe", lambda e, k=k, g=g, s=s: e.matmul(pk[:], lhsT=wq[:, s, k, 128:256], rhs=hT[:, k, g * 512:(g + 1) * 512],
                                                                    start=(k == 0), stop=(k == 7)), R=[b_wq[s]] + allT[g * 4:g * 4 + 4], W=[bpk])
                    for b2 in range(2):
                        P.add("act", lambda e, g=g, s=s, b2=b2: e.activation(out=kT[:, s, g * 512 + b2 * 256:g * 512 + (b2 + 1) * 256],
                                                                           in_=pk[:, b2 * 256:(b2 + 1) * 256], func=AF.Copy,
                                                                           accum_out=kms[:, 2 * g + b2:2 * g + b2 + 1]), R=[bpk], W=[b_k[s], b_km])
                P.add("dve", lambda e: e.tensor_scalar(out=kmb[:], in0=kms[:], scalar1=1.0 / 256.0, scalar2=None, op0=ALU.mult), R=[b_km], W=[b_km])
                for g in range(4):
                    pv, bpv = C.ps[5], C.bps[5]
                    for tt in range(4):
                        t = g * 4 + tt
                        for k in range(8):
                            P.add("pe", lambda e, k=k, t=t, tt=tt, s=s: e.matmul(pv[:, tt * 128:(tt + 1) * 128], lhsT=hT[:, k, t * 128:(t + 1) * 128],
                                                                               rhs=wq[:, s, k, 256:384], start=(k == 0), stop=(k == 7)),
                                  R=[b_wq[s], C.bhT[t]], W=[bpv])
                    P.add("act", lambda e, g=g, s=s: e.copy(out=V[:, s, g * 4:g * 4 + 4, :], in_=pv[:].rearrange("p (a c) -> p a c", a=4)),
                          R=[bpv], W=[b_v[s]])
                MD = 9
                if MD < 2:
                    continue
                pg, bpg = C.ps[6], C.bps[6]
                for qt in range(NT):
                    P.add("pe", lambda e, qt=qt, s=s: e.matmul(pg[:, qt * 8:(qt + 1) * 8], lhsT=qT[:, s, qt * 128:(qt + 1) * 128], rhs=kmb[:],
                                                             start=True, stop=True), R=[b_q[s], b_km], W=[bpg])
                P.add("dve", lambda e: e.tensor_tensor(out=g2[:], in0=pg[:, 0:128], in1=vm[:], op=ALU.add), R=[bpg, b_cst], W=[b_gate])
                for qt in range(NT):
                    P.add("dve", lambda e, qt=qt: e.max(out=m8[:], in_=g2[:, qt * 8:(qt + 1) * 8]), R=[b_gate], W=[b_gate])
                    P.add("dve", lambda e, qt=qt: e.tensor_scalar(out=bm[:, qt * 8:(qt + 1) * 8], in0=g2[:, qt * 8:(qt + 1) * 8], scalar1=m8[:, 2:3],
                                                                  scalar2=BIG, op0=ALU.is_ge, op1=ALU.mult), R=[b_gate], W=[b_gate])
                P.add("dve", lambda e: e.tensor_tensor(out=bm[:], in0=bm[:], in1=vm2[:], op=ALU.add), R=[b_gate, b_cst], W=[b_gate])
                P.add("dve", lambda e, hh=hh: e.tensor_scalar(out=cm[:], in0=bm[:], scalar1=cb[:, hh:hh + 1], scalar2=None, op0=ALU.add),
                      R=[b_gate, b_cst], W=[b_gate])
                for qt in range(NT if MD >= 3 else 0):
                    qb = qt // 2
                    nk = qt + 1
                    z = qt % 2
                    for c in range((nk + 3) // 4):
                        w = min(4, nk - c * 4)
                        pss, bpss = C.ps[3 + (C.q_rr % 2)], C.bps[3 + (C.q_rr % 2)]
                        C.q_rr += 1
                        P.add("pe", lambda e, c=c, w=w, qt=qt, s=s, pss=pss: e.matmul(pss[:, 0:w * 128], lhsT=qT[:, s, qt * 128:(qt + 1) * 128],
                                                                                   rhs=kT[:, s, c * 512:c * 512 + w * 128], start=True, stop=True),
                              R=[b_q[s], b_k[s]], W=[bpss])
                        for i in range(w):
                            kt = c * 4 + i
                            n = kt // 2
                            src = pss[:, i * 128:(i + 1) * 128]
                            dst = sc[:, z, kt * 128:(kt + 1) * 128]
                            if kt == qt:
                                P.add("dve", lambda e, src=src, dst=dst, hh=hh: e.scalar_tensor_tensor(out=dst, in0=src, scalar=SCALE, in1=rel[:, hh, 0, :],
                                                                                                     op0=ALU.mult, op1=ALU.add), R=[bpss, b_cst], W=[b_s[z]])
                            elif kt == qt - 1:
                                P.add("dve", lambda e, src=src, dst=dst, hh=hh: e.scalar_tensor_tensor(out=dst, in0=src, scalar=SCALE, in1=rel[:, hh, 1, :],
                                                                                                     op0=ALU.mult, op1=ALU.add), R=[bpss, b_cst], W=[b_s[z]])
                                if n < qb:
                                    P.add("dve", lambda e, dst=dst, qt=qt, n=n: e.tensor_scalar(out=dst, in0=dst, scalar1=bm[:, qt * 8 + n:qt * 8 + n + 1],
                                                                                              scalar2=None, op0=ALU.add), R=[b_s[z], b_gate], W=[b_s[z]])
                            else:
                                P.add("dve", lambda e, src=src, dst=dst, qt=qt, n=n: e.tensor_scalar(out=dst, in0=src, scalar1=SCALE,
                                                                                                   scalar2=cm[:, qt * 8 + n:qt * 8 + n + 1],
                                                                                                   op0=ALU.mult, op1=ALU.add), R=[bpss, b_gate], W=[b_s[z]])
                    L = nk * 128
                    if MD < 4:
                        continue
                    P.add("dve", lambda e, z=z, L=L: e.reduce_max(out=stt[:, z, 0:1], in_=sc[:, z, 0:L], axis=AX.X), R=[b_s[z]], W=[b_st[z]])
                    P.add("dve", lambda e, z=z: e.tensor_scalar(out=stt[:, z, 1:2], in0=stt[:, z, 0:1], scalar1=-1.0, scalar2=None, op0=ALU.mult),
                          R=[b_st[z]], W=[b_st[z]])
                    P.add("act", lambda e, z=z, L=L: e.activation(out=sc[:, z, 0:L], in_=sc[:, z, 0:L], func=AF.Exp, bias=stt[:, z, 1:2], scale=1.0,
                                                                  accum_out=stt[:, z, 2:3]), R=[b_s[z], b_st[z]], W=[b_s[z], b_st[z]])
                    P.add("dve", lambda e, z=z: e.reciprocal(out=stt[:, z, 3:4], in_=stt[:, z, 2:3]), R=[b_st[z]], W=[b_st[z]])
                    P.add("act", lambda e, z=z, L=L: e.activation(out=pb[:, 0:L], in_=sc[:, z, 0:L], func=AF.Copy, scale=stt[:, z, 3:4]),
                          R=[b_s[z], b_st[z]], W=[b_p[z]])
                    po, bpo = C.ps[5 + z], C.bps[5 + z]
                    if MD < 5:
                        continue
                    for c in range((nk + 3) // 4):
                        w = min(4, nk - c * 4)
                        y = C.y_rr % 2
                        C.y_rr += 1
                        ptp, bptp = C.ps[(0, 1)[y]], C.bps[(0, 1)[y]]
                        ptb = ptp[:].bitcast(BF16)
                        for i in range(w):
                            kt = c * 4 + i
                            P.add("pe", lambda e, i=i, kt=kt, z=z, ptb=ptb: e.transpose(out=ptb[:, i * 128:(i + 1) * 128], in_=pb[:, kt * 128:(kt + 1) * 128],
                                                                                      identity=C.identb[:]), R=[b_p[z], C.b_const], W=[bptp])
                        P.add("act", lambda e, w=w, y=y, ptb=ptb: e.copy(out=pT[:, y, 0:w, :], in_=ptb[:, 0:w * 128].rearrange("p (a c) -> p a c", a=w)),
                              R=[bptp], W=[b_pT[y]])
                        for i in range(w):
                            kt = c * 4 + i
                            P.add("pe", lambda e, i=i, kt=kt, y=y, s=s, qt=qt, nk=nk, po=po: e.matmul(po[:, 0:128], lhsT=V[:, s, kt, :], rhs=pT[:, y, i, :],
                                                                                                   start=(kt == 0), stop=(kt == nk - 1)),
                                  R=[b_v[s], b_pT[y]], W=[bpo])
                    P.add("act", lambda e, po=po, hh=hh, qt=qt: e.copy(out=aT[:, hh, qt * 128:(qt + 1) * 128], in_=po[:, 0:128]), R=[bpo], W=[C.b_srcT])
            P.flush()
        out_proj_ln(C, li, T, aT, 8, T["a_w_out_%d" % j], R_)


def gmlp_phase(C, li, T, R_):
    P, nc = C.P, C.nc
    h, hT = C.h, C.hT
    j = li // 3
    w_in = T["b_w_in_%d" % j]
    w_out = T["b_w_out_%d" % j]
    with ExitStack() as es:
        lng = es.enter_context(nc.sbuf_tensor("g_lng_%d" % li, [128, D], F32))
        lnb = es.enter_context(nc.sbuf_tensor("g_lnb_%d" % li, [128, D], F32))
        wi = es.enter_context(nc.sbuf_tensor("g_wi_%d" % li, [128, 2, 8, 512], BF16))
        wo = es.enter_context(nc.sbuf_tensor("g_wo_%d" % li, [128, 2, 4, 1024], BF16))
        yT = es.enter_context(nc.sbuf_tensor("g_yT_%d" % li, [128, 16, 256], BF16))
        v = es.enter_context(nc.sbuf_tensor("g_v_%d" % li, [128, 2, 2048], F32))
        vn = es.enter_context(nc.sbuf_tensor("g_vn_%d" % li, [128, 2, 2048], BF16))
        gb = es.enter_context(nc.sbuf_tensor("g_gb_%d" % li, [128, 2, 2048], F32))
        ws = es.enter_context(nc.sbuf_tensor("g_ws_%d" % li, [128, 8, 128], F32))
        wsT = es.enter_context(nc.sbuf_tensor("g_wsT_%d" % li, [128, 8, 128], BF16))
        tril = es.enter_context(nc.sbuf_tensor("g_tril_%d" % li, [128, 128], F32))
        bs = es.enter_context(nc.sbuf_tensor("g_bs_%d" % li, [1, 1024], F32))
        ones = es.enter_context(nc.sbuf_tensor("g_ones_%d" % li, [1, 128], F32))
        st = es.enter_context(nc.sbuf_tensor("g_st_%d" % li, [128, 4, 6], F32))
        mv = es.enter_context(nc.sbuf_tensor("g_mv_%d" % li, [128, 2], F32))
        rs = es.enter_context(nc.sbuf_tensor("g_rs_%d" % li, [128, 1], F32))
        blnp, b_cst, b_wsT, b_yT, b_st = Buf(), Buf(), Buf(), Buf(), Buf()
        b_wi = [Buf(), Buf()]
        b_wo = [Buf(), Buf()]
        b_v = [Buf(), Buf()]
        b_vn = [Buf(), Buf()]
        load_ln_params(C, lng, lnb, blnp, T["ln_g"][li, 0, :], T["ln_b"][li, 0, :])
        P.add("sp", lambda e: e.dma_start(out=gb[:, 0, :], in_=bcast_rows(T["b_ln_g_%d" % j])), W=[b_cst], dma=True)
        P.add("sp", lambda e: e.dma_start(out=gb[:, 1, :], in_=bcast_rows(T["b_ln_b_%d" % j])), W=[b_cst], dma=True)
        P.add("sp", lambda e: e.dma_start(out=ws[:], in_=T["b_w_s_%d" % j].rearrange("g t s -> t g s")), W=[b_cst], dma=True)
        P.add("sp", lambda e: e.dma_start(out=tril[:], in_=T["tril"]), W=[b_cst], dma=True)
        P.add("sp", lambda e: e.dma_start(out=bs[:], in_=T["b_b_s_%d" % j].rearrange("(o g) t -> o (g t)", o=1)), W=[b_cst], dma=True)
        P.add("pool", lambda e: e.memset(ones[:], 1.0), W=[b_cst])
        for g in range(8):
            P.add("dve", lambda e, g=g: e.tensor_tensor(out=ws[:, g, :], in0=ws[:, g, :], in1=tril[:], op=ALU.mult), R=[b_cst], W=[b_cst])
        for half in range(2):
            ps, bps = C.ps[half], C.bps[half]
            for gg in range(4):
                g = half * 4 + gg
                P.add("pe", lambda e, g=g, gg=gg, ps=ps: e.transpose(out=ps[:, gg * 128:(gg + 1) * 128], in_=ws[:, g, :], identity=C.ident[:]),
                      R=[b_cst, C.b_const], W=[bps])
            P.add("act", lambda e, ps=ps, half=half: e.copy(out=wsT[:, half * 4:half * 4 + 4, :], in_=ps[:].rearrange("p (a c) -> p a c", a=4)),
                  R=[bps], W=[b_wsT])
        it = 0
        iw = 0
        acc_banks = (5, 6, 7, 2)
        DBG = 9
        for tg in range(8 if DBG >= 2 else 0):
            c0 = tg * 256
            tiles = (2 * tg, 2 * tg + 1)
            bhTg = [C.bhT[t] for t in tiles]
            for cg in range(8):
                s = it % 2
                it += 1
                P.add("pool", lambda e, s=s, cg=cg: e.dma_start(out=wi[:, s], in_=w_in[:, cg * 512:(cg + 1) * 512].rearrange("(k p) n -> p k n", p=128)),
                      W=[b_wi[s]], dma=True)
                if cg < 4:
                    for jj in range(4):
                        fc = cg * 4 + jj
                        q = C.q_rr % 2
                        C.q_rr += 1
                        pu, bpu = C.ps[3 + q], C.bps[3 + q]
                        for k in range(8):
                            P.add("pe", lambda e, k=k, jj=jj, s=s, pu=pu, c0=c0: e.matmul(pu[:, 0:256], lhsT=wi[:, s, k, jj * 128:(jj + 1) * 128],
                                                                                      rhs=hT[:, k, c0:c0 + 256], start=(k == 0), stop=(k == 7)),
                                  R=[b_wi[s]] + bhTg, W=[bpu])
                        P.add("act", lambda e, fc=fc, pu=pu: e.activation(out=yT[:, fc, :], in_=pu[:, 0:256], func=AF.Gelu), R=[bpu], W=[b_yT])
                else:
                    for ti in range(2):
                        t = tiles[ti]
                        q = C.q_rr % 2
                        C.q_rr += 1
                        pv, bpv = C.ps[3 + q], C.bps[3 + q]
                        for k in range(8):
                            P.add("pe", lambda e, k=k, t=t, s=s, pv=pv: e.matmul(pv[:], lhsT=hT[:, k, t * 128:(t + 1) * 128], rhs=wi[:, s, k, :],
                                                                               start=(k == 0), stop=(k == 7)), R=[b_wi[s], C.bhT[t]], W=[bpv])
                        P.add("act", lambda e, ti=ti, cg=cg, pv=pv: e.activation(out=v[:, ti, (cg - 4) * 512:(cg - 3) * 512], in_=pv[:], func=AF.Gelu),
                              R=[bpv], W=[b_v[ti]])
            for ti in range(2 if DBG >= 3 else 0):
                for c in range(4):
                    P.add("dve", lambda e, c=c, ti=ti: e.bn_stats(out=st[:, c, :], in_=v[:, ti, c * 512:(c + 1) * 512]), R=[b_v[ti]], W=[b_st])
                P.add("dve", lambda e: e.bn_aggr(out=mv[:], in_=st[:]), R=[b_st], W=[b_st])
                P.add("act", lambda e: e.activation(out=rs[:], in_=mv[:, 1:2], func=AF.Sqrt, bias=EPS, scale=1.0), R=[b_st], W=[b_st])
                P.add("dve", lambda e: e.reciprocal(out=rs[:], in_=rs[:]), R=[b_st], W=[b_st])
                P.add("dve", lambda e, ti=ti: e.tensor_scalar(out=v[:, ti, :], in0=v[:, ti, :], scalar1=mv[:, 0:1], scalar2=rs[:, 0:1],
                                                              op0=ALU.subtract, op1=ALU.mult), R=[b_v[ti], b_st], W=[b_v[ti]])
                P.add("pool", lambda e, ti=ti: e.tensor_tensor(out=v[:, ti, :], in0=v[:, ti, :], in1=gb[:, 0, :], op=ALU.mult), R=[b_v[ti], b_cst], W=[b_v[ti]])
                P.add("pool", lambda e, ti=ti: e.tensor_tensor(out=vn[:, ti, :], in0=v[:, ti, :], in1=gb[:, 1, :], op=ALU.add), R=[b_v[ti], b_cst], W=[b_vn[ti]])
            for ti in range(2 if DBG >= 4 else 0):
                for bk in range(4):
                    q = C.q_rr % 2
                    C.q_rr += 1
                    pm, bpm = C.ps[3 + q], C.bps[3 + q]
                    for jj in range(4):
                        fc = bk * 4 + jj
                        g = fc // 2
                        P.add("pe", lambda e, jj=jj, fc=fc, g=g, ti=ti, pm=pm: e.matmul(pm[:, jj * 128:(jj + 1) * 128], lhsT=vn[:, ti, fc * 128:(fc + 1) * 128],
                                                                                      rhs=wsT[:, g, :], start=True, stop=False), R=[b_vn[ti], b_wsT], W=[bpm])
                        P.add("pe", lambda e, jj=jj, g=g, pm=pm: e.matmul(pm[:, jj * 128:(jj + 1) * 128], lhsT=ones[0:1, :], rhs=bs[0:1, g * 128:(g + 1) * 128],
                                                                        start=False, stop=True), R=[b_cst], W=[bpm])
                    P.add("dve", lambda e, bk=bk, ti=ti, pm=pm: e.tensor_tensor(out=yT[:, bk * 4:bk * 4 + 4, ti * 128:(ti + 1) * 128],
                                                                              in0=pm[:].rearrange("p (a c) -> p a c", a=4),
                                                                              in1=yT[:, bk * 4:bk * 4 + 4, ti * 128:(ti + 1) * 128], op=ALU.mult),
                          R=[bpm, b_yT], W=[b_yT])
            if DBG < 5:
                continue
            for wc in range(4):
                s2 = iw % 2
                iw += 1
                P.add("pool", lambda e, s2=s2, wc=wc: e.dma_start(out=wo[:, s2], in_=w_out[wc * 512:(wc + 1) * 512, :].rearrange("(f p) n -> p f n", p=128)),
                      W=[b_wo[s2]], dma=True)
                for ti in range(2):
                    for n in range(2):
                        bi = acc_banks[ti * 2 + n]
                        po, bpo = C.ps[bi], C.bps[bi]
                        for ff in range(4):
                            P.add("pe", lambda e, wc=wc, ff=ff, ti=ti, n=n, s2=s2, po=po: e.matmul(
                                po[:], lhsT=yT[:, wc * 4 + ff, ti * 128:(ti + 1) * 128], rhs=wo[:, s2, ff, n * 512:(n + 1) * 512],
                                start=(wc == 0 and ff == 0), stop=(wc == 3 and ff == 3)), R=[b_yT, b_wo[s2]], W=[bpo])
            for ti in range(2):
                t = tiles[ti]
                for n in range(2):
                    bi = acc_banks[ti * 2 + n]
                    po, bpo = C.ps[bi], C.bps[bi]
                    P.add("dve", lambda e, n=n, t=t, po=po: e.scalar_tensor_tensor(out=h[:, t, n * 512:(n + 1) * 512], in0=h[:, t, n * 512:(n + 1) * 512],
                                                                                 scalar=ALPHA, in1=po[:], op0=ALU.mult, op1=ALU.add),
                          R=[bpo, C.bh[t]], W=[C.bh[t]])
            for ti in range(2 if DBG >= 6 else 0):
                ln_tile(C, tiles[ti], lng, lnb, blnp, router=(R_ if DBG >= 7 else None))
        P.flush()


def gla_phase(C, li, T, R_):
    P, nc = C.P, C.nc
    h, hT = C.h, C.hT
    j = li // 3
    w_in = T["c_w_in_%d" % j]
    SCALE = 128 ** -0.5
    with nc.sbuf_tensor("c_oT_%d" % li, [128, 8, S], BF16) as oT:
        C.b_srcT = Buf("oT")
        with ExitStack() as es:
            wq = es.enter_context(nc.sbuf_tensor("c_wq_%d" % li, [128, 2, 8, 768], BF16))
            wg = es.enter_context(nc.sbuf_tensor("c_wg_%d" % li, [128, 8, 16], BF16))
            gkT = es.enter_context(nc.sbuf_tensor("c_gkT_%d" % li, [32, S], F32))
            wgk = es.enter_context(nc.sbuf_tensor("c_wgk_%d" % li, [32, 512], F32))
            ng = es.enter_context(nc.sbuf_tensor("c_ng_%d" % li, [128, 256], F32))
            tri = es.enter_context(nc.sbuf_tensor("c_tri_%d" % li, [128, 128], F32))
            su = es.enter_context(nc.sbuf_tensor("c_su_%d" % li, [128, 128], F32))
            cmk = es.enter_context(nc.sbuf_tensor("c_cm_%d" % li, [128, 128], F32))
            qk = es.enter_context(nc.sbuf_tensor("c_qk_%d" % li, [128, 2, 2, 128], F32))
            kt = es.enter_context(nc.sbuf_tensor("c_kt_%d" % li, [128, 2, 128], F32))
            vt = es.enter_context(nc.sbuf_tensor("c_vt_%d" % li, [128, 2, 256], BF16))
            gs = es.enter_context(nc.sbuf_tensor("c_gs_%d" % li, [128, 2, 256], F32))
            nl = es.enter_context(nc.sbuf_tensor("c_nl_%d" % li, [128, 2, 128], F32))
            ex = es.enter_context(nc.sbuf_tensor("c_ex_%d" % li, [128, 2, 3, 128], F32))
            qz = es.enter_context(nc.sbuf_tensor("c_qz_%d" % li, [128, 2, 2, 128], BF16))
            ke = es.enter_context(nc.sbuf_tensor("c_ke_%d" % li, [128, 2, 128], BF16))
            krz = es.enter_context(nc.sbuf_tensor("c_krz_%d" % li, [128, 2, 2, 128], BF16))
            att = es.enter_context(nc.sbuf_tensor("c_att_%d" % li, [128, 2, 128], BF16))
            St = es.enter_context(nc.sbuf_tensor("c_S_%d" % li, [128, 256], F32))
            Sb = es.enter_context(nc.sbuf_tensor("c_Sb_%d" % li, [128, 5, 256], BF16))
            jk = es.enter_context(nc.sbuf_tensor("c_jk_%d" % li, [128, 256], F32))
            ot = es.enter_context(nc.sbuf_tensor("c_ot_%d" % li, [128, 2, 256], F32))
            og = es.enter_context(nc.sbuf_tensor("c_og_%d" % li, [128, 2, 256], BF16))
            ss = es.enter_context(nc.sbuf_tensor("c_ss_%d" % li, [128, 2, 2], F32))
            b_cst, b_gk, b_S, b_jk = Buf(), Buf(), Buf(), Buf()
            b_wq = [Buf(), Buf()]
            b_qk = [Buf(), Buf()]
            b_kt = [Buf(), Buf()]
            b_vt = [Buf(), Buf()]
            b_gs = [Buf(), Buf()]
            b_nl = [Buf(), Buf()]
            b_ex = [Buf(), Buf()]
            b_qz = [Buf(), Buf()]
            b_ke = [Buf(), Buf()]
            b_krz = [Buf(), Buf()]
            b_att = [Buf(), Buf()]
            b_Sb = [Buf() for _ in range(5)]
            b_ot = [Buf(), Buf()]
            b_og = [Buf(), Buf()]
            b_ss = [Buf(), Buf()]
            P.add("pool", lambda e: e.dma_start(out=wg[:], in_=w_in[:, 3072:3088].rearrange("(k p) n -> p k n", p=128)), W=[b_cst], dma=True)
            P.add("sp", lambda e: e.dma_start(out=wgk[0:16, :], in_=T["c_w_gk_up_%d" % j]), W=[b_cst], dma=True)
            P.add("sp", lambda e: e.dma_start(out=wgk[16:17, :], in_=T["c_b_gk_%d" % j].rearrange("(o n) -> o n", o=1)), W=[b_cst], dma=True)
            P.add("sp", lambda e: e.dma_start(out=ng[:], in_=bcast_rows(T["c_norm_g_%d" % j])), W=[b_cst], dma=True)
            P.add("sp", lambda e: e.dma_start(out=tri[:], in_=T["gla_tri"]), W=[b_cst], dma=True)
            P.add("sp", lambda e: e.dma_start(out=su[:], in_=T["gla_su"]), W=[b_cst], dma=True)
            P.add("sp", lambda e: e.dma_start(out=cmk[:], in_=T["gla_cm"]), W=[b_cst], dma=True)
            P.add("pool", lambda e: e.memset(gkT[:], 1.0), W=[b_gk])
            for z in range(2):
                P.add("pool", lambda e, z=z: e.memset(qz[:, z], 0.0), W=[b_qz[z]])
                P.add("pool", lambda e, z=z: e.memset(krz[:, z], 0.0), W=[b_krz[z]])
            for g4 in range(4):
                pg, bpg = C.ps[3 + g4 % 2], C.bps[3 + g4 % 2]
                for k in range(8):
                    P.add("pe", lambda e, k=k, g4=g4, pg=pg: e.matmul(pg[0:16, :], lhsT=wg[:, k, :], rhs=hT[:, k, g4 * 512:(g4 + 1) * 512],
                                                                    start=(k == 0), stop=(k == 7)), R=[b_cst] + C.bhT[g4 * 4:g4 * 4 + 4], W=[bpg])
                P.add("act", lambda e, g4=g4, pg=pg: e.copy(out=gkT[0:16, g4 * 512:(g4 + 1) * 512], in_=pg[0:16, :]), R=[bpg], W=[b_gk])

            def front(hd, s, t):
                z = t % 2
                tc = slice(t * 128, (t + 1) * 128)
                bhT = C.bhT[t]
                pa, bpa = C.ps[3], C.bps[3]
                for k in range(8):
                    P.add("pe", lambda e, k=k: e.matmul(pa[:, 0:128], lhsT=wq[:, s, k, 0:128], rhs=hT[:, k, tc], start=(k == 0), stop=(k == 7)),
                          R=[b_wq[s], bhT], W=[bpa])
                for k in range(8):
                    P.add("pe", lambda e, k=k: e.matmul(pa[:, 128:256], lhsT=wq[:, s, k, 128:256], rhs=hT[:, k, tc], start=(k == 0), stop=(k == 7)),
                          R=[b_wq[s], bhT], W=[bpa])
                for k in range(8):
                    P.add("pe", lambda e, k=k: e.matmul(pa[:, 256:384], lhsT=hT[:, k, tc], rhs=wq[:, s, k, 128:256], start=(k == 0), stop=(k == 7)),
                          R=[b_wq[s], bhT], W=[bpa])
                P.add("pe", lambda e: e.matmul(pa[:, 384:512], lhsT=gkT[0:17, tc], rhs=wgk[0:17, hd * 128:(hd + 1) * 128], start=True, stop=True),
                      R=[b_gk, b_cst], W=[bpa])
                P.add("act", lambda e: e.copy(out=qk[:, z], in_=pa[:, 0:256].rearrange("p (a c) -> p a c", a=2)), R=[bpa], W=[b_qk[z]])
                P.add("act", lambda e: e.copy(out=kt[:, z], in_=pa[:, 256:384]), R=[bpa], W=[b_kt[z]])
                P.add("act", lambda e: e.activation(out=nl[:, z], in_=pa[:, 384:512], func=AF.Exp, scale=-1.0), R=[bpa], W=[b_nl[z]])
                P.add("act", lambda e: e.activation(out=nl[:, z], in_=nl[:, z], func=AF.Ln, bias=1.0, scale=1.0), R=[b_nl[z]], W=[b_nl[z]])
                pb_, bpb = C.ps[4], C.bps[4]
                for k in range(8):
                    P.add("pe", lambda e, k=k: e.matmul(pb_[:], lhsT=hT[:, k, tc], rhs=wq[:, s, k, 256:768], start=(k == 0), stop=(k == 7)),
                          R=[b_wq[s], bhT], W=[bpb])
                P.add("act", lambda e: e.copy(out=vt[:, z], in_=pb_[:, 0:256]), R=[bpb], W=[b_vt[z]])
                P.add("act", lambda e: e.activation(out=gs[:, z], in_=pb_[:, 256:512], func=AF.Silu), R=[bpb], W=[b_gs[z]])
                pc, bpc = C.ps[5], C.bps[5]
                P.add("pe", lambda e: e.matmul(pc[:, 0:128], lhsT=nl[:, z], rhs=tri[:], start=True, stop=True), R=[b_nl[z], b_cst], W=[bpc])
                P.add("pe", lambda e: e.matmul(pc[:, 128:256], lhsT=su[:], rhs=nl[:, z], start=True, stop=True), R=[b_nl[z], b_cst], W=[bpc])
                P.add("act", lambda e: e.activation(out=ex[:, z, 0, :], in_=pc[:, 0:128], func=AF.Exp, scale=-1.0 / 16.0), R=[bpc], W=[b_ex[z]])
                P.add("act", lambda e: e.activation(out=ex[:, z, 1, :], in_=pc[:, 0:128], func=AF.Exp, scale=1.0 / 16.0), R=[bpc], W=[b_ex[z]])
                P.add("act", lambda e: e.activation(out=ex[:, z, 2, :], in_=pc[:, 128:256], func=AF.Exp, scale=-1.0 / 16.0), R=[bpc], W=[b_ex[z]])
                for c in range(2):
                    cs = slice(c * 64, (c + 1) * 64)
                    P.add("dve", lambda e, c=c, cs=cs: e.scalar_tensor_tensor(out=qz[:, z, c, cs], in0=qk[:, z, 0, cs], scalar=SCALE, in1=ex[:, z, 0, cs],
                                                                            op0=ALU.mult, op1=ALU.mult), R=[b_qk[z], b_ex[z]], W=[b_qz[z]])
                P.add("pool", lambda e: e.tensor_tensor(out=ke[:, z], in0=qk[:, z, 1], in1=ex[:, z, 1, :], op=ALU.mult), R=[b_qk[z], b_ex[z]], W=[b_ke[z]])
                for c in range(2):
                    rows = slice(c * 64, (c + 1) * 64)
                    P.add("pool", lambda e, c=c, rows=rows: e.tensor_tensor(out=krz[rows, z, c, :], in0=kt[rows, z], in1=ex[rows, z, 2, :], op=ALU.mult),
                          R=[b_kt[z], b_ex[z]], W=[b_krz[z]])
                P.add("pe", lambda e: e.matmul(pc[:, 256:384], lhsT=ke[:, z], rhs=qz[:, z, 0, :], start=True, stop=False), R=[b_ke[z], b_qz[z]], W=[bpc])
                P.add("pe", lambda e: e.matmul(pc[:, 256:384], lhsT=ke[:, z], rhs=qz[:, z, 1, :], start=False, stop=True), R=[b_ke[z], b_qz[z]], W=[bpc])
                P.add("dve", lambda e: e.tensor_tensor(out=att[:, z], in0=pc[:, 256:384], in1=cmk[:], op=ALU.mult), R=[bpc, b_cst], W=[b_att[z]])
                pd, bpd = C.ps[6], C.bps[6]
                P.add("pe", lambda e: e.matmul(pd[:, 0:256], lhsT=krz[:, z, 0, :], rhs=vt[:, z], start=True, stop=True), R=[b_krz[z], b_vt[z]], W=[bpd])
                P.add("pe", lambda e: e.matmul(pd[:, 256:512], lhsT=krz[:, z, 1, :], rhs=vt[:, z], start=True, stop=True), R=[b_krz[z], b_vt[z]], W=[bpd])
                P.add("dve", lambda e: e.scalar_tensor_tensor(out=St[:], in0=St[:], scalar=ex[:, z, 0, 63:64], in1=pd[:, 0:256], op0=ALU.mult, op1=ALU.add),
                      R=[b_S, b_ex[z], bpd], W=[b_S])
                P.add("act", lambda e: e.copy(out=Sb[:, 3 + z, :], in_=St[:]), R=[b_S], W=[b_Sb[3 + z]])
                P.add("dve", lambda e: e.scalar_tensor_tensor(out=St[:], in0=St[:], scalar=ex[:, z, 0, 127:128], in1=pd[:, 256:512], op0=ALU.mult, op1=ALU.add),
                      R=[b_S, b_ex[z], bpd], W=[b_S])
                P.add("act", lambda e: e.copy(out=Sb[:, (t + 1) % 3, :], in_=St[:]), R=[b_S], W=[b_Sb[(t + 1) % 3]])

            def back(hd, s, t):
                z = t % 2
                tc = slice(t * 128, (t + 1) * 128)
                po, bpo = C.ps[7], C.bps[7]
                P.add("pe", lambda e: e.matmul(po[:, 0:256], lhsT=qz[:, z, 0, :], rhs=Sb[:, t % 3, :], start=True, stop=False), R=[b_qz[z], b_Sb[t % 3]], W=[bpo])
                P.add("pe", lambda e: e.matmul(po[:, 0:256], lhsT=qz[:, z, 1, :], rhs=Sb[:, 3 + z, :], start=False, stop=False), R=[b_qz[z], b_Sb[3 + z]], W=[bpo])
                P.add("pe", lambda e: e.matmul(po[:, 0:256], lhsT=att[:, z], rhs=vt[:, z], start=False, stop=True), R=[b_att[z], b_vt[z]], W=[bpo])
                P.add("act", lambda e: e.activation(out=jk[:], in_=po[:, 0:256], func=AF.Square, accum_out=ss[:, z, 0:1]), R=[bpo], W=[b_jk, b_ss[z]])
                P.add("act", lambda e: e.activation(out=ss[:, z, 1:2], in_=ss[:, z, 0:1], func=AF.Sqrt, bias=EPS, scale=1.0 / 256.0), R=[b_ss[z]], W=[b_ss[z]])
                P.add("dve", lambda e: e.reciprocal(out=ss[:, z, 1:2], in_=ss[:, z, 1:2]), R=[b_ss[z]], W=[b_ss[z]])
                P.add("dve", lambda e: e.scalar_tensor_tensor(out=ot[:, z], in0=po[:, 0:256], scalar=ss[:, z, 1:2], in1=ng[:], op0=ALU.mult, op1=ALU.mult),
                      R=[bpo, b_ss[z], b_cst], W=[b_ot[z]])
                P.add("pool", lambda e: e.tensor_tensor(out=og[:, z], in0=ot[:, z], in1=gs[:, z], op=ALU.mult), R=[b_ot[z], b_gs[z]], W=[b_og[z]])
                ptp, bptp = C.ps[z], C.bps[z]
                ptb = ptp[:].bitcast(BF16)
                for c2 in range(2):
                    P.add("pe", lambda e, c2=c2: e.transpose(out=ptb[:, c2 * 128:(c2 + 1) * 128], in_=og[:, z, c2 * 128:(c2 + 1) * 128], identity=C.identb[:]),
                          R=[b_og[z], C.b_const], W=[bptp])
                P.add("act", lambda e: e.copy(out=oT[:, hd * 2:hd * 2 + 2, tc], in_=ptb[:, 0:256].rearrange("p (a c) -> p a c", a=2)), R=[bptp], W=[C.b_srcT])

            for hd in range(4):
                s = hd % 2
                for (c0_, w_, d0) in ((hd * 128, 128, 0), (512 + hd * 128, 128, 128), (1024 + hd * 256, 256, 256), (2048 + hd * 256, 256, 512)):
                    P.add("pool", lambda e, c0_=c0_, w_=w_, d0=d0, s=s: e.dma_start(
                        out=wq[:, s, :, d0:d0 + w_], in_=w_in[:, c0_:c0_ + w_].rearrange("(k p) n -> p k n", p=128)), W=[b_wq[s]], dma=True)
                P.add("pool", lambda e: e.memset(St[:], 0.0), W=[b_S])
                P.add("pool", lambda e: e.memset(Sb[:, 0, :], 0.0), W=[b_Sb[0]])
                front(hd, s, 0)
                for t in range(NT):
                    if t + 1 < NT:
                        front(hd, s, t + 1)
                    back(hd, s, t)
            P.flush()
        out_proj_ln(C, li, T, oT, 8, T["c_w_out_%d" % j], R_)


def build_program(mode="full", n_experts=NE, layers=(0, 1, 2, 3)):
    nc = bass.Bass("TRN2", target_bir_lowering=False)
    T = {}

    def din(name, shape, dt=F32):
        T[name] = nc.dram_tensor(name, list(shape), dt, kind="ExternalInput").ap()

    do_mixer = mode in ("full", "mixer_only")
    do_moe = mode in ("full", "moe_only")
    din("x", (S, D))
    din("ident", (128, 128))
    din("identb", (128, 128), BF16)
    din("ln_g", (DEPTH, 2, D))
    din("ln_b", (DEPTH, 2, D))
    mixers = sorted(set(li % 3 for li in layers)) if do_mixer else []
    if 0 in mixers:
        din("rel_bias", (32, 8))
        din("relT", (8, 2, 128, 128))
        din("causal", (128, 128))
        din("vmask", (128, 128))
        din("vmask2", (128, 128))
    for li in layers:
        if do_mixer:
            j = li // 3
            if li % 3 == 0:
                din("a_w_in_%d" % j, (D, 3 * D))
                din("a_w_out_%d" % j, (D, D))
            elif li % 3 == 1:
                din("b_w_in_%d" % j, (D, 4 * D))
                din("b_ln_g_%d" % j, (2 * D,))
                din("b_ln_b_%d" % j, (2 * D,))
                din("b_w_s_%d" % j, (8, 128, 128))
                din("b_b_s_%d" % j, (8, 128))
                din("b_w_out_%d" % j, (2 * D, D))
                din("tril", (128, 128))
            else:
                din("c_w_in_%d" % j, (D, 3088))
                din("c_w_gk_up_%d" % j, (16, 512))
                din("c_b_gk_%d" % j, (512,))
                din("c_norm_g_%d" % j, (256,))
                din("c_w_out_%d" % j, (D, D))
                din("gla_tri", (128, 128))
                din("gla_su", (128, 128))
                din("gla_cm", (128, 128))
        din("moe_w_router_%d" % li, (D, NE))
        din("moe_b_router_%d" % li, (NE,))
        if do_moe:
            din("moe_w_gate_up_%d" % li, (NE, D, 2 * D))
            din("moe_b_gate_up_%d" % li, (NE, 2 * D))
            din("moe_w_down_%d" % li, (NE, D, D))
            din("moe_b_down_%d" % li, (NE, D))
    out = nc.dram_tensor("out", [S, D], F32, kind="ExternalOutput").ap()

    C = Ctx()
    C.nc = nc
    C.n_experts = n_experts
    C.P = P = Prog(nc)
    C.tp_rr = C.act_rr = C.q_rr = C.y_rr = 0
    with ExitStack() as es:
        h = es.enter_context(nc.sbuf_tensor("h", [128, NT, D], F32))
        hT = es.enter_context(nc.sbuf_tensor("hT", [128, 8, S], BF16))
        ident = es.enter_context(nc.sbuf_tensor("ident_s", [128, 128], F32))
        identb = es.enter_context(nc.sbuf_tensor("identb_s", [128, 128], BF16))
        hT32 = es.enter_context(nc.sbuf_tensor("hT32", [128, 8, 128], F32))
        gates = es.enter_context(nc.sbuf_tensor("gates", [128, NT, NE], F32))
        ln_st = es.enter_context(nc.sbuf_tensor("ln_st", [128, 2, 6], F32))
        ln_mv = es.enter_context(nc.sbuf_tensor("ln_mv", [128, 2], F32))
        ln_rs = es.enter_context(nc.sbuf_tensor("ln_rs", [128, 1], F32))
        r_lg = es.enter_context(nc.sbuf_tensor("r_lg", [128, NE], F32))
        r_m8 = es.enter_context(nc.sbuf_tensor("r_m8", [128, 8], F32))
        r_ex = es.enter_context(nc.sbuf_tensor("r_ex", [128, NE], F32))
        r_msk = es.enter_context(nc.sbuf_tensor("r_msk", [128, NE], F32))
        r_sm = es.enter_context(nc.sbuf_tensor("r_sm", [128, 2], F32))
        r_wr = es.enter_context(nc.sbuf_tensor("r_wr", [128, 8, NE], F32))
        r_brb = es.enter_context(nc.sbuf_tensor("r_brb", [128, NE], F32))
        C.h, C.hT, C.ident, C.identb, C.hT32, C.gates = h, hT, ident, identb, hT32, gates
        C.ln_st, C.ln_mv, C.ln_rs, C.ln_nb = ln_st, ln_mv, ln_rs, None
        C.r_lg, C.r_m8, C.r_ex, C.r_msk, C.r_sm = r_lg, r_m8, r_ex, r_msk, r_sm
        C.bh = [Buf("h%d" % t) for t in range(NT)]
        C.bhT = [Buf("hT%d" % t) for t in range(NT)]
        C.b_gates = [Buf() for t in range(NT)]
        C.b_const = Buf("const")
        C.b_lnst = Buf()
        C.b_hT32 = Buf()
        C.b_rt = Buf()
        ps_cms = [nc.psum_tensor("ps%d" % i, [128, 512], F32) for i in range(8)]
        C.ps = [cm.__enter__() for cm in ps_cms]
        C.bps = [Buf("ps%d" % i) for i in range(8)]

        P.add("sp", lambda e: e.dma_start(out=ident[:], in_=T["ident"]), W=[C.b_const], dma=True)
        P.add("sp", lambda e: e.dma_start(out=identb[:], in_=T["identb"]), W=[C.b_const], dma=True)
        for t in range(NT):
            P.add("sp", lambda e, t=t: e.dma_start(out=h[:, t, :], in_=T["x"][t * 128:(t + 1) * 128, :]), W=[C.bh[t]], dma=True)
        first = True
        for li in layers:
            b_r = Buf()
            P.add("sp", lambda e, li=li: e.dma_start(out=r_wr[:], in_=T["moe_w_router_%d" % li].rearrange("(k p) n -> p k n", p=128)), W=[b_r], dma=True)
            P.add("sp", lambda e, li=li: e.dma_start(out=r_brb[:], in_=bcast_rows(T["moe_b_router_%d" % li])), W=[b_r], dma=True)
            R_ = {"wr": r_wr, "brb": r_brb, "b": b_r}
            if first:
                for t in range(NT):
                    transpose_tile(C, t, None if do_mixer else R_)
                P.flush()
                first = False
            if do_mixer:
                if li % 3 == 0:
                    moba_phase(C, li, T, R_)
                elif li % 3 == 1:
                    gmlp_phase(C, li, T, R_)
                else:
                    gla_phase(C, li, T, R_)
            if do_moe:
                moe_phase(C, li, T)

        for t in range(NT):
            P.add("sp", lambda e, t=t: e.dma_start(out=out[t * 128:(t + 1) * 128, :], in_=h[:, t, :]), R=[C.bh[t]], dma=True)
        P.flush()
        for cm in reversed(ps_cms):
            cm.__exit__(None, None, None)
    P.close()
    return nc


def t5_bucket_np(rel):
    n = np.maximum(rel, 0)
    nf = np.maximum(n, 1).astype(np.float32)
    large = 16 + (np.log(nf / np.float32(16)) / np.float32(math.log(128 / 16)) * np.float32(16)).astype(np.int32)
    large = np.minimum(large, 31)
    return np.where(n < 16, n, large)


def host_constants(rel_bias=None):
    import ml_dtypes
    c = {"ident": np.eye(128, dtype=np.float32), "identb": np.eye(128, dtype=np.float32).astype(ml_dtypes.bfloat16)}
    i = np.arange(128)[:, None]
    jj = np.arange(128)[None, :]
    c["causal"] = np.where(jj <= i, 0.0, NEG).astype(np.float32)
    c["tril"] = (jj <= i).astype(np.float32)
    qt = np.arange(16)[:, None]
    n = np.arange(8)[None, :]
    valid = n < (qt // 2)
    vm = np.where(valid, 0.0, -1e30).astype(np.float32).reshape(1, 128)
    c["vmask"] = np.broadcast_to(vm, (128, 128)).copy()
    vm2 = np.where(valid, -30000.0, -1e30).astype(np.float32).reshape(1, 128)
    c["vmask2"] = np.broadcast_to(vm2, (128, 128)).copy()
    if rel_bias is not None:
        relT = np.empty((8, 2, 128, 128), np.float32)
        for d in range(2):
            bk = t5_bucket_np(d * 128 + i - jj)
            for hh in range(8):
                relT[hh, d] = rel_bias[bk, hh]
        c["relT"] = relT
    same = (i // 64) == (jj // 64)
    c["gla_tri"] = (same & (i <= jj)).astype(np.float32)
    c["gla_su"] = (same & (i > jj)).astype(np.float32)
    c["gla_cm"] = (same & (i <= jj)).astype(np.float32)
    return c


def make_in_map(A, x_core, consts, layers=(0, 1, 2, 3), mode="full"):
    do_mixer = mode in ("full", "mixer_only")
    do_moe = mode in ("full", "moe_only")
    im = {"x": np.ascontiguousarray(x_core), "ln_g": A["ln_g"], "ln_b": A["ln_b"]}
    im.update(consts)
    if "rel_bias" in A:
        im["rel_bias"] = A["rel_bias"]
    for li in layers:
        j = li // 3
        if do_mixer:
            if li % 3 == 0:
                im["a_w_in_%d" % j] = A["a_w_in"][j]
                im["a_w_out_%d" % j] = A["a_w_out"][j]
            elif li % 3 == 1:
                for n in ("b_w_in", "b_ln_g", "b_ln_b", "b_w_s", "b_b_s", "b_w_out"):
                    im["%s_%d" % (n, j)] = A[n][j]
            else:
                for n in ("c_w_in", "c_w_gk_up", "c_b_gk", "c_norm_g", "c_w_out"):
                    im["%s_%d" % (n, j)] = A[n][j]
        im["moe_w_router_%d" % li] = A["moe_w_router"][li]
        im["moe_b_router_%d" % li] = A["moe_b_router"][li]
        if do_moe:
            for n in ("moe_w_gate_up", "moe_b_gate_up", "moe_w_down", "moe_b_down"):
                im["%s_%d" % (n, li)] = A[n][li]
    return im


def kernel(**inputs):
    A = {k: np.asarray(v) for k, v in inputs.items()}
    nc = build_program("full")
    consts = host_constants(A["rel_bias"])
    in_maps = [make_in_map(A, A["x"][c], consts) for c in range(8)]
    res = run_bass_kernel_spmd(nc, in_maps, core_ids=list(range(8)))
    out = np.stack([np.asarray(res.results[c]["out"]) for c in range(8)], axis=0)
    return out.astype(np.float32)
```

```python
import math
from contextlib import ExitStack
import numpy as np
import concourse.bass as bass
import concourse.mybir as mybir
from concourse.bass_utils import run_bass_kernel_spmd

F32 = mybir.dt.float32
BF16 = mybir.dt.bfloat16
ALU = mybir.AluOpType
AF = mybir.ActivationFunctionType
AX = mybir.AxisListType

ENGS = ("pe", "act", "dve", "pool", "sp")

D = 1024
S = 2048
NT = 16
DEPTH = 4
ALPHA = (2.0 * DEPTH) ** 0.25
EPS = 1e-5
NE = 32
NEG = -30000.0


class Buf:
    __slots__ = ("name", "lw", "rd")

    def __init__(self, name="b"):
        self.name = name
        self.lw = None
        self.rd = []


class Op:
    __slots__ = ("eng", "fn", "dma", "deps", "sem", "val", "needs_sig", "epoch")

    def __init__(self, eng, fn, dma, epoch=0):
        self.epoch = epoch
        self.eng = eng
        self.fn = fn
        self.dma = dma
        self.deps = []
        self.sem = None
        self.val = 0
        self.needs_sig = False


class Prog:
    def __init__(self, nc, n_dma_sems=(("sp", 24), ("pool", 24), ("act", 8))):
        self.nc = nc
        self.pending = {e: [] for e in ENGS}
        self.esem = {}
        self.ecount = {e: 0 for e in ENGS}
        self._ctx = []
        for e in ENGS:
            cm = nc.semaphore("s_" + e)
            self.esem[e] = cm.__enter__()
            self._ctx.append(cm)
        self.dsems = {}
        self.dcount = {}
        self.dlast = {}
        self.dnext = {}
        for q, n in n_dma_sems:
            lst = []
            for i in range(n):
                cm = nc.semaphore("d_%s_%d" % (q, i))
                lst.append(cm.__enter__())
                self._ctx.append(cm)
            self.dsems[q] = lst
            self.dcount[q] = [0] * n
            self.dlast[q] = [None] * n
            self.dnext[q] = 0
        self.known = {e: {} for e in ENGS}
        self.all_dma_since_barrier = []
        self.n_ops = 0
        self.epoch = 0

    def close(self):
        for cm in reversed(self._ctx):
            cm.__exit__(None, None, None)

    def add(self, eng, fn, R=(), W=(), dma=False):
        op = Op(eng, fn, dma, self.epoch)
        self.n_ops += 1
        deps = []
        rset = set(id(b) for b in R)
        for b in R:
            if b.lw is not None:
                deps.append((b.lw, True))
        for b in W:
            if b.lw is not None:
                deps.append((b.lw, id(b) in rset))
            for r in b.rd:
                deps.append((r, False))
        seen = set()
        for d, raw in deps:
            if d is op or d.epoch < self.epoch:
                continue
            key = id(d)
            if d.dma or dma:
                pass
            elif d.eng == eng:
                if eng == "pe" or not raw:
                    continue
            if key in seen:
                continue
            seen.add(key)
            op.deps.append(d)
            d.needs_sig = True
        for b in R:
            b.rd.append(op)
        for b in W:
            b.lw = op
            b.rd = []
        if dma:
            q = eng
            k = self.dnext[q]
            self.dnext[q] = (k + 1) % len(self.dsems[q])
            prev = self.dlast[q][k]
            if prev is not None and prev.epoch == self.epoch:
                op.deps.append(prev)
            self.dcount[q][k] += 16
            op.sem = self.dsems[q][k]
            op.val = self.dcount[q][k]
            self.dlast[q][k] = op
            op.needs_sig = True
            self.all_dma_since_barrier.append(op)
        self.pending[eng].append(op)
        return op

    def barrier(self):
        lasts = []
        for e in ENGS:
            for o in reversed(self.pending[e]):
                if not o.dma and o.fn is not None:
                    lasts.append(o)
                    break
        dmas = list(self.all_dma_since_barrier)
        self.all_dma_since_barrier = []
        for e in ENGS:
            op = Op(e, None, False, self.epoch)
            for d in lasts:
                if d.eng != e:
                    op.deps.append(d)
                    d.needs_sig = True
            for d in dmas:
                op.deps.append(d)
            self.pending[e].append(op)
        self.epoch += 1

    def flush(self):
        nc = self.nc
        self.barrier()
        for e in ENGS:
            for op in self.pending[e]:
                if op.dma or op.fn is None:
                    continue
                if op.needs_sig:
                    self.ecount[e] += 1
                    op.sem = self.esem[e]
                    op.val = self.ecount[e]
        pend = self.pending
        self.pending = {e: [] for e in ENGS}
        known = self.known

        def emit(e, eng):
            kn = known[e]
            for op in pend[e]:
                need = {}
                for d in op.deps:
                    assert d.sem is not None, "dep without signal"
                    sid = id(d.sem)
                    if kn.get(sid, 0) >= d.val:
                        continue
                    if sid not in need or need[sid][1] < d.val:
                        need[sid] = (d.sem, d.val)
                for sid, (sem, val) in need.items():
                    eng.wait_ge(sem, val)
                    kn[sid] = val
                if op.fn is None:
                    continue
                ins = op.fn(eng)
                if op.dma:
                    ins.then_inc(op.sem, 16)
                elif op.needs_sig:
                    ins.then_inc(op.sem, 1)

        with nc.Block() as blk:
            @blk.tensor
            def _(eng):
                emit("pe", eng)

            @blk.scalar
            def _(eng):
                emit("act", eng)

            @blk.vector
            def _(eng):
                emit("dve", eng)

            @blk.gpsimd
            def _(eng):
                emit("pool", eng)

            @blk.sync
            def _(eng):
                emit("sp", eng)


class Ctx:
    pass


def bcast_rows(ap_row, nparts=128):
    return ap_row.partition_broadcast(nparts)


def ln_tile(C, t, lng, lnb, blnp, router=None):
    P, nc = C.P, C.nc
    h, hT = C.h, C.hT
    bh, bhT = C.bh[t], C.bhT[t]
    st, mv, rs, nb = C.ln_st, C.ln_mv, C.ln_rs, C.ln_nb
    b_st = C.b_lnst
    for c in range(2):
        P.add("dve", lambda e, c=c: e.bn_stats(out=st[:, c, :], in_=h[:, t, c * 512:(c + 1) * 512]), R=[bh], W=[b_st])
    P.add("dve", lambda e: e.bn_aggr(out=mv[:], in_=st[:]), R=[b_st], W=[b_st])
    P.add("act", lambda e: e.activation(out=rs[:], in_=mv[:, 1:2], func=AF.Sqrt, bias=EPS, scale=1.0), R=[b_st], W=[b_st])
    P.add("dve", lambda e: e.reciprocal(out=rs[:], in_=rs[:]), R=[b_st], W=[b_st])
    P.add("dve", lambda e: e.tensor_scalar(out=h[:, t, :], in0=h[:, t, :], scalar1=mv[:, 0:1], scalar2=rs[:, 0:1],
                                           op0=ALU.subtract, op1=ALU.mult), R=[bh, b_st], W=[bh])
    P.add("pool", lambda e: e.tensor_tensor(out=h[:, t, :], in0=h[:, t, :], in1=lng[:], op=ALU.mult), R=[bh, blnp], W=[bh])
    P.add("pool", lambda e: e.tensor_tensor(out=h[:, t, :], in0=h[:, t, :], in1=lnb[:], op=ALU.add), R=[bh, blnp], W=[bh])
    transpose_tile(C, t, router)


def transpose_tile(C, t, router=None):
    P = C.P
    h, hT = C.h, C.hT
    bh, bhT = C.bh[t], C.bhT[t]
    for half in range(2):
        ps, bps = C.ps[C.tp_rr % 2], C.bps[C.tp_rr % 2]
        C.tp_rr += 1
        for kk in range(4):
            k = half * 4 + kk
            P.add("pe", lambda e, k=k, kk=kk, ps=ps: e.transpose(out=ps[:, kk * 128:(kk + 1) * 128], in_=h[:, t, k * 128:(k + 1) * 128],
                                                                  identity=C.ident[:]), R=[bh, C.b_const], W=[bps])
        P.add("act", lambda e, ps=ps, half=half: e.copy(out=hT[:, half * 4:half * 4 + 4, t * 128:(t + 1) * 128],
                                                        in_=ps[:].rearrange("p (k c) -> p k c", k=4)), R=[bps], W=[bhT])
        if router is not None:
            P.add("act", lambda e, ps=ps, half=half: e.copy(out=C.hT32[:, half * 4:half * 4 + 4, :],
                                                            in_=ps[:].rearrange("p (k c) -> p k c", k=4)), R=[bps], W=[C.b_hT32])
    if router is not None:
        router_tile(C, t, router)


def router_tile(C, t, R_):
    P = C.P
    ps, bps = C.ps[2], C.bps[2]
    wr, brb, b_r = R_["wr"], R_["brb"], R_["b"]
    lg, m8, ex, msk, sm = C.r_lg, C.r_m8, C.r_ex, C.r_msk, C.r_sm
    b = C.b_rt
    for k in range(8):
        P.add("pe", lambda e, k=k: e.matmul(ps[:, 0:NE], lhsT=C.hT32[:, k, :], rhs=wr[:, k, :], start=(k == 0), stop=(k == 7)),
              R=[C.b_hT32, b_r], W=[bps])
    P.add("dve", lambda e: e.tensor_tensor(out=lg[:], in0=ps[:, 0:NE], in1=brb[:], op=ALU.add), R=[bps, b_r], W=[b])
    P.add("dve", lambda e: e.max(out=m8[:], in_=lg[:]), R=[b], W=[b])
    P.add("dve", lambda e: e.tensor_scalar(out=msk[:], in0=lg[:], scalar1=m8[:, 3:4], scalar2=None, op0=ALU.is_ge), R=[b], W=[b])
    P.add("dve", lambda e: e.tensor_scalar(out=sm[:, 0:1], in0=m8[:, 0:1], scalar1=-1.0, scalar2=None, op0=ALU.mult), R=[b], W=[b])
    P.add("act", lambda e: e.activation(out=ex[:], in_=lg[:], func=AF.Exp, bias=sm[:, 0:1], scale=1.0), R=[b], W=[b])
    P.add("dve", lambda e: e.tensor_tensor(out=ex[:], in0=ex[:], in1=msk[:], op=ALU.mult), R=[b], W=[b])
    P.add("dve", lambda e: e.reduce_sum(out=sm[:, 1:2], in_=ex[:], axis=AX.X), R=[b], W=[b])
    P.add("dve", lambda e: e.reciprocal(out=sm[:, 1:2], in_=sm[:, 1:2]), R=[b], W=[b])
    P.add("dve", lambda e: e.tensor_scalar(out=C.gates[:, t, :], in0=ex[:], scalar1=sm[:, 1:2], scalar2=None, op0=ALU.mult),
          R=[b], W=[C.b_gates[t]])


def load_ln_params(C, lng, lnb, blnp, g_row, b_row):
    P = C.P
    P.add("sp", lambda e: e.dma_start(out=lng[:], in_=bcast_rows(g_row)), W=[blnp], dma=True)
    P.add("sp", lambda e: e.dma_start(out=lnb[:], in_=bcast_rows(b_row)), W=[blnp], dma=True)


def moe_phase(C, li, T):
    P, nc = C.P, C.nc
    h, hT = C.h, C.hT
    hb = hT[:].rearrange("p k s -> p (k s)").rearrange("p (t d) -> p t d", t=NT)
    BANKS = (0, 1, 7, 2)
    with ExitStack() as es:
        w1g = es.enter_context(nc.sbuf_tensor("m_w1g_%d" % li, [128, 2, 8, 512], BF16))
        w1u = es.enter_context(nc.sbuf_tensor("m_w1u_%d" % li, [128, 2, 8, 512], BF16))
        w2 = es.enter_context(nc.sbuf_tensor("m_w2_%d" % li, [128, 2, 4, 1024], BF16))
        actT = es.enter_context(nc.sbuf_tensor("m_actT_%d" % li, [128, 2, 4, 512], BF16))
        g1 = es.enter_context(nc.sbuf_tensor("m_g1_%d" % li, [128, 2, 512], F32))
        sg = es.enter_context(nc.sbuf_tensor("m_sg_%d" % li, [128, 2, 512], F32))
        u1 = es.enter_context(nc.sbuf_tensor("m_u1_%d" % li, [128, 2, 512], F32))
        braw = es.enter_context(nc.sbuf_tensor("m_braw_%d" % li, [128, 4, 128], F32))
        bguT = es.enter_context(nc.sbuf_tensor("m_bguT_%d" % li, [128, 512], F32))
        bd = es.enter_context(nc.sbuf_tensor("m_bd_%d" % li, [NE, D], F32))
        gT = es.enter_context(nc.sbuf_tensor("m_gT_%d" % li, [NE, 128], F32))
        xgT = es.enter_context(nc.sbuf_tensor("m_xgT_%d" % li, [128, 8, 512], BF16))
        Pm = es.enter_context(nc.sbuf_tensor("m_P_%d" % li, [128, NT, 128], BF16))
        PTm = es.enter_context(nc.sbuf_tensor("m_PT_%d" % li, [128, 4, 512], BF16))
        ys = es.enter_context(nc.sbuf_tensor("m_ys_%d" % li, [128, 2, 1024], BF16))
        mask = es.enter_context(nc.sbuf_tensor("m_mask_%d" % li, [128, NT, NE], BF16))
        posm = es.enter_context(nc.sbuf_tensor("m_pos_%d" % li, [128, NT, NE], F32))
        iota = es.enter_context(nc.sbuf_tensor("m_iota_%d" % li, [128, 128], F32))
        onesb = es.enter_context(nc.sbuf_tensor("m_ones_%d" % li, [128, 128], BF16))
        tris = es.enter_context(nc.sbuf_tensor("m_tris_%d" % li, [128, 128], BF16))
        b_w = [Buf("w%d" % i) for i in range(2)]
        b_act = [Buf() for _ in range(2)]
        b_g1 = [Buf() for _ in range(2)]
        b_sg = [Buf() for _ in range(2)]
        b_u1 = [Buf() for _ in range(2)]
        b_ys = [Buf() for _ in range(2)]
        b_misc, b_gT, b_cst, b_mask, b_pos, b_xg, b_P, b_PT = Buf(), Buf(), Buf(), Buf(), Buf(), Buf(), Buf(), Buf()
        allhT = list(C.bhT)
        P.add("sp", lambda e: e.dma_start(out=iota[:], in_=T["iota"]), W=[b_cst], dma=True)
        P.add("sp", lambda e: e.dma_start(out=onesb[:], in_=T["ones_b"]), W=[b_cst], dma=True)
        P.add("sp", lambda e: e.dma_start(out=tris[:], in_=T["tris_b"]), W=[b_cst], dma=True)
        P.add("sp", lambda e: e.dma_start(out=braw[:], in_=T["moe_b_gate_up_%d" % li].rearrange("e (c p) -> (e c) p", p=128)
                                          .rearrange("(b r) p -> r b p", r=128)), W=[b_misc], dma=True)
        P.add("sp", lambda e: e.dma_start(out=bd[:], in_=T["moe_b_down_%d" % li]), W=[b_misc], dma=True)
        for bb in range(4):
            ps, bps = C.ps[bb % 2], C.bps[bb % 2]
            P.add("pe", lambda e, bb=bb, ps=ps: e.transpose(out=ps[:, 0:128], in_=braw[:, bb, :], identity=C.ident[:]),
                  R=[b_misc, C.b_const], W=[bps])
            P.add("act", lambda e, bb=bb, ps=ps: e.copy(out=bguT[:, bb * 128:(bb + 1) * 128], in_=ps[:, 0:128]), R=[bps], W=[b_misc])
        bg3 = bguT[:].rearrange("p (e c) -> p e c", c=16)
        P.add("dve", lambda e: e.tensor_scalar(out=bg3[:, :, 8:16], in0=bg3[:, :, 8:16], scalar1=1.0, scalar2=None, op0=ALU.add), R=[b_misc], W=[b_misc])
        for t in range(NT):
            P.add("act", lambda e, t=t: e.copy(out=hb[:, t, :], in_=h[:, t, :]), R=[C.bh[t]], W=allhT)
        for t in range(NT):
            ps, bps = C.ps[2], C.bps[2]
            P.add("pe", lambda e, t=t: e.transpose(out=ps[0:NE, 0:128], in_=C.gates[:, t, :], identity=C.ident[:]),
                  R=[C.b_gates[t], C.b_const], W=[bps])
            P.add("act", lambda e: e.copy(out=gT[:], in_=ps[0:NE, 0:128]), R=[bps], W=[b_gT])
            for n in range(2):
                po, bpo = C.ps[n], C.bps[n]
                P.add("pe", lambda e, n=n, po=po: e.matmul(po[:], lhsT=gT[:], rhs=bd[:, n * 512:(n + 1) * 512], start=True, stop=True),
                      R=[b_gT, b_misc], W=[bpo])
                P.add("dve", lambda e, n=n, po=po, t=t: e.scalar_tensor_tensor(out=h[:, t, n * 512:(n + 1) * 512], in0=h[:, t, n * 512:(n + 1) * 512],
                                                                               scalar=ALPHA, in1=po[:], op0=ALU.mult, op1=ALU.add),
                      R=[bpo, C.bh[t]], W=[C.bh[t]])
        P.add("dve", lambda e: e.tensor_scalar(out=mask[:], in0=C.gates[:], scalar1=0.0, scalar2=None, op0=ALU.is_gt), R=list(C.b_gates), W=[b_mask])
        pp, bpp = C.ps[2], C.bps[2]
        for t in range(NT):
            gi, i = divmod(t, 4)
            for i2 in range(i):
                P.add("pe", lambda e, t=t, gi=gi, i2=i2: e.matmul(pp[:, t * NE:(t + 1) * NE], lhsT=onesb[:], rhs=mask[:, 4 * gi + i2, :],
                                                                start=(i2 == 0), stop=False), R=[b_mask, b_cst], W=[bpp])
            P.add("pe", lambda e, t=t, i=i: e.matmul(pp[:, t * NE:(t + 1) * NE], lhsT=tris[:], rhs=mask[:, t, :], start=(i == 0), stop=True),
                  R=[b_mask, b_cst], W=[bpp])
        P.add("dve", lambda e: e.scalar_tensor_tensor(out=posm[:], in0=pp[:].rearrange("p (t x) -> p t x", t=NT), scalar=1.0, in1=mask[:],
                                                      op0=ALU.add, op1=ALU.mult), R=[bpp, b_mask], W=[b_pos])
        P.add("dve", lambda e: e.tensor_scalar(out=posm[:], in0=posm[:], scalar1=-1.0, scalar2=None, op0=ALU.add), R=[b_pos], W=[b_pos])

        wgu = T["moe_w_gate_up_%d" % li]
        wd = T["moe_w_down_%d" % li]

        def bank():
            y = C.y_rr % 4
            C.y_rr += 1
            return C.ps[BANKS[y]], C.bps[BANKS[y]]

        def gather_unit(ex):
            for t in range(NT):
                P.add("dve", lambda e, t=t: e.tensor_scalar(out=Pm[:, t, :], in0=iota[:], scalar1=posm[:, t, ex:ex + 1], scalar2=None, op0=ALU.is_equal),
                      R=[b_pos, b_cst], W=[b_P])
            for gi in range(4):
                for kh in range(2):
                    px, bpx = bank()
                    for kk in range(4):
                        k = kh * 4 + kk
                        for i in range(4):
                            t = 4 * gi + i
                            P.add("pe", lambda e, kk=kk, k=k, t=t, i=i, px=px: e.matmul(px[:, kk * 128:(kk + 1) * 128], lhsT=hb[:, t, k * 128:(k + 1) * 128],
                                                                                      rhs=Pm[:, t, :], start=(i == 0), stop=(i == 3)), R=[b_P] + allhT, W=[bpx])
                    P.add("act", lambda e, kh=kh, gi=gi, px=px: e.copy(out=xgT[:, kh * 4:kh * 4 + 4, gi * 128:(gi + 1) * 128],
                                                                      in_=px[:].rearrange("p (a c) -> p a c", a=4)), R=[bpx], W=[b_xg])

        def pt_unit(ex):
            for gi in range(4):
                ptp, bptp = bank()
                ptb = ptp[:].bitcast(BF16)
                for i in range(4):
                    P.add("pe", lambda e, i=i, gi=gi, ptb=ptb: e.transpose(out=ptb[:, i * 128:(i + 1) * 128], in_=Pm[:, 4 * gi + i, :], identity=C.identb[:]),
                          R=[b_P, C.b_const], W=[bptp])
                P.add("act", lambda e, gi=gi, ptb=ptb: e.copy(out=PTm[:, gi, :], in_=ptb[:, 0:512]), R=[bptp], W=[b_PT])

        def w1_unit(ex, hf, s, a):
            bw = b_w[s]
            for j in range(4):
                q = C.q_rr % 2
                C.q_rr += 1
                pg, bpg = C.ps[3 + q], C.bps[3 + q]
                pu, bpu = C.ps[5 + q], C.bps[5 + q]
                for k in range(8):
                    P.add("pe", lambda e, k=k, j=j, pg=pg: e.matmul(pg[:], lhsT=w1g[:, s, k, j * 128:(j + 1) * 128], rhs=xgT[:, k, :],
                                                                  start=(k == 0), stop=(k == 7)), R=[bw, b_xg], W=[bpg])
                for k in range(8):
                    P.add("pe", lambda e, k=k, j=j, pu=pu: e.matmul(pu[:], lhsT=w1u[:, s, k, j * 128:(j + 1) * 128], rhs=xgT[:, k, :],
                                                                  start=(k == 0), stop=(k == 7)), R=[bw, b_xg], W=[bpu])
                cg = ex * 16 + hf * 4 + j
                cu = ex * 16 + 8 + hf * 4 + j
                P.add("dve", lambda e, q=q, pg=pg, cg=cg: e.tensor_scalar(out=g1[:, q, :], in0=pg[:], scalar1=bguT[:, cg:cg + 1], scalar2=7.0,
                                                                         op0=ALU.add, op1=ALU.min), R=[bpg, b_misc], W=[b_g1[q]])
                P.add("act", lambda e, q=q: e.activation(out=sg[:, q, :], in_=g1[:, q, :], func=AF.Sigmoid, scale=1.702), R=[b_g1[q]], W=[b_sg[q]])
                P.add("dve", lambda e, q=q, pu=pu, cu=cu: e.tensor_scalar(out=u1[:, q, :], in0=pu[:], scalar1=bguT[:, cu:cu + 1], scalar2=8.0,
                                                                         op0=ALU.add, op1=ALU.min), R=[bpu, b_misc], W=[b_u1[q]])
                P.add("dve", lambda e, q=q: e.tensor_tensor(out=sg[:, q, :], in0=sg[:, q, :], in1=g1[:, q, :], op=ALU.mult),
                      R=[b_sg[q], b_g1[q]], W=[b_sg[q]])
                P.add("dve", lambda e, q=q, j=j: e.scalar_tensor_tensor(out=actT[:, a, j, :], in0=u1[:, q, :], scalar=-6.0, in1=sg[:, q, :],
                                                                       op0=ALU.max, op1=ALU.mult), R=[b_sg[q], b_u1[q]], W=[b_act[a]])

        def w2_unit(ex, hf, s, a):
            bw = b_w[s]
            for tt in range(4):
                yb = C.ys_rr % 2
                C.ys_rr += 1
                for n in range(2):
                    py, bpy = bank()
                    for j in range(4):
                        P.add("pe", lambda e, j=j, tt=tt, n=n, py=py: e.matmul(py[:], lhsT=actT[:, a, j, tt * 128:(tt + 1) * 128],
                                                                             rhs=w2[:, s, j, n * 512:(n + 1) * 512], start=(j == 0), stop=(j == 3)),
                              R=[b_act[a], bw], W=[bpy])
                    P.add("act", lambda e, n=n, yb=yb, py=py: e.copy(out=ys[:, yb, n * 512:(n + 1) * 512], in_=py[:]), R=[bpy], W=[b_ys[yb]])
                for i in range(4):
                    t = 4 * tt + i
                    for n in range(2):
                        pz, bpz = bank()
                        P.add("pe", lambda e, i=i, tt=tt, n=n, yb=yb, pz=pz: e.matmul(pz[:], lhsT=PTm[:, tt, i * 128:(i + 1) * 128],
                                                                                    rhs=ys[:, yb, n * 512:(n + 1) * 512], start=True, stop=True),
                              R=[b_PT, b_ys[yb]], W=[bpz])
                        P.add("dve", lambda e, t=t, n=n, pz=pz: e.scalar_tensor_tensor(
                            out=h[:, t, n * 512:(n + 1) * 512], in0=pz[:], scalar=C.gates[:, t, ex:ex + 1], in1=h[:, t, n * 512:(n + 1) * 512],
                            op0=ALU.mult, op1=ALU.add), R=[bpz, C.b_gates[t], C.bh[t]], W=[C.bh[t]])

        it = 0
        prev = None
        for ex in range(C.n_experts):
            gather_unit(ex)
            for hf in range(2):
                s = it % 2
                it += 1
                bw = b_w[s]
                P.add("pool", lambda e, s=s, ex=ex, hf=hf: e.dma_start(
                    out=w1g[:, s], in_=wgu[ex, :, hf * 512:(hf + 1) * 512].rearrange("(k p) n -> p k n", p=128)), W=[bw], dma=True)
                P.add("pool", lambda e, s=s, ex=ex, hf=hf: e.dma_start(
                    out=w1u[:, s], in_=wgu[ex, :, 1024 + hf * 512:1024 + (hf + 1) * 512].rearrange("(k p) n -> p k n", p=128)), W=[bw], dma=True)
                P.add("pool", lambda e, s=s, ex=ex, hf=hf: e.dma_start(
                    out=w2[:, s], in_=wd[ex, hf * 512:(hf + 1) * 512, :].rearrange("(j p) n -> p j n", p=128)), W=[bw], dma=True)
                a = C.act_rr % 2
                C.act_rr += 1
                w1_unit(ex, hf, s, a)
                if prev is not None:
                    w2_unit(*prev)
                if hf == 0:
                    pt_unit(ex)
                prev = (ex, hf, s, a)
        if prev is not None:
            w2_unit(*prev)
        P.flush()
    with ExitStack() as es:
        lng = es.enter_context(nc.sbuf_tensor("m_lng_%d" % li, [128, D], F32))
        lnb = es.enter_context(nc.sbuf_tensor("m_lnb_%d" % li, [128, D], F32))
        blnp = Buf()
        load_ln_params(C, lng, lnb, blnp, T["ln_g"][li, 1, :], T["ln_b"][li, 1, :])
        for t in range(NT):
            ln_tile(C, t, lng, lnb, blnp, router=None)
        P.flush()


def out_proj_ln(C, li, T, srcT, nchunks, w_dram, R_):
    P, nc = C.P, C.nc
    h = C.h
    with ExitStack() as es:
        wo = es.enter_context(nc.sbuf_tensor("o_w_%d" % li, [128, nchunks, D], BF16))
        lng = es.enter_context(nc.sbuf_tensor("o_lng_%d" % li, [128, D], F32))
        lnb = es.enter_context(nc.sbuf_tensor("o_lnb_%d" % li, [128, D], F32))
        bw, blnp = Buf(), Buf()
        P.add("pool", lambda e: e.dma_start(out=wo[:], in_=w_dram.rearrange("(k p) n -> p k n", p=128)), W=[bw], dma=True)
        load_ln_params(C, lng, lnb, blnp, T["ln_g"][li, 0, :], T["ln_b"][li, 0, :])
        for t in range(NT):
            for n in range(2):
                po, bpo = C.ps[3 + n + 2 * (t % 2)], C.bps[3 + n + 2 * (t % 2)]
                for f in range(nchunks):
                    P.add("pe", lambda e, f=f, n=n, t=t, po=po: e.matmul(po[:], lhsT=srcT[:, f, t * 128:(t + 1) * 128], rhs=wo[:, f, n * 512:(n + 1) * 512],
                                                                      start=(f == 0), stop=(f == nchunks - 1)), R=[C.b_srcT, bw], W=[bpo])
                P.add("dve", lambda e, n=n, t=t, po=po: e.scalar_tensor_tensor(out=h[:, t, n * 512:(n + 1) * 512], in0=h[:, t, n * 512:(n + 1) * 512],
                                                                             scalar=ALPHA, in1=po[:], op0=ALU.mult, op1=ALU.add),
                      R=[bpo, C.bh[t]], W=[C.bh[t]])
            ln_tile(C, t, lng, lnb, blnp, router=R_)
        P.flush()


def moba_phase(C, li, T, R_):
    P, nc = C.P, C.nc
    h, hT = C.h, C.hT
    j = li // 3
    w_in = T["a_w_in_%d" % j]
    H = 8
    SCALE = 128 ** -0.5
    BIG = 30000.0
    with nc.sbuf_tensor("a_aT_%d" % li, [128, 8, S], BF16) as aT:
        C.b_srcT = Buf("aT")
        with ExitStack() as es:
            wq = es.enter_context(nc.sbuf_tensor("a_wq_%d" % li, [128, 2, 8, 384], BF16))
            qT = es.enter_context(nc.sbuf_tensor("a_qT_%d" % li, [128, 2, S], BF16))
            kT = es.enter_context(nc.sbuf_tensor("a_kT_%d" % li, [128, 2, S], BF16))
            V = es.enter_context(nc.sbuf_tensor("a_V_%d" % li, [128, 2, NT, 128], BF16))
            kms = es.enter_context(nc.sbuf_tensor("a_kms_%d" % li, [128, 8], F32))
            kmb = es.enter_context(nc.sbuf_tensor("a_kmb_%d" % li, [128, 8], BF16))
            g2 = es.enter_context(nc.sbuf_tensor("a_g2_%d" % li, [128, 128], F32))
            m8 = es.enter_context(nc.sbuf_tensor("a_m8_%d" % li, [128, 8], F32))
            bm = es.enter_context(nc.sbuf_tensor("a_bm_%d" % li, [128, 128], F32))
            cm = es.enter_context(nc.sbuf_tensor("a_cm_%d" % li, [128, 128], F32))
            vm = es.enter_context(nc.sbuf_tensor("a_vm_%d" % li, [128, 128], F32))
            vm2 = es.enter_context(nc.sbuf_tensor("a_vm2_%d" % li, [128, 128], F32))
            rel = es.enter_context(nc.sbuf_tensor("a_rel_%d" % li, [128, 8, 2, 128], F32))
            caus = es.enter_context(nc.sbuf_tensor("a_caus_%d" % li, [128, 128], F32))
            cb = es.enter_context(nc.sbuf_tensor("a_cb_%d" % li, [128, 8], F32))
            sc = es.enter_context(nc.sbuf_tensor("a_s_%d" % li, [128, 2, S], F32))
            pb = es.enter_context(nc.sbuf_tensor("a_p_%d" % li, [128, S], BF16))
            pT = es.enter_context(nc.sbuf_tensor("a_pT_%d" % li, [128, 2, 4, 128], BF16))
            stt = es.enter_context(nc.sbuf_tensor("a_st_%d" % li, [128, 2, 4], F32))
            b_wq = [Buf() for _ in range(2)]
            b_q = [Buf() for _ in range(2)]
            b_k = [Buf() for _ in range(2)]
            b_v = [Buf() for _ in range(2)]
            b_km, b_gate, b_cst = Buf(), Buf(), Buf()
            b_s = [Buf() for _ in range(2)]
            b_p = [Buf()] * 2
            b_pT = [Buf() for _ in range(2)]
            b_st = [Buf() for _ in range(2)]
            SK = ""
            if "rel" not in SK:
                P.add("sp", lambda e: e.dma_start(out=rel[:], in_=T["relT"].rearrange("h d i j -> i h d j")), W=[b_cst], dma=True)
            P.add("sp", lambda e: e.dma_start(out=caus[:], in_=T["causal"]), W=[b_cst], dma=True)
            P.add("sp", lambda e: e.dma_start(out=vm[:], in_=T["vmask"]), W=[b_cst], dma=True)
            P.add("sp", lambda e: e.dma_start(out=vm2[:], in_=T["vmask2"]), W=[b_cst], dma=True)
            if "cb" not in SK:
                P.add("sp", lambda e: e.dma_start(out=cb[:], in_=bcast_rows(T["rel_bias"][31, :])), W=[b_cst], dma=True)
            for hh in range(H):
                P.add("pool", lambda e, hh=hh: e.tensor_tensor(out=rel[:, hh, 0, :], in0=rel[:, hh, 0, :], in1=caus[:], op=ALU.add), R=[b_cst], W=[b_cst])
            allT = list(C.bhT)
            for hh in range(H):
                s = hh % 2
                for c3 in range(3):
                    P.add("pool", lambda e, s=s, c3=c3, hh=hh: e.dma_start(
                        out=wq[:, s, :, c3 * 128:(c3 + 1) * 128],
                        in_=w_in[:, c3 * 1024 + hh * 128:c3 * 1024 + (hh + 1) * 128].rearrange("(k p) n -> p k n", p=128)), W=[b_wq[s]], dma=True)
                for g in range(4):
                    pq, bpq = C.ps[3], C.bps[3]
                    pk, bpk = C.ps[4], C.bps[4]
                    for k in range(8):
                        P.add("pe", lambda e, k=k, g=g, s=s: e.matmul(pq[:], lhsT=wq[:, s, k, 0:128], rhs=hT[:, k, g * 512:(g + 1) * 512],
                                                                    start=(k == 0), stop=(k == 7)), R=[b_wq[s]] + allT[g * 4:g * 4 + 4], W=[bpq])
                    P.add("act", lambda e, g=g, s=s: e.copy(out=qT[:, s, g * 512:(g + 1) * 512], in_=pq[:]), R=[bpq], W=[b_q[s]])
                    for k in range(8):
                        P.add("pe", lambda e, k=k, g=g, s=s: e.matmul(pk[:], lhsT=wq[:, s, k, 128:256], rhs=hT[:, k, g * 512:(g + 1) * 512],
                                                                    start=(k == 0), stop=(k == 7)), R=[b_wq[s]] + allT[g * 4:g * 4 + 4], W=[bpk])
                    for b2 in range(2):
                        P.add("act", lambda e, g=g, s=s, b2=b2: e.activation(out=kT[:, s, g * 512 + b2 * 256:g * 512 + (b2 + 1) * 256],
                                                                           in_=pk[:, b2 * 256:(b2 + 1) * 256], func=AF.Copy,
                                                                           accum_out=kms[:, 2 * g + b2:2 * g + b2 + 1]), R=[bpk], W=[b_k[s], b_km])
                P.add("dve", lambda e: e.tensor_scalar(out=kmb[:], in0=kms[:], scalar1=1.0 / 256.0, scalar2=None, op0=ALU.mult), R=[b_km], W=[b_km])
                for g in range(4):
                    pv, bpv = C.ps[5], C.bps[5]
                    for tt in range(4):
                        t = g * 4 + tt
                        for k in range(8):
                            P.add("pe", lambda e, k=k, t=t, tt=tt, s=s: e.matmul(pv[:, tt * 128:(tt + 1) * 128], lhsT=hT[:, k, t * 128:(t + 1) * 128],
                                                                               rhs=wq[:, s, k, 256:384], start=(k == 0), stop=(k == 7)),
                                  R=[b_wq[s], C.bhT[t]], W=[bpv])
                    P.add("act", lambda e, g=g, s=s: e.copy(out=V[:, s, g * 4:g * 4 + 4, :], in_=pv[:].rearrange("p (a c) -> p a c", a=4)),
                          R=[bpv], W=[b_v[s]])
                MD = 9
                if MD < 2:
                    continue
                pg, bpg = C.ps[6], C.bps[6]
                for qt in range(NT):
                    P.add("pe", lambda e, qt=qt, s=s: e.matmul(pg[:, qt * 8:(qt + 1) * 8], lhsT=qT[:, s, qt * 128:(qt + 1) * 128], rhs=kmb[:],
                                                             start=True, stop=True), R=[b_q[s], b_km], W=[bpg])
                P.add("dve", lambda e: e.tensor_tensor(out=g2[:], in0=pg[:, 0:128], in1=vm[:], op=ALU.add), R=[bpg, b_cst], W=[b_gate])
                for qt in range(NT):
                    P.add("dve", lambda e, qt=qt: e.max(out=m8[:], in_=g2[:, qt * 8:(qt + 1) * 8]), R=[b_gate], W=[b_gate])
                    P.add("dve", lambda e, qt=qt: e.tensor_scalar(out=bm[:, qt * 8:(qt + 1) * 8], in0=g2[:, qt * 8:(qt + 1) * 8], scalar1=m8[:, 2:3],
                                                                  scalar2=BIG, op0=ALU.is_ge, op1=ALU.mult), R=[b_gate], W=[b_gate])
                P.add("dve", lambda e: e.tensor_tensor(out=bm[:], in0=bm[:], in1=vm2[:], op=ALU.add), R=[b_gate, b_cst], W=[b_gate])
                P.add("dve", lambda e, hh=hh: e.tensor_scalar(out=cm[:], in0=bm[:], scalar1=cb[:, hh:hh + 1], scalar2=None, op0=ALU.add),
                      R=[b_gate, b_cst], W=[b_gate])
                for qt in range(NT if MD >= 3 else 0):
                    qb = qt // 2
                    nk = qt + 1
                    z = qt % 2
                    for c in range((nk + 3) // 4):
                        w = min(4, nk - c * 4)
                        pss, bpss = C.ps[3 + (C.q_rr % 2)], C.bps[3 + (C.q_rr % 2)]
                        C.q_rr += 1
                        P.add("pe", lambda e, c=c, w=w, qt=qt, s=s, pss=pss: e.matmul(pss[:, 0:w * 128], lhsT=qT[:, s, qt * 128:(qt + 1) * 128],
                                                                                   rhs=kT[:, s, c * 512:c * 512 + w * 128], start=True, stop=True),
                              R=[b_q[s], b_k[s]], W=[bpss])
                        for i in range(w):
                            kt = c * 4 + i
                            n = kt // 2
                            src = pss[:, i * 128:(i + 1) * 128]
                            dst = sc[:, z, kt * 128:(kt + 1) * 128]
                            if kt == qt:
                                P.add("dve", lambda e, src=src, dst=dst, hh=hh: e.scalar_tensor_tensor(out=dst, in0=src, scalar=SCALE, in1=rel[:, hh, 0, :],
                                                                                                     op0=ALU.mult, op1=ALU.add), R=[bpss, b_cst], W=[b_s[z]])
                            elif kt == qt - 1:
                                P.add("dve", lambda e, src=src, dst=dst, hh=hh: e.scalar_tensor_tensor(out=dst, in0=src, scalar=SCALE, in1=rel[:, hh, 1, :],
                                                                                                     op0=ALU.mult, op1=ALU.add), R=[bpss, b_cst], W=[b_s[z]])
                                if n < qb:
                                    P.add("dve", lambda e, dst=dst, qt=qt, n=n: e.tensor_scalar(out=dst, in0=dst, scalar1=bm[:, qt * 8 + n:qt * 8 + n + 1],
                                                                                              scalar2=None, op0=ALU.add), R=[b_s[z], b_gate], W=[b_s[z]])
                            else:
                                P.add("dve", lambda e, src=src, dst=dst, qt=qt, n=n: e.tensor_scalar(out=dst, in0=src, scalar1=SCALE,
                                                                                                   scalar2=cm[:, qt * 8 + n:qt * 8 + n + 1],
                                                                                                   op0=ALU.mult, op1=ALU.add), R=[bpss, b_gate], W=[b_s[z]])
                    L = nk * 128
                    if MD < 4:
                        continue
                    P.add("dve", lambda e, z=z, L=L: e.reduce_max(out=stt[:, z, 0:1], in_=sc[:, z, 0:L], axis=AX.X), R=[b_s[z]], W=[b_st[z]])
                    P.add("dve", lambda e, z=z: e.tensor_scalar(out=stt[:, z, 1:2], in0=stt[:, z, 0:1], scalar1=-1.0, scalar2=None, op0=ALU.mult),
                          R=[b_st[z]], W=[b_st[z]])
                    P.add("act", lambda e, z=z, L=L: e.activation(out=sc[:, z, 0:L], in_=sc[:, z, 0:L], func=AF.Exp, bias=stt[:, z, 1:2], scale=1.0,
                                                                  accum_out=stt[:, z, 2:3]), R=[b_s[z], b_st[z]], W=[b_s[z], b_st[z]])
                    P.add("dve", lambda e, z=z: e.reciprocal(out=stt[:, z, 3:4], in_=stt[:, z, 2:3]), R=[b_st[z]], W=[b_st[z]])
                    P.add("act", lambda e, z=z, L=L: e.activation(out=pb[:, 0:L], in_=sc[:, z, 0:L], func=AF.Copy, scale=stt[:, z, 3:4]),
                          R=[b_s[z], b_st[z]], W=[b_p[z]])
                    po, bpo = C.ps[5 + z], C.bps[5 + z]
                    if MD < 5:
                        continue
                    for c in range((nk + 3) // 4):
                        w = min(4, nk - c * 4)
                        y = C.y_rr % 2
                        C.y_rr += 1
                        ptp, bptp = C.ps[(0, 1)[y]], C.bps[(0, 1)[y]]
                        ptb = ptp[:].bitcast(BF16)
                        for i in range(w):
                            kt = c * 4 + i
                            P.add("pe", lambda e, i=i, kt=kt, z=z, ptb=ptb: e.transpose(out=ptb[:, i * 128:(i + 1) * 128], in_=pb[:, kt * 128:(kt + 1) * 128],
                                                                                      identity=C.identb[:]), R=[b_p[z], C.b_const], W=[bptp])
                        P.add("act", lambda e, w=w, y=y, ptb=ptb: e.copy(out=pT[:, y, 0:w, :], in_=ptb[:, 0:w * 128].rearrange("p (a c) -> p a c", a=w)),
                              R=[bptp], W=[b_pT[y]])
                        for i in range(w):
                            kt = c * 4 + i
                            P.add("pe", lambda e, i=i, kt=kt, y=y, s=s, qt=qt, nk=nk, po=po: e.matmul(po[:, 0:128], lhsT=V[:, s, kt, :], rhs=pT[:, y, i, :],
                                                                                                   start=(kt == 0), stop=(kt == nk - 1)),
                                  R=[b_v[s], b_pT[y]], W=[bpo])
                    P.add("act", lambda e, po=po, hh=hh, qt=qt: e.copy(out=aT[:, hh, qt * 128:(qt + 1) * 128], in_=po[:, 0:128]), R=[bpo], W=[C.b_srcT])
            P.flush()
        out_proj_ln(C, li, T, aT, 8, T["a_w_out_%d" % j], R_)


def gmlp_phase(C, li, T, R_):
    P, nc = C.P, C.nc
    h, hT = C.h, C.hT
    j = li // 3
    w_in = T["b_w_in_%d" % j]
    w_out = T["b_w_out_%d" % j]
    with ExitStack() as es:
        lng = es.enter_context(nc.sbuf_tensor("g_lng_%d" % li, [128, D], F32))
        lnb = es.enter_context(nc.sbuf_tensor("g_lnb_%d" % li, [128, D], F32))
        wi = es.enter_context(nc.sbuf_tensor("g_wi_%d" % li, [128, 2, 8, 512], BF16))
        wo = es.enter_context(nc.sbuf_tensor("g_wo_%d" % li, [128, 2, 4, 1024], BF16))
        yT = es.enter_context(nc.sbuf_tensor("g_yT_%d" % li, [128, 16, 256], BF16))
        v = es.enter_context(nc.sbuf_tensor("g_v_%d" % li, [128, 2, 2048], F32))
        vn = es.enter_context(nc.sbuf_tensor("g_vn_%d" % li, [128, 2, 2048], BF16))
        gb = es.enter_context(nc.sbuf_tensor("g_gb_%d" % li, [128, 2, 2048], F32))
        ws = es.enter_context(nc.sbuf_tensor("g_ws_%d" % li, [128, 8, 128], F32))
        wsT = es.enter_context(nc.sbuf_tensor("g_wsT_%d" % li, [128, 8, 128], BF16))
        tril = es.enter_context(nc.sbuf_tensor("g_tril_%d" % li, [128, 128], F32))
        bs = es.enter_context(nc.sbuf_tensor("g_bs_%d" % li, [1, 1024], F32))
        ones = es.enter_context(nc.sbuf_tensor("g_ones_%d" % li, [1, 128], F32))
        st = es.enter_context(nc.sbuf_tensor("g_st_%d" % li, [128, 4, 6], F32))
        mv = es.enter_context(nc.sbuf_tensor("g_mv_%d" % li, [128, 2], F32))
        rs = es.enter_context(nc.sbuf_tensor("g_rs_%d" % li, [128, 1], F32))
        blnp, b_cst, b_wsT, b_yT, b_st = Buf(), Buf(), Buf(), Buf(), Buf()
        b_wi = [Buf(), Buf()]
        b_wo = [Buf(), Buf()]
        b_v = [Buf(), Buf()]
        b_vn = [Buf(), Buf()]
        load_ln_params(C, lng, lnb, blnp, T["ln_g"][li, 0, :], T["ln_b"][li, 0, :])
        P.add("sp", lambda e: e.dma_start(out=gb[:, 0, :], in_=bcast_rows(T["b_ln_g_%d" % j])), W=[b_cst], dma=True)
        P.add("sp", lambda e: e.dma_start(out=gb[:, 1, :], in_=bcast_rows(T["b_ln_b_%d" % j])), W=[b_cst], dma=True)
        P.add("sp", lambda e: e.dma_start(out=ws[:], in_=T["b_w_s_%d" % j].rearrange("g t s -> t g s")), W=[b_cst], dma=True)
        P.add("sp", lambda e: e.dma_start(out=tril[:], in_=T["tril"]), W=[b_cst], dma=True)
        P.add("sp", lambda e: e.dma_start(out=bs[:], in_=T["b_b_s_%d" % j].rearrange("(o g) t -> o (g t)", o=1)), W=[b_cst], dma=True)
        P.add("pool", lambda e: e.memset(ones[:], 1.0), W=[b_cst])
        for g in range(8):
            P.add("dve", lambda e, g=g: e.tensor_tensor(out=ws[:, g, :], in0=ws[:, g, :], in1=tril[:], op=ALU.mult), R=[b_cst], W=[b_cst])
        for half in range(2):
            ps, bps = C.ps[half], C.bps[half]
            for gg in range(4):
                g = half * 4 + gg
                P.add("pe", lambda e, g=g, gg=gg, ps=ps: e.transpose(out=ps[:, gg * 128:(gg + 1) * 128], in_=ws[:, g, :], identity=C.ident[:]),
                      R=[b_cst, C.b_const], W=[bps])
            P.add("act", lambda e, ps=ps, half=half: e.copy(out=wsT[:, half * 4:half * 4 + 4, :], in_=ps[:].rearrange("p (a c) -> p a c", a=4)),
                  R=[bps], W=[b_wsT])
        it = 0
        iw = 0
        acc_banks = (5, 6, 7, 2)
        DBG = 9
        for tg in range(8 if DBG >= 2 else 0):
            c0 = tg * 256
            tiles = (2 * tg, 2 * tg + 1)
            bhTg = [C.bhT[t] for t in tiles]
            for cg in range(8):
                s = it % 2
                it += 1
                P.add("pool", lambda e, s=s, cg=cg: e.dma_start(out=wi[:, s], in_=w_in[:, cg * 512:(cg + 1) * 512].rearrange("(k p) n -> p k n", p=128)),
                      W=[b_wi[s]], dma=True)
                if cg < 4:
                    for jj in range(4):
                        fc = cg * 4 + jj
                        q = C.q_rr % 2
                        C.q_rr += 1
                        pu, bpu = C.ps[3 + q], C.bps[3 + q]
                        for k in range(8):
                            P.add("pe", lambda e, k=k, jj=jj, s=s, pu=pu, c0=c0: e.matmul(pu[:, 0:256], lhsT=wi[:, s, k, jj * 128:(jj + 1) * 128],
                                                                                      rhs=hT[:, k, c0:c0 + 256], start=(k == 0), stop=(k == 7)),
                                  R=[b_wi[s]] + bhTg, W=[bpu])
                        P.add("act", lambda e, fc=fc, pu=pu: e.activation(out=yT[:, fc, :], in_=pu[:, 0:256], func=AF.Gelu), R=[bpu], W=[b_yT])
                else:
                    for ti in range(2):
                        t = tiles[ti]
                        q = C.q_rr % 2
                        C.q_rr += 1
                        pv, bpv = C.ps[3 + q], C.bps[3 + q]
                        for k in range(8):
                            P.add("pe", lambda e, k=k, t=t, s=s, pv=pv: e.matmul(pv[:], lhsT=hT[:, k, t * 128:(t + 1) * 128], rhs=wi[:, s, k, :],
                                                                               start=(k == 0), stop=(k == 7)), R=[b_wi[s], C.bhT[t]], W=[bpv])
                        P.add("act", lambda e, ti=ti, cg=cg, pv=pv: e.activation(out=v[:, ti, (cg - 4) * 512:(cg - 3) * 512], in_=pv[:], func=AF.Gelu),
                              R=[bpv], W=[b_v[ti]])
            for ti in range(2 if DBG >= 3 else 0):
                for c in range(4):
                    P.add("dve", lambda e, c=c, ti=ti: e.bn_stats(out=st[:, c, :], in_=v[:, ti, c * 512:(c + 1) * 512]), R=[b_v[ti]], W=[b_st])
                P.add("dve", lambda e: e.bn_aggr(out=mv[:], in_=st[:]), R=[b_st], W=[b_st])
                P.add("act", lambda e: e.activation(out=rs[:], in_=mv[:, 1:2], func=AF.Sqrt, bias=EPS, scale=1.0), R=[b_st], W=[b_st])
                P.add("dve", lambda e: e.reciprocal(out=rs[:], in_=rs[:]), R=[b_st], W=[b_st])
                P.add("dve", lambda e, ti=ti: e.tensor_scalar(out=v[:, ti, :], in0=v[:, ti, :], scalar1=mv[:, 0:1], scalar2=rs[:, 0:1],
                                                              op0=ALU.subtract, op1=ALU.mult), R=[b_v[ti], b_st], W=[b_v[ti]])
                P.add("pool", lambda e, ti=ti: e.tensor_tensor(out=v[:, ti, :], in0=v[:, ti, :], in1=gb[:, 0, :], op=ALU.mult), R=[b_v[ti], b_cst], W=[b_v[ti]])
                P.add("pool", lambda e, ti=ti: e.tensor_tensor(out=vn[:, ti, :], in0=v[:, ti, :], in1=gb[:, 1, :], op=ALU.add), R=[b_v[ti], b_cst], W=[b_vn[ti]])
            for ti in range(2 if DBG >= 4 else 0):
                for bk in range(4):
                    q = C.q_rr % 2
                    C.q_rr += 1
                    pm, bpm = C.ps[3 + q], C.bps[3 + q]
                    for jj in range(4):
                        fc = bk * 4 + jj
                        g = fc // 2
                        P.add("pe", lambda e, jj=jj, fc=fc, g=g, ti=ti, pm=pm: e.matmul(pm[:, jj * 128:(jj + 1) * 128], lhsT=vn[:, ti, fc * 128:(fc + 1) * 128],
                                                                                      rhs=wsT[:, g, :], start=True, stop=False), R=[b_vn[ti], b_wsT], W=[bpm])
                        P.add("pe", lambda e, jj=jj, g=g, pm=pm: e.matmul(pm[:, jj * 128:(jj + 1) * 128], lhsT=ones[0:1, :], rhs=bs[0:1, g * 128:(g + 1) * 128],
                                                                        start=False, stop=True), R=[b_cst], W=[bpm])
                    P.add("dve", lambda e, bk=bk, ti=ti, pm=pm: e.tensor_tensor(out=yT[:, bk * 4:bk * 4 + 4, ti * 128:(ti + 1) * 128],
                                                                              in0=pm[:].rearrange("p (a c) -> p a c", a=4),
                                                                              in1=yT[:, bk * 4:bk * 4 + 4, ti * 128:(ti + 1) * 128], op=ALU.mult),
                          R=[bpm, b_yT], W=[b_yT])
            if DBG < 5:
                continue
            for wc in range(4):
                s2 = iw % 2
                iw += 1
                P.add("pool", lambda e, s2=s2, wc=wc: e.dma_start(out=wo[:, s2], in_=w_out[wc * 512:(wc + 1) * 512, :].rearrange("(f p) n -> p f n", p=128)),
                      W=[b_wo[s2]], dma=True)
                for ti in range(2):
                    for n in range(2):
                        bi = acc_banks[ti * 2 + n]
                        po, bpo = C.ps[bi], C.bps[bi]
                        for ff in range(4):
                            P.add("pe", lambda e, wc=wc, ff=ff, ti=ti, n=n, s2=s2, po=po: e.matmul(
                                po[:], lhsT=yT[:, wc * 4 + ff, ti * 128:(ti + 1) * 128], rhs=wo[:, s2, ff, n * 512:(n + 1) * 512],
                                start=(wc == 0 and ff == 0), stop=(wc == 3 and ff == 3)), R=[b_yT, b_wo[s2]], W=[bpo])
            for ti in range(2):
                t = tiles[ti]
                for n in range(2):
                    bi = acc_banks[ti * 2 + n]
                    po, bpo = C.ps[bi], C.bps[bi]
                    P.add("dve", lambda e, n=n, t=t, po=po: e.scalar_tensor_tensor(out=h[:, t, n * 512:(n + 1) * 512], in0=h[:, t, n * 512:(n + 1) * 512],
                                                                                 scalar=ALPHA, in1=po[:], op0=ALU.mult, op1=ALU.add),
                          R=[bpo, C.bh[t]], W=[C.bh[t]])
            for ti in range(2 if DBG >= 6 else 0):
                ln_tile(C, tiles[ti], lng, lnb, blnp, router=(R_ if DBG >= 7 else None))
        P.flush()


def gla_phase(C, li, T, R_):
    P, nc = C.P, C.nc
    h, hT = C.h, C.hT
    j = li // 3
    w_in = T["c_w_in_%d" % j]
    SCALE = 128 ** -0.5
    with nc.sbuf_tensor("c_oT_%d" % li, [128, 8, S], BF16) as oT:
        C.b_srcT = Buf("oT")
        with ExitStack() as es:
            wq = es.enter_context(nc.sbuf_tensor("c_wq_%d" % li, [128, 2, 8, 768], BF16))
            wg = es.enter_context(nc.sbuf_tensor("c_wg_%d" % li, [128, 8, 16], BF16))
            gkT = es.enter_context(nc.sbuf_tensor("c_gkT_%d" % li, [32, S], F32))
            wgk = es.enter_context(nc.sbuf_tensor("c_wgk_%d" % li, [32, 512], F32))
            ng = es.enter_context(nc.sbuf_tensor("c_ng_%d" % li, [128, 256], F32))
            tri = es.enter_context(nc.sbuf_tensor("c_tri_%d" % li, [128, 128], F32))
            su = es.enter_context(nc.sbuf_tensor("c_su_%d" % li, [128, 128], F32))
            cmk = es.enter_context(nc.sbuf_tensor("c_cm_%d" % li, [128, 128], F32))
            qk = es.enter_context(nc.sbuf_tensor("c_qk_%d" % li, [128, 2, 2, 128], F32))
            kt = es.enter_context(nc.sbuf_tensor("c_kt_%d" % li, [128, 2, 128], F32))
            vt = es.enter_context(nc.sbuf_tensor("c_vt_%d" % li, [128, 2, 256], BF16))
            gs = es.enter_context(nc.sbuf_tensor("c_gs_%d" % li, [128, 2, 256], F32))
            nl = es.enter_context(nc.sbuf_tensor("c_nl_%d" % li, [128, 2, 128], F32))
            ex = es.enter_context(nc.sbuf_tensor("c_ex_%d" % li, [128, 2, 3, 128], F32))
            qz = es.enter_context(nc.sbuf_tensor("c_qz_%d" % li, [128, 2, 2, 128], BF16))
            ke = es.enter_context(nc.sbuf_tensor("c_ke_%d" % li, [128, 2, 128], BF16))
            krz = es.enter_context(nc.sbuf_tensor("c_krz_%d" % li, [128, 2, 2, 128], BF16))
            att = es.enter_context(nc.sbuf_tensor("c_att_%d" % li, [128, 2, 128], BF16))
            St = es.enter_context(nc.sbuf_tensor("c_S_%d" % li, [128, 256], F32))
            Sb = es.enter_context(nc.sbuf_tensor("c_Sb_%d" % li, [128, 5, 256], BF16))
            jk = es.enter_context(nc.sbuf_tensor("c_jk_%d" % li, [128, 256], F32))
            ot = es.enter_context(nc.sbuf_tensor("c_ot_%d" % li, [128, 2, 256], F32))
            og = es.enter_context(nc.sbuf_tensor("c_og_%d" % li, [128, 2, 256], BF16))
            ss = es.enter_context(nc.sbuf_tensor("c_ss_%d" % li, [128, 2, 2], F32))
            b_cst, b_gk, b_S, b_jk = Buf(), Buf(), Buf(), Buf()
            b_wq = [Buf(), Buf()]
            b_qk = [Buf(), Buf()]
            b_kt = [Buf(), Buf()]
            b_vt = [Buf(), Buf()]
            b_gs = [Buf(), Buf()]
            b_nl = [Buf(), Buf()]
            b_ex = [Buf(), Buf()]
            b_qz = [Buf(), Buf()]
            b_ke = [Buf(), Buf()]
            b_krz = [Buf(), Buf()]
            b_att = [Buf(), Buf()]
            b_Sb = [Buf() for _ in range(5)]
            b_ot = [Buf(), Buf()]
            b_og = [Buf(), Buf()]
            b_ss = [Buf(), Buf()]
            P.add("pool", lambda e: e.dma_start(out=wg[:], in_=w_in[:, 3072:3088].rearrange("(k p) n -> p k n", p=128)), W=[b_cst], dma=True)
            P.add("sp", lambda e: e.dma_start(out=wgk[0:16, :], in_=T["c_w_gk_up_%d" % j]), W=[b_cst], dma=True)
            P.add("sp", lambda e: e.dma_start(out=wgk[16:17, :], in_=T["c_b_gk_%d" % j].rearrange("(o n) -> o n", o=1)), W=[b_cst], dma=True)
            P.add("sp", lambda e: e.dma_start(out=ng[:], in_=bcast_rows(T["c_norm_g_%d" % j])), W=[b_cst], dma=True)
            P.add("sp", lambda e: e.dma_start(out=tri[:], in_=T["gla_tri"]), W=[b_cst], dma=True)
            P.add("sp", lambda e: e.dma_start(out=su[:], in_=T["gla_su"]), W=[b_cst], dma=True)
            P.add("sp", lambda e: e.dma_start(out=cmk[:], in_=T["gla_cm"]), W=[b_cst], dma=True)
            P.add("pool", lambda e: e.memset(gkT[:], 1.0), W=[b_gk])
            for z in range(2):
                P.add("pool", lambda e, z=z: e.memset(qz[:, z], 0.0), W=[b_qz[z]])
                P.add("pool", lambda e, z=z: e.memset(krz[:, z], 0.0), W=[b_krz[z]])
            for g4 in range(4):
                pg, bpg = C.ps[3 + g4 % 2], C.bps[3 + g4 % 2]
                for k in range(8):
                    P.add("pe", lambda e, k=k, g4=g4, pg=pg: e.matmul(pg[0:16, :], lhsT=wg[:, k, :], rhs=hT[:, k, g4 * 512:(g4 + 1) * 512],
                                                                    start=(k == 0), stop=(k == 7)), R=[b_cst] + C.bhT[g4 * 4:g4 * 4 + 4], W=[bpg])
                P.add("act", lambda e, g4=g4, pg=pg: e.copy(out=gkT[0:16, g4 * 512:(g4 + 1) * 512], in_=pg[0:16, :]), R=[bpg], W=[b_gk])

            def front(hd, s, t):
                z = t % 2
                tc = slice(t * 128, (t + 1) * 128)
                bhT = C.bhT[t]
                pa, bpa = C.ps[3], C.bps[3]
                for k in range(8):
                    P.add("pe", lambda e, k=k: e.matmul(pa[:, 0:128], lhsT=wq[:, s, k, 0:128], rhs=hT[:, k, tc], start=(k == 0), stop=(k == 7)),
                          R=[b_wq[s], bhT], W=[bpa])
                for k in range(8):
                    P.add("pe", lambda e, k=k: e.matmul(pa[:, 128:256], lhsT=wq[:, s, k, 128:256], rhs=hT[:, k, tc], start=(k == 0), stop=(k == 7)),
                          R=[b_wq[s], bhT], W=[bpa])
                for k in range(8):
                    P.add("pe", lambda e, k=k: e.matmul(pa[:, 256:384], lhsT=hT[:, k, tc], rhs=wq[:, s, k, 128:256], start=(k == 0), stop=(k == 7)),
                          R=[b_wq[s], bhT], W=[bpa])
                P.add("pe", lambda e: e.matmul(pa[:, 384:512], lhsT=gkT[0:17, tc], rhs=wgk[0:17, hd * 128:(hd + 1) * 128], start=True, stop=True),
                      R=[b_gk, b_cst], W=[bpa])
                P.add("act", lambda e: e.copy(out=qk[:, z], in_=pa[:, 0:256].rearrange("p (a c) -> p a c", a=2)), R=[bpa], W=[b_qk[z]])
                P.add("act", lambda e: e.copy(out=kt[:, z], in_=pa[:, 256:384]), R=[bpa], W=[b_kt[z]])
                P.add("act", lambda e: e.activation(out=nl[:, z], in_=pa[:, 384:512], func=AF.Exp, scale=-1.0), R=[bpa], W=[b_nl[z]])
                P.add("act", lambda e: e.activation(out=nl[:, z], in_=nl[:, z], func=AF.Ln, bias=1.0, scale=1.0), R=[b_nl[z]], W=[b_nl[z]])
                pb_, bpb = C.ps[4], C.bps[4]
                for k in range(8):
                    P.add("pe", lambda e, k=k: e.matmul(pb_[:], lhsT=hT[:, k, tc], rhs=wq[:, s, k, 256:768], start=(k == 0), stop=(k == 7)),
                          R=[b_wq[s], bhT], W=[bpb])
                P.add("act", lambda e: e.copy(out=vt[:, z], in_=pb_[:, 0:256]), R=[bpb], W=[b_vt[z]])
                P.add("act", lambda e: e.activation(out=gs[:, z], in_=pb_[:, 256:512], func=AF.Silu), R=[bpb], W=[b_gs[z]])
                pc, bpc = C.ps[5], C.bps[5]
                P.add("pe", lambda e: e.matmul(pc[:, 0:128], lhsT=nl[:, z], rhs=tri[:], start=True, stop=True), R=[b_nl[z], b_cst], W=[bpc])
                P.add("pe", lambda e: e.matmul(pc[:, 128:256], lhsT=su[:], rhs=nl[:, z], start=True, stop=True), R=[b_nl[z], b_cst], W=[bpc])
                P.add("act", lambda e: e.activation(out=ex[:, z, 0, :], in_=pc[:, 0:128], func=AF.Exp, scale=-1.0 / 16.0), R=[bpc], W=[b_ex[z]])
                P.add("act", lambda e: e.activation(out=ex[:, z, 1, :], in_=pc[:, 0:128], func=AF.Exp, scale=1.0 / 16.0), R=[bpc], W=[b_ex[z]])
                P.add("act", lambda e: e.activation(out=ex[:, z, 2, :], in_=pc[:, 128:256], func=AF.Exp, scale=-1.0 / 16.0), R=[bpc], W=[b_ex[z]])
                for c in range(2):
                    cs = slice(c * 64, (c + 1) * 64)
                    P.add("dve", lambda e, c=c, cs=cs: e.scalar_tensor_tensor(out=qz[:, z, c, cs], in0=qk[:, z, 0, cs], scalar=SCALE, in1=ex[:, z, 0, cs],
                                                                            op0=ALU.mult, op1=ALU.mult), R=[b_qk[z], b_ex[z]], W=[b_qz[z]])
                P.add("pool", lambda e: e.tensor_tensor(out=ke[:, z], in0=qk[:, z, 1], in1=ex[:, z, 1, :], op=ALU.mult), R=[b_qk[z], b_ex[z]], W=[b_ke[z]])
                for c in range(2):
                    rows = slice(c * 64, (c + 1) * 64)
                    P.add("pool", lambda e, c=c, rows=rows: e.tensor_tensor(out=krz[rows, z, c, :], in0=kt[rows, z], in1=ex[rows, z, 2, :], op=ALU.mult),
                          R=[b_kt[z], b_ex[z]], W=[b_krz[z]])
                P.add("pe", lambda e: e.matmul(pc[:, 256:384], lhsT=ke[:, z], rhs=qz[:, z, 0, :], start=True, stop=False), R=[b_ke[z], b_qz[z]], W=[bpc])
                P.add("pe", lambda e: e.matmul(pc[:, 256:384], lhsT=ke[:, z], rhs=qz[:, z, 1, :], start=False, stop=True), R=[b_ke[z], b_qz[z]], W=[bpc])
                P.add("dve", lambda e: e.tensor_tensor(out=att[:, z], in0=pc[:, 256:384], in1=cmk[:], op=ALU.mult), R=[bpc, b_cst], W=[b_att[z]])
                pd, bpd = C.ps[6], C.bps[6]
                P.add("pe", lambda e: e.matmul(pd[:, 0:256], lhsT=krz[:, z, 0, :], rhs=vt[:, z], start=True, stop=True), R=[b_krz[z], b_vt[z]], W=[bpd])
                P.add("pe", lambda e: e.matmul(pd[:, 256:512], lhsT=krz[:, z, 1, :], rhs=vt[:, z], start=True, stop=True), R=[b_krz[z], b_vt[z]], W=[bpd])
                P.add("dve", lambda e: e.scalar_tensor_tensor(out=St[:], in0=St[:], scalar=ex[:, z, 0, 63:64], in1=pd[:, 0:256], op0=ALU.mult, op1=ALU.add),
                      R=[b_S, b_ex[z], bpd], W=[b_S])
                P.add("act", lambda e: e.copy(out=Sb[:, 3 + z, :], in_=St[:]), R=[b_S], W=[b_Sb[3 + z]])
                P.add("dve", lambda e: e.scalar_tensor_tensor(out=St[:], in0=St[:], scalar=ex[:, z, 0, 127:128], in1=pd[:, 256:512], op0=ALU.mult, op1=ALU.add),
                      R=[b_S, b_ex[z], bpd], W=[b_S])
                P.add("act", lambda e: e.copy(out=Sb[:, (t + 1) % 3, :], in_=St[:]), R=[b_S], W=[b_Sb[(t + 1) % 3]])

            def back(hd, s, t):
                z = t % 2
                tc = slice(t * 128, (t + 1) * 128)
                po, bpo = C.ps[7], C.bps[7]
                P.add("pe", lambda e: e.matmul(po[:, 0:256], lhsT=qz[:, z, 0, :], rhs=Sb[:, t % 3, :], start=True, stop=False), R=[b_qz[z], b_Sb[t % 3]], W=[bpo])
                P.add("pe", lambda e: e.matmul(po[:, 0:256], lhsT=qz[:, z, 1, :], rhs=Sb[:, 3 + z, :], start=False, stop=False), R=[b_qz[z], b_Sb[3 + z]], W=[bpo])
                P.add("pe", lambda e: e.matmul(po[:, 0:256], lhsT=att[:, z], rhs=vt[:, z], start=False, stop=True), R=[b_att[z], b_vt[z]], W=[bpo])
                P.add("act", lambda e: e.activation(out=jk[:], in_=po[:, 0:256], func=AF.Square, accum_out=ss[:, z, 0:1]), R=[bpo], W=[b_jk, b_ss[z]])
                P.add("act", lambda e: e.activation(out=ss[:, z, 1:2], in_=ss[:, z, 0:1], func=AF.Sqrt, bias=EPS, scale=1.0 / 256.0), R=[b_ss[z]], W=[b_ss[z]])
                P.add("dve", lambda e: e.reciprocal(out=ss[:, z, 1:2], in_=ss[:, z, 1:2]), R=[b_ss[z]], W=[b_ss[z]])
                P.add("dve", lambda e: e.scalar_tensor_tensor(out=ot[:, z], in0=po[:, 0:256], scalar=ss[:, z, 1:2], in1=ng[:], op0=ALU.mult, op1=ALU.mult),
                      R=[bpo, b_ss[z], b_cst], W=[b_ot[z]])
                P.add("pool", lambda e: e.tensor_tensor(out=og[:, z], in0=ot[:, z], in1=gs[:, z], op=ALU.mult), R=[b_ot[z], b_gs[z]], W=[b_og[z]])
                ptp, bptp = C.ps[z], C.bps[z]
                ptb = ptp[:].bitcast(BF16)
                for c2 in range(2):
                    P.add("pe", lambda e, c2=c2: e.transpose(out=ptb[:, c2 * 128:(c2 + 1) * 128], in_=og[:, z, c2 * 128:(c2 + 1) * 128], identity=C.identb[:]),
                          R=[b_og[z], C.b_const], W=[bptp])
                P.add("act", lambda e: e.copy(out=oT[:, hd * 2:hd * 2 + 2, tc], in_=ptb[:, 0:256].rearrange("p (a c) -> p a c", a=2)), R=[bptp], W=[C.b_srcT])

            for hd in range(4):
                s = hd % 2
                for (c0_, w_, d0) in ((hd * 128, 128, 0), (512 + hd * 128, 128, 128), (1024 + hd * 256, 256, 256), (2048 + hd * 256, 256, 512)):
                    P.add("pool", lambda e, c0_=c0_, w_=w_, d0=d0, s=s: e.dma_start(
                        out=wq[:, s, :, d0:d0 + w_], in_=w_in[:, c0_:c0_ + w_].rearrange("(k p) n -> p k n", p=128)), W=[b_wq[s]], dma=True)
                P.add("pool", lambda e: e.memset(St[:], 0.0), W=[b_S])
                P.add("pool", lambda e: e.memset(Sb[:, 0, :], 0.0), W=[b_Sb[0]])
                front(hd, s, 0)
                for t in range(NT):
                    if t + 1 < NT:
                        front(hd, s, t + 1)
                    back(hd, s, t)
            P.flush()
        out_proj_ln(C, li, T, oT, 8, T["c_w_out_%d" % j], R_)


def build_program(mode="full", n_experts=NE, layers=(0, 1, 2, 3)):
    nc = bass.Bass("TRN2", target_bir_lowering=False)
    T = {}

    def din(name, shape, dt=F32):
        T[name] = nc.dram_tensor(name, list(shape), dt, kind="ExternalInput").ap()

    do_mixer = mode in ("full", "mixer_only")
    do_moe = mode in ("full", "moe_only")
    din("x", (S, D))
    din("ident", (128, 128))
    din("identb", (128, 128), BF16)
    din("ln_g", (DEPTH, 2, D))
    din("ln_b", (DEPTH, 2, D))
    mixers = sorted(set(li % 3 for li in layers)) if do_mixer else []
    if 0 in mixers:
        din("rel_bias", (32, 8))
        din("relT", (8, 2, 128, 128))
        din("causal", (128, 128))
        din("vmask", (128, 128))
        din("vmask2", (128, 128))
    for li in layers:
        if do_mixer:
            j = li // 3
            if li % 3 == 0:
                din("a_w_in_%d" % j, (D, 3 * D))
                din("a_w_out_%d" % j, (D, D))
            elif li % 3 == 1:
                din("b_w_in_%d" % j, (D, 4 * D))
                din("b_ln_g_%d" % j, (2 * D,))
                din("b_ln_b_%d" % j, (2 * D,))
                din("b_w_s_%d" % j, (8, 128, 128))
                din("b_b_s_%d" % j, (8, 128))
                din("b_w_out_%d" % j, (2 * D, D))
                din("tril", (128, 128))
            else:
                din("c_w_in_%d" % j, (D, 3088))
                din("c_w_gk_up_%d" % j, (16, 512))
                din("c_b_gk_%d" % j, (512,))
                din("c_norm_g_%d" % j, (256,))
                din("c_w_out_%d" % j, (D, D))
                din("gla_tri", (128, 128))
                din("gla_su", (128, 128))
                din("gla_cm", (128, 128))
        din("moe_w_router_%d" % li, (D, NE))
        din("moe_b_router_%d" % li, (NE,))
        if do_moe:
            if "iota" not in T:
                din("iota", (128, 128))
                din("ones_b", (128, 128), BF16)
                din("tris_b", (128, 128), BF16)
            din("moe_w_gate_up_%d" % li, (NE, D, 2 * D))
            din("moe_b_gate_up_%d" % li, (NE, 2 * D))
            din("moe_w_down_%d" % li, (NE, D, D))
            din("moe_b_down_%d" % li, (NE, D))
    out = nc.dram_tensor("out", [S, D], F32, kind="ExternalOutput").ap()

    C = Ctx()
    C.nc = nc
    C.n_experts = n_experts
    C.P = P = Prog(nc)
    C.tp_rr = C.act_rr = C.q_rr = C.y_rr = C.ys_rr = 0
    with ExitStack() as es:
        h = es.enter_context(nc.sbuf_tensor("h", [128, NT, D], F32))
        hT = es.enter_context(nc.sbuf_tensor("hT", [128, 8, S], BF16))
        ident = es.enter_context(nc.sbuf_tensor("ident_s", [128, 128], F32))
        identb = es.enter_context(nc.sbuf_tensor("identb_s", [128, 128], BF16))
        hT32 = es.enter_context(nc.sbuf_tensor("hT32", [128, 8, 128], F32))
        gates = es.enter_context(nc.sbuf_tensor("gates", [128, NT, NE], F32))
        ln_st = es.enter_context(nc.sbuf_tensor("ln_st", [128, 2, 6], F32))
        ln_mv = es.enter_context(nc.sbuf_tensor("ln_mv", [128, 2], F32))
        ln_rs = es.enter_context(nc.sbuf_tensor("ln_rs", [128, 1], F32))
        r_lg = es.enter_context(nc.sbuf_tensor("r_lg", [128, NE], F32))
        r_m8 = es.enter_context(nc.sbuf_tensor("r_m8", [128, 8], F32))
        r_ex = es.enter_context(nc.sbuf_tensor("r_ex", [128, NE], F32))
        r_msk = es.enter_context(nc.sbuf_tensor("r_msk", [128, NE], F32))
        r_sm = es.enter_context(nc.sbuf_tensor("r_sm", [128, 2], F32))
        r_wr = es.enter_context(nc.sbuf_tensor("r_wr", [128, 8, NE], F32))
        r_brb = es.enter_context(nc.sbuf_tensor("r_brb", [128, NE], F32))
        C.h, C.hT, C.ident, C.identb, C.hT32, C.gates = h, hT, ident, identb, hT32, gates
        C.ln_st, C.ln_mv, C.ln_rs, C.ln_nb = ln_st, ln_mv, ln_rs, None
        C.r_lg, C.r_m8, C.r_ex, C.r_msk, C.r_sm = r_lg, r_m8, r_ex, r_msk, r_sm
        C.bh = [Buf("h%d" % t) for t in range(NT)]
        C.bhT = [Buf("hT%d" % t) for t in range(NT)]
        C.b_gates = [Buf() for t in range(NT)]
        C.b_const = Buf("const")
        C.b_lnst = Buf()
        C.b_hT32 = Buf()
        C.b_rt = Buf()
        ps_cms = [nc.psum_tensor("ps%d" % i, [128, 512], F32) for i in range(8)]
        C.ps = [cm.__enter__() for cm in ps_cms]
        C.bps = [Buf("ps%d" % i) for i in range(8)]

        P.add("sp", lambda e: e.dma_start(out=ident[:], in_=T["ident"]), W=[C.b_const], dma=True)
        P.add("sp", lambda e: e.dma_start(out=identb[:], in_=T["identb"]), W=[C.b_const], dma=True)
        for t in range(NT):
            P.add("sp", lambda e, t=t: e.dma_start(out=h[:, t, :], in_=T["x"][t * 128:(t + 1) * 128, :]), W=[C.bh[t]], dma=True)
        first = True
        for li in layers:
            b_r = Buf()
            P.add("sp", lambda e, li=li: e.dma_start(out=r_wr[:], in_=T["moe_w_router_%d" % li].rearrange("(k p) n -> p k n", p=128)), W=[b_r], dma=True)
            P.add("sp", lambda e, li=li: e.dma_start(out=r_brb[:], in_=bcast_rows(T["moe_b_router_%d" % li])), W=[b_r], dma=True)
            R_ = {"wr": r_wr, "brb": r_brb, "b": b_r}
            if first:
                for t in range(NT):
                    transpose_tile(C, t, None if do_mixer else R_)
                P.flush()
                first = False
            if do_mixer:
                if li % 3 == 0:
                    moba_phase(C, li, T, R_)
                elif li % 3 == 1:
                    gmlp_phase(C, li, T, R_)
                else:
                    gla_phase(C, li, T, R_)
            if do_moe:
                moe_phase(C, li, T)

        for t in range(NT):
            P.add("sp", lambda e, t=t: e.dma_start(out=out[t * 128:(t + 1) * 128, :], in_=h[:, t, :]), R=[C.bh[t]], dma=True)
        P.flush()
        for cm in reversed(ps_cms):
            cm.__exit__(None, None, None)
    P.close()
    return nc


def t5_bucket_np(rel):
    n = np.maximum(rel, 0)
    nf = np.maximum(n, 1).astype(np.float32)
    large = 16 + (np.log(nf / np.float32(16)) / np.float32(math.log(128 / 16)) * np.float32(16)).astype(np.int32)
    large = np.minimum(large, 31)
    return np.where(n < 16, n, large)


def host_constants(rel_bias=None):
    import ml_dtypes
    c = {"ident": np.eye(128, dtype=np.float32), "identb": np.eye(128, dtype=np.float32).astype(ml_dtypes.bfloat16)}
    i = np.arange(128)[:, None]
    jj = np.arange(128)[None, :]
    c["causal"] = np.where(jj <= i, 0.0, NEG).astype(np.float32)
    c["iota"] = np.broadcast_to(np.arange(128, dtype=np.float32)[None, :], (128, 128)).copy()
    c["ones_b"] = np.ones((128, 128), np.float32).astype(ml_dtypes.bfloat16)
    c["tris_b"] = (i < jj).astype(np.float32).astype(ml_dtypes.bfloat16)
    c["tril"] = (jj <= i).astype(np.float32)
    qt = np.arange(16)[:, None]
    n = np.arange(8)[None, :]
    valid = n < (qt // 2)
    vm = np.where(valid, 0.0, -1e30).astype(np.float32).reshape(1, 128)
    c["vmask"] = np.broadcast_to(vm, (128, 128)).copy()
    vm2 = np.where(valid, -30000.0, -1e30).astype(np.float32).reshape(1, 128)
    c["vmask2"] = np.broadcast_to(vm2, (128, 128)).copy()
    if rel_bias is not None:
        relT = np.empty((8, 2, 128, 128), np.float32)
        for d in range(2):
            bk = t5_bucket_np(d * 128 + i - jj)
            for hh in range(8):
                relT[hh, d] = rel_bias[bk, hh]
        c["relT"] = relT
    same = (i // 64) == (jj // 64)
    c["gla_tri"] = (same & (i <= jj)).astype(np.float32)
    c["gla_su"] = (same & (i > jj)).astype(np.float32)
    c["gla_cm"] = (same & (i <= jj)).astype(np.float32)
    return c


def make_in_map(A, x_core, consts, layers=(0, 1, 2, 3), mode="full"):
    do_mixer = mode in ("full", "mixer_only")
    do_moe = mode in ("full", "moe_only")
    im = {"x": np.ascontiguousarray(x_core), "ln_g": A["ln_g"], "ln_b": A["ln_b"]}
    im.update(consts)
    if "rel_bias" in A:
        im["rel_bias"] = A["rel_bias"]
    for li in layers:
        j = li // 3
        if do_mixer:
            if li % 3 == 0:
                im["a_w_in_%d" % j] = A["a_w_in"][j]
                im["a_w_out_%d" % j] = A["a_w_out"][j]
            elif li % 3 == 1:
                for n in ("b_w_in", "b_ln_g", "b_ln_b", "b_w_s", "b_b_s", "b_w_out"):
                    im["%s_%d" % (n, j)] = A[n][j]
            else:
                for n in ("c_w_in", "c_w_gk_up", "c_b_gk", "c_norm_g", "c_w_out"):
                    im["%s_%d" % (n, j)] = A[n][j]
        im["moe_w_router_%d" % li] = A["moe_w_router"][li]
        im["moe_b_router_%d" % li] = A["moe_b_router"][li]
        if do_moe:
            for n in ("moe_w_gate_up", "moe_b_gate_up", "moe_w_down", "moe_b_down"):
                im["%s_%d" % (n, li)] = A[n][li]
    return im


def kernel(**inputs):
    A = {k: np.asarray(v) for k, v in inputs.items()}
    nc = build_program("full")
    consts = host_constants(A["rel_bias"])
    in_maps = [make_in_map(A, A["x"][c], consts) for c in range(8)]
    res = run_bass_kernel_spmd(nc, in_maps, core_ids=list(range(8)))
    out = np.stack([np.asarray(res.results[c]["out"]) for c in range(8)], axis=0)
    return out.astype(np.float32)
```

```python
import math
from contextlib import ExitStack
import numpy as np
import concourse.bass as bass
import concourse.mybir as mybir
from concourse.bass_utils import run_bass_kernel_spmd

F32 = mybir.dt.float32
BF16 = mybir.dt.bfloat16
ALU = mybir.AluOpType
AF = mybir.ActivationFunctionType
AX = mybir.AxisListType

ENGS = ("pe", "act", "dve", "pool", "sp")

D = 1024
S = 2048
NT = 16
DEPTH = 4
ALPHA = (2.0 * DEPTH) ** 0.25
EPS = 1e-5
NE = 32
NEG = -30000.0


class Buf:
    __slots__ = ("name", "lw", "rd")

    def __init__(self, name="b"):
        self.name = name
        self.lw = None
        self.rd = []


class Op:
    __slots__ = ("eng", "fn", "dma", "deps", "sem", "val", "needs_sig", "epoch")

    def __init__(self, eng, fn, dma, epoch=0):
        self.epoch = epoch
        self.eng = eng
        self.fn = fn
        self.dma = dma
        self.deps = []
        self.sem = None
        self.val = 0
        self.needs_sig = False


class Prog:
    def __init__(self, nc, n_dma_sems=(("sp", 24), ("pool", 24), ("act", 8))):
        self.nc = nc
        self.pending = {e: [] for e in ENGS}
        self.esem = {}
        self.ecount = {e: 0 for e in ENGS}
        self._ctx = []
        for e in ENGS:
            cm = nc.semaphore("s_" + e)
            self.esem[e] = cm.__enter__()
            self._ctx.append(cm)
        self.dsems = {}
        self.dcount = {}
        self.dlast = {}
        self.dnext = {}
        for q, n in n_dma_sems:
            lst = []
            for i in range(n):
                cm = nc.semaphore("d_%s_%d" % (q, i))
                lst.append(cm.__enter__())
                self._ctx.append(cm)
            self.dsems[q] = lst
            self.dcount[q] = [0] * n
            self.dlast[q] = [None] * n
            self.dnext[q] = 0
        self.known = {e: {} for e in ENGS}
        self.all_dma_since_barrier = []
        self.n_ops = 0
        self.epoch = 0

    def close(self):
        for cm in reversed(self._ctx):
            cm.__exit__(None, None, None)

    def add(self, eng, fn, R=(), W=(), dma=False):
        op = Op(eng, fn, dma, self.epoch)
        self.n_ops += 1
        deps = []
        rset = set(id(b) for b in R)
        for b in R:
            if b.lw is not None:
                deps.append((b.lw, True))
        for b in W:
            if b.lw is not None:
                deps.append((b.lw, id(b) in rset))
            for r in b.rd:
                deps.append((r, False))
        seen = set()
        for d, raw in deps:
            if d is op or d.epoch < self.epoch:
                continue
            key = id(d)
            if d.dma or dma:
                pass
            elif d.eng == eng:
                if eng == "pe" or not raw:
                    continue
            if key in seen:
                continue
            seen.add(key)
            op.deps.append(d)
            d.needs_sig = True
        for b in R:
            b.rd.append(op)
        for b in W:
            b.lw = op
            b.rd = []
        if dma:
            q = eng
            k = self.dnext[q]
            self.dnext[q] = (k + 1) % len(self.dsems[q])
            prev = self.dlast[q][k]
            if prev is not None and prev.epoch == self.epoch:
                op.deps.append(prev)
            self.dcount[q][k] += 16
            op.sem = self.dsems[q][k]
            op.val = self.dcount[q][k]
            self.dlast[q][k] = op
            op.needs_sig = True
            self.all_dma_since_barrier.append(op)
        self.pending[eng].append(op)
        return op

    def barrier(self):
        lasts = []
        for e in ENGS:
            for o in reversed(self.pending[e]):
                if not o.dma and o.fn is not None:
                    lasts.append(o)
                    break
        dmas = list(self.all_dma_since_barrier)
        self.all_dma_since_barrier = []
        for e in ENGS:
            op = Op(e, None, False, self.epoch)
            for d in lasts:
                if d.eng != e:
                    op.deps.append(d)
                    d.needs_sig = True
            for d in dmas:
                op.deps.append(d)
            self.pending[e].append(op)
        self.epoch += 1

    def flush(self):
        nc = self.nc
        self.barrier()
        for e in ENGS:
            for op in self.pending[e]:
                if op.dma or op.fn is None:
                    continue
                if op.needs_sig:
                    self.ecount[e] += 1
                    op.sem = self.esem[e]
                    op.val = self.ecount[e]
        pend = self.pending
        self.pending = {e: [] for e in ENGS}
        known = self.known

        def emit(e, eng):
            kn = known[e]
            for op in pend[e]:
                need = {}
                for d in op.deps:
                    assert d.sem is not None, "dep without signal"
                    sid = id(d.sem)
                    if kn.get(sid, 0) >= d.val:
                        continue
                    if sid not in need or need[sid][1] < d.val:
                        need[sid] = (d.sem, d.val)
                for sid, (sem, val) in need.items():
                    eng.wait_ge(sem, val)
                    kn[sid] = val
                if op.fn is None:
                    continue
                ins = op.fn(eng)
                if op.dma:
                    ins.then_inc(op.sem, 16)
                elif op.needs_sig:
                    ins.then_inc(op.sem, 1)

        with nc.Block() as blk:
            @blk.tensor
            def _(eng):
                emit("pe", eng)

            @blk.scalar
            def _(eng):
                emit("act", eng)

            @blk.vector
            def _(eng):
                emit("dve", eng)

            @blk.gpsimd
            def _(eng):
                emit("pool", eng)

            @blk.sync
            def _(eng):
                emit("sp", eng)


class Ctx:
    pass


def bcast_rows(ap_row, nparts=128):
    return ap_row.partition_broadcast(nparts)


def ln_tile(C, t, lng, lnb, blnp, router=None):
    P, nc = C.P, C.nc
    h, hT = C.h, C.hT
    bh, bhT = C.bh[t], C.bhT[t]
    st, mv, rs, nb = C.ln_st, C.ln_mv, C.ln_rs, C.ln_nb
    b_st = C.b_lnst
    for c in range(2):
        P.add("dve", lambda e, c=c: e.bn_stats(out=st[:, c, :], in_=h[:, t, c * 512:(c + 1) * 512]), R=[bh], W=[b_st])
    P.add("dve", lambda e: e.bn_aggr(out=mv[:], in_=st[:]), R=[b_st], W=[b_st])
    P.add("act", lambda e: e.activation(out=rs[:], in_=mv[:, 1:2], func=AF.Sqrt, bias=EPS, scale=1.0), R=[b_st], W=[b_st])
    P.add("dve", lambda e: e.reciprocal(out=rs[:], in_=rs[:]), R=[b_st], W=[b_st])
    P.add("dve", lambda e: e.tensor_scalar(out=h[:, t, :], in0=h[:, t, :], scalar1=mv[:, 0:1], scalar2=rs[:, 0:1],
                                           op0=ALU.subtract, op1=ALU.mult), R=[bh, b_st], W=[bh])
    P.add("pool", lambda e: e.tensor_tensor(out=h[:, t, :], in0=h[:, t, :], in1=lng[:], op=ALU.mult), R=[bh, blnp], W=[bh])
    P.add("pool", lambda e: e.tensor_tensor(out=h[:, t, :], in0=h[:, t, :], in1=lnb[:], op=ALU.add), R=[bh, blnp], W=[bh])
    transpose_tile(C, t, router)


def transpose_tile(C, t, router=None):
    P = C.P
    h, hT = C.h, C.hT
    bh, bhT = C.bh[t], C.bhT[t]
    for half in range(2):
        ps, bps = C.ps[C.tp_rr % 2], C.bps[C.tp_rr % 2]
        C.tp_rr += 1
        for kk in range(4):
            k = half * 4 + kk
            P.add("pe", lambda e, k=k, kk=kk, ps=ps: e.transpose(out=ps[:, kk * 128:(kk + 1) * 128], in_=h[:, t, k * 128:(k + 1) * 128],
                                                                  identity=C.ident[:]), R=[bh, C.b_const], W=[bps])
        P.add("act", lambda e, ps=ps, half=half: e.copy(out=hT[:, half * 4:half * 4 + 4, t * 128:(t + 1) * 128],
                                                        in_=ps[:].rearrange("p (k c) -> p k c", k=4)), R=[bps], W=[bhT])
        if router is not None:
            P.add("act", lambda e, ps=ps, half=half: e.copy(out=C.hT32[:, half * 4:half * 4 + 4, :],
                                                            in_=ps[:].rearrange("p (k c) -> p k c", k=4)), R=[bps], W=[C.b_hT32])
    if router is not None:
        router_tile(C, t, router)


def router_tile(C, t, R_):
    P = C.P
    ps, bps = C.ps[2], C.bps[2]
    wr, brb, b_r = R_["wr"], R_["brb"], R_["b"]
    lg, m8, ex, msk, sm = C.r_lg, C.r_m8, C.r_ex, C.r_msk, C.r_sm
    b = C.b_rt
    for k in range(8):
        P.add("pe", lambda e, k=k: e.matmul(ps[:, 0:NE], lhsT=C.hT32[:, k, :], rhs=wr[:, k, :], start=(k == 0), stop=(k == 7)),
              R=[C.b_hT32, b_r], W=[bps])
    P.add("dve", lambda e: e.tensor_tensor(out=lg[:], in0=ps[:, 0:NE], in1=brb[:], op=ALU.add), R=[bps, b_r], W=[b])
    P.add("dve", lambda e: e.max(out=m8[:], in_=lg[:]), R=[b], W=[b])
    P.add("dve", lambda e: e.tensor_scalar(out=msk[:], in0=lg[:], scalar1=m8[:, 3:4], scalar2=None, op0=ALU.is_ge), R=[b], W=[b])
    P.add("dve", lambda e: e.tensor_scalar(out=sm[:, 0:1], in0=m8[:, 0:1], scalar1=-1.0, scalar2=None, op0=ALU.mult), R=[b], W=[b])
    P.add("act", lambda e: e.activation(out=ex[:], in_=lg[:], func=AF.Exp, bias=sm[:, 0:1], scale=1.0), R=[b], W=[b])
    P.add("dve", lambda e: e.tensor_tensor(out=ex[:], in0=ex[:], in1=msk[:], op=ALU.mult), R=[b], W=[b])
    P.add("dve", lambda e: e.reduce_sum(out=sm[:, 1:2], in_=ex[:], axis=AX.X), R=[b], W=[b])
    P.add("dve", lambda e: e.reciprocal(out=sm[:, 1:2], in_=sm[:, 1:2]), R=[b], W=[b])
    P.add("dve", lambda e: e.tensor_scalar(out=C.gates[:, t, :], in0=ex[:], scalar1=sm[:, 1:2], scalar2=None, op0=ALU.mult),
          R=[b], W=[C.b_gates[t]])


def load_ln_params(C, lng, lnb, blnp, g_row, b_row):
    P = C.P
    P.add("sp", lambda e: e.dma_start(out=lng[:], in_=bcast_rows(g_row)), W=[blnp], dma=True)
    P.add("sp", lambda e: e.dma_start(out=lnb[:], in_=bcast_rows(b_row)), W=[blnp], dma=True)


def moe_phase(C, li, T):
    P, nc = C.P, C.nc
    h, hT = C.h, C.hT
    hb = hT[:].rearrange("p k s -> p (k s)").rearrange("p (t d) -> p t d", t=NT)
    BANKS = (0, 1, 7, 2)
    with ExitStack() as es:
        w1g = es.enter_context(nc.sbuf_tensor("m_w1g_%d" % li, [128, 2, 8, 512], BF16))
        w1u = es.enter_context(nc.sbuf_tensor("m_w1u_%d" % li, [128, 2, 8, 512], BF16))
        w2 = es.enter_context(nc.sbuf_tensor("m_w2_%d" % li, [128, 2, 4, 1024], BF16))
        actT = es.enter_context(nc.sbuf_tensor("m_actT_%d" % li, [128, 2, 4, 512], BF16))
        g1 = es.enter_context(nc.sbuf_tensor("m_g1_%d" % li, [128, 2, 512], F32))
        sg = es.enter_context(nc.sbuf_tensor("m_sg_%d" % li, [128, 2, 512], F32))
        u1 = es.enter_context(nc.sbuf_tensor("m_u1_%d" % li, [128, 2, 512], F32))
        braw = es.enter_context(nc.sbuf_tensor("m_braw_%d" % li, [128, 4, 128], F32))
        bguT = es.enter_context(nc.sbuf_tensor("m_bguT_%d" % li, [128, 512], F32))
        bd = es.enter_context(nc.sbuf_tensor("m_bd_%d" % li, [NE, D], F32))
        gT = es.enter_context(nc.sbuf_tensor("m_gT_%d" % li, [NE, 128], F32))
        xgT = es.enter_context(nc.sbuf_tensor("m_xgT_%d" % li, [128, 8, 512], BF16))
        Pm = es.enter_context(nc.sbuf_tensor("m_P_%d" % li, [128, NT, 128], BF16))
        PTm = es.enter_context(nc.sbuf_tensor("m_PT_%d" % li, [128, 4, 512], BF16))
        ys = es.enter_context(nc.sbuf_tensor("m_ys_%d" % li, [128, 2, 1024], BF16))
        mask = es.enter_context(nc.sbuf_tensor("m_mask_%d" % li, [128, NT, NE], BF16))
        posm = es.enter_context(nc.sbuf_tensor("m_pos_%d" % li, [128, NT, NE], F32))
        iota = es.enter_context(nc.sbuf_tensor("m_iota_%d" % li, [128, 128], F32))
        onesb = es.enter_context(nc.sbuf_tensor("m_ones_%d" % li, [128, 128], BF16))
        tris = es.enter_context(nc.sbuf_tensor("m_tris_%d" % li, [128, 128], BF16))
        b_w1 = [Buf("w1_%d" % i) for i in range(2)]
        b_w2 = [Buf("w2_%d" % i) for i in range(2)]
        b_act = [Buf() for _ in range(2)]
        b_g1 = [Buf() for _ in range(2)]
        b_sg = [Buf() for _ in range(2)]
        b_u1 = [Buf() for _ in range(2)]
        b_ys = [Buf() for _ in range(2)]
        b_misc, b_gT, b_cst, b_mask, b_pos, b_xg, b_P, b_PT = Buf(), Buf(), Buf(), Buf(), Buf(), Buf(), Buf(), Buf()
        allhT = list(C.bhT)
        P.add("sp", lambda e: e.dma_start(out=iota[:], in_=T["iota"]), W=[b_cst], dma=True)
        P.add("sp", lambda e: e.dma_start(out=onesb[:], in_=T["ones_b"]), W=[b_cst], dma=True)
        P.add("sp", lambda e: e.dma_start(out=tris[:], in_=T["tris_b"]), W=[b_cst], dma=True)
        P.add("sp", lambda e: e.dma_start(out=braw[:], in_=T["moe_b_gate_up_%d" % li].rearrange("e (c p) -> (e c) p", p=128)
                                          .rearrange("(b r) p -> r b p", r=128)), W=[b_misc], dma=True)
        P.add("sp", lambda e: e.dma_start(out=bd[:], in_=T["moe_b_down_%d" % li]), W=[b_misc], dma=True)
        for bb in range(4):
            ps, bps = C.ps[bb % 2], C.bps[bb % 2]
            P.add("pe", lambda e, bb=bb, ps=ps: e.transpose(out=ps[:, 0:128], in_=braw[:, bb, :], identity=C.ident[:]),
                  R=[b_misc, C.b_const], W=[bps])
            P.add("act", lambda e, bb=bb, ps=ps: e.copy(out=bguT[:, bb * 128:(bb + 1) * 128], in_=ps[:, 0:128]), R=[bps], W=[b_misc])
        bg3 = bguT[:].rearrange("p (e c) -> p e c", c=16)
        P.add("dve", lambda e: e.tensor_scalar(out=bg3[:, :, 8:16], in0=bg3[:, :, 8:16], scalar1=1.0, scalar2=None, op0=ALU.add), R=[b_misc], W=[b_misc])
        for t in range(NT):
            P.add("act", lambda e, t=t: e.copy(out=hb[:, t, :], in_=h[:, t, :]), R=[C.bh[t]], W=allhT)
        for t in range(NT):
            ps, bps = C.ps[2], C.bps[2]
            P.add("pe", lambda e, t=t: e.transpose(out=ps[0:NE, 0:128], in_=C.gates[:, t, :], identity=C.ident[:]),
                  R=[C.b_gates[t], C.b_const], W=[bps])
            P.add("act", lambda e: e.copy(out=gT[:], in_=ps[0:NE, 0:128]), R=[bps], W=[b_gT])
            for n in range(2):
                po, bpo = C.ps[n], C.bps[n]
                P.add("pe", lambda e, n=n, po=po: e.matmul(po[:], lhsT=gT[:], rhs=bd[:, n * 512:(n + 1) * 512], start=True, stop=True),
                      R=[b_gT, b_misc], W=[bpo])
                P.add("dve", lambda e, n=n, po=po, t=t: e.scalar_tensor_tensor(out=h[:, t, n * 512:(n + 1) * 512], in0=h[:, t, n * 512:(n + 1) * 512],
                                                                               scalar=ALPHA, in1=po[:], op0=ALU.mult, op1=ALU.add),
                      R=[bpo, C.bh[t]], W=[C.bh[t]])
        P.add("dve", lambda e: e.tensor_scalar(out=mask[:], in0=C.gates[:], scalar1=0.0, scalar2=None, op0=ALU.is_gt), R=list(C.b_gates), W=[b_mask])
        pp, bpp = C.ps[2], C.bps[2]
        for t in range(NT):
            gi, i = divmod(t, 4)
            for i2 in range(i):
                P.add("pe", lambda e, t=t, gi=gi, i2=i2: e.matmul(pp[:, t * NE:(t + 1) * NE], lhsT=onesb[:], rhs=mask[:, 4 * gi + i2, :],
                                                                start=(i2 == 0), stop=False), R=[b_mask, b_cst], W=[bpp])
            P.add("pe", lambda e, t=t, i=i: e.matmul(pp[:, t * NE:(t + 1) * NE], lhsT=tris[:], rhs=mask[:, t, :], start=(i == 0), stop=True),
                  R=[b_mask, b_cst], W=[bpp])
        P.add("dve", lambda e: e.scalar_tensor_tensor(out=posm[:], in0=pp[:].rearrange("p (t x) -> p t x", t=NT), scalar=1.0, in1=mask[:],
                                                      op0=ALU.add, op1=ALU.mult), R=[bpp, b_mask], W=[b_pos])
        P.add("dve", lambda e: e.tensor_scalar(out=posm[:], in0=posm[:], scalar1=-1.0, scalar2=None, op0=ALU.add), R=[b_pos], W=[b_pos])

        wgu = T["moe_w_gate_up_%d" % li]
        wd = T["moe_w_down_%d" % li]

        def bank():
            y = C.y_rr % 4
            C.y_rr += 1
            return C.ps[BANKS[y]], C.bps[BANKS[y]]

        def p_build(ex):
            for t in range(NT):
                P.add("dve", lambda e, t=t: e.tensor_scalar(out=Pm[:, t, :], in0=iota[:], scalar1=posm[:, t, ex:ex + 1], scalar2=None, op0=ALU.is_equal),
                      R=[b_pos, b_cst], W=[b_P])

        def gather_unit(ex):
            for gi in range(4):
                for kh in range(2):
                    px, bpx = bank()
                    for kk in range(4):
                        k = kh * 4 + kk
                        for i in range(4):
                            t = 4 * gi + i
                            P.add("pe", lambda e, kk=kk, k=k, t=t, i=i, px=px: e.matmul(px[:, kk * 128:(kk + 1) * 128], lhsT=hb[:, t, k * 128:(k + 1) * 128],
                                                                                      rhs=Pm[:, t, :], start=(i == 0), stop=(i == 3)), R=[b_P] + allhT, W=[bpx])
                    P.add("act", lambda e, kh=kh, gi=gi, px=px: e.copy(out=xgT[:, kh * 4:kh * 4 + 4, gi * 128:(gi + 1) * 128],
                                                                      in_=px[:].rearrange("p (a c) -> p a c", a=4)), R=[bpx], W=[b_xg])

        def pt_unit(ex):
            for gi in range(4):
                ptp, bptp = bank()
                ptb = ptp[:].bitcast(BF16)
                for i in range(4):
                    P.add("pe", lambda e, i=i, gi=gi, ptb=ptb: e.transpose(out=ptb[:, i * 128:(i + 1) * 128], in_=Pm[:, 4 * gi + i, :], identity=C.identb[:]),
                          R=[b_P, C.b_const], W=[bptp])
                P.add("act", lambda e, gi=gi, ptb=ptb: e.copy(out=PTm[:, gi, :], in_=ptb[:, 0:512]), R=[bptp], W=[b_PT])

        def w1_unit(ex, hf, s, a):
            bw = b_w1[s]
            for j in range(4):
                q = C.q_rr % 2
                C.q_rr += 1
                pg, bpg = C.ps[3 + q], C.bps[3 + q]
                pu, bpu = C.ps[5 + q], C.bps[5 + q]
                for k in range(8):
                    P.add("pe", lambda e, k=k, j=j, pg=pg: e.matmul(pg[:], lhsT=w1g[:, s, k, j * 128:(j + 1) * 128], rhs=xgT[:, k, :],
                                                                  start=(k == 0), stop=(k == 7)), R=[bw, b_xg], W=[bpg])
                for k in range(8):
                    P.add("pe", lambda e, k=k, j=j, pu=pu: e.matmul(pu[:], lhsT=w1u[:, s, k, j * 128:(j + 1) * 128], rhs=xgT[:, k, :],
                                                                  start=(k == 0), stop=(k == 7)), R=[bw, b_xg], W=[bpu])
                cg = ex * 16 + hf * 4 + j
                cu = ex * 16 + 8 + hf * 4 + j
                P.add("dve", lambda e, q=q, pg=pg, cg=cg: e.tensor_scalar(out=g1[:, q, :], in0=pg[:], scalar1=bguT[:, cg:cg + 1], scalar2=7.0,
                                                                         op0=ALU.add, op1=ALU.min), R=[bpg, b_misc], W=[b_g1[q]])
                P.add("act", lambda e, q=q: e.activation(out=sg[:, q, :], in_=g1[:, q, :], func=AF.Sigmoid, scale=1.702), R=[b_g1[q]], W=[b_sg[q]])
                P.add("dve", lambda e, q=q, pu=pu, cu=cu: e.tensor_scalar(out=u1[:, q, :], in0=pu[:], scalar1=bguT[:, cu:cu + 1], scalar2=8.0,
                                                                         op0=ALU.add, op1=ALU.min), R=[bpu, b_misc], W=[b_u1[q]])
                P.add("dve", lambda e, q=q: e.tensor_tensor(out=sg[:, q, :], in0=sg[:, q, :], in1=g1[:, q, :], op=ALU.mult),
                      R=[b_sg[q], b_g1[q]], W=[b_sg[q]])
                P.add("dve", lambda e, q=q, j=j: e.scalar_tensor_tensor(out=actT[:, a, j, :], in0=u1[:, q, :], scalar=-6.0, in1=sg[:, q, :],
                                                                       op0=ALU.max, op1=ALU.mult), R=[b_sg[q], b_u1[q]], W=[b_act[a]])

        def w2_unit(ex, hf, s, a):
            bw = b_w2[s]
            ybs = {}

            def down(tt):
                yb = C.ys_rr % 2
                C.ys_rr += 1
                ybs[tt] = yb
                for n in range(2):
                    py, bpy = bank()
                    for j in range(4):
                        P.add("pe", lambda e, j=j, n=n, py=py: e.matmul(py[:], lhsT=actT[:, a, j, tt * 128:(tt + 1) * 128],
                                                                      rhs=w2[:, s, j, n * 512:(n + 1) * 512], start=(j == 0), stop=(j == 3)),
                              R=[b_act[a], bw], W=[bpy])
                    P.add("act", lambda e, n=n, yb=yb, py=py: e.copy(out=ys[:, yb, n * 512:(n + 1) * 512], in_=py[:]), R=[bpy], W=[b_ys[yb]])

            def scatter(tt):
                yb = ybs[tt]
                for i in range(4):
                    t = 4 * tt + i
                    for n in range(2):
                        pz, bpz = bank()
                        P.add("pe", lambda e, i=i, n=n, pz=pz: e.matmul(pz[:], lhsT=PTm[:, tt, i * 128:(i + 1) * 128],
                                                                      rhs=ys[:, yb, n * 512:(n + 1) * 512], start=True, stop=True),
                              R=[b_PT, b_ys[yb]], W=[bpz])
                        P.add("dve", lambda e, t=t, n=n, pz=pz: e.scalar_tensor_tensor(
                            out=h[:, t, n * 512:(n + 1) * 512], in0=pz[:], scalar=C.gates[:, t, ex:ex + 1], in1=h[:, t, n * 512:(n + 1) * 512],
                            op0=ALU.mult, op1=ALU.add), R=[bpz, C.b_gates[t], C.bh[t]], W=[C.bh[t]])

            down(0)
            down(1)
            scatter(0)
            down(2)
            scatter(1)
            down(3)
            scatter(2)
            scatter(3)

        def dma_w1(ex, hf, s):
            P.add("pool", lambda e: e.dma_start(
                out=w1g[:, s], in_=wgu[ex, :, hf * 512:(hf + 1) * 512].rearrange("(k p) n -> p k n", p=128)), W=[b_w1[s]], dma=True)
            P.add("pool", lambda e: e.dma_start(
                out=w1u[:, s], in_=wgu[ex, :, 1024 + hf * 512:1024 + (hf + 1) * 512].rearrange("(k p) n -> p k n", p=128)), W=[b_w1[s]], dma=True)

        def dma_w2(ex, hf, s):
            P.add("pool", lambda e: e.dma_start(
                out=w2[:, s], in_=wd[ex, hf * 512:(hf + 1) * 512, :].rearrange("(j p) n -> p j n", p=128)), W=[b_w2[s]], dma=True)

        steps = [(ex, hf) for ex in range(C.n_experts) for hf in range(2)]
        prev = None
        if steps:
            dma_w1(steps[0][0], steps[0][1], 0)
            p_build(0)
            gather_unit(0)
        for it, (ex, hf) in enumerate(steps):
            s = it % 2
            dma_w2(ex, hf, s)
            if it + 1 < len(steps):
                dma_w1(steps[it + 1][0], steps[it + 1][1], 1 - s)
            a = C.act_rr % 2
            C.act_rr += 1
            if hf == 1 and ex + 1 < C.n_experts:
                p_build(ex + 1)
            w1_unit(ex, hf, s, a)
            if hf == 1 and ex + 1 < C.n_experts:
                gather_unit(ex + 1)
            if prev is not None:
                w2_unit(*prev)
            if hf == 0:
                pt_unit(ex)
            prev = (ex, hf, s, a)
        if prev is not None:
            w2_unit(*prev)
        P.flush()
    with ExitStack() as es:
        lng = es.enter_context(nc.sbuf_tensor("m_lng_%d" % li, [128, D], F32))
        lnb = es.enter_context(nc.sbuf_tensor("m_lnb_%d" % li, [128, D], F32))
        blnp = Buf()
        load_ln_params(C, lng, lnb, blnp, T["ln_g"][li, 1, :], T["ln_b"][li, 1, :])
        for t in range(NT):
            ln_tile(C, t, lng, lnb, blnp, router=None)
        P.flush()


def out_proj_ln(C, li, T, srcT, nchunks, w_dram, R_):
    P, nc = C.P, C.nc
    h = C.h
    with ExitStack() as es:
        wo = es.enter_context(nc.sbuf_tensor("o_w_%d" % li, [128, nchunks, D], BF16))
        lng = es.enter_context(nc.sbuf_tensor("o_lng_%d" % li, [128, D], F32))
        lnb = es.enter_context(nc.sbuf_tensor("o_lnb_%d" % li, [128, D], F32))
        bw, blnp = Buf(), Buf()
        P.add("pool", lambda e: e.dma_start(out=wo[:], in_=w_dram.rearrange("(k p) n -> p k n", p=128)), W=[bw], dma=True)
        load_ln_params(C, lng, lnb, blnp, T["ln_g"][li, 0, :], T["ln_b"][li, 0, :])
        for t in range(NT):
            for n in range(2):
                po, bpo = C.ps[3 + n + 2 * (t % 2)], C.bps[3 + n + 2 * (t % 2)]
                for f in range(nchunks):
                    P.add("pe", lambda e, f=f, n=n, t=t, po=po: e.matmul(po[:], lhsT=srcT[:, f, t * 128:(t + 1) * 128], rhs=wo[:, f, n * 512:(n + 1) * 512],
                                                                      start=(f == 0), stop=(f == nchunks - 1)), R=[C.b_srcT, bw], W=[bpo])
                P.add("dve", lambda e, n=n, t=t, po=po: e.scalar_tensor_tensor(out=h[:, t, n * 512:(n + 1) * 512], in0=h[:, t, n * 512:(n + 1) * 512],
                                                                             scalar=ALPHA, in1=po[:], op0=ALU.mult, op1=ALU.add),
                      R=[bpo, C.bh[t]], W=[C.bh[t]])
            ln_tile(C, t, lng, lnb, blnp, router=R_)
        P.flush()


def moba_phase(C, li, T, R_):
    P, nc = C.P, C.nc
    h, hT = C.h, C.hT
    j = li // 3
    w_in = T["a_w_in_%d" % j]
    H = 8
    SCALE = 128 ** -0.5
    BIG = 30000.0
    with nc.sbuf_tensor("a_aT_%d" % li, [128, 8, S], BF16) as aT:
        C.b_srcT = Buf("aT")
        with ExitStack() as es:
            wq = es.enter_context(nc.sbuf_tensor("a_wq_%d" % li, [128, 2, 8, 384], BF16))
            qT = es.enter_context(nc.sbuf_tensor("a_qT_%d" % li, [128, 2, S], BF16))
            kT = es.enter_context(nc.sbuf_tensor("a_kT_%d" % li, [128, 2, S], BF16))
            V = es.enter_context(nc.sbuf_tensor("a_V_%d" % li, [128, 2, NT, 128], BF16))
            kms = es.enter_context(nc.sbuf_tensor("a_kms_%d" % li, [128, 8], F32))
            kmb = es.enter_context(nc.sbuf_tensor("a_kmb_%d" % li, [128, 8], BF16))
            g2 = es.enter_context(nc.sbuf_tensor("a_g2_%d" % li, [128, 128], F32))
            m8 = es.enter_context(nc.sbuf_tensor("a_m8_%d" % li, [128, 8], F32))
            bm = es.enter_context(nc.sbuf_tensor("a_bm_%d" % li, [128, 128], F32))
            cm = es.enter_context(nc.sbuf_tensor("a_cm_%d" % li, [128, 128], F32))
            vm = es.enter_context(nc.sbuf_tensor("a_vm_%d" % li, [128, 128], F32))
            vm2 = es.enter_context(nc.sbuf_tensor("a_vm2_%d" % li, [128, 128], F32))
            rel = es.enter_context(nc.sbuf_tensor("a_rel_%d" % li, [128, 8, 2, 128], F32))
            caus = es.enter_context(nc.sbuf_tensor("a_caus_%d" % li, [128, 128], F32))
            cb = es.enter_context(nc.sbuf_tensor("a_cb_%d" % li, [128, 8], F32))
            sc = es.enter_context(nc.sbuf_tensor("a_s_%d" % li, [128, 2, S], F32))
            pb = es.enter_context(nc.sbuf_tensor("a_p_%d" % li, [128, S], BF16))
            pT = es.enter_context(nc.sbuf_tensor("a_pT_%d" % li, [128, 2, 4, 128], BF16))
            stt = es.enter_context(nc.sbuf_tensor("a_st_%d" % li, [128, 2, 4], F32))
            b_wq = [Buf() for _ in range(2)]
            b_q = [Buf() for _ in range(2)]
            b_k = [Buf() for _ in range(2)]
            b_v = [Buf() for _ in range(2)]
            b_km, b_gate, b_cst = Buf(), Buf(), Buf()
            b_s = [Buf() for _ in range(2)]
            b_p = [Buf()] * 2
            b_pT = [Buf() for _ in range(2)]
            b_st = [Buf() for _ in range(2)]
            SK = ""
            if "rel" not in SK:
                P.add("sp", lambda e: e.dma_start(out=rel[:], in_=T["relT"].rearrange("h d i j -> i h d j")), W=[b_cst], dma=True)
            P.add("sp", lambda e: e.dma_start(out=caus[:], in_=T["causal"]), W=[b_cst], dma=True)
            P.add("sp", lambda e: e.dma_start(out=vm[:], in_=T["vmask"]), W=[b_cst], dma=True)
            P.add("sp", lambda e: e.dma_start(out=vm2[:], in_=T["vmask2"]), W=[b_cst], dma=True)
            if "cb" not in SK:
                P.add("sp", lambda e: e.dma_start(out=cb[:], in_=bcast_rows(T["rel_bias"][31, :])), W=[b_cst], dma=True)
            for hh in range(H):
                P.add("pool", lambda e, hh=hh: e.tensor_tensor(out=rel[:, hh, 0, :], in0=rel[:, hh, 0, :], in1=caus[:], op=ALU.add), R=[b_cst], W=[b_cst])
            allT = list(C.bhT)
            for hh in range(H):
                s = hh % 2
                for c3 in range(3):
                    P.add("pool", lambda e, s=s, c3=c3, hh=hh: e.dma_start(
                        out=wq[:, s, :, c3 * 128:(c3 + 1) * 128],
                        in_=w_in[:, c3 * 1024 + hh * 128:c3 * 1024 + (hh + 1) * 128].rearrange("(k p) n -> p k n", p=128)), W=[b_wq[s]], dma=True)
                for g in range(4):
                    pq, bpq = C.ps[3], C.bps[3]
                    pk, bpk = C.ps[4], C.bps[4]
                    for k in range(8):
                        P.add("pe", lambda e, k=k, g=g, s=s: e.matmul(pq[:], lhsT=wq[:, s, k, 0:128], rhs=hT[:, k, g * 512:(g + 1) * 512],
                                                                    start=(k == 0), stop=(k == 7)), R=[b_wq[s]] + allT[g * 4:g * 4 + 4], W=[bpq])
                    P.add("act", lambda e, g=g, s=s: e.copy(out=qT[:, s, g * 512:(g + 1) * 512], in_=pq[:]), R=[bpq], W=[b_q[s]])
                    for k in range(8):
                        P.add("pe", lambda e, k=k, g=g, s=s: e.matmul(pk[:], lhsT=wq[:, s, k, 128:256], rhs=hT[:, k, g * 512:(g + 1) * 512],
                                                                    start=(k == 0), stop=(k == 7)), R=[b_wq[s]] + allT[g * 4:g * 4 + 4], W=[bpk])
                    for b2 in range(2):
                        P.add("act", lambda e, g=g, s=s, b2=b2: e.activation(out=kT[:, s, g * 512 + b2 * 256:g * 512 + (b2 + 1) * 256],
                                                                           in_=pk[:, b2 * 256:(b2 + 1) * 256], func=AF.Copy,
                                                                           accum_out=kms[:, 2 * g + b2:2 * g + b2 + 1]), R=[bpk], W=[b_k[s], b_km])
                P.add("dve", lambda e: e.tensor_scalar(out=kmb[:], in0=kms[:], scalar1=1.0 / 256.0, scalar2=None, op0=ALU.mult), R=[b_km], W=[b_km])
                for g in range(4):
                    pv, bpv = C.ps[5], C.bps[5]
                    for tt in range(4):
                        t = g * 4 + tt
                        for k in range(8):
                            P.add("pe", lambda e, k=k, t=t, tt=tt, s=s: e.matmul(pv[:, tt * 128:(tt + 1) * 128], lhsT=hT[:, k, t * 128:(t + 1) * 128],
                                                                               rhs=wq[:, s, k, 256:384], start=(k == 0), stop=(k == 7)),
                                  R=[b_wq[s], C.bhT[t]], W=[bpv])
                    P.add("act", lambda e, g=g, s=s: e.copy(out=V[:, s, g * 4:g * 4 + 4, :], in_=pv[:].rearrange("p (a c) -> p a c", a=4)),
                          R=[bpv], W=[b_v[s]])
                MD = 9
                if MD < 2:
                    continue
                pg, bpg = C.ps[6], C.bps[6]
                for qt in range(NT):
                    P.add("pe", lambda e, qt=qt, s=s: e.matmul(pg[:, qt * 8:(qt + 1) * 8], lhsT=qT[:, s, qt * 128:(qt + 1) * 128], rhs=kmb[:],
                                                             start=True, stop=True), R=[b_q[s], b_km], W=[bpg])
                P.add("dve", lambda e: e.tensor_tensor(out=g2[:], in0=pg[:, 0:128], in1=vm[:], op=ALU.add), R=[bpg, b_cst], W=[b_gate])
                for qt in range(NT):
                    P.add("dve", lambda e, qt=qt: e.max(out=m8[:], in_=g2[:, qt * 8:(qt + 1) * 8]), R=[b_gate], W=[b_gate])
                    P.add("dve", lambda e, qt=qt: e.tensor_scalar(out=bm[:, qt * 8:(qt + 1) * 8], in0=g2[:, qt * 8:(qt + 1) * 8], scalar1=m8[:, 2:3],
                                                                  scalar2=BIG, op0=ALU.is_ge, op1=ALU.mult), R=[b_gate], W=[b_gate])
                P.add("dve", lambda e: e.tensor_tensor(out=bm[:], in0=bm[:], in1=vm2[:], op=ALU.add), R=[b_gate, b_cst], W=[b_gate])
                P.add("dve", lambda e, hh=hh: e.tensor_scalar(out=cm[:], in0=bm[:], scalar1=cb[:, hh:hh + 1], scalar2=None, op0=ALU.add),
                      R=[b_gate, b_cst], W=[b_gate])
                for qt in range(NT if MD >= 3 else 0):
                    qb = qt // 2
                    nk = qt + 1
                    z = qt % 2
                    for c in range((nk + 3) // 4):
                        w = min(4, nk - c * 4)
                        pss, bpss = C.ps[3 + (C.q_rr % 2)], C.bps[3 + (C.q_rr % 2)]
                        C.q_rr += 1
                        P.add("pe", lambda e, c=c, w=w, qt=qt, s=s, pss=pss: e.matmul(pss[:, 0:w * 128], lhsT=qT[:, s, qt * 128:(qt + 1) * 128],
                                                                                   rhs=kT[:, s, c * 512:c * 512 + w * 128], start=True, stop=True),
                              R=[b_q[s], b_k[s]], W=[bpss])
                        for i in range(w):
                            kt = c * 4 + i
                            n = kt // 2
                            src = pss[:, i * 128:(i + 1) * 128]
                            dst = sc[:, z, kt * 128:(kt + 1) * 128]
                            if kt == qt:
                                P.add("dve", lambda e, src=src, dst=dst, hh=hh: e.scalar_tensor_tensor(out=dst, in0=src, scalar=SCALE, in1=rel[:, hh, 0, :],
                                                                                                     op0=ALU.mult, op1=ALU.add), R=[bpss, b_cst], W=[b_s[z]])
                            elif kt == qt - 1:
                                P.add("dve", lambda e, src=src, dst=dst, hh=hh: e.scalar_tensor_tensor(out=dst, in0=src, scalar=SCALE, in1=rel[:, hh, 1, :],
                                                                                                     op0=ALU.mult, op1=ALU.add), R=[bpss, b_cst], W=[b_s[z]])
                                if n < qb:
                                    P.add("dve", lambda e, dst=dst, qt=qt, n=n: e.tensor_scalar(out=dst, in0=dst, scalar1=bm[:, qt * 8 + n:qt * 8 + n + 1],
                                                                                              scalar2=None, op0=ALU.add), R=[b_s[z], b_gate], W=[b_s[z]])
                            else:
                                P.add("dve", lambda e, src=src, dst=dst, qt=qt, n=n: e.tensor_scalar(out=dst, in0=src, scalar1=SCALE,
                                                                                                   scalar2=cm[:, qt * 8 + n:qt * 8 + n + 1],
                                                                                                   op0=ALU.mult, op1=ALU.add), R=[bpss, b_gate], W=[b_s[z]])
                    L = nk * 128
                    if MD < 4:
                        continue
                    P.add("dve", lambda e, z=z, L=L: e.reduce_max(out=stt[:, z, 0:1], in_=sc[:, z, 0:L], axis=AX.X), R=[b_s[z]], W=[b_st[z]])
                    P.add("dve", lambda e, z=z: e.tensor_scalar(out=stt[:, z, 1:2], in0=stt[:, z, 0:1], scalar1=-1.0, scalar2=None, op0=ALU.mult),
                          R=[b_st[z]], W=[b_st[z]])
                    P.add("act", lambda e, z=z, L=L: e.activation(out=sc[:, z, 0:L], in_=sc[:, z, 0:L], func=AF.Exp, bias=stt[:, z, 1:2], scale=1.0,
                                                                  accum_out=stt[:, z, 2:3]), R=[b_s[z], b_st[z]], W=[b_s[z], b_st[z]])
                    P.add("dve", lambda e, z=z: e.reciprocal(out=stt[:, z, 3:4], in_=stt[:, z, 2:3]), R=[b_st[z]], W=[b_st[z]])
                    P.add("act", lambda e, z=z, L=L: e.activation(out=pb[:, 0:L], in_=sc[:, z, 0:L], func=AF.Copy, scale=stt[:, z, 3:4]),
                          R=[b_s[z], b_st[z]], W=[b_p[z]])
                    po, bpo = C.ps[5 + z], C.bps[5 + z]
                    if MD < 5:
                        continue
                    for c in range((nk + 3) // 4):
                        w = min(4, nk - c * 4)
                        y = C.y_rr % 2
                        C.y_rr += 1
                        ptp, bptp = C.ps[(0, 1)[y]], C.bps[(0, 1)[y]]
                        ptb = ptp[:].bitcast(BF16)
                        for i in range(w):
                            kt = c * 4 + i
                            P.add("pe", lambda e, i=i, kt=kt, z=z, ptb=ptb: e.transpose(out=ptb[:, i * 128:(i + 1) * 128], in_=pb[:, kt * 128:(kt + 1) * 128],
                                                                                      identity=C.identb[:]), R=[b_p[z], C.b_const], W=[bptp])
                        P.add("act", lambda e, w=w, y=y, ptb=ptb: e.copy(out=pT[:, y, 0:w, :], in_=ptb[:, 0:w * 128].rearrange("p (a c) -> p a c", a=w)),
                              R=[bptp], W=[b_pT[y]])
                        for i in range(w):
                            kt = c * 4 + i
                            P.add("pe", lambda e, i=i, kt=kt, y=y, s=s, qt=qt, nk=nk, po=po: e.matmul(po[:, 0:128], lhsT=V[:, s, kt, :], rhs=pT[:, y, i, :],
                                                                                                   start=(kt == 0), stop=(kt == nk - 1)),
                                  R=[b_v[s], b_pT[y]], W=[bpo])
                    P.add("act", lambda e, po=po, hh=hh, qt=qt: e.copy(out=aT[:, hh, qt * 128:(qt + 1) * 128], in_=po[:, 0:128]), R=[bpo], W=[C.b_srcT])
            P.flush()
        out_proj_ln(C, li, T, aT, 8, T["a_w_out_%d" % j], R_)


def gmlp_phase(C, li, T, R_):
    P, nc = C.P, C.nc
    h, hT = C.h, C.hT
    j = li // 3
    w_in = T["b_w_in_%d" % j]
    w_out = T["b_w_out_%d" % j]
    with ExitStack() as es:
        lng = es.enter_context(nc.sbuf_tensor("g_lng_%d" % li, [128, D], F32))
        lnb = es.enter_context(nc.sbuf_tensor("g_lnb_%d" % li, [128, D], F32))
        wi = es.enter_context(nc.sbuf_tensor("g_wi_%d" % li, [128, 2, 8, 512], BF16))
        wo = es.enter_context(nc.sbuf_tensor("g_wo_%d" % li, [128, 2, 4, 1024], BF16))
        yT = es.enter_context(nc.sbuf_tensor("g_yT_%d" % li, [128, 16, 256], BF16))
        v = es.enter_context(nc.sbuf_tensor("g_v_%d" % li, [128, 2, 2048], F32))
        vn = es.enter_context(nc.sbuf_tensor("g_vn_%d" % li, [128, 2, 2048], BF16))
        gb = es.enter_context(nc.sbuf_tensor("g_gb_%d" % li, [128, 2, 2048], F32))
        ws = es.enter_context(nc.sbuf_tensor("g_ws_%d" % li, [128, 8, 128], F32))
        wsT = es.enter_context(nc.sbuf_tensor("g_wsT_%d" % li, [128, 8, 128], BF16))
        tril = es.enter_context(nc.sbuf_tensor("g_tril_%d" % li, [128, 128], F32))
        bs = es.enter_context(nc.sbuf_tensor("g_bs_%d" % li, [1, 1024], F32))
        ones = es.enter_context(nc.sbuf_tensor("g_ones_%d" % li, [1, 128], F32))
        st = es.enter_context(nc.sbuf_tensor("g_st_%d" % li, [128, 4, 6], F32))
        mv = es.enter_context(nc.sbuf_tensor("g_mv_%d" % li, [128, 2], F32))
        rs = es.enter_context(nc.sbuf_tensor("g_rs_%d" % li, [128, 1], F32))
        blnp, b_cst, b_wsT, b_yT, b_st = Buf(), Buf(), Buf(), Buf(), Buf()
        b_wi = [Buf(), Buf()]
        b_wo = [Buf(), Buf()]
        b_v = [Buf(), Buf()]
        b_vn = [Buf(), Buf()]
        load_ln_params(C, lng, lnb, blnp, T["ln_g"][li, 0, :], T["ln_b"][li, 0, :])
        P.add("sp", lambda e: e.dma_start(out=gb[:, 0, :], in_=bcast_rows(T["b_ln_g_%d" % j])), W=[b_cst], dma=True)
        P.add("sp", lambda e: e.dma_start(out=gb[:, 1, :], in_=bcast_rows(T["b_ln_b_%d" % j])), W=[b_cst], dma=True)
        P.add("sp", lambda e: e.dma_start(out=ws[:], in_=T["b_w_s_%d" % j].rearrange("g t s -> t g s")), W=[b_cst], dma=True)
        P.add("sp", lambda e: e.dma_start(out=tril[:], in_=T["tril"]), W=[b_cst], dma=True)
        P.add("sp", lambda e: e.dma_start(out=bs[:], in_=T["b_b_s_%d" % j].rearrange("(o g) t -> o (g t)", o=1)), W=[b_cst], dma=True)
        P.add("pool", lambda e: e.memset(ones[:], 1.0), W=[b_cst])
        for g in range(8):
            P.add("dve", lambda e, g=g: e.tensor_tensor(out=ws[:, g, :], in0=ws[:, g, :], in1=tril[:], op=ALU.mult), R=[b_cst], W=[b_cst])
        for half in range(2):
            ps, bps = C.ps[half], C.bps[half]
            for gg in range(4):
                g = half * 4 + gg
                P.add("pe", lambda e, g=g, gg=gg, ps=ps: e.transpose(out=ps[:, gg * 128:(gg + 1) * 128], in_=ws[:, g, :], identity=C.ident[:]),
                      R=[b_cst, C.b_const], W=[bps])
            P.add("act", lambda e, ps=ps, half=half: e.copy(out=wsT[:, half * 4:half * 4 + 4, :], in_=ps[:].rearrange("p (a c) -> p a c", a=4)),
                  R=[bps], W=[b_wsT])
        it = 0
        iw = 0
        acc_banks = (5, 6, 7, 2)
        DBG = 9
        for tg in range(8 if DBG >= 2 else 0):
            c0 = tg * 256
            tiles = (2 * tg, 2 * tg + 1)
            bhTg = [C.bhT[t] for t in tiles]
            for cg in range(8):
                s = it % 2
                it += 1
                P.add("pool", lambda e, s=s, cg=cg: e.dma_start(out=wi[:, s], in_=w_in[:, cg * 512:(cg + 1) * 512].rearrange("(k p) n -> p k n", p=128)),
                      W=[b_wi[s]], dma=True)
                if cg < 4:
                    for jj in range(4):
                        fc = cg * 4 + jj
                        q = C.q_rr % 2
                        C.q_rr += 1
                        pu, bpu = C.ps[3 + q], C.bps[3 + q]
                        for k in range(8):
                            P.add("pe", lambda e, k=k, jj=jj, s=s, pu=pu, c0=c0: e.matmul(pu[:, 0:256], lhsT=wi[:, s, k, jj * 128:(jj + 1) * 128],
                                                                                      rhs=hT[:, k, c0:c0 + 256], start=(k == 0), stop=(k == 7)),
                                  R=[b_wi[s]] + bhTg, W=[bpu])
                        P.add("act", lambda e, fc=fc, pu=pu: e.activation(out=yT[:, fc, :], in_=pu[:, 0:256], func=AF.Gelu), R=[bpu], W=[b_yT])
                else:
                    for ti in range(2):
                        t = tiles[ti]
                        q = C.q_rr % 2
                        C.q_rr += 1
                        pv, bpv = C.ps[3 + q], C.bps[3 + q]
                        for k in range(8):
                            P.add("pe", lambda e, k=k, t=t, s=s, pv=pv: e.matmul(pv[:], lhsT=hT[:, k, t * 128:(t + 1) * 128], rhs=wi[:, s, k, :],
                                                                               start=(k == 0), stop=(k == 7)), R=[b_wi[s], C.bhT[t]], W=[bpv])
                        P.add("act", lambda e, ti=ti, cg=cg, pv=pv: e.activation(out=v[:, ti, (cg - 4) * 512:(cg - 3) * 512], in_=pv[:], func=AF.Gelu),
                              R=[bpv], W=[b_v[ti]])
            for ti in range(2 if DBG >= 3 else 0):
                for c in range(4):
                    P.add("dve", lambda e, c=c, ti=ti: e.bn_stats(out=st[:, c, :], in_=v[:, ti, c * 512:(c + 1) * 512]), R=[b_v[ti]], W=[b_st])
                P.add("dve", lambda e: e.bn_aggr(out=mv[:], in_=st[:]), R=[b_st], W=[b_st])
                P.add("act", lambda e: e.activation(out=rs[:], in_=mv[:, 1:2], func=AF.Sqrt, bias=EPS, scale=1.0), R=[b_st], W=[b_st])
                P.add("dve", lambda e: e.reciprocal(out=rs[:], in_=rs[:]), R=[b_st], W=[b_st])
                P.add("dve", lambda e, ti=ti: e.tensor_scalar(out=v[:, ti, :], in0=v[:, ti, :], scalar1=mv[:, 0:1], scalar2=rs[:, 0:1],
                                                              op0=ALU.subtract, op1=ALU.mult), R=[b_v[ti], b_st], W=[b_v[ti]])
                P.add("pool", lambda e, ti=ti: e.tensor_tensor(out=v[:, ti, :], in0=v[:, ti, :], in1=gb[:, 0, :], op=ALU.mult), R=[b_v[ti], b_cst], W=[b_v[ti]])
                P.add("pool", lambda e, ti=ti: e.tensor_tensor(out=vn[:, ti, :], in0=v[:, ti, :], in1=gb[:, 1, :], op=ALU.add), R=[b_v[ti], b_cst], W=[b_vn[ti]])
            for ti in range(2 if DBG >= 4 else 0):
                for bk in range(4):
                    q = C.q_rr % 2
                    C.q_rr += 1
                    pm, bpm = C.ps[3 + q], C.bps[3 + q]
                    for jj in range(4):
                        fc = bk * 4 + jj
                        g = fc // 2
                        P.add("pe", lambda e, jj=jj, fc=fc, g=g, ti=ti, pm=pm: e.matmul(pm[:, jj * 128:(jj + 1) * 128], lhsT=vn[:, ti, fc * 128:(fc + 1) * 128],
                                                                                      rhs=wsT[:, g, :], start=True, stop=False), R=[b_vn[ti], b_wsT], W=[bpm])
                        P.add("pe", lambda e, jj=jj, g=g, pm=pm: e.matmul(pm[:, jj * 128:(jj + 1) * 128], lhsT=ones[0:1, :], rhs=bs[0:1, g * 128:(g + 1) * 128],
                                                                        start=False, stop=True), R=[b_cst], W=[bpm])
                    P.add("dve", lambda e, bk=bk, ti=ti, pm=pm: e.tensor_tensor(out=yT[:, bk * 4:bk * 4 + 4, ti * 128:(ti + 1) * 128],
                                                                              in0=pm[:].rearrange("p (a c) -> p a c", a=4),
                                                                              in1=yT[:, bk * 4:bk * 4 + 4, ti * 128:(ti + 1) * 128], op=ALU.mult),
                          R=[bpm, b_yT], W=[b_yT])
            if DBG < 5:
                continue
            for wc in range(4):
                s2 = iw % 2
                iw += 1
                P.add("pool", lambda e, s2=s2, wc=wc: e.dma_start(out=wo[:, s2], in_=w_out[wc * 512:(wc + 1) * 512, :].rearrange("(f p) n -> p f n", p=128)),
                      W=[b_wo[s2]], dma=True)
                for ti in range(2):
                    for n in range(2):
                        bi = acc_banks[ti * 2 + n]
                        po, bpo = C.ps[bi], C.bps[bi]
                        for ff in range(4):
                            P.add("pe", lambda e, wc=wc, ff=ff, ti=ti, n=n, s2=s2, po=po: e.matmul(
                                po[:], lhsT=yT[:, wc * 4 + ff, ti * 128:(ti + 1) * 128], rhs=wo[:, s2, ff, n * 512:(n + 1) * 512],
                                start=(wc == 0 and ff == 0), stop=(wc == 3 and ff == 3)), R=[b_yT, b_wo[s2]], W=[bpo])
            for ti in range(2):
                t = tiles[ti]
                for n in range(2):
                    bi = acc_banks[ti * 2 + n]
                    po, bpo = C.ps[bi], C.bps[bi]
                    P.add("dve", lambda e, n=n, t=t, po=po: e.scalar_tensor_tensor(out=h[:, t, n * 512:(n + 1) * 512], in0=h[:, t, n * 512:(n + 1) * 512],
                                                                                 scalar=ALPHA, in1=po[:], op0=ALU.mult, op1=ALU.add),
                          R=[bpo, C.bh[t]], W=[C.bh[t]])
            for ti in range(2 if DBG >= 6 else 0):
                ln_tile(C, tiles[ti], lng, lnb, blnp, router=(R_ if DBG >= 7 else None))
        P.flush()


def gla_phase(C, li, T, R_):
    P, nc = C.P, C.nc
    h, hT = C.h, C.hT
    j = li // 3
    w_in = T["c_w_in_%d" % j]
    SCALE = 128 ** -0.5
    with nc.sbuf_tensor("c_oT_%d" % li, [128, 8, S], BF16) as oT:
        C.b_srcT = Buf("oT")
        with ExitStack() as es:
            wq = es.enter_context(nc.sbuf_tensor("c_wq_%d" % li, [128, 2, 8, 768], BF16))
            wg = es.enter_context(nc.sbuf_tensor("c_wg_%d" % li, [128, 8, 16], BF16))
            gkT = es.enter_context(nc.sbuf_tensor("c_gkT_%d" % li, [32, S], F32))
            wgk = es.enter_context(nc.sbuf_tensor("c_wgk_%d" % li, [32, 512], F32))
            ng = es.enter_context(nc.sbuf_tensor("c_ng_%d" % li, [128, 256], F32))
            tri = es.enter_context(nc.sbuf_tensor("c_tri_%d" % li, [128, 128], F32))
            su = es.enter_context(nc.sbuf_tensor("c_su_%d" % li, [128, 128], F32))
            cmk = es.enter_context(nc.sbuf_tensor("c_cm_%d" % li, [128, 128], F32))
            qk = es.enter_context(nc.sbuf_tensor("c_qk_%d" % li, [128, 2, 2, 128], F32))
            kt = es.enter_context(nc.sbuf_tensor("c_kt_%d" % li, [128, 2, 128], F32))
            vt = es.enter_context(nc.sbuf_tensor("c_vt_%d" % li, [128, 2, 256], BF16))
            gs = es.enter_context(nc.sbuf_tensor("c_gs_%d" % li, [128, 2, 256], F32))
            nl = es.enter_context(nc.sbuf_tensor("c_nl_%d" % li, [128, 2, 128], F32))
            ex = es.enter_context(nc.sbuf_tensor("c_ex_%d" % li, [128, 2, 3, 128], F32))
            qz = es.enter_context(nc.sbuf_tensor("c_qz_%d" % li, [128, 2, 2, 128], BF16))
            ke = es.enter_context(nc.sbuf_tensor("c_ke_%d" % li, [128, 2, 128], BF16))
            krz = es.enter_context(nc.sbuf_tensor("c_krz_%d" % li, [128, 2, 2, 128], BF16))
            att = es.enter_context(nc.sbuf_tensor("c_att_%d" % li, [128, 2, 128], BF16))
            St = es.enter_context(nc.sbuf_tensor("c_S_%d" % li, [128, 256], F32))
            Sb = es.enter_context(nc.sbuf_tensor("c_Sb_%d" % li, [128, 5, 256], BF16))
            jk = es.enter_context(nc.sbuf_tensor("c_jk_%d" % li, [128, 256], F32))
            ot = es.enter_context(nc.sbuf_tensor("c_ot_%d" % li, [128, 2, 256], F32))
            og = es.enter_context(nc.sbuf_tensor("c_og_%d" % li, [128, 2, 256], BF16))
            ss = es.enter_context(nc.sbuf_tensor("c_ss_%d" % li, [128, 2, 2], F32))
            b_cst, b_gk, b_S, b_jk = Buf(), Buf(), Buf(), Buf()
            b_wq = [Buf(), Buf()]
            b_qk = [Buf(), Buf()]
            b_kt = [Buf(), Buf()]
            b_vt = [Buf(), Buf()]
            b_gs = [Buf(), Buf()]
            b_nl = [Buf(), Buf()]
            b_ex = [Buf(), Buf()]
            b_qz = [Buf(), Buf()]
            b_ke = [Buf(), Buf()]
            b_krz = [Buf(), Buf()]
            b_att = [Buf(), Buf()]
            b_Sb = [Buf() for _ in range(5)]
            b_ot = [Buf(), Buf()]
            b_og = [Buf(), Buf()]
            b_ss = [Buf(), Buf()]
            P.add("pool", lambda e: e.dma_start(out=wg[:], in_=w_in[:, 3072:3088].rearrange("(k p) n -> p k n", p=128)), W=[b_cst], dma=True)
            P.add("sp", lambda e: e.dma_start(out=wgk[0:16, :], in_=T["c_w_gk_up_%d" % j]), W=[b_cst], dma=True)
            P.add("sp", lambda e: e.dma_start(out=wgk[16:17, :], in_=T["c_b_gk_%d" % j].rearrange("(o n) -> o n", o=1)), W=[b_cst], dma=True)
            P.add("sp", lambda e: e.dma_start(out=ng[:], in_=bcast_rows(T["c_norm_g_%d" % j])), W=[b_cst], dma=True)
            P.add("sp", lambda e: e.dma_start(out=tri[:], in_=T["gla_tri"]), W=[b_cst], dma=True)
            P.add("sp", lambda e: e.dma_start(out=su[:], in_=T["gla_su"]), W=[b_cst], dma=True)
            P.add("sp", lambda e: e.dma_start(out=cmk[:], in_=T["gla_cm"]), W=[b_cst], dma=True)
            P.add("pool", lambda e: e.memset(gkT[:], 1.0), W=[b_gk])
            for z in range(2):
                P.add("pool", lambda e, z=z: e.memset(qz[:, z], 0.0), W=[b_qz[z]])
                P.add("pool", lambda e, z=z: e.memset(krz[:, z], 0.0), W=[b_krz[z]])
            for g4 in range(4):
                pg, bpg = C.ps[3 + g4 % 2], C.bps[3 + g4 % 2]
                for k in range(8):
                    P.add("pe", lambda e, k=k, g4=g4, pg=pg: e.matmul(pg[0:16, :], lhsT=wg[:, k, :], rhs=hT[:, k, g4 * 512:(g4 + 1) * 512],
                                                                    start=(k == 0), stop=(k == 7)), R=[b_cst] + C.bhT[g4 * 4:g4 * 4 + 4], W=[bpg])
                P.add("act", lambda e, g4=g4, pg=pg: e.copy(out=gkT[0:16, g4 * 512:(g4 + 1) * 512], in_=pg[0:16, :]), R=[bpg], W=[b_gk])

            def front(hd, s, t):
                z = t % 2
                tc = slice(t * 128, (t + 1) * 128)
                bhT = C.bhT[t]
                pa, bpa = C.ps[3], C.bps[3]
                for k in range(8):
                    P.add("pe", lambda e, k=k: e.matmul(pa[:, 0:128], lhsT=wq[:, s, k, 0:128], rhs=hT[:, k, tc], start=(k == 0), stop=(k == 7)),
                          R=[b_wq[s], bhT], W=[bpa])
                for k in range(8):
                    P.add("pe", lambda e, k=k: e.matmul(pa[:, 128:256], lhsT=wq[:, s, k, 128:256], rhs=hT[:, k, tc], start=(k == 0), stop=(k == 7)),
                          R=[b_wq[s], bhT], W=[bpa])
                for k in range(8):
                    P.add("pe", lambda e, k=k: e.matmul(pa[:, 256:384], lhsT=hT[:, k, tc], rhs=wq[:, s, k, 128:256], start=(k == 0), stop=(k == 7)),
                          R=[b_wq[s], bhT], W=[bpa])
                P.add("pe", lambda e: e.matmul(pa[:, 384:512], lhsT=gkT[0:17, tc], rhs=wgk[0:17, hd * 128:(hd + 1) * 128], start=True, stop=True),
                      R=[b_gk, b_cst], W=[bpa])
                P.add("act", lambda e: e.copy(out=qk[:, z], in_=pa[:, 0:256].rearrange("p (a c) -> p a c", a=2)), R=[bpa], W=[b_qk[z]])
                P.add("act", lambda e: e.copy(out=kt[:, z], in_=pa[:, 256:384]), R=[bpa], W=[b_kt[z]])
                P.add("act", lambda e: e.activation(out=nl[:, z], in_=pa[:, 384:512], func=AF.Exp, scale=-1.0), R=[bpa], W=[b_nl[z]])
                P.add("act", lambda e: e.activation(out=nl[:, z], in_=nl[:, z], func=AF.Ln, bias=1.0, scale=1.0), R=[b_nl[z]], W=[b_nl[z]])
                pb_, bpb = C.ps[4], C.bps[4]
                for k in range(8):
                    P.add("pe", lambda e, k=k: e.matmul(pb_[:], lhsT=hT[:, k, tc], rhs=wq[:, s, k, 256:768], start=(k == 0), stop=(k == 7)),
                          R=[b_wq[s], bhT], W=[bpb])
                P.add("act", lambda e: e.copy(out=vt[:, z], in_=pb_[:, 0:256]), R=[bpb], W=[b_vt[z]])
                P.add("act", lambda e: e.activation(out=gs[:, z], in_=pb_[:, 256:512], func=AF.Silu), R=[bpb], W=[b_gs[z]])
                pc, bpc = C.ps[5], C.bps[5]
                P.add("pe", lambda e: e.matmul(pc[:, 0:128], lhsT=nl[:, z], rhs=tri[:], start=True, stop=True), R=[b_nl[z], b_cst], W=[bpc])
                P.add("pe", lambda e: e.matmul(pc[:, 128:256], lhsT=su[:], rhs=nl[:, z], start=True, stop=True), R=[b_nl[z], b_cst], W=[bpc])
                P.add("act", lambda e: e.activation(out=ex[:, z, 0, :], in_=pc[:, 0:128], func=AF.Exp, scale=-1.0 / 16.0), R=[bpc], W=[b_ex[z]])
                P.add("act", lambda e: e.activation(out=ex[:, z, 1, :], in_=pc[:, 0:128], func=AF.Exp, scale=1.0 / 16.0), R=[bpc], W=[b_ex[z]])
                P.add("act", lambda e: e.activation(out=ex[:, z, 2, :], in_=pc[:, 128:256], func=AF.Exp, scale=-1.0 / 16.0), R=[bpc], W=[b_ex[z]])
                for c in range(2):
                    cs = slice(c * 64, (c + 1) * 64)
                    P.add("dve", lambda e, c=c, cs=cs: e.scalar_tensor_tensor(out=qz[:, z, c, cs], in0=qk[:, z, 0, cs], scalar=SCALE, in1=ex[:, z, 0, cs],
                                                                            op0=ALU.mult, op1=ALU.mult), R=[b_qk[z], b_ex[z]], W=[b_qz[z]])
                P.add("pool", lambda e: e.tensor_tensor(out=ke[:, z], in0=qk[:, z, 1], in1=ex[:, z, 1, :], op=ALU.mult), R=[b_qk[z], b_ex[z]], W=[b_ke[z]])
                for c in range(2):
                    rows = slice(c * 64, (c + 1) * 64)
                    P.add("pool", lambda e, c=c, rows=rows: e.tensor_tensor(out=krz[rows, z, c, :], in0=kt[rows, z], in1=ex[rows, z, 2, :], op=ALU.mult),
                          R=[b_kt[z], b_ex[z]], W=[b_krz[z]])
                P.add("pe", lambda e: e.matmul(pc[:, 256:384], lhsT=ke[:, z], rhs=qz[:, z, 0, :], start=True, stop=False), R=[b_ke[z], b_qz[z]], W=[bpc])
                P.add("pe", lambda e: e.matmul(pc[:, 256:384], lhsT=ke[:, z], rhs=qz[:, z, 1, :], start=False, stop=True), R=[b_ke[z], b_qz[z]], W=[bpc])
                P.add("dve", lambda e: e.tensor_tensor(out=att[:, z], in0=pc[:, 256:384], in1=cmk[:], op=ALU.mult), R=[bpc, b_cst], W=[b_att[z]])
                pd, bpd = C.ps[6], C.bps[6]
                P.add("pe", lambda e: e.matmul(pd[:, 0:256], lhsT=krz[:, z, 0, :], rhs=vt[:, z], start=True, stop=True), R=[b_krz[z], b_vt[z]], W=[bpd])
                P.add("pe", lambda e: e.matmul(pd[:, 256:512], lhsT=krz[:, z, 1, :], rhs=vt[:, z], start=True, stop=True), R=[b_krz[z], b_vt[z]], W=[bpd])
                P.add("dve", lambda e: e.scalar_tensor_tensor(out=St[:], in0=St[:], scalar=ex[:, z, 0, 63:64], in1=pd[:, 0:256], op0=ALU.mult, op1=ALU.add),
                      R=[b_S, b_ex[z], bpd], W=[b_S])
                P.add("act", lambda e: e.copy(out=Sb[:, 3 + z, :], in_=St[:]), R=[b_S], W=[b_Sb[3 + z]])
                P.add("dve", lambda e: e.scalar_tensor_tensor(out=St[:], in0=St[:], scalar=ex[:, z, 0, 127:128], in1=pd[:, 256:512], op0=ALU.mult, op1=ALU.add),
                      R=[b_S, b_ex[z], bpd], W=[b_S])
                P.add("act", lambda e: e.copy(out=Sb[:, (t + 1) % 3, :], in_=St[:]), R=[b_S], W=[b_Sb[(t + 1) % 3]])

            def back(hd, s, t):
                z = t % 2
                tc = slice(t * 128, (t + 1) * 128)
                po, bpo = C.ps[7], C.bps[7]
                P.add("pe", lambda e: e.matmul(po[:, 0:256], lhsT=qz[:, z, 0, :], rhs=Sb[:, t % 3, :], start=True, stop=False), R=[b_qz[z], b_Sb[t % 3]], W=[bpo])
                P.add("pe", lambda e: e.matmul(po[:, 0:256], lhsT=qz[:, z, 1, :], rhs=Sb[:, 3 + z, :], start=False, stop=False), R=[b_qz[z], b_Sb[3 + z]], W=[bpo])
                P.add("pe", lambda e: e.matmul(po[:, 0:256], lhsT=att[:, z], rhs=vt[:, z], start=False, stop=True), R=[b_att[z], b_vt[z]], W=[bpo])
                P.add("act", lambda e: e.activation(out=jk[:], in_=po[:, 0:256], func=AF.Square, accum_out=ss[:, z, 0:1]), R=[bpo], W=[b_jk, b_ss[z]])
                P.add("act", lambda e: e.activation(out=ss[:, z, 1:2], in_=ss[:, z, 0:1], func=AF.Sqrt, bias=EPS, scale=1.0 / 256.0), R=[b_ss[z]], W=[b_ss[z]])
                P.add("dve", lambda e: e.reciprocal(out=ss[:, z, 1:2], in_=ss[:, z, 1:2]), R=[b_ss[z]], W=[b_ss[z]])
                P.add("dve", lambda e: e.scalar_tensor_tensor(out=ot[:, z], in0=po[:, 0:256], scalar=ss[:, z, 1:2], in1=ng[:], op0=ALU.mult, op1=ALU.mult),
                      R=[bpo, b_ss[z], b_cst], W=[b_ot[z]])
                P.add("pool", lambda e: e.tensor_tensor(out=og[:, z], in0=ot[:, z], in1=gs[:, z], op=ALU.mult), R=[b_ot[z], b_gs[z]], W=[b_og[z]])
                ptp, bptp = C.ps[z], C.bps[z]
                ptb = ptp[:].bitcast(BF16)
                for c2 in range(2):
                    P.add("pe", lambda e, c2=c2: e.transpose(out=ptb[:, c2 * 128:(c2 + 1) * 128], in_=og[:, z, c2 * 128:(c2 + 1) * 128], identity=C.identb[:]),
                          R=[b_og[z], C.b_const], W=[bptp])
                P.add("act", lambda e: e.copy(out=oT[:, hd * 2:hd * 2 + 2, tc], in_=ptb[:, 0:256].rearrange("p (a c) -> p a c", a=2)), R=[bptp], W=[C.b_srcT])

            for hd in range(4):
                s = hd % 2
                for (c0_, w_, d0) in ((hd * 128, 128, 0), (512 + hd * 128, 128, 128), (1024 + hd * 256, 256, 256), (2048 + hd * 256, 256, 512)):
                    P.add("pool", lambda e, c0_=c0_, w_=w_, d0=d0, s=s: e.dma_start(
                        out=wq[:, s, :, d0:d0 + w_], in_=w_in[:, c0_:c0_ + w_].rearrange("(k p) n -> p k n", p=128)), W=[b_wq[s]], dma=True)
                P.add("pool", lambda e: e.memset(St[:], 0.0), W=[b_S])
                P.add("pool", lambda e: e.memset(Sb[:, 0, :], 0.0), W=[b_Sb[0]])
                front(hd, s, 0)
                for t in range(NT):
                    if t + 1 < NT:
                        front(hd, s, t + 1)
                    back(hd, s, t)
            P.flush()
        out_proj_ln(C, li, T, oT, 8, T["c_w_out_%d" % j], R_)


def build_program(mode="full", n_experts=NE, layers=(0, 1, 2, 3)):
    nc = bass.Bass("TRN2", target_bir_lowering=False)
    T = {}

    def din(name, shape, dt=F32):
        T[name] = nc.dram_tensor(name, list(shape), dt, kind="ExternalInput").ap()

    do_mixer = mode in ("full", "mixer_only")
    do_moe = mode in ("full", "moe_only")
    din("x", (S, D))
    din("ident", (128, 128))
    din("identb", (128, 128), BF16)
    din("ln_g", (DEPTH, 2, D))
    din("ln_b", (DEPTH, 2, D))
    mixers = sorted(set(li % 3 for li in layers)) if do_mixer else []
    if 0 in mixers:
        din("rel_bias", (32, 8))
        din("relT", (8, 2, 128, 128))
        din("causal", (128, 128))
        din("vmask", (128, 128))
        din("vmask2", (128, 128))
    for li in layers:
        if do_mixer:
            j = li // 3
            if li % 3 == 0:
                din("a_w_in_%d" % j, (D, 3 * D))
                din("a_w_out_%d" % j, (D, D))
            elif li % 3 == 1:
                din("b_w_in_%d" % j, (D, 4 * D))
                din("b_ln_g_%d" % j, (2 * D,))
                din("b_ln_b_%d" % j, (2 * D,))
                din("b_w_s_%d" % j, (8, 128, 128))
                din("b_b_s_%d" % j, (8, 128))
                din("b_w_out_%d" % j, (2 * D, D))
                din("tril", (128, 128))
            else:
                din("c_w_in_%d" % j, (D, 3088))
                din("c_w_gk_up_%d" % j, (16, 512))
                din("c_b_gk_%d" % j, (512,))
                din("c_norm_g_%d" % j, (256,))
                din("c_w_out_%d" % j, (D, D))
                din("gla_tri", (128, 128))
                din("gla_su", (128, 128))
                din("gla_cm", (128, 128))
        din("moe_w_router_%d" % li, (D, NE))
        din("moe_b_router_%d" % li, (NE,))
        if do_moe:
            if "iota" not in T:
                din("iota", (128, 128))
                din("ones_b", (128, 128), BF16)
                din("tris_b", (128, 128), BF16)
            din("moe_w_gate_up_%d" % li, (NE, D, 2 * D))
            din("moe_b_gate_up_%d" % li, (NE, 2 * D))
            din("moe_w_down_%d" % li, (NE, D, D))
            din("moe_b_down_%d" % li, (NE, D))
    out = nc.dram_tensor("out", [S, D], F32, kind="ExternalOutput").ap()

    C = Ctx()
    C.nc = nc
    C.n_experts = n_experts
    C.P = P = Prog(nc)
    C.tp_rr = C.act_rr = C.q_rr = C.y_rr = C.ys_rr = 0
    with ExitStack() as es:
        h = es.enter_context(nc.sbuf_tensor("h", [128, NT, D], F32))
        hT = es.enter_context(nc.sbuf_tensor("hT", [128, 8, S], BF16))
        ident = es.enter_context(nc.sbuf_tensor("ident_s", [128, 128], F32))
        identb = es.enter_context(nc.sbuf_tensor("identb_s", [128, 128], BF16))
        hT32 = es.enter_context(nc.sbuf_tensor("hT32", [128, 8, 128], F32))
        gates = es.enter_context(nc.sbuf_tensor("gates", [128, NT, NE], F32))
        ln_st = es.enter_context(nc.sbuf_tensor("ln_st", [128, 2, 6], F32))
        ln_mv = es.enter_context(nc.sbuf_tensor("ln_mv", [128, 2], F32))
        ln_rs = es.enter_context(nc.sbuf_tensor("ln_rs", [128, 1], F32))
        r_lg = es.enter_context(nc.sbuf_tensor("r_lg", [128, NE], F32))
        r_m8 = es.enter_context(nc.sbuf_tensor("r_m8", [128, 8], F32))
        r_ex = es.enter_context(nc.sbuf_tensor("r_ex", [128, NE], F32))
        r_msk = es.enter_context(nc.sbuf_tensor("r_msk", [128, NE], F32))
        r_sm = es.enter_context(nc.sbuf_tensor("r_sm", [128, 2], F32))
        r_wr = es.enter_context(nc.sbuf_tensor("r_wr", [128, 8, NE], F32))
        r_brb = es.enter_context(nc.sbuf_tensor("r_brb", [128, NE], F32))
        C.h, C.hT, C.ident, C.identb, C.hT32, C.gates = h, hT, ident, identb, hT32, gates
        C.ln_st, C.ln_mv, C.ln_rs, C.ln_nb = ln_st, ln_mv, ln_rs, None
        C.r_lg, C.r_m8, C.r_ex, C.r_msk, C.r_sm = r_lg, r_m8, r_ex, r_msk, r_sm
        C.bh = [Buf("h%d" % t) for t in range(NT)]
        C.bhT = [Buf("hT%d" % t) for t in range(NT)]
        C.b_gates = [Buf() for t in range(NT)]
        C.b_const = Buf("const")
        C.b_lnst = Buf()
        C.b_hT32 = Buf()
        C.b_rt = Buf()
        ps_cms = [nc.psum_tensor("ps%d" % i, [128, 512], F32) for i in range(8)]
        C.ps = [cm.__enter__() for cm in ps_cms]
        C.bps = [Buf("ps%d" % i) for i in range(8)]

        P.add("sp", lambda e: e.dma_start(out=ident[:], in_=T["ident"]), W=[C.b_const], dma=True)
        P.add("sp", lambda e: e.dma_start(out=identb[:], in_=T["identb"]), W=[C.b_const], dma=True)
        for t in range(NT):
            P.add("sp", lambda e, t=t: e.dma_start(out=h[:, t, :], in_=T["x"][t * 128:(t + 1) * 128, :]), W=[C.bh[t]], dma=True)
        first = True
        for li in layers:
            b_r = Buf()
            P.add("sp", lambda e, li=li: e.dma_start(out=r_wr[:], in_=T["moe_w_router_%d" % li].rearrange("(k p) n -> p k n", p=128)), W=[b_r], dma=True)
            P.add("sp", lambda e, li=li: e.dma_start(out=r_brb[:], in_=bcast_rows(T["moe_b_router_%d" % li])), W=[b_r], dma=True)
            R_ = {"wr": r_wr, "brb": r_brb, "b": b_r}
            if first:
                for t in range(NT):
                    transpose_tile(C, t, None if do_mixer else R_)
                P.flush()
                first = False
            if do_mixer:
                if li % 3 == 0:
                    moba_phase(C, li, T, R_)
                elif li % 3 == 1:
                    gmlp_phase(C, li, T, R_)
                else:
                    gla_phase(C, li, T, R_)
            if do_moe:
                moe_phase(C, li, T)

        for t in range(NT):
            P.add("sp", lambda e, t=t: e.dma_start(out=out[t * 128:(t + 1) * 128, :], in_=h[:, t, :]), R=[C.bh[t]], dma=True)
        P.flush()
        for cm in reversed(ps_cms):
            cm.__exit__(None, None, None)
    P.close()
    return nc


def t5_bucket_np(rel):
    n = np.maximum(rel, 0)
    nf = np.maximum(n, 1).astype(np.float32)
    large = 16 + (np.log(nf / np.float32(16)) / np.float32(math.log(128 / 16)) * np.float32(16)).astype(np.int32)
    large = np.minimum(large, 31)
    return np.where(n < 16, n, large)


def host_constants(rel_bias=None):
    import ml_dtypes
    c = {"ident": np.eye(128, dtype=np.float32), "identb": np.eye(128, dtype=np.float32).astype(ml_dtypes.bfloat16)}
    i = np.arange(128)[:, None]
    jj = np.arange(128)[None, :]
    c["causal"] = np.where(jj <= i, 0.0, NEG).astype(np.float32)
    c["iota"] = np.broadcast_to(np.arange(128, dtype=np.float32)[None, :], (128, 128)).copy()
    c["ones_b"] = np.ones((128, 128), np.float32).astype(ml_dtypes.bfloat16)
    c["tris_b"] = (i < jj).astype(np.float32).astype(ml_dtypes.bfloat16)
    c["tril"] = (jj <= i).astype(np.float32)
    qt = np.arange(16)[:, None]
    n = np.arange(8)[None, :]
    valid = n < (qt // 2)
    vm = np.where(valid, 0.0, -1e30).astype(np.float32).reshape(1, 128)
    c["vmask"] = np.broadcast_to(vm, (128, 128)).copy()
    vm2 = np.where(valid, -30000.0, -1e30).astype(np.float32).reshape(1, 128)
    c["vmask2"] = np.broadcast_to(vm2, (128, 128)).copy()
    if rel_bias is not None:
        relT = np.empty((8, 2, 128, 128), np.float32)
        for d in range(2):
            bk = t5_bucket_np(d * 128 + i - jj)
            for hh in range(8):
                relT[hh, d] = rel_bias[bk, hh]
        c["relT"] = relT
    same = (i // 64) == (jj // 64)
    c["gla_tri"] = (same & (i <= jj)).astype(np.float32)
    c["gla_su"] = (same & (i > jj)).astype(np.float32)
    c["gla_cm"] = (same & (i <= jj)).astype(np.float32)
    return c


def make_in_map(A, x_core, consts, layers=(0, 1, 2, 3), mode="full"):
    do_mixer = mode in ("full", "mixer_only")
    do_moe = mode in ("full", "moe_only")
    im = {"x": np.ascontiguousarray(x_core), "ln_g": A["ln_g"], "ln_b": A["ln_b"]}
    im.update(consts)
    if "rel_bias" in A:
        im["rel_bias"] = A["rel_bias"]
    for li in layers:
        j = li // 3
        if do_mixer:
            if li % 3 == 0:
                im["a_w_in_%d" % j] = A["a_w_in"][j]
                im["a_w_out_%d" % j] = A["a_w_out"][j]
            elif li % 3 == 1:
                for n in ("b_w_in", "b_ln_g", "b_ln_b", "b_w_s", "b_b_s", "b_w_out"):
                    im["%s_%d" % (n, j)] = A[n][j]
            else:
                for n in ("c_w_in", "c_w_gk_up", "c_b_gk", "c_norm_g", "c_w_out"):
                    im["%s_%d" % (n, j)] = A[n][j]
        im["moe_w_router_%d" % li] = A["moe_w_router"][li]
        im["moe_b_router_%d" % li] = A["moe_b_router"][li]
        if do_moe:
            for n in ("moe_w_gate_up", "moe_b_gate_up", "moe_w_down", "moe_b_down"):
                im["%s_%d" % (n, li)] = A[n][li]
    return im


def kernel(**inputs):
    A = {k: np.asarray(v) for k, v in inputs.items()}
    nc = build_program("full")
    consts = host_constants(A["rel_bias"])
    in_maps = [make_in_map(A, A["x"][c], consts) for c in range(8)]
    res = run_bass_kernel_spmd(nc, in_maps, core_ids=list(range(8)))
    out = np.stack([np.asarray(res.results[c]["out"]) for c in range(8)], axis=0)
    return out.astype(np.float32)
```

```python
import math
from contextlib import ExitStack
import numpy as np
import concourse.bass as bass
import concourse.mybir as mybir
from concourse.bass_utils import run_bass_kernel_spmd

F32 = mybir.dt.float32
BF16 = mybir.dt.bfloat16
ALU = mybir.AluOpType
AF = mybir.ActivationFunctionType
AX = mybir.AxisListType

ENGS = ("pe", "act", "dve", "pool", "sp")

D = 1024
S = 2048
NT = 16
DEPTH = 4
ALPHA = (2.0 * DEPTH) ** 0.25
EPS = 1e-5
NE = 32
NEG = -30000.0


class Buf:
    __slots__ = ("name", "lw", "rd")

    def __init__(self, name="b"):
        self.name = name
        self.lw = None
        self.rd = []


class Op:
    __slots__ = ("eng", "fn", "dma", "deps", "sem", "val", "needs_sig", "epoch")

    def __init__(self, eng, fn, dma, epoch=0):
        self.epoch = epoch
        self.eng = eng
        self.fn = fn
        self.dma = dma
        self.deps = []
        self.sem = None
        self.val = 0
        self.needs_sig = False


class Prog:
    def __init__(self, nc, n_dma_sems=(("sp", 24), ("pool", 24), ("act", 8))):
        self.nc = nc
        self.pending = {e: [] for e in ENGS}
        self.esem = {}
        self.ecount = {e: 0 for e in ENGS}
        self._ctx = []
        for e in ENGS:
            cm = nc.semaphore("s_" + e)
            self.esem[e] = cm.__enter__()
            self._ctx.append(cm)
        self.dsems = {}
        self.dcount = {}
        self.dlast = {}
        self.dnext = {}
        for q, n in n_dma_sems:
            lst = []
            for i in range(n):
                cm = nc.semaphore("d_%s_%d" % (q, i))
                lst.append(cm.__enter__())
                self._ctx.append(cm)
            self.dsems[q] = lst
            self.dcount[q] = [0] * n
            self.dlast[q] = [None] * n
            self.dnext[q] = 0
        self.known = {e: {} for e in ENGS}
        self.all_dma_since_barrier = []
        self.n_ops = 0
        self.epoch = 0

    def close(self):
        for cm in reversed(self._ctx):
            cm.__exit__(None, None, None)

    def add(self, eng, fn, R=(), W=(), dma=False):
        op = Op(eng, fn, dma, self.epoch)
        self.n_ops += 1
        deps = []
        rset = set(id(b) for b in R)
        for b in R:
            if b.lw is not None:
                deps.append((b.lw, True))
        for b in W:
            if b.lw is not None:
                deps.append((b.lw, id(b) in rset))
            for r in b.rd:
                deps.append((r, False))
        seen = set()
        for d, raw in deps:
            if d is op or d.epoch < self.epoch:
                continue
            key = id(d)
            if d.dma or dma:
                pass
            elif d.eng == eng:
                if eng == "pe" or not raw:
                    continue
            if key in seen:
                continue
            seen.add(key)
            op.deps.append(d)
            d.needs_sig = True
        for b in R:
            b.rd.append(op)
        for b in W:
            b.lw = op
            b.rd = []
        if dma:
            q = eng
            k = self.dnext[q]
            self.dnext[q] = (k + 1) % len(self.dsems[q])
            prev = self.dlast[q][k]
            if prev is not None and prev.epoch == self.epoch:
                op.deps.append(prev)
            self.dcount[q][k] += 16
            op.sem = self.dsems[q][k]
            op.val = self.dcount[q][k]
            self.dlast[q][k] = op
            op.needs_sig = True
            self.all_dma_since_barrier.append(op)
        self.pending[eng].append(op)
        return op

    def barrier(self):
        lasts = []
        for e in ENGS:
            for o in reversed(self.pending[e]):
                if not o.dma and o.fn is not None:
                    lasts.append(o)
                    break
        dmas = list(self.all_dma_since_barrier)
        self.all_dma_since_barrier = []
        for e in ENGS:
            op = Op(e, None, False, self.epoch)
            for d in lasts:
                if d.eng != e:
                    op.deps.append(d)
                    d.needs_sig = True
            for d in dmas:
                op.deps.append(d)
            self.pending[e].append(op)
        self.epoch += 1

    def flush(self):
        nc = self.nc
        self.barrier()
        for e in ENGS:
            for op in self.pending[e]:
                if op.dma or op.fn is None:
                    continue
                if op.needs_sig:
                    self.ecount[e] += 1
                    op.sem = self.esem[e]
                    op.val = self.ecount[e]
        pend = self.pending
        self.pending = {e: [] for e in ENGS}
        known = self.known

        def emit(e, eng):
            kn = known[e]
            for op in pend[e]:
                need = {}
                for d in op.deps:
                    assert d.sem is not None, "dep without signal"
                    sid = id(d.sem)
                    if kn.get(sid, 0) >= d.val:
                        continue
                    if sid not in need or need[sid][1] < d.val:
                        need[sid] = (d.sem, d.val)
                for sid, (sem, val) in need.items():
                    eng.wait_ge(sem, val)
                    kn[sid] = val
                if op.fn is None:
                    continue
                ins = op.fn(eng)
                if op.dma:
                    ins.then_inc(op.sem, 16)
                elif op.needs_sig:
                    ins.then_inc(op.sem, 1)

        with nc.Block() as blk:
            @blk.tensor
            def _(eng):
                emit("pe", eng)

            @blk.scalar
            def _(eng):
                emit("act", eng)

            @blk.vector
            def _(eng):
                emit("dve", eng)

            @blk.gpsimd
            def _(eng):
                emit("pool", eng)

            @blk.sync
            def _(eng):
                emit("sp", eng)


class Ctx:
    pass


def bcast_rows(ap_row, nparts=128):
    return ap_row.partition_broadcast(nparts)


def ln_tile(C, t, lng, lnb, blnp, router=None):
    P, nc = C.P, C.nc
    h, hT = C.h, C.hT
    bh, bhT = C.bh[t], C.bhT[t]
    st, mv, rs, nb = C.ln_st, C.ln_mv, C.ln_rs, C.ln_nb
    b_st = C.b_lnst
    for c in range(2):
        P.add("dve", lambda e, c=c: e.bn_stats(out=st[:, c, :], in_=h[:, t, c * 512:(c + 1) * 512]), R=[bh], W=[b_st])
    P.add("dve", lambda e: e.bn_aggr(out=mv[:], in_=st[:]), R=[b_st], W=[b_st])
    P.add("act", lambda e: e.activation(out=rs[:], in_=mv[:, 1:2], func=AF.Sqrt, bias=EPS, scale=1.0), R=[b_st], W=[b_st])
    P.add("dve", lambda e: e.reciprocal(out=rs[:], in_=rs[:]), R=[b_st], W=[b_st])
    P.add("dve", lambda e: e.tensor_scalar(out=h[:, t, :], in0=h[:, t, :], scalar1=mv[:, 0:1], scalar2=rs[:, 0:1],
                                           op0=ALU.subtract, op1=ALU.mult), R=[bh, b_st], W=[bh])
    P.add("pool", lambda e: e.tensor_tensor(out=h[:, t, :], in0=h[:, t, :], in1=lng[:], op=ALU.mult), R=[bh, blnp], W=[bh])
    P.add("pool", lambda e: e.tensor_tensor(out=h[:, t, :], in0=h[:, t, :], in1=lnb[:], op=ALU.add), R=[bh, blnp], W=[bh])
    transpose_tile(C, t, router)


def transpose_tile(C, t, router=None):
    P = C.P
    h, hT = C.h, C.hT
    bh, bhT = C.bh[t], C.bhT[t]
    for half in range(2):
        ps, bps = C.ps[C.tp_rr % 2], C.bps[C.tp_rr % 2]
        C.tp_rr += 1
        for kk in range(4):
            k = half * 4 + kk
            P.add("pe", lambda e, k=k, kk=kk, ps=ps: e.transpose(out=ps[:, kk * 128:(kk + 1) * 128], in_=h[:, t, k * 128:(k + 1) * 128],
                                                                  identity=C.ident[:]), R=[bh, C.b_const], W=[bps])
        P.add("act", lambda e, ps=ps, half=half: e.copy(out=hT[:, half * 4:half * 4 + 4, t * 128:(t + 1) * 128],
                                                        in_=ps[:].rearrange("p (k c) -> p k c", k=4)), R=[bps], W=[bhT])
        if router is not None:
            P.add("act", lambda e, ps=ps, half=half: e.copy(out=C.hT32[:, half * 4:half * 4 + 4, :],
                                                            in_=ps[:].rearrange("p (k c) -> p k c", k=4)), R=[bps], W=[C.b_hT32])
    if router is not None:
        router_tile(C, t, router)


def router_tile(C, t, R_):
    P = C.P
    ps, bps = C.ps[2], C.bps[2]
    wr, brb, b_r = R_["wr"], R_["brb"], R_["b"]
    lg, m8, ex, msk, sm = C.r_lg, C.r_m8, C.r_ex, C.r_msk, C.r_sm
    b = C.b_rt
    for k in range(8):
        P.add("pe", lambda e, k=k: e.matmul(ps[:, 0:NE], lhsT=C.hT32[:, k, :], rhs=wr[:, k, :], start=(k == 0), stop=(k == 7)),
              R=[C.b_hT32, b_r], W=[bps])
    P.add("dve", lambda e: e.tensor_tensor(out=lg[:], in0=ps[:, 0:NE], in1=brb[:], op=ALU.add), R=[bps, b_r], W=[b])
    P.add("dve", lambda e: e.max(out=m8[:], in_=lg[:]), R=[b], W=[b])
    P.add("dve", lambda e: e.tensor_scalar(out=msk[:], in0=lg[:], scalar1=m8[:, 3:4], scalar2=None, op0=ALU.is_ge), R=[b], W=[b])
    P.add("dve", lambda e: e.tensor_scalar(out=sm[:, 0:1], in0=m8[:, 0:1], scalar1=-1.0, scalar2=None, op0=ALU.mult), R=[b], W=[b])
    P.add("act", lambda e: e.activation(out=ex[:], in_=lg[:], func=AF.Exp, bias=sm[:, 0:1], scale=1.0), R=[b], W=[b])
    P.add("dve", lambda e: e.tensor_tensor(out=ex[:], in0=ex[:], in1=msk[:], op=ALU.mult), R=[b], W=[b])
    P.add("dve", lambda e: e.reduce_sum(out=sm[:, 1:2], in_=ex[:], axis=AX.X), R=[b], W=[b])
    P.add("dve", lambda e: e.reciprocal(out=sm[:, 1:2], in_=sm[:, 1:2]), R=[b], W=[b])
    P.add("dve", lambda e: e.tensor_scalar(out=C.gates[:, t, :], in0=ex[:], scalar1=sm[:, 1:2], scalar2=None, op0=ALU.mult),
          R=[b], W=[C.b_gates[t]])


def load_ln_params(C, lng, lnb, blnp, g_row, b_row):
    P = C.P
    P.add("sp", lambda e: e.dma_start(out=lng[:], in_=bcast_rows(g_row)), W=[blnp], dma=True)
    P.add("sp", lambda e: e.dma_start(out=lnb[:], in_=bcast_rows(b_row)), W=[blnp], dma=True)


def moe_phase(C, li, T):
    P, nc = C.P, C.nc
    h, hT = C.h, C.hT
    hb = hT[:].rearrange("p k s -> p (k s)").rearrange("p (t d) -> p t d", t=NT)
    BANKS = (0, 1, 7, 2)
    with ExitStack() as es:
        w1g = es.enter_context(nc.sbuf_tensor("m_w1g_%d" % li, [128, 2, 8, 512], BF16))
        w1u = es.enter_context(nc.sbuf_tensor("m_w1u_%d" % li, [128, 2, 8, 512], BF16))
        w2 = es.enter_context(nc.sbuf_tensor("m_w2_%d" % li, [128, 2, 4, 1024], BF16))
        actT = es.enter_context(nc.sbuf_tensor("m_actT_%d" % li, [128, 2, 4, 512], BF16))
        g1 = es.enter_context(nc.sbuf_tensor("m_g1_%d" % li, [128, 2, 512], F32))
        sg = es.enter_context(nc.sbuf_tensor("m_sg_%d" % li, [128, 2, 512], F32))
        u1 = es.enter_context(nc.sbuf_tensor("m_u1_%d" % li, [128, 2, 512], F32))
        braw = es.enter_context(nc.sbuf_tensor("m_braw_%d" % li, [128, 4, 128], F32))
        bguT = es.enter_context(nc.sbuf_tensor("m_bguT_%d" % li, [128, 512], F32))
        bd = es.enter_context(nc.sbuf_tensor("m_bd_%d" % li, [NE, D], F32))
        gT = es.enter_context(nc.sbuf_tensor("m_gT_%d" % li, [NE, 128], F32))
        xgT = es.enter_context(nc.sbuf_tensor("m_xgT_%d" % li, [128, 8, 512], BF16))
        Pm = es.enter_context(nc.sbuf_tensor("m_P_%d" % li, [128, NT, 128], BF16))
        PTm = es.enter_context(nc.sbuf_tensor("m_PT_%d" % li, [128, 4, 512], BF16))
        ys = es.enter_context(nc.sbuf_tensor("m_ys_%d" % li, [128, 2, 1024], BF16))
        mask = es.enter_context(nc.sbuf_tensor("m_mask_%d" % li, [128, NT, NE], BF16))
        posm = es.enter_context(nc.sbuf_tensor("m_pos_%d" % li, [128, NT, NE], F32))
        iota = es.enter_context(nc.sbuf_tensor("m_iota_%d" % li, [128, 128], F32))
        onesb = es.enter_context(nc.sbuf_tensor("m_ones_%d" % li, [128, 128], BF16))
        tris = es.enter_context(nc.sbuf_tensor("m_tris_%d" % li, [128, 128], BF16))
        b_w1 = [Buf("w1_%d" % i) for i in range(2)]
        b_w2 = [Buf("w2_%d" % i) for i in range(2)]
        b_act = [Buf() for _ in range(2)]
        b_g1 = [Buf() for _ in range(2)]
        b_sg = [Buf() for _ in range(2)]
        b_u1 = [Buf() for _ in range(2)]
        b_ys = [Buf() for _ in range(2)]
        b_misc, b_gT, b_cst, b_mask, b_pos, b_xg, b_P, b_PT = Buf(), Buf(), Buf(), Buf(), Buf(), Buf(), Buf(), Buf()
        allhT = list(C.bhT)
        P.add("sp", lambda e: e.dma_start(out=iota[:], in_=T["iota"]), W=[b_cst], dma=True)
        P.add("sp", lambda e: e.dma_start(out=onesb[:], in_=T["ones_b"]), W=[b_cst], dma=True)
        P.add("sp", lambda e: e.dma_start(out=tris[:], in_=T["tris_b"]), W=[b_cst], dma=True)
        P.add("sp", lambda e: e.dma_start(out=braw[:], in_=T["moe_b_gate_up_%d" % li].rearrange("e (c p) -> (e c) p", p=128)
                                          .rearrange("(b r) p -> r b p", r=128)), W=[b_misc], dma=True)
        P.add("sp", lambda e: e.dma_start(out=bd[:], in_=T["moe_b_down_%d" % li]), W=[b_misc], dma=True)
        for bb in range(4):
            ps, bps = C.ps[bb % 2], C.bps[bb % 2]
            P.add("pe", lambda e, bb=bb, ps=ps: e.transpose(out=ps[:, 0:128], in_=braw[:, bb, :], identity=C.ident[:]),
                  R=[b_misc, C.b_const], W=[bps])
            P.add("act", lambda e, bb=bb, ps=ps: e.copy(out=bguT[:, bb * 128:(bb + 1) * 128], in_=ps[:, 0:128]), R=[bps], W=[b_misc])
        bg3 = bguT[:].rearrange("p (e c) -> p e c", c=16)
        P.add("dve", lambda e: e.tensor_scalar(out=bg3[:, :, 8:16], in0=bg3[:, :, 8:16], scalar1=1.0, scalar2=None, op0=ALU.add), R=[b_misc], W=[b_misc])
        for t in range(NT):
            P.add("act", lambda e, t=t: e.copy(out=hb[:, t, :], in_=h[:, t, :]), R=[C.bh[t]], W=allhT)
        for t in range(NT):
            ps, bps = C.ps[2], C.bps[2]
            P.add("pe", lambda e, t=t: e.transpose(out=ps[0:NE, 0:128], in_=C.gates[:, t, :], identity=C.ident[:]),
                  R=[C.b_gates[t], C.b_const], W=[bps])
            P.add("act", lambda e: e.copy(out=gT[:], in_=ps[0:NE, 0:128]), R=[bps], W=[b_gT])
            for n in range(2):
                po, bpo = C.ps[n], C.bps[n]
                P.add("pe", lambda e, n=n, po=po: e.matmul(po[:], lhsT=gT[:], rhs=bd[:, n * 512:(n + 1) * 512], start=True, stop=True),
                      R=[b_gT, b_misc], W=[bpo])
                P.add("dve", lambda e, n=n, po=po, t=t: e.scalar_tensor_tensor(out=h[:, t, n * 512:(n + 1) * 512], in0=h[:, t, n * 512:(n + 1) * 512],
                                                                               scalar=ALPHA, in1=po[:], op0=ALU.mult, op1=ALU.add),
                      R=[bpo, C.bh[t]], W=[C.bh[t]])
        P.add("dve", lambda e: e.tensor_scalar(out=mask[:], in0=C.gates[:], scalar1=0.0, scalar2=None, op0=ALU.is_gt), R=list(C.b_gates), W=[b_mask])
        pp, bpp = C.ps[2], C.bps[2]
        for t in range(NT):
            gi, i = divmod(t, 4)
            for i2 in range(i):
                P.add("pe", lambda e, t=t, gi=gi, i2=i2: e.matmul(pp[:, t * NE:(t + 1) * NE], lhsT=onesb[:], rhs=mask[:, 4 * gi + i2, :],
                                                                start=(i2 == 0), stop=False), R=[b_mask, b_cst], W=[bpp])
            P.add("pe", lambda e, t=t, i=i: e.matmul(pp[:, t * NE:(t + 1) * NE], lhsT=tris[:], rhs=mask[:, t, :], start=(i == 0), stop=True),
                  R=[b_mask, b_cst], W=[bpp])
        P.add("dve", lambda e: e.scalar_tensor_tensor(out=posm[:], in0=pp[:].rearrange("p (t x) -> p t x", t=NT), scalar=1.0, in1=mask[:],
                                                      op0=ALU.add, op1=ALU.mult), R=[bpp, b_mask], W=[b_pos])
        P.add("dve", lambda e: e.tensor_scalar(out=posm[:], in0=posm[:], scalar1=-1.0, scalar2=None, op0=ALU.add), R=[b_pos], W=[b_pos])

        wgu = T["moe_w_gate_up_%d" % li]
        wd = T["moe_w_down_%d" % li]

        def bank():
            y = C.y_rr % 4
            C.y_rr += 1
            return C.ps[BANKS[y]], C.bps[BANKS[y]]

        def p_build(ex):
            for t in range(NT):
                P.add("dve", lambda e, t=t: e.tensor_scalar(out=Pm[:, t, :], in0=iota[:], scalar1=posm[:, t, ex:ex + 1], scalar2=None, op0=ALU.is_equal),
                      R=[b_pos, b_cst], W=[b_P])

        def gather_unit(ex):
            for gi in range(4):
                for kh in range(2):
                    px, bpx = bank()
                    for kk in range(4):
                        k = kh * 4 + kk
                        for i in range(4):
                            t = 4 * gi + i
                            P.add("pe", lambda e, kk=kk, k=k, t=t, i=i, px=px: e.matmul(px[:, kk * 128:(kk + 1) * 128], lhsT=hb[:, t, k * 128:(k + 1) * 128],
                                                                                      rhs=Pm[:, t, :], start=(i == 0), stop=(i == 3)), R=[b_P] + allhT, W=[bpx])
                    P.add("act", lambda e, kh=kh, gi=gi, px=px: e.copy(out=xgT[:, kh * 4:kh * 4 + 4, gi * 128:(gi + 1) * 128],
                                                                      in_=px[:].rearrange("p (a c) -> p a c", a=4)), R=[bpx], W=[b_xg])

        def pt_unit(ex):
            for gi in range(4):
                ptp, bptp = bank()
                ptb = ptp[:].bitcast(BF16)
                for i in range(4):
                    P.add("pe", lambda e, i=i, gi=gi, ptb=ptb: e.transpose(out=ptb[:, i * 128:(i + 1) * 128], in_=Pm[:, 4 * gi + i, :], identity=C.identb[:]),
                          R=[b_P, C.b_const], W=[bptp])
                P.add("act", lambda e, gi=gi, ptb=ptb: e.copy(out=PTm[:, gi, :], in_=ptb[:, 0:512]), R=[bptp], W=[b_PT])

        def w1_unit(ex, hf, s, a):
            bw = b_w1[s]
            for j in range(4):
                q = C.q_rr % 2
                C.q_rr += 1
                pg, bpg = C.ps[3 + q], C.bps[3 + q]
                pu, bpu = C.ps[5 + q], C.bps[5 + q]
                for k in range(8):
                    P.add("pe", lambda e, k=k, j=j, pg=pg: e.matmul(pg[:], lhsT=w1g[:, s, k, j * 128:(j + 1) * 128], rhs=xgT[:, k, :],
                                                                  start=(k == 0), stop=(k == 7)), R=[bw, b_xg], W=[bpg])
                for k in range(8):
                    P.add("pe", lambda e, k=k, j=j, pu=pu: e.matmul(pu[:], lhsT=w1u[:, s, k, j * 128:(j + 1) * 128], rhs=xgT[:, k, :],
                                                                  start=(k == 0), stop=(k == 7)), R=[bw, b_xg], W=[bpu])
                cg = ex * 16 + hf * 4 + j
                cu = ex * 16 + 8 + hf * 4 + j
                P.add("dve", lambda e, q=q, pg=pg, cg=cg: e.tensor_scalar(out=g1[:, q, :], in0=pg[:], scalar1=bguT[:, cg:cg + 1], scalar2=7.0,
                                                                         op0=ALU.add, op1=ALU.min), R=[bpg, b_misc], W=[b_g1[q]])
                P.add("act", lambda e, q=q: e.activation(out=sg[:, q, :], in_=g1[:, q, :], func=AF.Sigmoid, scale=1.702), R=[b_g1[q]], W=[b_sg[q]])
                P.add("dve", lambda e, q=q, pu=pu, cu=cu: e.tensor_scalar(out=u1[:, q, :], in0=pu[:], scalar1=bguT[:, cu:cu + 1], scalar2=8.0,
                                                                         op0=ALU.add, op1=ALU.min), R=[bpu, b_misc], W=[b_u1[q]])
                P.add("dve", lambda e, q=q: e.tensor_tensor(out=sg[:, q, :], in0=sg[:, q, :], in1=g1[:, q, :], op=ALU.mult),
                      R=[b_sg[q], b_g1[q]], W=[b_sg[q]])
                P.add("dve", lambda e, q=q, j=j: e.scalar_tensor_tensor(out=actT[:, a, j, :], in0=u1[:, q, :], scalar=-6.0, in1=sg[:, q, :],
                                                                       op0=ALU.max, op1=ALU.mult), R=[b_sg[q], b_u1[q]], W=[b_act[a]])

        def w2_unit(ex):
            ybs = {}

            def down(tt):
                yb = C.ys_rr % 2
                C.ys_rr += 1
                ybs[tt] = yb
                for n in range(2):
                    py, bpy = bank()
                    for hf in range(2):
                        for j in range(4):
                            P.add("pe", lambda e, j=j, n=n, hf=hf, py=py: e.matmul(py[:], lhsT=actT[:, hf, j, tt * 128:(tt + 1) * 128],
                                                                                 rhs=w2[:, hf, j, n * 512:(n + 1) * 512],
                                                                                 start=(hf == 0 and j == 0), stop=(hf == 1 and j == 3)),
                                  R=[b_act[hf], b_w2[hf]], W=[bpy])
                    P.add("act", lambda e, n=n, yb=yb, py=py: e.copy(out=ys[:, yb, n * 512:(n + 1) * 512], in_=py[:]), R=[bpy], W=[b_ys[yb]])

            def scatter(tt):
                yb = ybs[tt]
                for i in range(4):
                    t = 4 * tt + i
                    for n in range(2):
                        pz, bpz = bank()
                        P.add("pe", lambda e, i=i, n=n, pz=pz: e.matmul(pz[:], lhsT=PTm[:, tt, i * 128:(i + 1) * 128],
                                                                      rhs=ys[:, yb, n * 512:(n + 1) * 512], start=True, stop=True),
                              R=[b_PT, b_ys[yb]], W=[bpz])
                        P.add("dve", lambda e, t=t, n=n, pz=pz: e.scalar_tensor_tensor(
                            out=h[:, t, n * 512:(n + 1) * 512], in0=pz[:], scalar=C.gates[:, t, ex:ex + 1], in1=h[:, t, n * 512:(n + 1) * 512],
                            op0=ALU.mult, op1=ALU.add), R=[bpz, C.b_gates[t], C.bh[t]], W=[C.bh[t]])

            down(0)
            down(1)
            scatter(0)
            down(2)
            scatter(1)
            down(3)
            scatter(2)
            scatter(3)

        def dma_w1(ex, hf):
            P.add("pool", lambda e: e.dma_start(
                out=w1g[:, hf], in_=wgu[ex, :, hf * 512:(hf + 1) * 512].rearrange("(k p) n -> p k n", p=128)), W=[b_w1[hf]], dma=True)
            P.add("pool", lambda e: e.dma_start(
                out=w1u[:, hf], in_=wgu[ex, :, 1024 + hf * 512:1024 + (hf + 1) * 512].rearrange("(k p) n -> p k n", p=128)), W=[b_w1[hf]], dma=True)

        def dma_w2(ex, hf):
            P.add("pool", lambda e: e.dma_start(
                out=w2[:, hf], in_=wd[ex, hf * 512:(hf + 1) * 512, :].rearrange("(j p) n -> p j n", p=128)), W=[b_w2[hf]], dma=True)

        NX = C.n_experts
        if NX > 0:
            dma_w1(0, 0)
            dma_w1(0, 1)
            dma_w2(0, 0)
            dma_w2(0, 1)
            p_build(0)
            gather_unit(0)
        for ex in range(NX):
            nxt = ex + 1 < NX
            w1_unit(ex, 0, 0, 0)
            if nxt:
                dma_w1(ex + 1, 0)
            pt_unit(ex)
            if nxt:
                p_build(ex + 1)
            w1_unit(ex, 1, 1, 1)
            if nxt:
                dma_w1(ex + 1, 1)
                gather_unit(ex + 1)
            w2_unit(ex)
            if nxt:
                dma_w2(ex + 1, 0)
                dma_w2(ex + 1, 1)
        P.flush()
    with ExitStack() as es:
        lng = es.enter_context(nc.sbuf_tensor("m_lng_%d" % li, [128, D], F32))
        lnb = es.enter_context(nc.sbuf_tensor("m_lnb_%d" % li, [128, D], F32))
        blnp = Buf()
        load_ln_params(C, lng, lnb, blnp, T["ln_g"][li, 1, :], T["ln_b"][li, 1, :])
        for t in range(NT):
            ln_tile(C, t, lng, lnb, blnp, router=None)
        P.flush()


def out_proj_ln(C, li, T, srcT, nchunks, w_dram, R_):
    P, nc = C.P, C.nc
    h = C.h
    with ExitStack() as es:
        wo = es.enter_context(nc.sbuf_tensor("o_w_%d" % li, [128, nchunks, D], BF16))
        lng = es.enter_context(nc.sbuf_tensor("o_lng_%d" % li, [128, D], F32))
        lnb = es.enter_context(nc.sbuf_tensor("o_lnb_%d" % li, [128, D], F32))
        bw, blnp = Buf(), Buf()
        P.add("pool", lambda e: e.dma_start(out=wo[:], in_=w_dram.rearrange("(k p) n -> p k n", p=128)), W=[bw], dma=True)
        load_ln_params(C, lng, lnb, blnp, T["ln_g"][li, 0, :], T["ln_b"][li, 0, :])
        for t in range(NT):
            for n in range(2):
                po, bpo = C.ps[3 + n + 2 * (t % 2)], C.bps[3 + n + 2 * (t % 2)]
                for f in range(nchunks):
                    P.add("pe", lambda e, f=f, n=n, t=t, po=po: e.matmul(po[:], lhsT=srcT[:, f, t * 128:(t + 1) * 128], rhs=wo[:, f, n * 512:(n + 1) * 512],
                                                                      start=(f == 0), stop=(f == nchunks - 1)), R=[C.b_srcT, bw], W=[bpo])
                P.add("dve", lambda e, n=n, t=t, po=po: e.scalar_tensor_tensor(out=h[:, t, n * 512:(n + 1) * 512], in0=h[:, t, n * 512:(n + 1) * 512],
                                                                             scalar=ALPHA, in1=po[:], op0=ALU.mult, op1=ALU.add),
                      R=[bpo, C.bh[t]], W=[C.bh[t]])
            ln_tile(C, t, lng, lnb, blnp, router=R_)
        P.flush()


def moba_phase(C, li, T, R_):
    P, nc = C.P, C.nc
    h, hT = C.h, C.hT
    j = li // 3
    w_in = T["a_w_in_%d" % j]
    H = 8
    SCALE = 128 ** -0.5
    BIG = 30000.0
    with nc.sbuf_tensor("a_aT_%d" % li, [128, 8, S], BF16) as aT:
        C.b_srcT = Buf("aT")
        with ExitStack() as es:
            wq = es.enter_context(nc.sbuf_tensor("a_wq_%d" % li, [128, 2, 8, 384], BF16))
            qT = es.enter_context(nc.sbuf_tensor("a_qT_%d" % li, [128, 2, S], BF16))
            kT = es.enter_context(nc.sbuf_tensor("a_kT_%d" % li, [128, 2, S], BF16))
            V = es.enter_context(nc.sbuf_tensor("a_V_%d" % li, [128, 2, NT, 128], BF16))
            kms = es.enter_context(nc.sbuf_tensor("a_kms_%d" % li, [128, 8], F32))
            kmb = es.enter_context(nc.sbuf_tensor("a_kmb_%d" % li, [128, 8], BF16))
            g2 = es.enter_context(nc.sbuf_tensor("a_g2_%d" % li, [128, 128], F32))
            m8 = es.enter_context(nc.sbuf_tensor("a_m8_%d" % li, [128, 8], F32))
            bm = es.enter_context(nc.sbuf_tensor("a_bm_%d" % li, [128, 128], F32))
            cm = es.enter_context(nc.sbuf_tensor("a_cm_%d" % li, [128, 128], F32))
            vm = es.enter_context(nc.sbuf_tensor("a_vm_%d" % li, [128, 128], F32))
            vm2 = es.enter_context(nc.sbuf_tensor("a_vm2_%d" % li, [128, 128], F32))
            rel = es.enter_context(nc.sbuf_tensor("a_rel_%d" % li, [128, 8, 2, 128], F32))
            caus = es.enter_context(nc.sbuf_tensor("a_caus_%d" % li, [128, 128], F32))
            cb = es.enter_context(nc.sbuf_tensor("a_cb_%d" % li, [128, 8], F32))
            sc = es.enter_context(nc.sbuf_tensor("a_s_%d" % li, [128, 2, S], F32))
            pb = es.enter_context(nc.sbuf_tensor("a_p_%d" % li, [128, S], BF16))
            pT = es.enter_context(nc.sbuf_tensor("a_pT_%d" % li, [128, 2, 4, 128], BF16))
            stt = es.enter_context(nc.sbuf_tensor("a_st_%d" % li, [128, 2, 4], F32))
            b_wq = [Buf() for _ in range(2)]
            b_q = [Buf() for _ in range(2)]
            b_k = [Buf() for _ in range(2)]
            b_v = [Buf() for _ in range(2)]
            b_km, b_gate, b_cst = Buf(), Buf(), Buf()
            b_s = [Buf() for _ in range(2)]
            b_p = [Buf()] * 2
            b_pT = [Buf() for _ in range(2)]
            b_st = [Buf() for _ in range(2)]
            SK = ""
            if "rel" not in SK:
                P.add("sp", lambda e: e.dma_start(out=rel[:], in_=T["relT"].rearrange("h d i j -> i h d j")), W=[b_cst], dma=True)
            P.add("sp", lambda e: e.dma_start(out=caus[:], in_=T["causal"]), W=[b_cst], dma=True)
            P.add("sp", lambda e: e.dma_start(out=vm[:], in_=T["vmask"]), W=[b_cst], dma=True)
            P.add("sp", lambda e: e.dma_start(out=vm2[:], in_=T["vmask2"]), W=[b_cst], dma=True)
            if "cb" not in SK:
                P.add("sp", lambda e: e.dma_start(out=cb[:], in_=bcast_rows(T["rel_bias"][31, :])), W=[b_cst], dma=True)
            for hh in range(H):
                P.add("pool", lambda e, hh=hh: e.tensor_tensor(out=rel[:, hh, 0, :], in0=rel[:, hh, 0, :], in1=caus[:], op=ALU.add), R=[b_cst], W=[b_cst])
            allT = list(C.bhT)
            for hh in range(H):
                s = hh % 2
                for c3 in range(3):
                    P.add("pool", lambda e, s=s, c3=c3, hh=hh: e.dma_start(
                        out=wq[:, s, :, c3 * 128:(c3 + 1) * 128],
                        in_=w_in[:, c3 * 1024 + hh * 128:c3 * 1024 + (hh + 1) * 128].rearrange("(k p) n -> p k n", p=128)), W=[b_wq[s]], dma=True)
                for g in range(4):
                    pq, bpq = C.ps[3], C.bps[3]
                    pk, bpk = C.ps[4], C.bps[4]
                    for k in range(8):
                        P.add("pe", lambda e, k=k, g=g, s=s: e.matmul(pq[:], lhsT=wq[:, s, k, 0:128], rhs=hT[:, k, g * 512:(g + 1) * 512],
                                                                    start=(k == 0), stop=(k == 7)), R=[b_wq[s]] + allT[g * 4:g * 4 + 4], W=[bpq])
                    P.add("act", lambda e, g=g, s=s: e.copy(out=qT[:, s, g * 512:(g + 1) * 512], in_=pq[:]), R=[bpq], W=[b_q[s]])
                    for k in range(8):
                        P.add("pe", lambda e, k=k, g=g, s=s: e.matmul(pk[:], lhsT=wq[:, s, k, 128:256], rhs=hT[:, k, g * 512:(g + 1) * 512],
                                                                    start=(k == 0), stop=(k == 7)), R=[b_wq[s]] + allT[g * 4:g * 4 + 4], W=[bpk])
                    for b2 in range(2):
                        P.add("act", lambda e, g=g, s=s, b2=b2: e.activation(out=kT[:, s, g * 512 + b2 * 256:g * 512 + (b2 + 1) * 256],
                                                                           in_=pk[:, b2 * 256:(b2 + 1) * 256], func=AF.Copy,
                                                                           accum_out=kms[:, 2 * g + b2:2 * g + b2 + 1]), R=[bpk], W=[b_k[s], b_km])
                P.add("dve", lambda e: e.tensor_scalar(out=kmb[:], in0=kms[:], scalar1=1.0 / 256.0, scalar2=None, op0=ALU.mult), R=[b_km], W=[b_km])
                for g in range(4):
                    pv, bpv = C.ps[5], C.bps[5]
                    for tt in range(4):
                        t = g * 4 + tt
                        for k in range(8):
                            P.add("pe", lambda e, k=k, t=t, tt=tt, s=s: e.matmul(pv[:, tt * 128:(tt + 1) * 128], lhsT=hT[:, k, t * 128:(t + 1) * 128],
                                                                               rhs=wq[:, s, k, 256:384], start=(k == 0), stop=(k == 7)),
                                  R=[b_wq[s], C.bhT[t]], W=[bpv])
                    P.add("act", lambda e, g=g, s=s: e.copy(out=V[:, s, g * 4:g * 4 + 4, :], in_=pv[:].rearrange("p (a c) -> p a c", a=4)),
                          R=[bpv], W=[b_v[s]])
                MD = 9
                if MD < 2:
                    continue
                pg, bpg = C.ps[6], C.bps[6]
                for qt in range(NT):
                    P.add("pe", lambda e, qt=qt, s=s: e.matmul(pg[:, qt * 8:(qt + 1) * 8], lhsT=qT[:, s, qt * 128:(qt + 1) * 128], rhs=kmb[:],
                                                             start=True, stop=True), R=[b_q[s], b_km], W=[bpg])
                P.add("dve", lambda e: e.tensor_tensor(out=g2[:], in0=pg[:, 0:128], in1=vm[:], op=ALU.add), R=[bpg, b_cst], W=[b_gate])
                for qt in range(NT):
                    P.add("dve", lambda e, qt=qt: e.max(out=m8[:], in_=g2[:, qt * 8:(qt + 1) * 8]), R=[b_gate], W=[b_gate])
                    P.add("dve", lambda e, qt=qt: e.tensor_scalar(out=bm[:, qt * 8:(qt + 1) * 8], in0=g2[:, qt * 8:(qt + 1) * 8], scalar1=m8[:, 2:3],
                                                                  scalar2=BIG, op0=ALU.is_ge, op1=ALU.mult), R=[b_gate], W=[b_gate])
                P.add("dve", lambda e: e.tensor_tensor(out=bm[:], in0=bm[:], in1=vm2[:], op=ALU.add), R=[b_gate, b_cst], W=[b_gate])
                P.add("dve", lambda e, hh=hh: e.tensor_scalar(out=cm[:], in0=bm[:], scalar1=cb[:, hh:hh + 1], scalar2=None, op0=ALU.add),
                      R=[b_gate, b_cst], W=[b_gate])
                for qt in range(NT if MD >= 3 else 0):
                    qb = qt // 2
                    nk = qt + 1
                    z = qt % 2
                    for c in range((nk + 3) // 4):
                        w = min(4, nk - c * 4)
                        pss, bpss = C.ps[3 + (C.q_rr % 2)], C.bps[3 + (C.q_rr % 2)]
                        C.q_rr += 1
                        P.add("pe", lambda e, c=c, w=w, qt=qt, s=s, pss=pss: e.matmul(pss[:, 0:w * 128], lhsT=qT[:, s, qt * 128:(qt + 1) * 128],
                                                                                   rhs=kT[:, s, c * 512:c * 512 + w * 128], start=True, stop=True),
                              R=[b_q[s], b_k[s]], W=[bpss])
                        for i in range(w):
                            kt = c * 4 + i
                            n = kt // 2
                            src = pss[:, i * 128:(i + 1) * 128]
                            dst = sc[:, z, kt * 128:(kt + 1) * 128]
                            if kt == qt:
                                P.add("dve", lambda e, src=src, dst=dst, hh=hh: e.scalar_tensor_tensor(out=dst, in0=src, scalar=SCALE, in1=rel[:, hh, 0, :],
                                                                                                     op0=ALU.mult, op1=ALU.add), R=[bpss, b_cst], W=[b_s[z]])
                            elif kt == qt - 1:
                                P.add("dve", lambda e, src=src, dst=dst, hh=hh: e.scalar_tensor_tensor(out=dst, in0=src, scalar=SCALE, in1=rel[:, hh, 1, :],
                                                                                                     op0=ALU.mult, op1=ALU.add), R=[bpss, b_cst], W=[b_s[z]])
                                if n < qb:
                                    P.add("dve", lambda e, dst=dst, qt=qt, n=n: e.tensor_scalar(out=dst, in0=dst, scalar1=bm[:, qt * 8 + n:qt * 8 + n + 1],
                                                                                              scalar2=None, op0=ALU.add), R=[b_s[z], b_gate], W=[b_s[z]])
                            else:
                                P.add("dve", lambda e, src=src, dst=dst, qt=qt, n=n: e.tensor_scalar(out=dst, in0=src, scalar1=SCALE,
                                                                                                   scalar2=cm[:, qt * 8 + n:qt * 8 + n + 1],
                                                                                                   op0=ALU.mult, op1=ALU.add), R=[bpss, b_gate], W=[b_s[z]])
                    L = nk * 128
                    if MD < 4:
                        continue
                    P.add("dve", lambda e, z=z, L=L: e.reduce_max(out=stt[:, z, 0:1], in_=sc[:, z, 0:L], axis=AX.X), R=[b_s[z]], W=[b_st[z]])
                    P.add("dve", lambda e, z=z: e.tensor_scalar(out=stt[:, z, 1:2], in0=stt[:, z, 0:1], scalar1=-1.0, scalar2=None, op0=ALU.mult),
                          R=[b_st[z]], W=[b_st[z]])
                    P.add("act", lambda e, z=z, L=L: e.activation(out=sc[:, z, 0:L], in_=sc[:, z, 0:L], func=AF.Exp, bias=stt[:, z, 1:2], scale=1.0,
                                                                  accum_out=stt[:, z, 2:3]), R=[b_s[z], b_st[z]], W=[b_s[z], b_st[z]])
                    P.add("dve", lambda e, z=z: e.reciprocal(out=stt[:, z, 3:4], in_=stt[:, z, 2:3]), R=[b_st[z]], W=[b_st[z]])
                    P.add("act", lambda e, z=z, L=L: e.activation(out=pb[:, 0:L], in_=sc[:, z, 0:L], func=AF.Copy, scale=stt[:, z, 3:4]),
                          R=[b_s[z], b_st[z]], W=[b_p[z]])
                    po, bpo = C.ps[5 + z], C.bps[5 + z]
                    if MD < 5:
                        continue
                    for c in range((nk + 3) // 4):
                        w = min(4, nk - c * 4)
                        y = C.y_rr % 2
                        C.y_rr += 1
                        ptp, bptp = C.ps[(0, 1)[y]], C.bps[(0, 1)[y]]
                        ptb = ptp[:].bitcast(BF16)
                        for i in range(w):
                            kt = c * 4 + i
                            P.add("pe", lambda e, i=i, kt=kt, z=z, ptb=ptb: e.transpose(out=ptb[:, i * 128:(i + 1) * 128], in_=pb[:, kt * 128:(kt + 1) * 128],
                                                                                      identity=C.identb[:]), R=[b_p[z], C.b_const], W=[bptp])
                        P.add("act", lambda e, w=w, y=y, ptb=ptb: e.copy(out=pT[:, y, 0:w, :], in_=ptb[:, 0:w * 128].rearrange("p (a c) -> p a c", a=w)),
                              R=[bptp], W=[b_pT[y]])
                        for i in range(w):
                            kt = c * 4 + i
                            P.add("pe", lambda e, i=i, kt=kt, y=y, s=s, qt=qt, nk=nk, po=po: e.matmul(po[:, 0:128], lhsT=V[:, s, kt, :], rhs=pT[:, y, i, :],
                                                                                                   start=(kt == 0), stop=(kt == nk - 1)),
                                  R=[b_v[s], b_pT[y]], W=[bpo])
                    P.add("act", lambda e, po=po, hh=hh, qt=qt: e.copy(out=aT[:, hh, qt * 128:(qt + 1) * 128], in_=po[:, 0:128]), R=[bpo], W=[C.b_srcT])
            P.flush()
        out_proj_ln(C, li, T, aT, 8, T["a_w_out_%d" % j], R_)


def gmlp_phase(C, li, T, R_):
    P, nc = C.P, C.nc
    h, hT = C.h, C.hT
    j = li // 3
    w_in = T["b_w_in_%d" % j]
    w_out = T["b_w_out_%d" % j]
    with ExitStack() as es:
        lng = es.enter_context(nc.sbuf_tensor("g_lng_%d" % li, [128, D], F32))
        lnb = es.enter_context(nc.sbuf_tensor("g_lnb_%d" % li, [128, D], F32))
        wi = es.enter_context(nc.sbuf_tensor("g_wi_%d" % li, [128, 2, 8, 512], BF16))
        wo = es.enter_context(nc.sbuf_tensor("g_wo_%d" % li, [128, 2, 4, 1024], BF16))
        yT = es.enter_context(nc.sbuf_tensor("g_yT_%d" % li, [128, 16, 256], BF16))
        v = es.enter_context(nc.sbuf_tensor("g_v_%d" % li, [128, 2, 2048], F32))
        vn = es.enter_context(nc.sbuf_tensor("g_vn_%d" % li, [128, 2, 2048], BF16))
        gb = es.enter_context(nc.sbuf_tensor("g_gb_%d" % li, [128, 2, 2048], F32))
        ws = es.enter_context(nc.sbuf_tensor("g_ws_%d" % li, [128, 8, 128], F32))
        wsT = es.enter_context(nc.sbuf_tensor("g_wsT_%d" % li, [128, 8, 128], BF16))
        tril = es.enter_context(nc.sbuf_tensor("g_tril_%d" % li, [128, 128], F32))
        bs = es.enter_context(nc.sbuf_tensor("g_bs_%d" % li, [1, 1024], F32))
        ones = es.enter_context(nc.sbuf_tensor("g_ones_%d" % li, [1, 128], F32))
        st = es.enter_context(nc.sbuf_tensor("g_st_%d" % li, [128, 4, 6], F32))
        mv = es.enter_context(nc.sbuf_tensor("g_mv_%d" % li, [128, 2], F32))
        rs = es.enter_context(nc.sbuf_tensor("g_rs_%d" % li, [128, 1], F32))
        blnp, b_cst, b_wsT, b_yT, b_st = Buf(), Buf(), Buf(), Buf(), Buf()
        b_wi = [Buf(), Buf()]
        b_wo = [Buf(), Buf()]
        b_v = [Buf(), Buf()]
        b_vn = [Buf(), Buf()]
        load_ln_params(C, lng, lnb, blnp, T["ln_g"][li, 0, :], T["ln_b"][li, 0, :])
        P.add("sp", lambda e: e.dma_start(out=gb[:, 0, :], in_=bcast_rows(T["b_ln_g_%d" % j])), W=[b_cst], dma=True)
        P.add("sp", lambda e: e.dma_start(out=gb[:, 1, :], in_=bcast_rows(T["b_ln_b_%d" % j])), W=[b_cst], dma=True)
        P.add("sp", lambda e: e.dma_start(out=ws[:], in_=T["b_w_s_%d" % j].rearrange("g t s -> t g s")), W=[b_cst], dma=True)
        P.add("sp", lambda e: e.dma_start(out=tril[:], in_=T["tril"]), W=[b_cst], dma=True)
        P.add("sp", lambda e: e.dma_start(out=bs[:], in_=T["b_b_s_%d" % j].rearrange("(o g) t -> o (g t)", o=1)), W=[b_cst], dma=True)
        P.add("pool", lambda e: e.memset(ones[:], 1.0), W=[b_cst])
        for g in range(8):
            P.add("dve", lambda e, g=g: e.tensor_tensor(out=ws[:, g, :], in0=ws[:, g, :], in1=tril[:], op=ALU.mult), R=[b_cst], W=[b_cst])
        for half in range(2):
            ps, bps = C.ps[half], C.bps[half]
            for gg in range(4):
                g = half * 4 + gg
                P.add("pe", lambda e, g=g, gg=gg, ps=ps: e.transpose(out=ps[:, gg * 128:(gg + 1) * 128], in_=ws[:, g, :], identity=C.ident[:]),
                      R=[b_cst, C.b_const], W=[bps])
            P.add("act", lambda e, ps=ps, half=half: e.copy(out=wsT[:, half * 4:half * 4 + 4, :], in_=ps[:].rearrange("p (a c) -> p a c", a=4)),
                  R=[bps], W=[b_wsT])
        it = 0
        iw = 0
        acc_banks = (5, 6, 7, 2)
        DBG = 9
        for tg in range(8 if DBG >= 2 else 0):
            c0 = tg * 256
            tiles = (2 * tg, 2 * tg + 1)
            bhTg = [C.bhT[t] for t in tiles]
            for cg in range(8):
                s = it % 2
                it += 1
                P.add("pool", lambda e, s=s, cg=cg: e.dma_start(out=wi[:, s], in_=w_in[:, cg * 512:(cg + 1) * 512].rearrange("(k p) n -> p k n", p=128)),
                      W=[b_wi[s]], dma=True)
                if cg < 4:
                    for jj in range(4):
                        fc = cg * 4 + jj
                        q = C.q_rr % 2
                        C.q_rr += 1
                        pu, bpu = C.ps[3 + q], C.bps[3 + q]
                        for k in range(8):
                            P.add("pe", lambda e, k=k, jj=jj, s=s, pu=pu, c0=c0: e.matmul(pu[:, 0:256], lhsT=wi[:, s, k, jj * 128:(jj + 1) * 128],
                                                                                      rhs=hT[:, k, c0:c0 + 256], start=(k == 0), stop=(k == 7)),
                                  R=[b_wi[s]] + bhTg, W=[bpu])
                        P.add("act", lambda e, fc=fc, pu=pu: e.activation(out=yT[:, fc, :], in_=pu[:, 0:256], func=AF.Gelu), R=[bpu], W=[b_yT])
                else:
                    for ti in range(2):
                        t = tiles[ti]
                        q = C.q_rr % 2
                        C.q_rr += 1
                        pv, bpv = C.ps[3 + q], C.bps[3 + q]
                        for k in range(8):
                            P.add("pe", lambda e, k=k, t=t, s=s, pv=pv: e.matmul(pv[:], lhsT=hT[:, k, t * 128:(t + 1) * 128], rhs=wi[:, s, k, :],
                                                                               start=(k == 0), stop=(k == 7)), R=[b_wi[s], C.bhT[t]], W=[bpv])
                        P.add("act", lambda e, ti=ti, cg=cg, pv=pv: e.activation(out=v[:, ti, (cg - 4) * 512:(cg - 3) * 512], in_=pv[:], func=AF.Gelu),
                              R=[bpv], W=[b_v[ti]])
            for ti in range(2 if DBG >= 3 else 0):
                for c in range(4):
                    P.add("dve", lambda e, c=c, ti=ti: e.bn_stats(out=st[:, c, :], in_=v[:, ti, c * 512:(c + 1) * 512]), R=[b_v[ti]], W=[b_st])
                P.add("dve", lambda e: e.bn_aggr(out=mv[:], in_=st[:]), R=[b_st], W=[b_st])
                P.add("act", lambda e: e.activation(out=rs[:], in_=mv[:, 1:2], func=AF.Sqrt, bias=EPS, scale=1.0), R=[b_st], W=[b_st])
                P.add("dve", lambda e: e.reciprocal(out=rs[:], in_=rs[:]), R=[b_st], W=[b_st])
                P.add("dve", lambda e, ti=ti: e.tensor_scalar(out=v[:, ti, :], in0=v[:, ti, :], scalar1=mv[:, 0:1], scalar2=rs[:, 0:1],
                                                              op0=ALU.subtract, op1=ALU.mult), R=[b_v[ti], b_st], W=[b_v[ti]])
                P.add("pool", lambda e, ti=ti: e.tensor_tensor(out=v[:, ti, :], in0=v[:, ti, :], in1=gb[:, 0, :], op=ALU.mult), R=[b_v[ti], b_cst], W=[b_v[ti]])
                P.add("pool", lambda e, ti=ti: e.tensor_tensor(out=vn[:, ti, :], in0=v[:, ti, :], in1=gb[:, 1, :], op=ALU.add), R=[b_v[ti], b_cst], W=[b_vn[ti]])
            for ti in range(2 if DBG >= 4 else 0):
                for bk in range(4):
                    q = C.q_rr % 2
                    C.q_rr += 1
                    pm, bpm = C.ps[3 + q], C.bps[3 + q]
                    for jj in range(4):
                        fc = bk * 4 + jj
                        g = fc // 2
                        P.add("pe", lambda e, jj=jj, fc=fc, g=g, ti=ti, pm=pm: e.matmul(pm[:, jj * 128:(jj + 1) * 128], lhsT=vn[:, ti, fc * 128:(fc + 1) * 128],
                                                                                      rhs=wsT[:, g, :], start=True, stop=False), R=[b_vn[ti], b_wsT], W=[bpm])
                        P.add("pe", lambda e, jj=jj, g=g, pm=pm: e.matmul(pm[:, jj * 128:(jj + 1) * 128], lhsT=ones[0:1, :], rhs=bs[0:1, g * 128:(g + 1) * 128],
                                                                        start=False, stop=True), R=[b_cst], W=[bpm])
                    P.add("dve", lambda e, bk=bk, ti=ti, pm=pm: e.tensor_tensor(out=yT[:, bk * 4:bk * 4 + 4, ti * 128:(ti + 1) * 128],
                                                                              in0=pm[:].rearrange("p (a c) -> p a c", a=4),
                                                                              in1=yT[:, bk * 4:bk * 4 + 4, ti * 128:(ti + 1) * 128], op=ALU.mult),
                          R=[bpm, b_yT], W=[b_yT])
            if DBG < 5:
                continue
            for wc in range(4):
                s2 = iw % 2
                iw += 1
                P.add("pool", lambda e, s2=s2, wc=wc: e.dma_start(out=wo[:, s2], in_=w_out[wc * 512:(wc + 1) * 512, :].rearrange("(f p) n -> p f n", p=128)),
                      W=[b_wo[s2]], dma=True)
                for ti in range(2):
                    for n in range(2):
                        bi = acc_banks[ti * 2 + n]
                        po, bpo = C.ps[bi], C.bps[bi]
                        for ff in range(4):
                            P.add("pe", lambda e, wc=wc, ff=ff, ti=ti, n=n, s2=s2, po=po: e.matmul(
                                po[:], lhsT=yT[:, wc * 4 + ff, ti * 128:(ti + 1) * 128], rhs=wo[:, s2, ff, n * 512:(n + 1) * 512],
                                start=(wc == 0 and ff == 0), stop=(wc == 3 and ff == 3)), R=[b_yT, b_wo[s2]], W=[bpo])
            for ti in range(2):
                t = tiles[ti]
                for n in range(2):
                    bi = acc_banks[ti * 2 + n]
                    po, bpo = C.ps[bi], C.bps[bi]
                    P.add("dve", lambda e, n=n, t=t, po=po: e.scalar_tensor_tensor(out=h[:, t, n * 512:(n + 1) * 512], in0=h[:, t, n * 512:(n + 1) * 512],
                                                                                 scalar=ALPHA, in1=po[:], op0=ALU.mult, op1=ALU.add),
                          R=[bpo, C.bh[t]], W=[C.bh[t]])
            for ti in range(2 if DBG >= 6 else 0):
                ln_tile(C, tiles[ti], lng, lnb, blnp, router=(R_ if DBG >= 7 else None))
        P.flush()


def gla_phase(C, li, T, R_):
    P, nc = C.P, C.nc
    h, hT = C.h, C.hT
    j = li // 3
    w_in = T["c_w_in_%d" % j]
    SCALE = 128 ** -0.5
    with nc.sbuf_tensor("c_oT_%d" % li, [128, 8, S], BF16) as oT:
        C.b_srcT = Buf("oT")
        with ExitStack() as es:
            wq = es.enter_context(nc.sbuf_tensor("c_wq_%d" % li, [128, 2, 8, 768], BF16))
            wg = es.enter_context(nc.sbuf_tensor("c_wg_%d" % li, [128, 8, 16], BF16))
            gkT = es.enter_context(nc.sbuf_tensor("c_gkT_%d" % li, [32, S], F32))
            wgk = es.enter_context(nc.sbuf_tensor("c_wgk_%d" % li, [32, 512], F32))
            ng = es.enter_context(nc.sbuf_tensor("c_ng_%d" % li, [128, 256], F32))
            tri = es.enter_context(nc.sbuf_tensor("c_tri_%d" % li, [128, 128], F32))
            su = es.enter_context(nc.sbuf_tensor("c_su_%d" % li, [128, 128], F32))
            cmk = es.enter_context(nc.sbuf_tensor("c_cm_%d" % li, [128, 128], F32))
            qk = es.enter_context(nc.sbuf_tensor("c_qk_%d" % li, [128, 2, 2, 128], F32))
            kt = es.enter_context(nc.sbuf_tensor("c_kt_%d" % li, [128, 2, 128], F32))
            vt = es.enter_context(nc.sbuf_tensor("c_vt_%d" % li, [128, 2, 256], BF16))
            gs = es.enter_context(nc.sbuf_tensor("c_gs_%d" % li, [128, 2, 256], F32))
            nl = es.enter_context(nc.sbuf_tensor("c_nl_%d" % li, [128, 2, 128], F32))
            ex = es.enter_context(nc.sbuf_tensor("c_ex_%d" % li, [128, 2, 3, 128], F32))
            qz = es.enter_context(nc.sbuf_tensor("c_qz_%d" % li, [128, 2, 2, 128], BF16))
            ke = es.enter_context(nc.sbuf_tensor("c_ke_%d" % li, [128, 2, 128], BF16))
            krz = es.enter_context(nc.sbuf_tensor("c_krz_%d" % li, [128, 2, 2, 128], BF16))
            att = es.enter_context(nc.sbuf_tensor("c_att_%d" % li, [128, 2, 128], BF16))
            St = es.enter_context(nc.sbuf_tensor("c_S_%d" % li, [128, 256], F32))
            Sb = es.enter_context(nc.sbuf_tensor("c_Sb_%d" % li, [128, 5, 256], BF16))
            jk = es.enter_context(nc.sbuf_tensor("c_jk_%d" % li, [128, 256], F32))
            ot = es.enter_context(nc.sbuf_tensor("c_ot_%d" % li, [128, 2, 256], F32))
            og = es.enter_context(nc.sbuf_tensor("c_og_%d" % li, [128, 2, 256], BF16))
            ss = es.enter_context(nc.sbuf_tensor("c_ss_%d" % li, [128, 2, 2], F32))
            b_cst, b_gk, b_S, b_jk = Buf(), Buf(), Buf(), Buf()
            b_wq = [Buf(), Buf()]
            b_qk = [Buf(), Buf()]
            b_kt = [Buf(), Buf()]
            b_vt = [Buf(), Buf()]
            b_gs = [Buf(), Buf()]
            b_nl = [Buf(), Buf()]
            b_ex = [Buf(), Buf()]
            b_qz = [Buf(), Buf()]
            b_ke = [Buf(), Buf()]
            b_krz = [Buf(), Buf()]
            b_att = [Buf(), Buf()]
            b_Sb = [Buf() for _ in range(5)]
            b_ot = [Buf(), Buf()]
            b_og = [Buf(), Buf()]
            b_ss = [Buf(), Buf()]
            P.add("pool", lambda e: e.dma_start(out=wg[:], in_=w_in[:, 3072:3088].rearrange("(k p) n -> p k n", p=128)), W=[b_cst], dma=True)
            P.add("sp", lambda e: e.dma_start(out=wgk[0:16, :], in_=T["c_w_gk_up_%d" % j]), W=[b_cst], dma=True)
            P.add("sp", lambda e: e.dma_start(out=wgk[16:17, :], in_=T["c_b_gk_%d" % j].rearrange("(o n) -> o n", o=1)), W=[b_cst], dma=True)
            P.add("sp", lambda e: e.dma_start(out=ng[:], in_=bcast_rows(T["c_norm_g_%d" % j])), W=[b_cst], dma=True)
            P.add("sp", lambda e: e.dma_start(out=tri[:], in_=T["gla_tri"]), W=[b_cst], dma=True)
            P.add("sp", lambda e: e.dma_start(out=su[:], in_=T["gla_su"]), W=[b_cst], dma=True)
            P.add("sp", lambda e: e.dma_start(out=cmk[:], in_=T["gla_cm"]), W=[b_cst], dma=True)
            P.add("pool", lambda e: e.memset(gkT[:], 1.0), W=[b_gk])
            for z in range(2):
                P.add("pool", lambda e, z=z: e.memset(qz[:, z], 0.0), W=[b_qz[z]])
                P.add("pool", lambda e, z=z: e.memset(krz[:, z], 0.0), W=[b_krz[z]])
            for g4 in range(4):
                pg, bpg = C.ps[3 + g4 % 2], C.bps[3 + g4 % 2]
                for k in range(8):
                    P.add("pe", lambda e, k=k, g4=g4, pg=pg: e.matmul(pg[0:16, :], lhsT=wg[:, k, :], rhs=hT[:, k, g4 * 512:(g4 + 1) * 512],
                                                                    start=(k == 0), stop=(k == 7)), R=[b_cst] + C.bhT[g4 * 4:g4 * 4 + 4], W=[bpg])
                P.add("act", lambda e, g4=g4, pg=pg: e.copy(out=gkT[0:16, g4 * 512:(g4 + 1) * 512], in_=pg[0:16, :]), R=[bpg], W=[b_gk])

            def front(hd, s, t):
                z = t % 2
                tc = slice(t * 128, (t + 1) * 128)
                bhT = C.bhT[t]
                pa, bpa = C.ps[3], C.bps[3]
                for k in range(8):
                    P.add("pe", lambda e, k=k: e.matmul(pa[:, 0:128], lhsT=wq[:, s, k, 0:128], rhs=hT[:, k, tc], start=(k == 0), stop=(k == 7)),
                          R=[b_wq[s], bhT], W=[bpa])
                for k in range(8):
                    P.add("pe", lambda e, k=k: e.matmul(pa[:, 128:256], lhsT=wq[:, s, k, 128:256], rhs=hT[:, k, tc], start=(k == 0), stop=(k == 7)),
                          R=[b_wq[s], bhT], W=[bpa])
                for k in range(8):
                    P.add("pe", lambda e, k=k: e.matmul(pa[:, 256:384], lhsT=hT[:, k, tc], rhs=wq[:, s, k, 128:256], start=(k == 0), stop=(k == 7)),
                          R=[b_wq[s], bhT], W=[bpa])
                P.add("pe", lambda e: e.matmul(pa[:, 384:512], lhsT=gkT[0:17, tc], rhs=wgk[0:17, hd * 128:(hd + 1) * 128], start=True, stop=True),
                      R=[b_gk, b_cst], W=[bpa])
                P.add("act", lambda e: e.copy(out=qk[:, z], in_=pa[:, 0:256].rearrange("p (a c) -> p a c", a=2)), R=[bpa], W=[b_qk[z]])
                P.add("act", lambda e: e.copy(out=kt[:, z], in_=pa[:, 256:384]), R=[bpa], W=[b_kt[z]])
                P.add("act", lambda e: e.activation(out=nl[:, z], in_=pa[:, 384:512], func=AF.Exp, scale=-1.0), R=[bpa], W=[b_nl[z]])
                P.add("act", lambda e: e.activation(out=nl[:, z], in_=nl[:, z], func=AF.Ln, bias=1.0, scale=1.0), R=[b_nl[z]], W=[b_nl[z]])
                pb_, bpb = C.ps[4], C.bps[4]
                for k in range(8):
                    P.add("pe", lambda e, k=k: e.matmul(pb_[:], lhsT=hT[:, k, tc], rhs=wq[:, s, k, 256:768], start=(k == 0), stop=(k == 7)),
                          R=[b_wq[s], bhT], W=[bpb])
                P.add("act", lambda e: e.copy(out=vt[:, z], in_=pb_[:, 0:256]), R=[bpb], W=[b_vt[z]])
                P.add("act", lambda e: e.activation(out=gs[:, z], in_=pb_[:, 256:512], func=AF.Silu), R=[bpb], W=[b_gs[z]])
                pc, bpc = C.ps[5], C.bps[5]
                P.add("pe", lambda e: e.matmul(pc[:, 0:128], lhsT=nl[:, z], rhs=tri[:], start=True, stop=True), R=[b_nl[z], b_cst], W=[bpc])
                P.add("pe", lambda e: e.matmul(pc[:, 128:256], lhsT=su[:], rhs=nl[:, z], start=True, stop=True), R=[b_nl[z], b_cst], W=[bpc])
                P.add("act", lambda e: e.activation(out=ex[:, z, 0, :], in_=pc[:, 0:128], func=AF.Exp, scale=-1.0 / 16.0), R=[bpc], W=[b_ex[z]])
                P.add("act", lambda e: e.activation(out=ex[:, z, 1, :], in_=pc[:, 0:128], func=AF.Exp, scale=1.0 / 16.0), R=[bpc], W=[b_ex[z]])
                P.add("act", lambda e: e.activation(out=ex[:, z, 2, :], in_=pc[:, 128:256], func=AF.Exp, scale=-1.0 / 16.0), R=[bpc], W=[b_ex[z]])
                for c in range(2):
                    cs = slice(c * 64, (c + 1) * 64)
                    P.add("dve", lambda e, c=c, cs=cs: e.scalar_tensor_tensor(out=qz[:, z, c, cs], in0=qk[:, z, 0, cs], scalar=SCALE, in1=ex[:, z, 0, cs],
                                                                            op0=ALU.mult, op1=ALU.mult), R=[b_qk[z], b_ex[z]], W=[b_qz[z]])
                P.add("pool", lambda e: e.tensor_tensor(out=ke[:, z], in0=qk[:, z, 1], in1=ex[:, z, 1, :], op=ALU.mult), R=[b_qk[z], b_ex[z]], W=[b_ke[z]])
                for c in range(2):
                    rows = slice(c * 64, (c + 1) * 64)
                    P.add("pool", lambda e, c=c, rows=rows: e.tensor_tensor(out=krz[rows, z, c, :], in0=kt[rows, z], in1=ex[rows, z, 2, :], op=ALU.mult),
                          R=[b_kt[z], b_ex[z]], W=[b_krz[z]])
                P.add("pe", lambda e: e.matmul(pc[:, 256:384], lhsT=ke[:, z], rhs=qz[:, z, 0, :], start=True, stop=False), R=[b_ke[z], b_qz[z]], W=[bpc])
                P.add("pe", lambda e: e.matmul(pc[:, 256:384], lhsT=ke[:, z], rhs=qz[:, z, 1, :], start=False, stop=True), R=[b_ke[z], b_qz[z]], W=[bpc])
                P.add("dve", lambda e: e.tensor_tensor(out=att[:, z], in0=pc[:, 256:384], in1=cmk[:], op=ALU.mult), R=[bpc, b_cst], W=[b_att[z]])
                pd, bpd = C.ps[6], C.bps[6]
                P.add("pe", lambda e: e.matmul(pd[:, 0:256], lhsT=krz[:, z, 0, :], rhs=vt[:, z], start=True, stop=True), R=[b_krz[z], b_vt[z]], W=[bpd])
                P.add("pe", lambda e: e.matmul(pd[:, 256:512], lhsT=krz[:, z, 1, :], rhs=vt[:, z], start=True, stop=True), R=[b_krz[z], b_vt[z]], W=[bpd])
                P.add("dve", lambda e: e.scalar_tensor_tensor(out=St[:], in0=St[:], scalar=ex[:, z, 0, 63:64], in1=pd[:, 0:256], op0=ALU.mult, op1=ALU.add),
                      R=[b_S, b_ex[z], bpd], W=[b_S])
                P.add("act", lambda e: e.copy(out=Sb[:, 3 + z, :], in_=St[:]), R=[b_S], W=[b_Sb[3 + z]])
                P.add("dve", lambda e: e.scalar_tensor_tensor(out=St[:], in0=St[:], scalar=ex[:, z, 0, 127:128], in1=pd[:, 256:512], op0=ALU.mult, op1=ALU.add),
                      R=[b_S, b_ex[z], bpd], W=[b_S])
                P.add("act", lambda e: e.copy(out=Sb[:, (t + 1) % 3, :], in_=St[:]), R=[b_S], W=[b_Sb[(t + 1) % 3]])

            def back(hd, s, t):
                z = t % 2
                tc = slice(t * 128, (t + 1) * 128)
                po, bpo = C.ps[7], C.bps[7]
                P.add("pe", lambda e: e.matmul(po[:, 0:256], lhsT=qz[:, z, 0, :], rhs=Sb[:, t % 3, :], start=True, stop=False), R=[b_qz[z], b_Sb[t % 3]], W=[bpo])
                P.add("pe", lambda e: e.matmul(po[:, 0:256], lhsT=qz[:, z, 1, :], rhs=Sb[:, 3 + z, :], start=False, stop=False), R=[b_qz[z], b_Sb[3 + z]], W=[bpo])
                P.add("pe", lambda e: e.matmul(po[:, 0:256], lhsT=att[:, z], rhs=vt[:, z], start=False, stop=True), R=[b_att[z], b_vt[z]], W=[bpo])
                P.add("act", lambda e: e.activation(out=jk[:], in_=po[:, 0:256], func=AF.Square, accum_out=ss[:, z, 0:1]), R=[bpo], W=[b_jk, b_ss[z]])
                P.add("act", lambda e: e.activation(out=ss[:, z, 1:2], in_=ss[:, z, 0:1], func=AF.Sqrt, bias=EPS, scale=1.0 / 256.0), R=[b_ss[z]], W=[b_ss[z]])
                P.add("dve", lambda e: e.reciprocal(out=ss[:, z, 1:2], in_=ss[:, z, 1:2]), R=[b_ss[z]], W=[b_ss[z]])
                P.add("dve", lambda e: e.scalar_tensor_tensor(out=ot[:, z], in0=po[:, 0:256], scalar=ss[:, z, 1:2], in1=ng[:], op0=ALU.mult, op1=ALU.mult),
                      R=[bpo, b_ss[z], b_cst], W=[b_ot[z]])
                P.add("pool", lambda e: e.tensor_tensor(out=og[:, z], in0=ot[:, z], in1=gs[:, z], op=ALU.mult), R=[b_ot[z], b_gs[z]], W=[b_og[z]])
                ptp, bptp = C.ps[z], C.bps[z]
                ptb = ptp[:].bitcast(BF16)
                for c2 in range(2):
                    P.add("pe", lambda e, c2=c2: e.transpose(out=ptb[:, c2 * 128:(c2 + 1) * 128], in_=og[:, z, c2 * 128:(c2 + 1) * 128], identity=C.identb[:]),
                          R=[b_og[z], C.b_const], W=[bptp])
                P.add("act", lambda e: e.copy(out=oT[:, hd * 2:hd * 2 + 2, tc], in_=ptb[:, 0:256].rearrange("p (a c) -> p a c", a=2)), R=[bptp], W=[C.b_srcT])

            for hd in range(4):
                s = hd % 2
                for (c0_, w_, d0) in ((hd * 128, 128, 0), (512 + hd * 128, 128, 128), (1024 + hd * 256, 256, 256), (2048 + hd * 256, 256, 512)):
                    P.add("pool", lambda e, c0_=c0_, w_=w_, d0=d0, s=s: e.dma_start(
                        out=wq[:, s, :, d0:d0 + w_], in_=w_in[:, c0_:c0_ + w_].rearrange("(k p) n -> p k n", p=128)), W=[b_wq[s]], dma=True)
                P.add("pool", lambda e: e.memset(St[:], 0.0), W=[b_S])
                P.add("pool", lambda e: e.memset(Sb[:, 0, :], 0.0), W=[b_Sb[0]])
                front(hd, s, 0)
                for t in range(NT):
                    if t + 1 < NT:
                        front(hd, s, t + 1)
                    back(hd, s, t)
            P.flush()
        out_proj_ln(C, li, T, oT, 8, T["c_w_out_%d" % j], R_)


def build_program(mode="full", n_experts=NE, layers=(0, 1, 2, 3)):
    nc = bass.Bass("TRN2", target_bir_lowering=False)
    T = {}

    def din(name, shape, dt=F32):
        T[name] = nc.dram_tensor(name, list(shape), dt, kind="ExternalInput").ap()

    do_mixer = mode in ("full", "mixer_only")
    do_moe = mode in ("full", "moe_only")
    din("x", (S, D))
    din("ident", (128, 128))
    din("identb", (128, 128), BF16)
    din("ln_g", (DEPTH, 2, D))
    din("ln_b", (DEPTH, 2, D))
    mixers = sorted(set(li % 3 for li in layers)) if do_mixer else []
    if 0 in mixers:
        din("rel_bias", (32, 8))
        din("relT", (8, 2, 128, 128))
        din("causal", (128, 128))
        din("vmask", (128, 128))
        din("vmask2", (128, 128))
    for li in layers:
        if do_mixer:
            j = li // 3
            if li % 3 == 0:
                din("a_w_in_%d" % j, (D, 3 * D))
                din("a_w_out_%d" % j, (D, D))
            elif li % 3 == 1:
                din("b_w_in_%d" % j, (D, 4 * D))
                din("b_ln_g_%d" % j, (2 * D,))
                din("b_ln_b_%d" % j, (2 * D,))
                din("b_w_s_%d" % j, (8, 128, 128))
                din("b_b_s_%d" % j, (8, 128))
                din("b_w_out_%d" % j, (2 * D, D))
                din("tril", (128, 128))
            else:
                din("c_w_in_%d" % j, (D, 3088))
                din("c_w_gk_up_%d" % j, (16, 512))
                din("c_b_gk_%d" % j, (512,))
                din("c_norm_g_%d" % j, (256,))
                din("c_w_out_%d" % j, (D, D))
                din("gla_tri", (128, 128))
                din("gla_su", (128, 128))
                din("gla_cm", (128, 128))
        din("moe_w_router_%d" % li, (D, NE))
        din("moe_b_router_%d" % li, (NE,))
        if do_moe:
            if "iota" not in T:
                din("iota", (128, 128))
                din("ones_b", (128, 128), BF16)
                din("tris_b", (128, 128), BF16)
            din("moe_w_gate_up_%d" % li, (NE, D, 2 * D))
            din("moe_b_gate_up_%d" % li, (NE, 2 * D))
            din("moe_w_down_%d" % li, (NE, D, D))
            din("moe_b_down_%d" % li, (NE, D))
    out = nc.dram_tensor("out", [S, D], F32, kind="ExternalOutput").ap()

    C = Ctx()
    C.nc = nc
    C.n_experts = n_experts
    C.P = P = Prog(nc)
    C.tp_rr = C.act_rr = C.q_rr = C.y_rr = C.ys_rr = 0
    with ExitStack() as es:
        h = es.enter_context(nc.sbuf_tensor("h", [128, NT, D], F32))
        hT = es.enter_context(nc.sbuf_tensor("hT", [128, 8, S], BF16))
        ident = es.enter_context(nc.sbuf_tensor("ident_s", [128, 128], F32))
        identb = es.enter_context(nc.sbuf_tensor("identb_s", [128, 128], BF16))
        hT32 = es.enter_context(nc.sbuf_tensor("hT32", [128, 8, 128], F32))
        gates = es.enter_context(nc.sbuf_tensor("gates", [128, NT, NE], F32))
        ln_st = es.enter_context(nc.sbuf_tensor("ln_st", [128, 2, 6], F32))
        ln_mv = es.enter_context(nc.sbuf_tensor("ln_mv", [128, 2], F32))
        ln_rs = es.enter_context(nc.sbuf_tensor("ln_rs", [128, 1], F32))
        r_lg = es.enter_context(nc.sbuf_tensor("r_lg", [128, NE], F32))
        r_m8 = es.enter_context(nc.sbuf_tensor("r_m8", [128, 8], F32))
        r_ex = es.enter_context(nc.sbuf_tensor("r_ex", [128, NE], F32))
        r_msk = es.enter_context(nc.sbuf_tensor("r_msk", [128, NE], F32))
        r_sm = es.enter_context(nc.sbuf_tensor("r_sm", [128, 2], F32))
        r_wr = es.enter_context(nc.sbuf_tensor("r_wr", [128, 8, NE], F32))
        r_brb = es.enter_context(nc.sbuf_tensor("r_brb", [128, NE], F32))
        C.h, C.hT, C.ident, C.identb, C.hT32, C.gates = h, hT, ident, identb, hT32, gates
        C.ln_st, C.ln_mv, C.ln_rs, C.ln_nb = ln_st, ln_mv, ln_rs, None
        C.r_lg, C.r_m8, C.r_ex, C.r_msk, C.r_sm = r_lg, r_m8, r_ex, r_msk, r_sm
        C.bh = [Buf("h%d" % t) for t in range(NT)]
        C.bhT = [Buf("hT%d" % t) for t in range(NT)]
        C.b_gates = [Buf() for t in range(NT)]
        C.b_const = Buf("const")
        C.b_lnst = Buf()
        C.b_hT32 = Buf()
        C.b_rt = Buf()
        ps_cms = [nc.psum_tensor("ps%d" % i, [128, 512], F32) for i in range(8)]
        C.ps = [cm.__enter__() for cm in ps_cms]
        C.bps = [Buf("ps%d" % i) for i in range(8)]

        P.add("sp", lambda e: e.dma_start(out=ident[:], in_=T["ident"]), W=[C.b_const], dma=True)
        P.add("sp", lambda e: e.dma_start(out=identb[:], in_=T["identb"]), W=[C.b_const], dma=True)
        for t in range(NT):
            P.add("sp", lambda e, t=t: e.dma_start(out=h[:, t, :], in_=T["x"][t * 128:(t + 1) * 128, :]), W=[C.bh[t]], dma=True)
        first = True
        for li in layers:
            b_r = Buf()
            P.add("sp", lambda e, li=li: e.dma_start(out=r_wr[:], in_=T["moe_w_router_%d" % li].rearrange("(k p) n -> p k n", p=128)), W=[b_r], dma=True)
            P.add("sp", lambda e, li=li: e.dma_start(out=r_brb[:], in_=bcast_rows(T["moe_b_router_%d" % li])), W=[b_r], dma=True)
            R_ = {"wr": r_wr, "brb": r_brb, "b": b_r}
            if first:
                for t in range(NT):
                    transpose_tile(C, t, None if do_mixer else R_)
                P.flush()
                first = False
            if do_mixer:
                if li % 3 == 0:
                    moba_phase(C, li, T, R_)
                elif li % 3 == 1:
                    gmlp_phase(C, li, T, R_)
                else:
                    gla_phase(C, li, T, R_)
            if do_moe:
                moe_phase(C, li, T)

        for t in range(NT):
            P.add("sp", lambda e, t=t: e.dma_start(out=out[t * 128:(t + 1) * 128, :], in_=h[:, t, :]), R=[C.bh[t]], dma=True)
        P.flush()
        for cm in reversed(ps_cms):
            cm.__exit__(None, None, None)
    P.close()
    return nc


def t5_bucket_np(rel):
    n = np.maximum(rel, 0)
    nf = np.maximum(n, 1).astype(np.float32)
    large = 16 + (np.log(nf / np.float32(16)) / np.float32(math.log(128 / 16)) * np.float32(16)).astype(np.int32)
    large = np.minimum(large, 31)
    return np.where(n < 16, n, large)


def host_constants(rel_bias=None):
    import ml_dtypes
    c = {"ident": np.eye(128, dtype=np.float32), "identb": np.eye(128, dtype=np.float32).astype(ml_dtypes.bfloat16)}
    i = np.arange(128)[:, None]
    jj = np.arange(128)[None, :]
    c["causal"] = np.where(jj <= i, 0.0, NEG).astype(np.float32)
    c["iota"] = np.broadcast_to(np.arange(128, dtype=np.float32)[None, :], (128, 128)).copy()
    c["ones_b"] = np.ones((128, 128), np.float32).astype(ml_dtypes.bfloat16)
    c["tris_b"] = (i < jj).astype(np.float32).astype(ml_dtypes.bfloat16)
    c["tril"] = (jj <= i).astype(np.float32)
    qt = np.arange(16)[:, None]
    n = np.arange(8)[None, :]
    valid = n < (qt // 2)
    vm = np.where(valid, 0.0, -1e30).astype(np.float32).reshape(1, 128)
    c["vmask"] = np.broadcast_to(vm, (128, 128)).copy()
    vm2 = np.where(valid, -30000.0, -1e30).astype(np.float32).reshape(1, 128)
    c["vmask2"] = np.broadcast_to(vm2, (128, 128)).copy()
    if rel_bias is not None:
        relT = np.empty((8, 2, 128, 128), np.float32)
        for d in range(2):
            bk = t5_bucket_np(d * 128 + i - jj)
            for hh in range(8):
                relT[hh, d] = rel_bias[bk, hh]
        c["relT"] = relT
    same = (i // 64) == (jj // 64)
    c["gla_tri"] = (same & (i <= jj)).astype(np.float32)
    c["gla_su"] = (same & (i > jj)).astype(np.float32)
    c["gla_cm"] = (same & (i <= jj)).astype(np.float32)
    return c


def make_in_map(A, x_core, consts, layers=(0, 1, 2, 3), mode="full"):
    do_mixer = mode in ("full", "mixer_only")
    do_moe = mode in ("full", "moe_only")
    im = {"x": np.ascontiguousarray(x_core), "ln_g": A["ln_g"], "ln_b": A["ln_b"]}
    im.update(consts)
    if "rel_bias" in A:
        im["rel_bias"] = A["rel_bias"]
    for li in layers:
        j = li // 3
        if do_mixer:
            if li % 3 == 0:
                im["a_w_in_%d" % j] = A["a_w_in"][j]
                im["a_w_out_%d" % j] = A["a_w_out"][j]
            elif li % 3 == 1:
                for n in ("b_w_in", "b_ln_g", "b_ln_b", "b_w_s", "b_b_s", "b_w_out"):
                    im["%s_%d" % (n, j)] = A[n][j]
            else:
                for n in ("c_w_in", "c_w_gk_up", "c_b_gk", "c_norm_g", "c_w_out"):
                    im["%s_%d" % (n, j)] = A[n][j]
        im["moe_w_router_%d" % li] = A["moe_w_router"][li]
        im["moe_b_router_%d" % li] = A["moe_b_router"][li]
        if do_moe:
            for n in ("moe_w_gate_up", "moe_b_gate_up", "moe_w_down", "moe_b_down"):
                im["%s_%d" % (n, li)] = A[n][li]
    return im


def kernel(**inputs):
    A = {k: np.asarray(v) for k, v in inputs.items()}
    nc = build_program("full")
    consts = host_constants(A["rel_bias"])
    in_maps = [make_in_map(A, A["x"][c], consts) for c in range(8)]
    res = run_bass_kernel_spmd(nc, in_maps, core_ids=list(range(8)))
    out = np.stack([np.asarray(res.results[c]["out"]) for c in range(8)], axis=0)
    return out.astype(np.float32)
```

```python
import math
from contextlib import ExitStack
import numpy as np
import concourse.bass as bass
import concourse.mybir as mybir
from concourse.bass_utils import run_bass_kernel_spmd

F32 = mybir.dt.float32
BF16 = mybir.dt.bfloat16
ALU = mybir.AluOpType
AF = mybir.ActivationFunctionType
AX = mybir.AxisListType

ENGS = ("pe", "act", "dve", "pool", "sp")

D = 1024
S = 2048
NT = 16
DEPTH = 4
ALPHA = (2.0 * DEPTH) ** 0.25
EPS = 1e-5
NE = 32
NEG = -30000.0


class Buf:
    __slots__ = ("name", "lw", "rd")

    def __init__(self, name="b"):
        self.name = name
        self.lw = None
        self.rd = []


class Op:
    __slots__ = ("eng", "fn", "dma", "deps", "sem", "val", "needs_sig", "epoch")

    def __init__(self, eng, fn, dma, epoch=0):
        self.epoch = epoch
        self.eng = eng
        self.fn = fn
        self.dma = dma
        self.deps = []
        self.sem = None
        self.val = 0
        self.needs_sig = False


class Prog:
    def __init__(self, nc, n_dma_sems=(("sp", 24), ("pool", 24), ("act", 8))):
        self.nc = nc
        self.pending = {e: [] for e in ENGS}
        self.esem = {}
        self.ecount = {e: 0 for e in ENGS}
        self._ctx = []
        for e in ENGS:
            cm = nc.semaphore("s_" + e)
            self.esem[e] = cm.__enter__()
            self._ctx.append(cm)
        self.dsems = {}
        self.dcount = {}
        self.dlast = {}
        self.dnext = {}
        for q, n in n_dma_sems:
            lst = []
            for i in range(n):
                cm = nc.semaphore("d_%s_%d" % (q, i))
                lst.append(cm.__enter__())
                self._ctx.append(cm)
            self.dsems[q] = lst
            self.dcount[q] = [0] * n
            self.dlast[q] = [None] * n
            self.dnext[q] = 0
        self.known = {e: {} for e in ENGS}
        self.all_dma_since_barrier = []
        self.n_ops = 0
        self.epoch = 0

    def close(self):
        for cm in reversed(self._ctx):
            cm.__exit__(None, None, None)

    def add(self, eng, fn, R=(), W=(), dma=False):
        op = Op(eng, fn, dma, self.epoch)
        self.n_ops += 1
        deps = []
        rset = set(id(b) for b in R)
        for b in R:
            if b.lw is not None:
                deps.append((b.lw, True))
        for b in W:
            if b.lw is not None:
                deps.append((b.lw, id(b) in rset))
            last = {}
            for r in b.rd:
                if r.dma:
                    deps.append((r, False))
                else:
                    last[r.eng] = r
            for r in last.values():
                deps.append((r, False))
        seen = set()
        for d, raw in deps:
            if d is op or d.epoch < self.epoch:
                continue
            key = id(d)
            if d.dma or dma:
                pass
            elif d.eng == eng:
                if eng == "pe" or not raw:
                    continue
            if key in seen:
                continue
            seen.add(key)
            op.deps.append(d)
            d.needs_sig = True
        for b in R:
            b.rd.append(op)
        for b in W:
            b.lw = op
            b.rd = []
        if dma:
            q = eng
            k = self.dnext[q]
            self.dnext[q] = (k + 1) % len(self.dsems[q])
            prev = self.dlast[q][k]
            if prev is not None and prev.epoch == self.epoch:
                op.deps.append(prev)
            self.dcount[q][k] += 16
            op.sem = self.dsems[q][k]
            op.val = self.dcount[q][k]
            self.dlast[q][k] = op
            op.needs_sig = True
            self.all_dma_since_barrier.append(op)
        self.pending[eng].append(op)
        return op

    def barrier(self):
        lasts = []
        for e in ENGS:
            for o in reversed(self.pending[e]):
                if not o.dma and o.fn is not None:
                    lasts.append(o)
                    break
        dmas = list(self.all_dma_since_barrier)
        self.all_dma_since_barrier = []
        for e in ENGS:
            op = Op(e, None, False, self.epoch)
            for d in lasts:
                if d.eng != e:
                    op.deps.append(d)
                    d.needs_sig = True
            for d in dmas:
                op.deps.append(d)
            self.pending[e].append(op)
        self.epoch += 1

    def flush(self):
        nc = self.nc
        self.barrier()
        for e in ENGS:
            for op in self.pending[e]:
                if op.dma or op.fn is None:
                    continue
                if op.needs_sig:
                    self.ecount[e] += 1
                    op.sem = self.esem[e]
                    op.val = self.ecount[e]
        pend = self.pending
        self.pending = {e: [] for e in ENGS}
        known = self.known

        def emit(e, eng):
            kn = known[e]
            for op in pend[e]:
                need = {}
                for d in op.deps:
                    assert d.sem is not None, "dep without signal"
                    sid = id(d.sem)
                    if kn.get(sid, 0) >= d.val:
                        continue
                    if sid not in need or need[sid][1] < d.val:
                        need[sid] = (d.sem, d.val)
                for sid, (sem, val) in need.items():
                    eng.wait_ge(sem, val)
                    kn[sid] = val
                if op.fn is None:
                    continue
                ins = op.fn(eng)
                if op.dma:
                    ins.then_inc(op.sem, 16)
                elif op.needs_sig:
                    ins.then_inc(op.sem, 1)

        with nc.Block() as blk:
            @blk.tensor
            def _(eng):
                emit("pe", eng)

            @blk.scalar
            def _(eng):
                emit("act", eng)

            @blk.vector
            def _(eng):
                emit("dve", eng)

            @blk.gpsimd
            def _(eng):
                emit("pool", eng)

            @blk.sync
            def _(eng):
                emit("sp", eng)


class Ctx:
    pass


def bcast_rows(ap_row, nparts=128):
    return ap_row.partition_broadcast(nparts)


def ln_tile(C, t, lng, lnb, blnp, router=None):
    P, nc = C.P, C.nc
    h, hT = C.h, C.hT
    bh, bhT = C.bh[t], C.bhT[t]
    st, mv, rs, nb = C.ln_st, C.ln_mv, C.ln_rs, C.ln_nb
    b_st = C.b_lnst
    for c in range(2):
        P.add("dve", lambda e, c=c: e.bn_stats(out=st[:, c, :], in_=h[:, t, c * 512:(c + 1) * 512]), R=[bh], W=[b_st])
    P.add("dve", lambda e: e.bn_aggr(out=mv[:], in_=st[:]), R=[b_st], W=[b_st])
    P.add("act", lambda e: e.activation(out=rs[:], in_=mv[:, 1:2], func=AF.Sqrt, bias=EPS, scale=1.0), R=[b_st], W=[b_st])
    P.add("dve", lambda e: e.reciprocal(out=rs[:], in_=rs[:]), R=[b_st], W=[b_st])
    P.add("dve", lambda e: e.tensor_scalar(out=h[:, t, :], in0=h[:, t, :], scalar1=mv[:, 0:1], scalar2=rs[:, 0:1],
                                           op0=ALU.subtract, op1=ALU.mult), R=[bh, b_st], W=[bh])
    P.add("pool", lambda e: e.tensor_tensor(out=h[:, t, :], in0=h[:, t, :], in1=lng[:], op=ALU.mult), R=[bh, blnp], W=[bh])
    P.add("pool", lambda e: e.tensor_tensor(out=h[:, t, :], in0=h[:, t, :], in1=lnb[:], op=ALU.add), R=[bh, blnp], W=[bh])
    transpose_tile(C, t, router)


def transpose_tile(C, t, router=None):
    P = C.P
    h, hT = C.h, C.hT
    bh, bhT = C.bh[t], C.bhT[t]
    for half in range(2):
        ps, bps = C.ps[C.tp_rr % 2], C.bps[C.tp_rr % 2]
        C.tp_rr += 1
        for kk in range(4):
            k = half * 4 + kk
            P.add("pe", lambda e, k=k, kk=kk, ps=ps: e.transpose(out=ps[:, kk * 128:(kk + 1) * 128], in_=h[:, t, k * 128:(k + 1) * 128],
                                                                  identity=C.ident[:]), R=[bh, C.b_const], W=[bps])
        P.add("act", lambda e, ps=ps, half=half: e.copy(out=hT[:, half * 4:half * 4 + 4, t * 128:(t + 1) * 128],
                                                        in_=ps[:].rearrange("p (k c) -> p k c", k=4)), R=[bps], W=[bhT])
        if router is not None:
            P.add("act", lambda e, ps=ps, half=half: e.copy(out=C.hT32[:, half * 4:half * 4 + 4, :],
                                                            in_=ps[:].rearrange("p (k c) -> p k c", k=4)), R=[bps], W=[C.b_hT32])
    if router is not None:
        router_tile(C, t, router)


def router_tile(C, t, R_):
    P = C.P
    ps, bps = C.ps[2], C.bps[2]
    wr, brb, b_r = R_["wr"], R_["brb"], R_["b"]
    lg, m8, ex, msk, sm = C.r_lg, C.r_m8, C.r_ex, C.r_msk, C.r_sm
    b = C.b_rt
    for k in range(8):
        P.add("pe", lambda e, k=k: e.matmul(ps[:, 0:NE], lhsT=C.hT32[:, k, :], rhs=wr[:, k, :], start=(k == 0), stop=(k == 7)),
              R=[C.b_hT32, b_r], W=[bps])
    P.add("dve", lambda e: e.tensor_tensor(out=lg[:], in0=ps[:, 0:NE], in1=brb[:], op=ALU.add), R=[bps, b_r], W=[b])
    P.add("dve", lambda e: e.max(out=m8[:], in_=lg[:]), R=[b], W=[b])
    P.add("dve", lambda e: e.tensor_scalar(out=msk[:], in0=lg[:], scalar1=m8[:, 3:4], scalar2=None, op0=ALU.is_ge), R=[b], W=[b])
    P.add("dve", lambda e: e.tensor_scalar(out=sm[:, 0:1], in0=m8[:, 0:1], scalar1=-1.0, scalar2=None, op0=ALU.mult), R=[b], W=[b])
    P.add("act", lambda e: e.activation(out=ex[:], in_=lg[:], func=AF.Exp, bias=sm[:, 0:1], scale=1.0), R=[b], W=[b])
    P.add("dve", lambda e: e.tensor_tensor(out=ex[:], in0=ex[:], in1=msk[:], op=ALU.mult), R=[b], W=[b])
    P.add("dve", lambda e: e.reduce_sum(out=sm[:, 1:2], in_=ex[:], axis=AX.X), R=[b], W=[b])
    P.add("dve", lambda e: e.reciprocal(out=sm[:, 1:2], in_=sm[:, 1:2]), R=[b], W=[b])
    P.add("dve", lambda e: e.tensor_scalar(out=C.gates[:, t, :], in0=ex[:], scalar1=sm[:, 1:2], scalar2=None, op0=ALU.mult),
          R=[b], W=[C.b_gates[t]])


def load_ln_params(C, lng, lnb, blnp, g_row, b_row):
    P = C.P
    P.add("sp", lambda e: e.dma_start(out=lng[:], in_=bcast_rows(g_row)), W=[blnp], dma=True)
    P.add("sp", lambda e: e.dma_start(out=lnb[:], in_=bcast_rows(b_row)), W=[blnp], dma=True)


def moe_phase(C, li, T):
    P, nc = C.P, C.nc
    h, hT = C.h, C.hT
    hb = hT[:].rearrange("p k s -> p (k s)").rearrange("p (t d) -> p t d", t=NT)
    BANKS = (0, 1, 7, 2)
    with ExitStack() as es:
        w1g = es.enter_context(nc.sbuf_tensor("m_w1g_%d" % li, [128, 2, 8, 512], BF16))
        w1u = es.enter_context(nc.sbuf_tensor("m_w1u_%d" % li, [128, 2, 8, 512], BF16))
        w2 = es.enter_context(nc.sbuf_tensor("m_w2_%d" % li, [128, 2, 4, 1024], BF16))
        actT = es.enter_context(nc.sbuf_tensor("m_actT_%d" % li, [128, 2, 4, 512], BF16))
        g1 = es.enter_context(nc.sbuf_tensor("m_g1_%d" % li, [128, 2, 512], F32))
        sg = es.enter_context(nc.sbuf_tensor("m_sg_%d" % li, [128, 2, 512], F32))
        u1 = es.enter_context(nc.sbuf_tensor("m_u1_%d" % li, [128, 2, 512], F32))
        braw = es.enter_context(nc.sbuf_tensor("m_braw_%d" % li, [128, 4, 128], F32))
        bguT = es.enter_context(nc.sbuf_tensor("m_bguT_%d" % li, [128, 512], F32))
        bd = es.enter_context(nc.sbuf_tensor("m_bd_%d" % li, [NE, D], F32))
        gT = es.enter_context(nc.sbuf_tensor("m_gT_%d" % li, [NE, 128], F32))
        xgT = es.enter_context(nc.sbuf_tensor("m_xgT_%d" % li, [128, 8, 512], BF16))
        Pm = es.enter_context(nc.sbuf_tensor("m_P_%d" % li, [128, NT, 128], BF16))
        PTm = es.enter_context(nc.sbuf_tensor("m_PT_%d" % li, [128, 4, 512], BF16))
        ys = es.enter_context(nc.sbuf_tensor("m_ys_%d" % li, [128, 2, 1024], BF16))
        mask = es.enter_context(nc.sbuf_tensor("m_mask_%d" % li, [128, NT, NE], BF16))
        posm = es.enter_context(nc.sbuf_tensor("m_pos_%d" % li, [128, NT, NE], F32))
        iota = es.enter_context(nc.sbuf_tensor("m_iota_%d" % li, [128, 128], F32))
        onesb = es.enter_context(nc.sbuf_tensor("m_ones_%d" % li, [128, 128], BF16))
        tris = es.enter_context(nc.sbuf_tensor("m_tris_%d" % li, [128, 128], BF16))
        b_w1 = [Buf("w1_%d" % i) for i in range(2)]
        b_w2 = [Buf("w2_%d" % i) for i in range(2)]
        b_act = [Buf() for _ in range(2)]
        b_g1 = [Buf() for _ in range(2)]
        b_sg = [Buf() for _ in range(2)]
        b_u1 = [Buf() for _ in range(2)]
        b_ys = [Buf() for _ in range(2)]
        b_misc, b_gT, b_cst, b_mask, b_pos, b_xg, b_P, b_PT = Buf(), Buf(), Buf(), Buf(), Buf(), Buf(), Buf(), Buf()
        allhT = list(C.bhT)
        P.add("sp", lambda e: e.dma_start(out=iota[:], in_=T["iota"]), W=[b_cst], dma=True)
        P.add("sp", lambda e: e.dma_start(out=onesb[:], in_=T["ones_b"]), W=[b_cst], dma=True)
        P.add("sp", lambda e: e.dma_start(out=tris[:], in_=T["tris_b"]), W=[b_cst], dma=True)
        P.add("sp", lambda e: e.dma_start(out=braw[:], in_=T["moe_b_gate_up_%d" % li].rearrange("e (c p) -> (e c) p", p=128)
                                          .rearrange("(b r) p -> r b p", r=128)), W=[b_misc], dma=True)
        P.add("sp", lambda e: e.dma_start(out=bd[:], in_=T["moe_b_down_%d" % li]), W=[b_misc], dma=True)
        for bb in range(4):
            ps, bps = C.ps[bb % 2], C.bps[bb % 2]
            P.add("pe", lambda e, bb=bb, ps=ps: e.transpose(out=ps[:, 0:128], in_=braw[:, bb, :], identity=C.ident[:]),
                  R=[b_misc, C.b_const], W=[bps])
            P.add("act", lambda e, bb=bb, ps=ps: e.copy(out=bguT[:, bb * 128:(bb + 1) * 128], in_=ps[:, 0:128]), R=[bps], W=[b_misc])
        bg3 = bguT[:].rearrange("p (e c) -> p e c", c=16)
        P.add("dve", lambda e: e.tensor_scalar(out=bg3[:, :, 8:16], in0=bg3[:, :, 8:16], scalar1=1.0, scalar2=None, op0=ALU.add), R=[b_misc], W=[b_misc])
        for t in range(NT):
            P.add("act", lambda e, t=t: e.copy(out=hb[:, t, :], in_=h[:, t, :]), R=[C.bh[t]], W=allhT)
        for t in range(NT):
            ps, bps = C.ps[2], C.bps[2]
            P.add("pe", lambda e, t=t: e.transpose(out=ps[0:NE, 0:128], in_=C.gates[:, t, :], identity=C.ident[:]),
                  R=[C.b_gates[t], C.b_const], W=[bps])
            P.add("act", lambda e: e.copy(out=gT[:], in_=ps[0:NE, 0:128]), R=[bps], W=[b_gT])
            for n in range(2):
                po, bpo = C.ps[n], C.bps[n]
                P.add("pe", lambda e, n=n, po=po: e.matmul(po[:], lhsT=gT[:], rhs=bd[:, n * 512:(n + 1) * 512], start=True, stop=True),
                      R=[b_gT, b_misc], W=[bpo])
                P.add("dve", lambda e, n=n, po=po, t=t: e.scalar_tensor_tensor(out=h[:, t, n * 512:(n + 1) * 512], in0=h[:, t, n * 512:(n + 1) * 512],
                                                                               scalar=ALPHA, in1=po[:], op0=ALU.mult, op1=ALU.add),
                      R=[bpo, C.bh[t]], W=[C.bh[t]])
        P.add("dve", lambda e: e.tensor_scalar(out=mask[:], in0=C.gates[:], scalar1=0.0, scalar2=None, op0=ALU.is_gt), R=list(C.b_gates), W=[b_mask])
        pp, bpp = C.ps[2], C.bps[2]
        for t in range(NT):
            gi, i = divmod(t, 4)
            for i2 in range(i):
                P.add("pe", lambda e, t=t, gi=gi, i2=i2: e.matmul(pp[:, t * NE:(t + 1) * NE], lhsT=onesb[:], rhs=mask[:, 4 * gi + i2, :],
                                                                start=(i2 == 0), stop=False), R=[b_mask, b_cst], W=[bpp])
            P.add("pe", lambda e, t=t, i=i: e.matmul(pp[:, t * NE:(t + 1) * NE], lhsT=tris[:], rhs=mask[:, t, :], start=(i == 0), stop=True),
                  R=[b_mask, b_cst], W=[bpp])
        P.add("dve", lambda e: e.scalar_tensor_tensor(out=posm[:], in0=pp[:].rearrange("p (t x) -> p t x", t=NT), scalar=1.0, in1=mask[:],
                                                      op0=ALU.add, op1=ALU.mult), R=[bpp, b_mask], W=[b_pos])
        P.add("dve", lambda e: e.tensor_scalar(out=posm[:], in0=posm[:], scalar1=-1.0, scalar2=None, op0=ALU.add), R=[b_pos], W=[b_pos])

        wgu = T["moe_w_gate_up_%d" % li]
        wd = T["moe_w_down_%d" % li]

        def bank():
            y = C.y_rr % 4
            C.y_rr += 1
            return C.ps[BANKS[y]], C.bps[BANKS[y]]

        def p_build(ex):
            for t in range(NT):
                P.add("dve", lambda e, t=t: e.tensor_scalar(out=Pm[:, t, :], in0=iota[:], scalar1=posm[:, t, ex:ex + 1], scalar2=None, op0=ALU.is_equal),
                      R=[b_pos, b_cst], W=[b_P])

        def gather_unit(ex):
            for gi in range(4):
                for kh in range(2):
                    px, bpx = bank()
                    for kk in range(4):
                        k = kh * 4 + kk
                        for i in range(4):
                            t = 4 * gi + i
                            P.add("pe", lambda e, kk=kk, k=k, t=t, i=i, px=px: e.matmul(px[:, kk * 128:(kk + 1) * 128], lhsT=hb[:, t, k * 128:(k + 1) * 128],
                                                                                      rhs=Pm[:, t, :], start=(i == 0), stop=(i == 3)), R=[b_P] + allhT, W=[bpx])
                    P.add("act", lambda e, kh=kh, gi=gi, px=px: e.copy(out=xgT[:, kh * 4:kh * 4 + 4, gi * 128:(gi + 1) * 128],
                                                                      in_=px[:].rearrange("p (a c) -> p a c", a=4)), R=[bpx], W=[b_xg])

        def pt_unit(ex):
            for gi in range(4):
                ptp, bptp = bank()
                ptb = ptp[:].bitcast(BF16)
                for i in range(4):
                    P.add("pe", lambda e, i=i, gi=gi, ptb=ptb: e.transpose(out=ptb[:, i * 128:(i + 1) * 128], in_=Pm[:, 4 * gi + i, :], identity=C.identb[:]),
                          R=[b_P, C.b_const], W=[bptp])
                P.add("act", lambda e, gi=gi, ptb=ptb: e.copy(out=PTm[:, gi, :], in_=ptb[:, 0:512]), R=[bptp], W=[b_PT])

        def w1_unit(ex, hf, s, a):
            bw = b_w1[s]
            for j in range(4):
                q = C.q_rr % 2
                C.q_rr += 1
                pg, bpg = C.ps[3 + q], C.bps[3 + q]
                pu, bpu = C.ps[5 + q], C.bps[5 + q]
                for k in range(8):
                    P.add("pe", lambda e, k=k, j=j, pg=pg: e.matmul(pg[:], lhsT=w1g[:, s, k, j * 128:(j + 1) * 128], rhs=xgT[:, k, :],
                                                                  start=(k == 0), stop=(k == 7)), R=[bw, b_xg], W=[bpg])
                for k in range(8):
                    P.add("pe", lambda e, k=k, j=j, pu=pu: e.matmul(pu[:], lhsT=w1u[:, s, k, j * 128:(j + 1) * 128], rhs=xgT[:, k, :],
                                                                  start=(k == 0), stop=(k == 7)), R=[bw, b_xg], W=[bpu])
                cg = ex * 16 + hf * 4 + j
                cu = ex * 16 + 8 + hf * 4 + j
                P.add("dve", lambda e, q=q, pg=pg, cg=cg: e.tensor_scalar(out=g1[:, q, :], in0=pg[:], scalar1=bguT[:, cg:cg + 1], scalar2=7.0,
                                                                         op0=ALU.add, op1=ALU.min), R=[bpg, b_misc], W=[b_g1[q]])
                P.add("act", lambda e, q=q: e.activation(out=sg[:, q, :], in_=g1[:, q, :], func=AF.Sigmoid, scale=1.702), R=[b_g1[q]], W=[b_sg[q]])
                P.add("dve", lambda e, q=q, pu=pu, cu=cu: e.tensor_scalar(out=u1[:, q, :], in0=pu[:], scalar1=bguT[:, cu:cu + 1], scalar2=8.0,
                                                                         op0=ALU.add, op1=ALU.min), R=[bpu, b_misc], W=[b_u1[q]])
                P.add("dve", lambda e, q=q: e.tensor_tensor(out=sg[:, q, :], in0=sg[:, q, :], in1=g1[:, q, :], op=ALU.mult),
                      R=[b_sg[q], b_g1[q]], W=[b_sg[q]])
                P.add("dve", lambda e, q=q, j=j: e.scalar_tensor_tensor(out=actT[:, a, j, :], in0=u1[:, q, :], scalar=-6.0, in1=sg[:, q, :],
                                                                       op0=ALU.max, op1=ALU.mult), R=[b_sg[q], b_u1[q]], W=[b_act[a]])

        def w2_unit(ex):
            ybs = {}

            def down(tt):
                yb = C.ys_rr % 2
                C.ys_rr += 1
                ybs[tt] = yb
                for n in range(2):
                    py, bpy = bank()
                    for hf in range(2):
                        for j in range(4):
                            P.add("pe", lambda e, j=j, n=n, hf=hf, py=py: e.matmul(py[:], lhsT=actT[:, hf, j, tt * 128:(tt + 1) * 128],
                                                                                 rhs=w2[:, hf, j, n * 512:(n + 1) * 512],
                                                                                 start=(hf == 0 and j == 0), stop=(hf == 1 and j == 3)),
                                  R=[b_act[hf], b_w2[hf]], W=[bpy])
                    P.add("act", lambda e, n=n, yb=yb, py=py: e.copy(out=ys[:, yb, n * 512:(n + 1) * 512], in_=py[:]), R=[bpy], W=[b_ys[yb]])

            def scatter(tt):
                yb = ybs[tt]
                for i in range(4):
                    t = 4 * tt + i
                    for n in range(2):
                        pz, bpz = bank()
                        P.add("pe", lambda e, i=i, n=n, pz=pz: e.matmul(pz[:], lhsT=PTm[:, tt, i * 128:(i + 1) * 128],
                                                                      rhs=ys[:, yb, n * 512:(n + 1) * 512], start=True, stop=True),
                              R=[b_PT, b_ys[yb]], W=[bpz])
                        P.add("dve", lambda e, t=t, n=n, pz=pz: e.scalar_tensor_tensor(
                            out=h[:, t, n * 512:(n + 1) * 512], in0=pz[:], scalar=C.gates[:, t, ex:ex + 1], in1=h[:, t, n * 512:(n + 1) * 512],
                            op0=ALU.mult, op1=ALU.add), R=[bpz, C.b_gates[t], C.bh[t]], W=[C.bh[t]])

            down(0)
            down(1)
            scatter(0)
            down(2)
            scatter(1)
            down(3)
            scatter(2)
            scatter(3)

        def dma_w1(ex, hf):
            P.add("pool", lambda e: e.dma_start(
                out=w1g[:, hf], in_=wgu[ex, :, hf * 512:(hf + 1) * 512].rearrange("(k p) n -> p k n", p=128)), W=[b_w1[hf]], dma=True)
            P.add("pool", lambda e: e.dma_start(
                out=w1u[:, hf], in_=wgu[ex, :, 1024 + hf * 512:1024 + (hf + 1) * 512].rearrange("(k p) n -> p k n", p=128)), W=[b_w1[hf]], dma=True)

        def dma_w2(ex, hf):
            P.add("pool", lambda e: e.dma_start(
                out=w2[:, hf], in_=wd[ex, hf * 512:(hf + 1) * 512, :].rearrange("(j p) n -> p j n", p=128)), W=[b_w2[hf]], dma=True)

        NX = C.n_experts
        if NX > 0:
            dma_w1(0, 0)
            dma_w1(0, 1)
            dma_w2(0, 0)
            dma_w2(0, 1)
            p_build(0)
            gather_unit(0)
        for ex in range(NX):
            nxt = ex + 1 < NX
            w1_unit(ex, 0, 0, 0)
            if nxt:
                dma_w1(ex + 1, 0)
            pt_unit(ex)
            if nxt:
                p_build(ex + 1)
            w1_unit(ex, 1, 1, 1)
            if nxt:
                dma_w1(ex + 1, 1)
                gather_unit(ex + 1)
            w2_unit(ex)
            if nxt:
                dma_w2(ex + 1, 0)
                dma_w2(ex + 1, 1)
        P.flush()
    with ExitStack() as es:
        lng = es.enter_context(nc.sbuf_tensor("m_lng_%d" % li, [128, D], F32))
        lnb = es.enter_context(nc.sbuf_tensor("m_lnb_%d" % li, [128, D], F32))
        blnp = Buf()
        load_ln_params(C, lng, lnb, blnp, T["ln_g"][li, 1, :], T["ln_b"][li, 1, :])
        for t in range(NT):
            ln_tile(C, t, lng, lnb, blnp, router=None)
        P.flush()


def out_proj_ln(C, li, T, srcT, nchunks, w_dram, R_):
    P, nc = C.P, C.nc
    h = C.h
    with ExitStack() as es:
        wo = es.enter_context(nc.sbuf_tensor("o_w_%d" % li, [128, nchunks, D], BF16))
        lng = es.enter_context(nc.sbuf_tensor("o_lng_%d" % li, [128, D], F32))
        lnb = es.enter_context(nc.sbuf_tensor("o_lnb_%d" % li, [128, D], F32))
        bw, blnp = Buf(), Buf()
        P.add("pool", lambda e: e.dma_start(out=wo[:], in_=w_dram.rearrange("(k p) n -> p k n", p=128)), W=[bw], dma=True)
        load_ln_params(C, lng, lnb, blnp, T["ln_g"][li, 0, :], T["ln_b"][li, 0, :])
        for t in range(NT):
            for n in range(2):
                po, bpo = C.ps[3 + n + 2 * (t % 2)], C.bps[3 + n + 2 * (t % 2)]
                for f in range(nchunks):
                    P.add("pe", lambda e, f=f, n=n, t=t, po=po: e.matmul(po[:], lhsT=srcT[:, f, t * 128:(t + 1) * 128], rhs=wo[:, f, n * 512:(n + 1) * 512],
                                                                      start=(f == 0), stop=(f == nchunks - 1)), R=[C.b_srcT, bw], W=[bpo])
                P.add("dve", lambda e, n=n, t=t, po=po: e.scalar_tensor_tensor(out=h[:, t, n * 512:(n + 1) * 512], in0=h[:, t, n * 512:(n + 1) * 512],
                                                                             scalar=ALPHA, in1=po[:], op0=ALU.mult, op1=ALU.add),
                      R=[bpo, C.bh[t]], W=[C.bh[t]])
            ln_tile(C, t, lng, lnb, blnp, router=R_)
        P.flush()


def moba_phase(C, li, T, R_):
    P, nc = C.P, C.nc
    h, hT = C.h, C.hT
    j = li // 3
    w_in = T["a_w_in_%d" % j]
    H = 8
    SCALE = 128 ** -0.5
    BIG = 30000.0
    with nc.sbuf_tensor("a_aT_%d" % li, [128, 8, S], BF16) as aT:
        C.b_srcT = Buf("aT")
        with ExitStack() as es:
            wq = es.enter_context(nc.sbuf_tensor("a_wq_%d" % li, [128, 2, 8, 384], BF16))
            qT = es.enter_context(nc.sbuf_tensor("a_qT_%d" % li, [128, 2, S], BF16))
            kT = es.enter_context(nc.sbuf_tensor("a_kT_%d" % li, [128, 2, S], BF16))
            V = es.enter_context(nc.sbuf_tensor("a_V_%d" % li, [128, 2, NT, 128], BF16))
            kms = es.enter_context(nc.sbuf_tensor("a_kms_%d" % li, [128, 8], F32))
            kmb = es.enter_context(nc.sbuf_tensor("a_kmb_%d" % li, [128, 8], BF16))
            g2 = es.enter_context(nc.sbuf_tensor("a_g2_%d" % li, [128, 128], F32))
            m8 = es.enter_context(nc.sbuf_tensor("a_m8_%d" % li, [128, 8], F32))
            bm = es.enter_context(nc.sbuf_tensor("a_bm_%d" % li, [128, 128], F32))
            cm = es.enter_context(nc.sbuf_tensor("a_cm_%d" % li, [128, 128], F32))
            vm = es.enter_context(nc.sbuf_tensor("a_vm_%d" % li, [128, 128], F32))
            vm2 = es.enter_context(nc.sbuf_tensor("a_vm2_%d" % li, [128, 128], F32))
            rel = es.enter_context(nc.sbuf_tensor("a_rel_%d" % li, [128, 8, 2, 128], F32))
            caus = es.enter_context(nc.sbuf_tensor("a_caus_%d" % li, [128, 128], F32))
            cb = es.enter_context(nc.sbuf_tensor("a_cb_%d" % li, [128, 8], F32))
            sc = es.enter_context(nc.sbuf_tensor("a_s_%d" % li, [128, 2, S], F32))
            pb = es.enter_context(nc.sbuf_tensor("a_p_%d" % li, [128, S], BF16))
            pT = es.enter_context(nc.sbuf_tensor("a_pT_%d" % li, [128, 2, 4, 128], BF16))
            stt = es.enter_context(nc.sbuf_tensor("a_st_%d" % li, [128, 2, 4], F32))
            b_wq = [Buf() for _ in range(2)]
            b_q = [Buf() for _ in range(2)]
            b_k = [Buf() for _ in range(2)]
            b_v = [Buf() for _ in range(2)]
            b_km, b_gate, b_cst = Buf(), Buf(), Buf()
            b_s = [Buf() for _ in range(2)]
            b_p = [Buf()] * 2
            b_pT = [Buf() for _ in range(2)]
            b_st = [Buf() for _ in range(2)]
            SK = ""
            if "rel" not in SK:
                P.add("sp", lambda e: e.dma_start(out=rel[:], in_=T["relT"].rearrange("h d i j -> i h d j")), W=[b_cst], dma=True)
            P.add("sp", lambda e: e.dma_start(out=caus[:], in_=T["causal"]), W=[b_cst], dma=True)
            P.add("sp", lambda e: e.dma_start(out=vm[:], in_=T["vmask"]), W=[b_cst], dma=True)
            P.add("sp", lambda e: e.dma_start(out=vm2[:], in_=T["vmask2"]), W=[b_cst], dma=True)
            if "cb" not in SK:
                P.add("sp", lambda e: e.dma_start(out=cb[:], in_=bcast_rows(T["rel_bias"][31, :])), W=[b_cst], dma=True)
            for hh in range(H):
                P.add("pool", lambda e, hh=hh: e.tensor_tensor(out=rel[:, hh, 0, :], in0=rel[:, hh, 0, :], in1=caus[:], op=ALU.add), R=[b_cst], W=[b_cst])
            allT = list(C.bhT)
            for hh in range(H):
                s = hh % 2
                for c3 in range(3):
                    P.add("pool", lambda e, s=s, c3=c3, hh=hh: e.dma_start(
                        out=wq[:, s, :, c3 * 128:(c3 + 1) * 128],
                        in_=w_in[:, c3 * 1024 + hh * 128:c3 * 1024 + (hh + 1) * 128].rearrange("(k p) n -> p k n", p=128)), W=[b_wq[s]], dma=True)
                for g in range(4):
                    pq, bpq = C.ps[3], C.bps[3]
                    pk, bpk = C.ps[4], C.bps[4]
                    for k in range(8):
                        P.add("pe", lambda e, k=k, g=g, s=s: e.matmul(pq[:], lhsT=wq[:, s, k, 0:128], rhs=hT[:, k, g * 512:(g + 1) * 512],
                                                                    start=(k == 0), stop=(k == 7)), R=[b_wq[s]] + allT[g * 4:g * 4 + 4], W=[bpq])
                    P.add("act", lambda e, g=g, s=s: e.copy(out=qT[:, s, g * 512:(g + 1) * 512], in_=pq[:]), R=[bpq], W=[b_q[s]])
                    for k in range(8):
                        P.add("pe", lambda e, k=k, g=g, s=s: e.matmul(pk[:], lhsT=wq[:, s, k, 128:256], rhs=hT[:, k, g * 512:(g + 1) * 512],
                                                                    start=(k == 0), stop=(k == 7)), R=[b_wq[s]] + allT[g * 4:g * 4 + 4], W=[bpk])
                    for b2 in range(2):
                        P.add("act", lambda e, g=g, s=s, b2=b2: e.activation(out=kT[:, s, g * 512 + b2 * 256:g * 512 + (b2 + 1) * 256],
                                                                           in_=pk[:, b2 * 256:(b2 + 1) * 256], func=AF.Copy,
                                                                           accum_out=kms[:, 2 * g + b2:2 * g + b2 + 1]), R=[bpk], W=[b_k[s], b_km])
                P.add("dve", lambda e: e.tensor_scalar(out=kmb[:], in0=kms[:], scalar1=1.0 / 256.0, scalar2=None, op0=ALU.mult), R=[b_km], W=[b_km])
                for g in range(4):
                    pv, bpv = C.ps[5], C.bps[5]
                    for tt in range(4):
                        t = g * 4 + tt
                        for k in range(8):
                            P.add("pe", lambda e, k=k, t=t, tt=tt, s=s: e.matmul(pv[:, tt * 128:(tt + 1) * 128], lhsT=hT[:, k, t * 128:(t + 1) * 128],
                                                                               rhs=wq[:, s, k, 256:384], start=(k == 0), stop=(k == 7)),
                                  R=[b_wq[s], C.bhT[t]], W=[bpv])
                    P.add("act", lambda e, g=g, s=s: e.copy(out=V[:, s, g * 4:g * 4 + 4, :], in_=pv[:].rearrange("p (a c) -> p a c", a=4)),
                          R=[bpv], W=[b_v[s]])
                MD = 9
                if MD < 2:
                    continue
                pg, bpg = C.ps[6], C.bps[6]
                for qt in range(NT):
                    P.add("pe", lambda e, qt=qt, s=s: e.matmul(pg[:, qt * 8:(qt + 1) * 8], lhsT=qT[:, s, qt * 128:(qt + 1) * 128], rhs=kmb[:],
                                                             start=True, stop=True), R=[b_q[s], b_km], W=[bpg])
                P.add("dve", lambda e: e.tensor_tensor(out=g2[:], in0=pg[:, 0:128], in1=vm[:], op=ALU.add), R=[bpg, b_cst], W=[b_gate])
                for qt in range(NT):
                    P.add("dve", lambda e, qt=qt: e.max(out=m8[:], in_=g2[:, qt * 8:(qt + 1) * 8]), R=[b_gate], W=[b_gate])
                    P.add("dve", lambda e, qt=qt: e.tensor_scalar(out=bm[:, qt * 8:(qt + 1) * 8], in0=g2[:, qt * 8:(qt + 1) * 8], scalar1=m8[:, 2:3],
                                                                  scalar2=BIG, op0=ALU.is_ge, op1=ALU.mult), R=[b_gate], W=[b_gate])
                P.add("dve", lambda e: e.tensor_tensor(out=bm[:], in0=bm[:], in1=vm2[:], op=ALU.add), R=[b_gate, b_cst], W=[b_gate])
                P.add("dve", lambda e, hh=hh: e.tensor_scalar(out=cm[:], in0=bm[:], scalar1=cb[:, hh:hh + 1], scalar2=None, op0=ALU.add),
                      R=[b_gate, b_cst], W=[b_gate])
                for qt in range(NT if MD >= 3 else 0):
                    qb = qt // 2
                    nk = qt + 1
                    z = qt % 2
                    for c in range((nk + 3) // 4):
                        w = min(4, nk - c * 4)
                        pss, bpss = C.ps[3 + (C.q_rr % 2)], C.bps[3 + (C.q_rr % 2)]
                        C.q_rr += 1
                        P.add("pe", lambda e, c=c, w=w, qt=qt, s=s, pss=pss: e.matmul(pss[:, 0:w * 128], lhsT=qT[:, s, qt * 128:(qt + 1) * 128],
                                                                                   rhs=kT[:, s, c * 512:c * 512 + w * 128], start=True, stop=True),
                              R=[b_q[s], b_k[s]], W=[bpss])
                        for i in range(w):
                            kt = c * 4 + i
                            n = kt // 2
                            src = pss[:, i * 128:(i + 1) * 128]
                            dst = sc[:, z, kt * 128:(kt + 1) * 128]
                            if kt == qt:
                                P.add("dve", lambda e, src=src, dst=dst, hh=hh: e.scalar_tensor_tensor(out=dst, in0=src, scalar=SCALE, in1=rel[:, hh, 0, :],
                                                                                                     op0=ALU.mult, op1=ALU.add), R=[bpss, b_cst], W=[b_s[z]])
                            elif kt == qt - 1:
                                P.add("dve", lambda e, src=src, dst=dst, hh=hh: e.scalar_tensor_tensor(out=dst, in0=src, scalar=SCALE, in1=rel[:, hh, 1, :],
                                                                                                     op0=ALU.mult, op1=ALU.add), R=[bpss, b_cst], W=[b_s[z]])
                                if n < qb:
                                    P.add("dve", lambda e, dst=dst, qt=qt, n=n: e.tensor_scalar(out=dst, in0=dst, scalar1=bm[:, qt * 8 + n:qt * 8 + n + 1],
                                                                                              scalar2=None, op0=ALU.add), R=[b_s[z], b_gate], W=[b_s[z]])
                            else:
                                P.add("dve", lambda e, src=src, dst=dst, qt=qt, n=n: e.tensor_scalar(out=dst, in0=src, scalar1=SCALE,
                                                                                                   scalar2=cm[:, qt * 8 + n:qt * 8 + n + 1],
                                                                                                   op0=ALU.mult, op1=ALU.add), R=[bpss, b_gate], W=[b_s[z]])
                    L = nk * 128
                    if MD < 4:
                        continue
                    P.add("dve", lambda e, z=z, L=L: e.reduce_max(out=stt[:, z, 0:1], in_=sc[:, z, 0:L], axis=AX.X), R=[b_s[z]], W=[b_st[z]])
                    P.add("dve", lambda e, z=z: e.tensor_scalar(out=stt[:, z, 1:2], in0=stt[:, z, 0:1], scalar1=-1.0, scalar2=None, op0=ALU.mult),
                          R=[b_st[z]], W=[b_st[z]])
                    P.add("act", lambda e, z=z, L=L: e.activation(out=sc[:, z, 0:L], in_=sc[:, z, 0:L], func=AF.Exp, bias=stt[:, z, 1:2], scale=1.0,
                                                                  accum_out=stt[:, z, 2:3]), R=[b_s[z], b_st[z]], W=[b_s[z], b_st[z]])
                    P.add("dve", lambda e, z=z: e.reciprocal(out=stt[:, z, 3:4], in_=stt[:, z, 2:3]), R=[b_st[z]], W=[b_st[z]])
                    P.add("act", lambda e, z=z, L=L: e.activation(out=pb[:, 0:L], in_=sc[:, z, 0:L], func=AF.Copy, scale=stt[:, z, 3:4]),
                          R=[b_s[z], b_st[z]], W=[b_p[z]])
                    po, bpo = C.ps[5 + z], C.bps[5 + z]
                    if MD < 5:
                        continue
                    for c in range((nk + 3) // 4):
                        w = min(4, nk - c * 4)
                        y = C.y_rr % 2
                        C.y_rr += 1
                        ptp, bptp = C.ps[(0, 1)[y]], C.bps[(0, 1)[y]]
                        ptb = ptp[:].bitcast(BF16)
                        for i in range(w):
                            kt = c * 4 + i
                            P.add("pe", lambda e, i=i, kt=kt, z=z, ptb=ptb: e.transpose(out=ptb[:, i * 128:(i + 1) * 128], in_=pb[:, kt * 128:(kt + 1) * 128],
                                                                                      identity=C.identb[:]), R=[b_p[z], C.b_const], W=[bptp])
                        P.add("act", lambda e, w=w, y=y, ptb=ptb: e.copy(out=pT[:, y, 0:w, :], in_=ptb[:, 0:w * 128].rearrange("p (a c) -> p a c", a=w)),
                              R=[bptp], W=[b_pT[y]])
                        for i in range(w):
                            kt = c * 4 + i
                            P.add("pe", lambda e, i=i, kt=kt, y=y, s=s, qt=qt, nk=nk, po=po: e.matmul(po[:, 0:128], lhsT=V[:, s, kt, :], rhs=pT[:, y, i, :],
                                                                                                   start=(kt == 0), stop=(kt == nk - 1)),
                                  R=[b_v[s], b_pT[y]], W=[bpo])
                    P.add("act", lambda e, po=po, hh=hh, qt=qt: e.copy(out=aT[:, hh, qt * 128:(qt + 1) * 128], in_=po[:, 0:128]), R=[bpo], W=[C.b_srcT])
            P.flush()
        out_proj_ln(C, li, T, aT, 8, T["a_w_out_%d" % j], R_)


def gmlp_phase(C, li, T, R_):
    P, nc = C.P, C.nc
    h, hT = C.h, C.hT
    j = li // 3
    w_in = T["b_w_in_%d" % j]
    w_out = T["b_w_out_%d" % j]
    with ExitStack() as es:
        lng = es.enter_context(nc.sbuf_tensor("g_lng_%d" % li, [128, D], F32))
        lnb = es.enter_context(nc.sbuf_tensor("g_lnb_%d" % li, [128, D], F32))
        wi = es.enter_context(nc.sbuf_tensor("g_wi_%d" % li, [128, 2, 8, 512], BF16))
        wo = es.enter_context(nc.sbuf_tensor("g_wo_%d" % li, [128, 2, 4, 1024], BF16))
        yT = es.enter_context(nc.sbuf_tensor("g_yT_%d" % li, [128, 16, 256], BF16))
        v = es.enter_context(nc.sbuf_tensor("g_v_%d" % li, [128, 2, 2048], F32))
        vn = es.enter_context(nc.sbuf_tensor("g_vn_%d" % li, [128, 2, 2048], BF16))
        gb = es.enter_context(nc.sbuf_tensor("g_gb_%d" % li, [128, 2, 2048], F32))
        ws = es.enter_context(nc.sbuf_tensor("g_ws_%d" % li, [128, 8, 128], F32))
        wsT = es.enter_context(nc.sbuf_tensor("g_wsT_%d" % li, [128, 8, 128], BF16))
        tril = es.enter_context(nc.sbuf_tensor("g_tril_%d" % li, [128, 128], F32))
        bs = es.enter_context(nc.sbuf_tensor("g_bs_%d" % li, [1, 1024], F32))
        ones = es.enter_context(nc.sbuf_tensor("g_ones_%d" % li, [1, 128], F32))
        st = es.enter_context(nc.sbuf_tensor("g_st_%d" % li, [128, 4, 6], F32))
        mv = es.enter_context(nc.sbuf_tensor("g_mv_%d" % li, [128, 2], F32))
        rs = es.enter_context(nc.sbuf_tensor("g_rs_%d" % li, [128, 1], F32))
        blnp, b_cst, b_wsT, b_yT, b_st = Buf(), Buf(), Buf(), Buf(), Buf()
        b_wi = [Buf(), Buf()]
        b_wo = [Buf(), Buf()]
        b_v = [Buf(), Buf()]
        b_vn = [Buf(), Buf()]
        load_ln_params(C, lng, lnb, blnp, T["ln_g"][li, 0, :], T["ln_b"][li, 0, :])
        P.add("sp", lambda e: e.dma_start(out=gb[:, 0, :], in_=bcast_rows(T["b_ln_g_%d" % j])), W=[b_cst], dma=True)
        P.add("sp", lambda e: e.dma_start(out=gb[:, 1, :], in_=bcast_rows(T["b_ln_b_%d" % j])), W=[b_cst], dma=True)
        P.add("sp", lambda e: e.dma_start(out=ws[:], in_=T["b_w_s_%d" % j].rearrange("g t s -> t g s")), W=[b_cst], dma=True)
        P.add("sp", lambda e: e.dma_start(out=tril[:], in_=T["tril"]), W=[b_cst], dma=True)
        P.add("sp", lambda e: e.dma_start(out=bs[:], in_=T["b_b_s_%d" % j].rearrange("(o g) t -> o (g t)", o=1)), W=[b_cst], dma=True)
        P.add("pool", lambda e: e.memset(ones[:], 1.0), W=[b_cst])
        for g in range(8):
            P.add("dve", lambda e, g=g: e.tensor_tensor(out=ws[:, g, :], in0=ws[:, g, :], in1=tril[:], op=ALU.mult), R=[b_cst], W=[b_cst])
        for half in range(2):
            ps, bps = C.ps[half], C.bps[half]
            for gg in range(4):
                g = half * 4 + gg
                P.add("pe", lambda e, g=g, gg=gg, ps=ps: e.transpose(out=ps[:, gg * 128:(gg + 1) * 128], in_=ws[:, g, :], identity=C.ident[:]),
                      R=[b_cst, C.b_const], W=[bps])
            P.add("act", lambda e, ps=ps, half=half: e.copy(out=wsT[:, half * 4:half * 4 + 4, :], in_=ps[:].rearrange("p (a c) -> p a c", a=4)),
                  R=[bps], W=[b_wsT])
        it = 0
        iw = 0
        acc_banks = (5, 6, 7, 2)
        DBG = 9
        for tg in range(8 if DBG >= 2 else 0):
            c0 = tg * 256
            tiles = (2 * tg, 2 * tg + 1)
            bhTg = [C.bhT[t] for t in tiles]
            for cg in range(8):
                s = it % 2
                it += 1
                P.add("pool", lambda e, s=s, cg=cg: e.dma_start(out=wi[:, s], in_=w_in[:, cg * 512:(cg + 1) * 512].rearrange("(k p) n -> p k n", p=128)),
                      W=[b_wi[s]], dma=True)
                if cg < 4:
                    for jj in range(4):
                        fc = cg * 4 + jj
                        q = C.q_rr % 2
                        C.q_rr += 1
                        pu, bpu = C.ps[3 + q], C.bps[3 + q]
                        for k in range(8):
                            P.add("pe", lambda e, k=k, jj=jj, s=s, pu=pu, c0=c0: e.matmul(pu[:, 0:256], lhsT=wi[:, s, k, jj * 128:(jj + 1) * 128],
                                                                                      rhs=hT[:, k, c0:c0 + 256], start=(k == 0), stop=(k == 7)),
                                  R=[b_wi[s]] + bhTg, W=[bpu])
                        P.add("act", lambda e, fc=fc, pu=pu: e.activation(out=yT[:, fc, :], in_=pu[:, 0:256], func=AF.Gelu), R=[bpu], W=[b_yT])
                else:
                    for ti in range(2):
                        t = tiles[ti]
                        q = C.q_rr % 2
                        C.q_rr += 1
                        pv, bpv = C.ps[3 + q], C.bps[3 + q]
                        for k in range(8):
                            P.add("pe", lambda e, k=k, t=t, s=s, pv=pv: e.matmul(pv[:], lhsT=hT[:, k, t * 128:(t + 1) * 128], rhs=wi[:, s, k, :],
                                                                               start=(k == 0), stop=(k == 7)), R=[b_wi[s], C.bhT[t]], W=[bpv])
                        P.add("act", lambda e, ti=ti, cg=cg, pv=pv: e.activation(out=v[:, ti, (cg - 4) * 512:(cg - 3) * 512], in_=pv[:], func=AF.Gelu),
                              R=[bpv], W=[b_v[ti]])
            for ti in range(2 if DBG >= 3 else 0):
                for c in range(4):
                    P.add("dve", lambda e, c=c, ti=ti: e.bn_stats(out=st[:, c, :], in_=v[:, ti, c * 512:(c + 1) * 512]), R=[b_v[ti]], W=[b_st])
                P.add("dve", lambda e: e.bn_aggr(out=mv[:], in_=st[:]), R=[b_st], W=[b_st])
                P.add("act", lambda e: e.activation(out=rs[:], in_=mv[:, 1:2], func=AF.Sqrt, bias=EPS, scale=1.0), R=[b_st], W=[b_st])
                P.add("dve", lambda e: e.reciprocal(out=rs[:], in_=rs[:]), R=[b_st], W=[b_st])
                P.add("dve", lambda e, ti=ti: e.tensor_scalar(out=v[:, ti, :], in0=v[:, ti, :], scalar1=mv[:, 0:1], scalar2=rs[:, 0:1],
                                                              op0=ALU.subtract, op1=ALU.mult), R=[b_v[ti], b_st], W=[b_v[ti]])
                P.add("pool", lambda e, ti=ti: e.tensor_tensor(out=v[:, ti, :], in0=v[:, ti, :], in1=gb[:, 0, :], op=ALU.mult), R=[b_v[ti], b_cst], W=[b_v[ti]])
                P.add("pool", lambda e, ti=ti: e.tensor_tensor(out=vn[:, ti, :], in0=v[:, ti, :], in1=gb[:, 1, :], op=ALU.add), R=[b_v[ti], b_cst], W=[b_vn[ti]])
            for ti in range(2 if DBG >= 4 else 0):
                for bk in range(4):
                    q = C.q_rr % 2
                    C.q_rr += 1
                    pm, bpm = C.ps[3 + q], C.bps[3 + q]
                    for jj in range(4):
                        fc = bk * 4 + jj
                        g = fc // 2
                        P.add("pe", lambda e, jj=jj, fc=fc, g=g, ti=ti, pm=pm: e.matmul(pm[:, jj * 128:(jj + 1) * 128], lhsT=vn[:, ti, fc * 128:(fc + 1) * 128],
                                                                                      rhs=wsT[:, g, :], start=True, stop=False), R=[b_vn[ti], b_wsT], W=[bpm])
                        P.add("pe", lambda e, jj=jj, g=g, pm=pm: e.matmul(pm[:, jj * 128:(jj + 1) * 128], lhsT=ones[0:1, :], rhs=bs[0:1, g * 128:(g + 1) * 128],
                                                                        start=False, stop=True), R=[b_cst], W=[bpm])
                    P.add("dve", lambda e, bk=bk, ti=ti, pm=pm: e.tensor_tensor(out=yT[:, bk * 4:bk * 4 + 4, ti * 128:(ti + 1) * 128],
                                                                              in0=pm[:].rearrange("p (a c) -> p a c", a=4),
                                                                              in1=yT[:, bk * 4:bk * 4 + 4, ti * 128:(ti + 1) * 128], op=ALU.mult),
                          R=[bpm, b_yT], W=[b_yT])
            if DBG < 5:
                continue
            for wc in range(4):
                s2 = iw % 2
                iw += 1
                P.add("pool", lambda e, s2=s2, wc=wc: e.dma_start(out=wo[:, s2], in_=w_out[wc * 512:(wc + 1) * 512, :].rearrange("(f p) n -> p f n", p=128)),
                      W=[b_wo[s2]], dma=True)
                for ti in range(2):
                    for n in range(2):
                        bi = acc_banks[ti * 2 + n]
                        po, bpo = C.ps[bi], C.bps[bi]
                        for ff in range(4):
                            P.add("pe", lambda e, wc=wc, ff=ff, ti=ti, n=n, s2=s2, po=po: e.matmul(
                                po[:], lhsT=yT[:, wc * 4 + ff, ti * 128:(ti + 1) * 128], rhs=wo[:, s2, ff, n * 512:(n + 1) * 512],
                                start=(wc == 0 and ff == 0), stop=(wc == 3 and ff == 3)), R=[b_yT, b_wo[s2]], W=[bpo])
            for ti in range(2):
                t = tiles[ti]
                for n in range(2):
                    bi = acc_banks[ti * 2 + n]
                    po, bpo = C.ps[bi], C.bps[bi]
                    P.add("dve", lambda e, n=n, t=t, po=po: e.scalar_tensor_tensor(out=h[:, t, n * 512:(n + 1) * 512], in0=h[:, t, n * 512:(n + 1) * 512],
                                                                                 scalar=ALPHA, in1=po[:], op0=ALU.mult, op1=ALU.add),
                          R=[bpo, C.bh[t]], W=[C.bh[t]])
            for ti in range(2 if DBG >= 6 else 0):
                ln_tile(C, tiles[ti], lng, lnb, blnp, router=(R_ if DBG >= 7 else None))
        P.flush()


def gla_phase(C, li, T, R_):
    P, nc = C.P, C.nc
    h, hT = C.h, C.hT
    j = li // 3
    w_in = T["c_w_in_%d" % j]
    SCALE = 128 ** -0.5
    with nc.sbuf_tensor("c_oT_%d" % li, [128, 8, S], BF16) as oT:
        C.b_srcT = Buf("oT")
        with ExitStack() as es:
            wq = es.enter_context(nc.sbuf_tensor("c_wq_%d" % li, [128, 2, 8, 768], BF16))
            wg = es.enter_context(nc.sbuf_tensor("c_wg_%d" % li, [128, 8, 16], BF16))
            gkT = es.enter_context(nc.sbuf_tensor("c_gkT_%d" % li, [32, S], F32))
            wgk = es.enter_context(nc.sbuf_tensor("c_wgk_%d" % li, [32, 512], F32))
            ng = es.enter_context(nc.sbuf_tensor("c_ng_%d" % li, [128, 256], F32))
            tri = es.enter_context(nc.sbuf_tensor("c_tri_%d" % li, [128, 128], F32))
            su = es.enter_context(nc.sbuf_tensor("c_su_%d" % li, [128, 128], F32))
            cmk = es.enter_context(nc.sbuf_tensor("c_cm_%d" % li, [128, 128], F32))
            qk = es.enter_context(nc.sbuf_tensor("c_qk_%d" % li, [128, 2, 2, 128], F32))
            kt = es.enter_context(nc.sbuf_tensor("c_kt_%d" % li, [128, 2, 128], F32))
            vt = es.enter_context(nc.sbuf_tensor("c_vt_%d" % li, [128, 2, 256], BF16))
            gs = es.enter_context(nc.sbuf_tensor("c_gs_%d" % li, [128, 2, 256], F32))
            nl = es.enter_context(nc.sbuf_tensor("c_nl_%d" % li, [128, 2, 128], F32))
            ex = es.enter_context(nc.sbuf_tensor("c_ex_%d" % li, [128, 2, 3, 128], F32))
            qz = es.enter_context(nc.sbuf_tensor("c_qz_%d" % li, [128, 2, 2, 128], BF16))
            ke = es.enter_context(nc.sbuf_tensor("c_ke_%d" % li, [128, 2, 128], BF16))
            krz = es.enter_context(nc.sbuf_tensor("c_krz_%d" % li, [128, 2, 2, 128], BF16))
            att = es.enter_context(nc.sbuf_tensor("c_att_%d" % li, [128, 2, 128], BF16))
            St = es.enter_context(nc.sbuf_tensor("c_S_%d" % li, [128, 256], F32))
            Sb = es.enter_context(nc.sbuf_tensor("c_Sb_%d" % li, [128, 5, 256], BF16))
            jk = es.enter_context(nc.sbuf_tensor("c_jk_%d" % li, [128, 256], F32))
            ot = es.enter_context(nc.sbuf_tensor("c_ot_%d" % li, [128, 2, 256], F32))
            og = es.enter_context(nc.sbuf_tensor("c_og_%d" % li, [128, 2, 256], BF16))
            ss = es.enter_context(nc.sbuf_tensor("c_ss_%d" % li, [128, 2, 2], F32))
            b_cst, b_gk, b_S, b_jk = Buf(), Buf(), Buf(), Buf()
            b_wq = [Buf(), Buf()]
            b_qk = [Buf(), Buf()]
            b_kt = [Buf(), Buf()]
            b_vt = [Buf(), Buf()]
            b_gs = [Buf(), Buf()]
            b_nl = [Buf(), Buf()]
            b_ex = [Buf(), Buf()]
            b_qz = [Buf(), Buf()]
            b_ke = [Buf(), Buf()]
            b_krz = [Buf(), Buf()]
            b_att = [Buf(), Buf()]
            b_Sb = [Buf() for _ in range(5)]
            b_ot = [Buf(), Buf()]
            b_og = [Buf(), Buf()]
            b_ss = [Buf(), Buf()]
            P.add("pool", lambda e: e.dma_start(out=wg[:], in_=w_in[:, 3072:3088].rearrange("(k p) n -> p k n", p=128)), W=[b_cst], dma=True)
            P.add("sp", lambda e: e.dma_start(out=wgk[0:16, :], in_=T["c_w_gk_up_%d" % j]), W=[b_cst], dma=True)
            P.add("sp", lambda e: e.dma_start(out=wgk[16:17, :], in_=T["c_b_gk_%d" % j].rearrange("(o n) -> o n", o=1)), W=[b_cst], dma=True)
            P.add("sp", lambda e: e.dma_start(out=ng[:], in_=bcast_rows(T["c_norm_g_%d" % j])), W=[b_cst], dma=True)
            P.add("sp", lambda e: e.dma_start(out=tri[:], in_=T["gla_tri"]), W=[b_cst], dma=True)
            P.add("sp", lambda e: e.dma_start(out=su[:], in_=T["gla_su"]), W=[b_cst], dma=True)
            P.add("sp", lambda e: e.dma_start(out=cmk[:], in_=T["gla_cm"]), W=[b_cst], dma=True)
            P.add("pool", lambda e: e.memset(gkT[:], 1.0), W=[b_gk])
            for z in range(2):
                P.add("pool", lambda e, z=z: e.memset(qz[:, z], 0.0), W=[b_qz[z]])
                P.add("pool", lambda e, z=z: e.memset(krz[:, z], 0.0), W=[b_krz[z]])
            for g4 in range(4):
                pg, bpg = C.ps[3 + g4 % 2], C.bps[3 + g4 % 2]
                for k in range(8):
                    P.add("pe", lambda e, k=k, g4=g4, pg=pg: e.matmul(pg[0:16, :], lhsT=wg[:, k, :], rhs=hT[:, k, g4 * 512:(g4 + 1) * 512],
                                                                    start=(k == 0), stop=(k == 7)), R=[b_cst] + C.bhT[g4 * 4:g4 * 4 + 4], W=[bpg])
                P.add("act", lambda e, g4=g4, pg=pg: e.copy(out=gkT[0:16, g4 * 512:(g4 + 1) * 512], in_=pg[0:16, :]), R=[bpg], W=[b_gk])

            def front(hd, s, t):
                z = t % 2
                tc = slice(t * 128, (t + 1) * 128)
                bhT = C.bhT[t]
                pa, bpa = C.ps[3], C.bps[3]
                for k in range(8):
                    P.add("pe", lambda e, k=k: e.matmul(pa[:, 0:128], lhsT=wq[:, s, k, 0:128], rhs=hT[:, k, tc], start=(k == 0), stop=(k == 7)),
                          R=[b_wq[s], bhT], W=[bpa])
                for k in range(8):
                    P.add("pe", lambda e, k=k: e.matmul(pa[:, 128:256], lhsT=wq[:, s, k, 128:256], rhs=hT[:, k, tc], start=(k == 0), stop=(k == 7)),
                          R=[b_wq[s], bhT], W=[bpa])
                for k in range(8):
                    P.add("pe", lambda e, k=k: e.matmul(pa[:, 256:384], lhsT=hT[:, k, tc], rhs=wq[:, s, k, 128:256], start=(k == 0), stop=(k == 7)),
                          R=[b_wq[s], bhT], W=[bpa])
                P.add("pe", lambda e: e.matmul(pa[:, 384:512], lhsT=gkT[0:17, tc], rhs=wgk[0:17, hd * 128:(hd + 1) * 128], start=True, stop=True),
                      R=[b_gk, b_cst], W=[bpa])
                P.add("act", lambda e: e.copy(out=qk[:, z], in_=pa[:, 0:256].rearrange("p (a c) -> p a c", a=2)), R=[bpa], W=[b_qk[z]])
                P.add("act", lambda e: e.copy(out=kt[:, z], in_=pa[:, 256:384]), R=[bpa], W=[b_kt[z]])
                P.add("act", lambda e: e.activation(out=nl[:, z], in_=pa[:, 384:512], func=AF.Exp, scale=-1.0), R=[bpa], W=[b_nl[z]])
                P.add("act", lambda e: e.activation(out=nl[:, z], in_=nl[:, z], func=AF.Ln, bias=1.0, scale=1.0), R=[b_nl[z]], W=[b_nl[z]])
                pb_, bpb = C.ps[4], C.bps[4]
                for k in range(8):
                    P.add("pe", lambda e, k=k: e.matmul(pb_[:], lhsT=hT[:, k, tc], rhs=wq[:, s, k, 256:768], start=(k == 0), stop=(k == 7)),
                          R=[b_wq[s], bhT], W=[bpb])
                P.add("act", lambda e: e.copy(out=vt[:, z], in_=pb_[:, 0:256]), R=[bpb], W=[b_vt[z]])
                P.add("act", lambda e: e.activation(out=gs[:, z], in_=pb_[:, 256:512], func=AF.Silu), R=[bpb], W=[b_gs[z]])
                pc, bpc = C.ps[5], C.bps[5]
                P.add("pe", lambda e: e.matmul(pc[:, 0:128], lhsT=nl[:, z], rhs=tri[:], start=True, stop=True), R=[b_nl[z], b_cst], W=[bpc])
                P.add("pe", lambda e: e.matmul(pc[:, 128:256], lhsT=su[:], rhs=nl[:, z], start=True, stop=True), R=[b_nl[z], b_cst], W=[bpc])
                P.add("act", lambda e: e.activation(out=ex[:, z, 0, :], in_=pc[:, 0:128], func=AF.Exp, scale=-1.0 / 16.0), R=[bpc], W=[b_ex[z]])
                P.add("act", lambda e: e.activation(out=ex[:, z, 1, :], in_=pc[:, 0:128], func=AF.Exp, scale=1.0 / 16.0), R=[bpc], W=[b_ex[z]])
                P.add("act", lambda e: e.activation(out=ex[:, z, 2, :], in_=pc[:, 128:256], func=AF.Exp, scale=-1.0 / 16.0), R=[bpc], W=[b_ex[z]])
                for c in range(2):
                    cs = slice(c * 64, (c + 1) * 64)
                    P.add("dve", lambda e, c=c, cs=cs: e.scalar_tensor_tensor(out=qz[:, z, c, cs], in0=qk[:, z, 0, cs], scalar=SCALE, in1=ex[:, z, 0, cs],
                                                                            op0=ALU.mult, op1=ALU.mult), R=[b_qk[z], b_ex[z]], W=[b_qz[z]])
                P.add("pool", lambda e: e.tensor_tensor(out=ke[:, z], in0=qk[:, z, 1], in1=ex[:, z, 1, :], op=ALU.mult), R=[b_qk[z], b_ex[z]], W=[b_ke[z]])
                for c in range(2):
                    rows = slice(c * 64, (c + 1) * 64)
                    P.add("pool", lambda e, c=c, rows=rows: e.tensor_tensor(out=krz[rows, z, c, :], in0=kt[rows, z], in1=ex[rows, z, 2, :], op=ALU.mult),
                          R=[b_kt[z], b_ex[z]], W=[b_krz[z]])
                P.add("pe", lambda e: e.matmul(pc[:, 256:384], lhsT=ke[:, z], rhs=qz[:, z, 0, :], start=True, stop=False), R=[b_ke[z], b_qz[z]], W=[bpc])
                P.add("pe", lambda e: e.matmul(pc[:, 256:384], lhsT=ke[:, z], rhs=qz[:, z, 1, :], start=False, stop=True), R=[b_ke[z], b_qz[z]], W=[bpc])
                P.add("dve", lambda e: e.tensor_tensor(out=att[:, z], in0=pc[:, 256:384], in1=cmk[:], op=ALU.mult), R=[bpc, b_cst], W=[b_att[z]])
                pd, bpd = C.ps[6], C.bps[6]
                P.add("pe", lambda e: e.matmul(pd[:, 0:256], lhsT=krz[:, z, 0, :], rhs=vt[:, z], start=True, stop=True), R=[b_krz[z], b_vt[z]], W=[bpd])
                P.add("pe", lambda e: e.matmul(pd[:, 256:512], lhsT=krz[:, z, 1, :], rhs=vt[:, z], start=True, stop=True), R=[b_krz[z], b_vt[z]], W=[bpd])
                P.add("dve", lambda e: e.scalar_tensor_tensor(out=St[:], in0=St[:], scalar=ex[:, z, 0, 63:64], in1=pd[:, 0:256], op0=ALU.mult, op1=ALU.add),
                      R=[b_S, b_ex[z], bpd], W=[b_S])
                P.add("act", lambda e: e.copy(out=Sb[:, 3 + z, :], in_=St[:]), R=[b_S], W=[b_Sb[3 + z]])
                P.add("dve", lambda e: e.scalar_tensor_tensor(out=St[:], in0=St[:], scalar=ex[:, z, 0, 127:128], in1=pd[:, 256:512], op0=ALU.mult, op1=ALU.add),
                      R=[b_S, b_ex[z], bpd], W=[b_S])
                P.add("act", lambda e: e.copy(out=Sb[:, (t + 1) % 3, :], in_=St[:]), R=[b_S], W=[b_Sb[(t + 1) % 3]])

            def back(hd, s, t):
                z = t % 2
                tc = slice(t * 128, (t + 1) * 128)
                po, bpo = C.ps[7], C.bps[7]
                P.add("pe", lambda e: e.matmul(po[:, 0:256], lhsT=qz[:, z, 0, :], rhs=Sb[:, t % 3, :], start=True, stop=False), R=[b_qz[z], b_Sb[t % 3]], W=[bpo])
                P.add("pe", lambda e: e.matmul(po[:, 0:256], lhsT=qz[:, z, 1, :], rhs=Sb[:, 3 + z, :], start=False, stop=False), R=[b_qz[z], b_Sb[3 + z]], W=[bpo])
                P.add("pe", lambda e: e.matmul(po[:, 0:256], lhsT=att[:, z], rhs=vt[:, z], start=False, stop=True), R=[b_att[z], b_vt[z]], W=[bpo])
                P.add("act", lambda e: e.activation(out=jk[:], in_=po[:, 0:256], func=AF.Square, accum_out=ss[:, z, 0:1]), R=[bpo], W=[b_jk, b_ss[z]])
                P.add("act", lambda e: e.activation(out=ss[:, z, 1:2], in_=ss[:, z, 0:1], func=AF.Sqrt, bias=EPS, scale=1.0 / 256.0), R=[b_ss[z]], W=[b_ss[z]])
                P.add("dve", lambda e: e.reciprocal(out=ss[:, z, 1:2], in_=ss[:, z, 1:2]), R=[b_ss[z]], W=[b_ss[z]])
                P.add("dve", lambda e: e.scalar_tensor_tensor(out=ot[:, z], in0=po[:, 0:256], scalar=ss[:, z, 1:2], in1=ng[:], op0=ALU.mult, op1=ALU.mult),
                      R=[bpo, b_ss[z], b_cst], W=[b_ot[z]])
                P.add("pool", lambda e: e.tensor_tensor(out=og[:, z], in0=ot[:, z], in1=gs[:, z], op=ALU.mult), R=[b_ot[z], b_gs[z]], W=[b_og[z]])
                ptp, bptp = C.ps[z], C.bps[z]
                ptb = ptp[:].bitcast(BF16)
                for c2 in range(2):
                    P.add("pe", lambda e, c2=c2: e.transpose(out=ptb[:, c2 * 128:(c2 + 1) * 128], in_=og[:, z, c2 * 128:(c2 + 1) * 128], identity=C.identb[:]),
                          R=[b_og[z], C.b_const], W=[bptp])
                P.add("act", lambda e: e.copy(out=oT[:, hd * 2:hd * 2 + 2, tc], in_=ptb[:, 0:256].rearrange("p (a c) -> p a c", a=2)), R=[bptp], W=[C.b_srcT])

            for hd in range(4):
                s = hd % 2
                for (c0_, w_, d0) in ((hd * 128, 128, 0), (512 + hd * 128, 128, 128), (1024 + hd * 256, 256, 256), (2048 + hd * 256, 256, 512)):
                    P.add("pool", lambda e, c0_=c0_, w_=w_, d0=d0, s=s: e.dma_start(
                        out=wq[:, s, :, d0:d0 + w_], in_=w_in[:, c0_:c0_ + w_].rearrange("(k p) n -> p k n", p=128)), W=[b_wq[s]], dma=True)
                P.add("pool", lambda e: e.memset(St[:], 0.0), W=[b_S])
                P.add("pool", lambda e: e.memset(Sb[:, 0, :], 0.0), W=[b_Sb[0]])
                front(hd, s, 0)
                for t in range(NT):
                    if t + 1 < NT:
                        front(hd, s, t + 1)
                    back(hd, s, t)
            P.flush()
        out_proj_ln(C, li, T, oT, 8, T["c_w_out_%d" % j], R_)


def build_program(mode="full", n_experts=NE, layers=(0, 1, 2, 3)):
    nc = bass.Bass("TRN2", target_bir_lowering=False)
    T = {}

    def din(name, shape, dt=F32):
        T[name] = nc.dram_tensor(name, list(shape), dt, kind="ExternalInput").ap()

    do_mixer = mode in ("full", "mixer_only")
    do_moe = mode in ("full", "moe_only")
    din("x", (S, D))
    din("ident", (128, 128))
    din("identb", (128, 128), BF16)
    din("ln_g", (DEPTH, 2, D))
    din("ln_b", (DEPTH, 2, D))
    mixers = sorted(set(li % 3 for li in layers)) if do_mixer else []
    if 0 in mixers:
        din("rel_bias", (32, 8))
        din("relT", (8, 2, 128, 128))
        din("causal", (128, 128))
        din("vmask", (128, 128))
        din("vmask2", (128, 128))
    for li in layers:
        if do_mixer:
            j = li // 3
            if li % 3 == 0:
                din("a_w_in_%d" % j, (D, 3 * D))
                din("a_w_out_%d" % j, (D, D))
            elif li % 3 == 1:
                din("b_w_in_%d" % j, (D, 4 * D))
                din("b_ln_g_%d" % j, (2 * D,))
                din("b_ln_b_%d" % j, (2 * D,))
                din("b_w_s_%d" % j, (8, 128, 128))
                din("b_b_s_%d" % j, (8, 128))
                din("b_w_out_%d" % j, (2 * D, D))
                din("tril", (128, 128))
            else:
                din("c_w_in_%d" % j, (D, 3088))
                din("c_w_gk_up_%d" % j, (16, 512))
                din("c_b_gk_%d" % j, (512,))
                din("c_norm_g_%d" % j, (256,))
                din("c_w_out_%d" % j, (D, D))
                din("gla_tri", (128, 128))
                din("gla_su", (128, 128))
                din("gla_cm", (128, 128))
        din("moe_w_router_%d" % li, (D, NE))
        din("moe_b_router_%d" % li, (NE,))
        if do_moe:
            if "iota" not in T:
                din("iota", (128, 128))
                din("ones_b", (128, 128), BF16)
                din("tris_b", (128, 128), BF16)
            din("moe_w_gate_up_%d" % li, (NE, D, 2 * D))
            din("moe_b_gate_up_%d" % li, (NE, 2 * D))
            din("moe_w_down_%d" % li, (NE, D, D))
            din("moe_b_down_%d" % li, (NE, D))
    out = nc.dram_tensor("out", [S, D], F32, kind="ExternalOutput").ap()

    C = Ctx()
    C.nc = nc
    C.n_experts = n_experts
    C.P = P = Prog(nc)
    C.tp_rr = C.act_rr = C.q_rr = C.y_rr = C.ys_rr = 0
    with ExitStack() as es:
        h = es.enter_context(nc.sbuf_tensor("h", [128, NT, D], F32))
        hT = es.enter_context(nc.sbuf_tensor("hT", [128, 8, S], BF16))
        ident = es.enter_context(nc.sbuf_tensor("ident_s", [128, 128], F32))
        identb = es.enter_context(nc.sbuf_tensor("identb_s", [128, 128], BF16))
        hT32 = es.enter_context(nc.sbuf_tensor("hT32", [128, 8, 128], F32))
        gates = es.enter_context(nc.sbuf_tensor("gates", [128, NT, NE], F32))
        ln_st = es.enter_context(nc.sbuf_tensor("ln_st", [128, 2, 6], F32))
        ln_mv = es.enter_context(nc.sbuf_tensor("ln_mv", [128, 2], F32))
        ln_rs = es.enter_context(nc.sbuf_tensor("ln_rs", [128, 1], F32))
        r_lg = es.enter_context(nc.sbuf_tensor("r_lg", [128, NE], F32))
        r_m8 = es.enter_context(nc.sbuf_tensor("r_m8", [128, 8], F32))
        r_ex = es.enter_context(nc.sbuf_tensor("r_ex", [128, NE], F32))
        r_msk = es.enter_context(nc.sbuf_tensor("r_msk", [128, NE], F32))
        r_sm = es.enter_context(nc.sbuf_tensor("r_sm", [128, 2], F32))
        r_wr = es.enter_context(nc.sbuf_tensor("r_wr", [128, 8, NE], F32))
        r_brb = es.enter_context(nc.sbuf_tensor("r_brb", [128, NE], F32))
        C.h, C.hT, C.ident, C.identb, C.hT32, C.gates = h, hT, ident, identb, hT32, gates
        C.ln_st, C.ln_mv, C.ln_rs, C.ln_nb = ln_st, ln_mv, ln_rs, None
        C.r_lg, C.r_m8, C.r_ex, C.r_msk, C.r_sm = r_lg, r_m8, r_ex, r_msk, r_sm
        C.bh = [Buf("h%d" % t) for t in range(NT)]
        C.bhT = [Buf("hT%d" % t) for t in range(NT)]
        C.b_gates = [Buf() for t in range(NT)]
        C.b_const = Buf("const")
        C.b_lnst = Buf()
        C.b_hT32 = Buf()
        C.b_rt = Buf()
        ps_cms = [nc.psum_tensor("ps%d" % i, [128, 512], F32) for i in range(8)]
        C.ps = [cm.__enter__() for cm in ps_cms]
        C.bps = [Buf("ps%d" % i) for i in range(8)]

        P.add("sp", lambda e: e.dma_start(out=ident[:], in_=T["ident"]), W=[C.b_const], dma=True)
        P.add("sp", lambda e: e.dma_start(out=identb[:], in_=T["identb"]), W=[C.b_const], dma=True)
        for t in range(NT):
            P.add("sp", lambda e, t=t: e.dma_start(out=h[:, t, :], in_=T["x"][t * 128:(t + 1) * 128, :]), W=[C.bh[t]], dma=True)
        first = True
        for li in layers:
            b_r = Buf()
            P.add("sp", lambda e, li=li: e.dma_start(out=r_wr[:], in_=T["moe_w_router_%d" % li].rearrange("(k p) n -> p k n", p=128)), W=[b_r], dma=True)
            P.add("sp", lambda e, li=li: e.dma_start(out=r_brb[:], in_=bcast_rows(T["moe_b_router_%d" % li])), W=[b_r], dma=True)
            R_ = {"wr": r_wr, "brb": r_brb, "b": b_r}
            if first:
                for t in range(NT):
                    transpose_tile(C, t, None if do_mixer else R_)
                P.flush()
                first = False
            if do_mixer:
                if li % 3 == 0:
                    moba_phase(C, li, T, R_)
                elif li % 3 == 1:
                    gmlp_phase(C, li, T, R_)
                else:
                    gla_phase(C, li, T, R_)
            if do_moe:
                moe_phase(C, li, T)

        for t in range(NT):
            P.add("sp", lambda e, t=t: e.dma_start(out=out[t * 128:(t + 1) * 128, :], in_=h[:, t, :]), R=[C.bh[t]], dma=True)
        P.flush()
        for cm in reversed(ps_cms):
            cm.__exit__(None, None, None)
    P.close()
    return nc


def t5_bucket_np(rel):
    n = np.maximum(rel, 0)
    nf = np.maximum(n, 1).astype(np.float32)
    large = 16 + (np.log(nf / np.float32(16)) / np.float32(math.log(128 / 16)) * np.float32(16)).astype(np.int32)
    large = np.minimum(large, 31)
    return np.where(n < 16, n, large)


def host_constants(rel_bias=None):
    import ml_dtypes
    c = {"ident": np.eye(128, dtype=np.float32), "identb": np.eye(128, dtype=np.float32).astype(ml_dtypes.bfloat16)}
    i = np.arange(128)[:, None]
    jj = np.arange(128)[None, :]
    c["causal"] = np.where(jj <= i, 0.0, NEG).astype(np.float32)
    c["iota"] = np.broadcast_to(np.arange(128, dtype=np.float32)[None, :], (128, 128)).copy()
    c["ones_b"] = np.ones((128, 128), np.float32).astype(ml_dtypes.bfloat16)
    c["tris_b"] = (i < jj).astype(np.float32).astype(ml_dtypes.bfloat16)
    c["tril"] = (jj <= i).astype(np.float32)
    qt = np.arange(16)[:, None]
    n = np.arange(8)[None, :]
    valid = n < (qt // 2)
    vm = np.where(valid, 0.0, -1e30).astype(np.float32).reshape(1, 128)
    c["vmask"] = np.broadcast_to(vm, (128, 128)).copy()
    vm2 = np.where(valid, -30000.0, -1e30).astype(np.float32).reshape(1, 128)
    c["vmask2"] = np.broadcast_to(vm2, (128, 128)).copy()
    if rel_bias is not None:
        relT = np.empty((8, 2, 128, 128), np.float32)
        for d in range(2):
            bk = t5_bucket_np(d * 128 + i - jj)
            for hh in range(8):
                relT[hh, d] = rel_bias[bk, hh]
        c["relT"] = relT
    same = (i // 64) == (jj // 64)
    c["gla_tri"] = (same & (i <= jj)).astype(np.float32)
    c["gla_su"] = (same & (i > jj)).astype(np.float32)
    c["gla_cm"] = (same & (i <= jj)).astype(np.float32)
    return c


def make_in_map(A, x_core, consts, layers=(0, 1, 2, 3), mode="full"):
    do_mixer = mode in ("full", "mixer_only")
    do_moe = mode in ("full", "moe_only")
    im = {"x": np.ascontiguousarray(x_core), "ln_g": A["ln_g"], "ln_b": A["ln_b"]}
    im.update(consts)
    if "rel_bias" in A:
        im["rel_bias"] = A["rel_bias"]
    for li in layers:
        j = li // 3
        if do_mixer:
            if li % 3 == 0:
                im["a_w_in_%d" % j] = A["a_w_in"][j]
                im["a_w_out_%d" % j] = A["a_w_out"][j]
            elif li % 3 == 1:
                for n in ("b_w_in", "b_ln_g", "b_ln_b", "b_w_s", "b_b_s", "b_w_out"):
                    im["%s_%d" % (n, j)] = A[n][j]
            else:
                for n in ("c_w_in", "c_w_gk_up", "c_b_gk", "c_norm_g", "c_w_out"):
                    im["%s_%d" % (n, j)] = A[n][j]
        im["moe_w_router_%d" % li] = A["moe_w_router"][li]
        im["moe_b_router_%d" % li] = A["moe_b_router"][li]
        if do_moe:
            for n in ("moe_w_gate_up", "moe_b_gate_up", "moe_w_down", "moe_b_down"):
                im["%s_%d" % (n, li)] = A[n][li]
    return im


def kernel(**inputs):
    A = {k: np.asarray(v) for k, v in inputs.items()}
    nc = build_program("full")
    consts = host_constants(A["rel_bias"])
    in_maps = [make_in_map(A, A["x"][c], consts) for c in range(8)]
    res = run_bass_kernel_spmd(nc, in_maps, core_ids=list(range(8)))
    out = np.stack([np.asarray(res.results[c]["out"]) for c in range(8)], axis=0)
    return out.astype(np.float32)
```
